# Optimizing a Trainium2 kernel written in Bass

```python
import jax, jax.numpy as jnp
from jax import lax
import numpy as np

D_MODEL = 1024
BATCH = 2
SEQ = 8192
DEPTH = 2

GLA_HEADS = 4
GLA_DK = D_MODEL // 2 // GLA_HEADS
GLA_DV = D_MODEL // GLA_HEADS
GLA_GATE_RANK = 16
GLA_TAU = 16.0
GLA_CHUNK = 64

NSA_HEADS = 16
NSA_GROUPS = 4
NSA_HPG = NSA_HEADS // NSA_GROUPS
NSA_HEAD_DIM = D_MODEL // NSA_HEADS
CMP_BLOCK = 32
CMP_STRIDE = 16
CMP_HIDDEN = 256
SLC_BLOCK = 64
SLC_TOP_N = 16
WINDOW = 512
Q_BLOCK = 128

ROPE_THETA = 500000.0
ROT_DIM = NSA_HEAD_DIM // 4

N_EXPERTS = 16
N_EXPERT_GROUPS = 4
EXPERTS_PER_GROUP = N_EXPERTS // N_EXPERT_GROUPS
TOP_K = 2
D_FF_EXPERT = D_MODEL // 4

DN_ALPHA = (2 * DEPTH) ** 0.25
DN_BETA = (8 * DEPTH) ** -0.25
LN_EPS = 1e-5
NEG = -1e30
BIG = 1e30

N_GLA_LAYERS = (DEPTH + 1) // 2
N_NSA_LAYERS = DEPTH // 2
GLA_IN = 2 * GLA_HEADS * GLA_DK + 2 * GLA_HEADS * GLA_DV + GLA_GATE_RANK
NSA_IN = NSA_HEADS * NSA_HEAD_DIM + 6 * NSA_GROUPS * NSA_HEAD_DIM + 3 * NSA_HEADS

kernel_name = 'hybrid_gla_nsa_moe_deepnorm'


def layer_norm(x, g, b):
    xf = x.astype(jnp.float32)
    mu = jnp.mean(xf, axis=-1, keepdims=True)
    var = jnp.mean(jnp.square(xf - mu), axis=-1, keepdims=True)
    y = (xf - mu) * lax.rsqrt(var + LN_EPS) * g.astype(jnp.float32) + b.astype(jnp.float32)
    return y.astype(x.dtype)


def partial_rope(t, pos):
    half = ROT_DIM // 2
    inv_freq = jnp.power(ROPE_THETA, -jnp.arange(half, dtype=jnp.float32) * (2.0 / ROT_DIM))
    ang = pos.astype(jnp.float32)[:, None] * inv_freq[None, :]
    cos = jnp.cos(ang).astype(t.dtype)
    sin = jnp.sin(ang).astype(t.dtype)
    x1 = t[..., :half]
    x2 = t[..., half:ROT_DIM]
    return jnp.concatenate([x1 * cos - x2 * sin, x2 * cos + x1 * sin, t[..., ROT_DIM:]], axis=-1)


def masked_softmax(scores, mask):
    return jax.nn.softmax(jnp.where(mask, scores.astype(jnp.float32), NEG), axis=-1)


def gla_mixer(x, w_in, w_gate_up, b_gate, norm_g, w_out):
    B, S, _ = x.shape
    H, DK, DV, C = GLA_HEADS, GLA_DK, GLA_DV, GLA_CHUNK
    N = S // C
    f32 = jnp.float32
    proj = x @ w_in
    cuts = np.cumsum([H * DK, H * DK, H * DV, GLA_GATE_RANK]).tolist()
    q, k, v, g_low, r = jnp.split(proj, cuts, axis=-1)
    log_a = jax.nn.log_sigmoid((g_low @ w_gate_up + b_gate).astype(f32)) / GLA_TAU

    def chunks(t, d):
        return t.reshape(B, N, C, H, d).transpose(0, 3, 1, 2, 4).astype(f32)

    q = chunks(q, DK) * (DK ** -0.5)
    k = chunks(k, DK)
    v = chunks(v, DV)
    b = jnp.cumsum(chunks(log_a, DK), axis=3)
    b_last = b[:, :, :, -1:, :]
    q_dec = q * jnp.exp(b)
    causal = jnp.tril(jnp.ones((C, C), dtype=bool))
    scores = jnp.einsum('bhnid,bhnjd->bhnij', q_dec, k * jnp.exp(-b))
    o_intra = jnp.einsum('bhnij,bhnjv->bhniv', jnp.where(causal, scores, 0.0), v)
    kv = jnp.einsum('bhncd,bhncv->bhndv', k * jnp.exp(b_last - b), v)
    decay = jnp.exp(b_last[:, :, :, 0, :])

    def step(state, inp):
        dec, kv_n = inp
        return dec[..., None] * state + kv_n, state

    _, states = lax.scan(step, jnp.zeros((B, H, DK, DV), f32),
                         (jnp.moveaxis(decay, 2, 0), jnp.moveaxis(kv, 2, 0)))
    o_inter = jnp.einsum('bhncd,nbhdv->bhncv', q_dec, states)
    o = (o_intra + o_inter).transpose(0, 2, 3, 1, 4).reshape(B, S, H, DV)
    o = o * lax.rsqrt(jnp.mean(jnp.square(o), axis=-1, keepdims=True) + LN_EPS) * norm_g.astype(f32)
    o = o.reshape(B, S, H * DV) * jax.nn.silu(r.astype(f32))
    return o.astype(x.dtype) @ w_out


def compress(t, w1, w2, pe):
    B, G, S, Dh = t.shape
    n_chunk = S // CMP_STRIDE
    span = CMP_BLOCK // CMP_STRIDE
    nc = n_chunk - span + 1
    ch = t.reshape(B, G, n_chunk, CMP_STRIDE, Dh)
    blocks = jnp.concatenate([ch[:, :, i:i + nc] for i in range(span)], axis=3)
    blocks = (blocks + pe).reshape(B, G, nc, CMP_BLOCK * Dh)
    return jax.nn.silu(blocks @ w1) @ w2


def nsa_mixer(x, w_in, w_ck1, w_ck2, w_cv1, w_cv2, cmp_pe, w_out):
    B, S, _ = x.shape
    H, G, HPG, Dh = NSA_HEADS, NSA_GROUPS, NSA_HPG, NSA_HEAD_DIM
    f32 = jnp.float32
    proj = x @ w_in
    cuts = np.cumsum([H * Dh] + [G * Dh] * 6).tolist()
    q, kc, vc, ks, vs, kw, vw, gates = jnp.split(proj, cuts, axis=-1)
    pos = jnp.arange(S, dtype=jnp.int32)

    def kv_heads(t):
        return t.reshape(B, S, G, Dh).transpose(0, 2, 1, 3)

    q = partial_rope(q.reshape(B, S, G, HPG, Dh).transpose(0, 2, 3, 1, 4), pos) * (Dh ** -0.5)
    gates = jax.nn.sigmoid(gates.astype(f32)).reshape(B, S, G, HPG, 3).transpose(0, 2, 3, 1, 4)

    span = CMP_BLOCK // CMP_STRIDE
    nc = S // CMP_STRIDE - span + 1
    cmp_end = jnp.arange(nc, dtype=jnp.int32) * CMP_STRIDE + (CMP_BLOCK - 1)
    k_cmp = partial_rope(compress(kv_heads(kc), w_ck1, w_ck2, cmp_pe), cmp_end)
    v_cmp = compress(kv_heads(vc), w_cv1, w_cv2, cmp_pe).astype(f32)

    ns = S // SLC_BLOCK
    n_sel = min(SLC_TOP_N, ns)
    k_slc = partial_rope(kv_heads(ks), pos).reshape(B, G, ns, SLC_BLOCK, Dh)
    v_slc = kv_heads(vs).reshape(B, G, ns, SLC_BLOCK, Dh)
    ratio = SLC_BLOCK // CMP_STRIDE
    pad_amt = ratio * ns + span - 1 - nc

    pad = ((0, 0), (0, 0), (WINDOW, 0), (0, 0))
    k_win = jnp.pad(partial_rope(kv_heads(kw), pos), pad)
    v_win = jnp.pad(kv_heads(vw), pad)

    bi = jnp.arange(B)[:, None, None, None]
    gi = jnp.arange(G)[None, :, None, None]
    blk_ids = jnp.arange(ns)
    in_blk = jnp.arange(SLC_BLOCK)
    win_off = jnp.arange(WINDOW + Q_BLOCK) - WINDOW

    def query_block(qi):
        s0 = qi * Q_BLOCK
        qpos = s0 + jnp.arange(Q_BLOCK)
        qb = lax.dynamic_slice_in_dim(q, s0, Q_BLOCK, axis=3)
        gb = lax.dynamic_slice_in_dim(gates, s0, Q_BLOCK, axis=3)

        cmask = cmp_end[None, :] <= qpos[:, None]
        p_cmp = masked_softmax(jnp.einsum('bghqd,bgcd->bghqc', qb, k_cmp), cmask)
        p_cmp = p_cmp * jnp.any(cmask, axis=-1)[:, None]
        o_cmp = jnp.einsum('bghqc,bgcd->bghqd', p_cmp, v_cmp)

        p_grp = jnp.pad(p_cmp.sum(axis=2), ((0, 0), (0, 0), (0, 0), (0, pad_amt)))
        imp = p_grp[..., 0:ratio * ns:ratio]
        for m in range(ratio):
            for n in range(span):
                if m + n > 0:
                    imp = imp + p_grp[..., m + n:m + n + ratio * ns:ratio]
        cur = (qpos // SLC_BLOCK)[:, None]
        forced = (blk_ids == 0) | (blk_ids == cur) | (blk_ids == cur - 1)
        imp = jnp.where(blk_ids <= cur, jnp.where(forced, BIG, imp), NEG)
        _, sel = lax.top_k(imp, n_sel)
        k_sel = k_slc[bi, gi, sel].reshape(B, G, Q_BLOCK, n_sel * SLC_BLOCK, Dh)
        v_sel = v_slc[bi, gi, sel].reshape(B, G, Q_BLOCK, n_sel * SLC_BLOCK, Dh)
        kpos = (sel[..., None] * SLC_BLOCK + in_blk).reshape(B, G, Q_BLOCK, n_sel * SLC_BLOCK)
        smask = (kpos <= qpos[:, None])[:, :, None]
        p_slc = masked_softmax(jnp.einsum('bghqd,bgqkd->bghqk', qb, k_sel), smask)
        o_slc = jnp.einsum('bghqk,bgqkd->bghqd', p_slc, v_sel.astype(f32))

        kw_b = lax.dynamic_slice_in_dim(k_win, s0, WINDOW + Q_BLOCK, axis=2)
        vw_b = lax.dynamic_slice_in_dim(v_win, s0, WINDOW + Q_BLOCK, axis=2)
        kwpos = s0 + win_off
        rel = qpos[:, None] - kwpos[None, :]
        wmask = (rel >= 0) & (rel < WINDOW) & (kwpos[None, :] >= 0)
        p_win = masked_softmax(jnp.einsum('bghqd,bgkd->bghqk', qb, kw_b), wmask)
        o_win = jnp.einsum('bghqk,bgkd->bghqd', p_win, vw_b.astype(f32))

        o = gb[..., 0:1] * o_cmp + gb[..., 1:2] * o_slc + gb[..., 2:3] * o_win
        return o.astype(x.dtype)

    outs = lax.map(query_block, jnp.arange(S // Q_BLOCK))
    o = outs.transpose(1, 0, 4, 2, 3, 5).reshape(B, S, H * Dh)
    return o @ w_out


def moe_ffn(x, w_router, b_router, w_gate, w_up, w_down):
    B, S, D = x.shape
    xt = x.reshape(B * S, D)
    n = xt.shape[0]
    s = jax.nn.sigmoid((xt @ w_router).astype(jnp.float32))
    biased = s + b_router.astype(jnp.float32)
    grp_score = lax.top_k(biased.reshape(n, N_EXPERT_GROUPS, EXPERTS_PER_GROUP), TOP_K)[0].sum(-1)
    grp = jnp.argmax(grp_score, axis=-1)
    in_grp = (jnp.arange(N_EXPERTS) // EXPERTS_PER_GROUP)[None, :] == grp[:, None]
    _, top_e = lax.top_k(jnp.where(in_grp, biased, NEG), TOP_K)
    w = jnp.take_along_axis(s, top_e, axis=-1)
    w = w / jnp.sum(w, axis=-1, keepdims=True)
    gate = jnp.zeros((n, N_EXPERTS), jnp.float32).at[jnp.arange(n)[:, None], top_e].set(w)
    h = jax.nn.silu(jnp.einsum('nd,edf->nef', xt, w_gate)) * jnp.einsum('nd,edf->nef', xt, w_up)
    h = h * gate[:, :, None].astype(h.dtype)
    return jnp.einsum('nef,efd->nd', h, w_down).reshape(B, S, D)


def setup_inputs(seed: int = 0) -> dict:
    key = jax.random.key(seed)
    ks = jax.random.split(key, 20)
    f32 = jnp.float32

    def nrm(k, shape, scale):
        return jax.random.normal(k, shape, f32) * scale

    D = D_MODEL
    nG, nN = N_GLA_LAYERS, N_NSA_LAYERS
    cmp_in = CMP_BLOCK * NSA_HEAD_DIM
    return {
        'x': nrm(ks[0], (BATCH, SEQ, D), 1.0),
        'gla_w_in': nrm(ks[1], (nG, D, GLA_IN), D ** -0.5),
        'gla_w_gate_up': nrm(ks[2], (nG, GLA_GATE_RANK, GLA_HEADS * GLA_DK), GLA_GATE_RANK ** -0.5),
        'gla_b_gate': nrm(ks[3], (nG, GLA_HEADS * GLA_DK), 0.1),
        'gla_norm_g': 1.0 + nrm(ks[4], (nG, GLA_DV), 0.02),
        'gla_w_out': nrm(ks[5], (nG, GLA_HEADS * GLA_DV, D), DN_BETA * (GLA_HEADS * GLA_DV) ** -0.5),
        'nsa_w_in': nrm(ks[6], (nN, D, NSA_IN), D ** -0.5),
        'nsa_w_cmp_k1': nrm(ks[7], (nN, cmp_in, CMP_HIDDEN), cmp_in ** -0.5),
        'nsa_w_cmp_k2': nrm(ks[8], (nN, CMP_HIDDEN, NSA_HEAD_DIM), CMP_HIDDEN ** -0.5),
        'nsa_w_cmp_v1': nrm(ks[9], (nN, cmp_in, CMP_HIDDEN), cmp_in ** -0.5),
        'nsa_w_cmp_v2': nrm(ks[10], (nN, CMP_HIDDEN, NSA_HEAD_DIM), CMP_HIDDEN ** -0.5),
        'nsa_cmp_pe': nrm(ks[11], (nN, CMP_BLOCK, NSA_HEAD_DIM), 0.1),
        'nsa_w_out': nrm(ks[12], (nN, NSA_HEADS * NSA_HEAD_DIM, D), DN_BETA * (NSA_HEADS * NSA_HEAD_DIM) ** -0.5),
        'moe_w_router': nrm(ks[13], (D, N_EXPERTS), D ** -0.5),
        'moe_b_router': nrm(ks[14], (N_EXPERTS,), 0.01),
        'moe_w_gate': nrm(ks[15], (DEPTH, N_EXPERTS, D, D_FF_EXPERT), D ** -0.5),
        'moe_w_up': nrm(ks[16], (DEPTH, N_EXPERTS, D, D_FF_EXPERT), D ** -0.5),
        'moe_w_down': nrm(ks[17], (DEPTH, N_EXPERTS, D_FF_EXPERT, D), DN_BETA * D_FF_EXPERT ** -0.5),
        'ln_g': 1.0 + nrm(ks[18], (DEPTH, 2, D), 0.02),
        'ln_b': nrm(ks[19], (DEPTH, 2, D), 0.02),
    }


def reference(x, gla_w_in, gla_w_gate_up, gla_b_gate, gla_norm_g, gla_w_out,
              nsa_w_in, nsa_w_cmp_k1, nsa_w_cmp_k2, nsa_w_cmp_v1, nsa_w_cmp_v2, nsa_cmp_pe, nsa_w_out,
              moe_w_router, moe_b_router, moe_w_gate, moe_w_up, moe_w_down, ln_g, ln_b):
    for i in range(DEPTH):
        j = i // 2
        if i % 2 == 0:
            h = gla_mixer(x, gla_w_in[j], gla_w_gate_up[j], gla_b_gate[j], gla_norm_g[j], gla_w_out[j])
        else:
            h = nsa_mixer(x, nsa_w_in[j], nsa_w_cmp_k1[j], nsa_w_cmp_k2[j], nsa_w_cmp_v1[j],
                          nsa_w_cmp_v2[j], nsa_cmp_pe[j], nsa_w_out[j])
        x = layer_norm(DN_ALPHA * x + h, ln_g[i, 0], ln_b[i, 0])
        f = moe_ffn(x, moe_w_router, moe_b_router, moe_w_gate[i], moe_w_up[i], moe_w_down[i])
        x = layer_norm(DN_ALPHA * x + f, ln_g[i, 1], ln_b[i, 1])
    return x
```

```python
from contextlib import ExitStack

import numpy as np
import concourse.bass as bass
import concourse.mybir as mybir
from concourse.bass_utils import run_bass_kernel_spmd

F32 = mybir.dt.float32
BF16 = mybir.dt.bfloat16
AF = mybir.ActivationFunctionType
ALU = mybir.AluOpType
AX = mybir.AxisListType

D = 1024
B = 2
S = 8192
NCORES = 8
DN_ALPHA = 4.0 ** 0.25
LN_EPS = 1e-5


class KB:
    NDMA = 12

    def __init__(self, nc, stack):
        self.nc = nc
        self.stack = stack
        self.engs = {"pe": nc.tensor, "act": nc.scalar, "dve": nc.vector,
                     "pool": nc.gpsimd, "sp": nc.sync}
        self.sems = {}
        self.cnt = {}
        for e in ["pe", "act", "dve", "pool"]:
            self.sems[e] = stack.enter_context(nc.semaphore("c_" + e))
            self.cnt[e] = 0
        self.dq = {}
        for q in ["sp", "act", "pool"]:
            for i in range(self.NDMA):
                nm = f"d_{q}{i}"
                self.sems[nm] = stack.enter_context(nc.semaphore(nm))
                self.cnt[nm] = 0
            self.dq[q] = 0
        self.waited = {e: {} for e in self.engs}
        self.last_w = {}
        self.readers = {}
        self.n_inst = 0
        self.limit = None

    def _need(self, eng, dep):
        sem, val = dep
        if eng == "pe" and sem == "pe":
            return
        if self.waited[eng].get(sem, 0) >= val:
            return
        self.engs[eng].wait_ge(self.sems[sem], val)
        self.waited[eng][sem] = val

    def _deps(self, eng, reads, writes):
        for k in reads:
            d = self.last_w.get(k)
            if d is not None:
                self._need(eng, d)
            if k.startswith("p"):
                for d in self.readers.get(k, ()):
                    if d[0] != eng:
                        self._need(eng, d)
        for k in writes:
            d = self.last_w.get(k)
            if d is not None:
                self._need(eng, d)
            for d in self.readers.get(k, ()):
                self._need(eng, d)

    def _commit(self, tok, reads, writes):
        for k in reads:
            self.readers.setdefault(k, []).append(tok)
        for k in writes:
            self.last_w[k] = tok
            self.readers[k] = []

    def op(self, eng, fn, reads=(), writes=()):
        if self.limit is not None and self.n_inst >= self.limit:
            return None
        self._deps(eng, reads, writes)
        ins = fn()
        self.cnt[eng] += 1
        ins.then_inc(self.sems[eng], 1)
        self._commit((eng, self.cnt[eng]), reads, writes)
        self.n_inst += 1
        return ins

    def dma(self, q, out, in_, reads=(), writes=(), **kw):
        if self.limit is not None and self.n_inst >= self.limit and not kw.pop("force", False):
            return None
        kw.pop("force", None)
        i = self.dq[q] % self.NDMA
        self.dq[q] += 1
        nm = f"d_{q}{i}"
        if self.cnt[nm] > 0:
            self._need(q, (nm, self.cnt[nm]))
        self._deps(q, reads, writes)
        ins = self.engs[q].dma_start(out=out, in_=in_, **kw)
        self.cnt[nm] += 16
        ins.then_inc(self.sems[nm], 16)
        self._commit((nm, self.cnt[nm]), reads, writes)
        self.n_inst += 1
        return ins

    def finish(self, keys, eng="sp"):
        for k in keys:
            d = self.last_w.get(k)
            if d is not None:
                self._need(eng, d)

    def barrier(self):
        for e in self.engs:
            for c, v in self.cnt.items():
                if v > 0:
                    self._need(e, (c, v))

    def mm(self, out, lhsT, rhs, start, stop, reads, writes):
        nc = self.nc
        return self.op("pe", lambda: nc.tensor.matmul(out, lhsT, rhs, start=start, stop=stop),
                       reads=reads, writes=writes)

    def act(self, out, in_, func, reads, writes, **kw):
        nc = self.nc
        return self.op("act", lambda: nc.scalar.activation(out, in_, func, **kw),
                       reads=reads, writes=writes)


class Ctx:
    def __init__(self, nc, kb, prefix="", over=None):
        self.nc, self.kb, self.prefix, self.over = nc, kb, prefix, dict(over or {})

    def din(self, name, shape, dt=F32):
        if name in self.over:
            return self.over[name]
        return self.nc.dram_tensor(self.prefix + name, list(shape), dt, kind="ExternalInput").ap()

    def dout(self, name, shape, dt=F32):
        if name in self.over:
            return self.over[name]
        return self.nc.dram_tensor(self.prefix + name, list(shape), dt, kind="ExternalOutput").ap()


def _standalone(emit, **kw):
    nc = bass.Bass("TRN2", target_bir_lowering=False)
    with ExitStack() as st0:
        kb = KB(nc, st0)
        kb.limit = kw.pop("limit", None)
        emit(Ctx(nc, kb), **kw)
    return nc


_UID = [0]


def _uniq(name):
    _UID[0] += 1
    return f"{name}_{_UID[0]}"


def sb(nc, st, name, shape, dt):
    return st.enter_context(nc.sbuf_tensor(_uniq(name), list(shape), dt))


def ps(nc, st, name, shape, dt=F32):
    return st.enter_context(nc.psum_tensor(_uniq(name), list(shape), dt))


GLA_DK = 128
GLA_DV = 256
GC = 128
TT = 512


def gla_consts():
    j = np.arange(128)[:, None]
    i = np.arange(128)[None, :]
    tri_i = np.where(j <= i, -1.0 / 16.0, 0.0).astype(np.float32)
    tri_u = np.where(j > i, -1.0 / 16.0, 0.0).astype(np.float32)
    mask = np.where(j <= i, 1.0, 0.0).astype(np.float32)
    return tri_i, tri_u, mask


def build_gla(n_tok=S, limit=None):
    return _standalone(emit_gla, n_tok=n_tok, limit=limit)


def emit_gla(ctx, n_tok=S):
    nc, kb = ctx.nc, ctx.kb
    limit = kb.limit
    xT = ctx.din("xT", [D, n_tok])
    wqk = ctx.din("wqk", [D, 256])
    wkvr = ctx.din("wkvr", [D, 640])
    wg = ctx.din("wg", [D, 16])
    wgu = ctx.din("wgu", [33, 128])
    normg = ctx.din("normg", [1, 256])
    tri_i_d = ctx.din("tri_i", [128, 128])
    tri_u_d = ctx.din("tri_u", [128, 128])
    mask_d = ctx.din("maskT", [128, 128])
    y = ctx.dout("y", [n_tok, 256], BF16)

    with ExitStack() as st:
        V, A = nc.vector, nc.scalar
        w_qk = sb(nc, st, "w_qk", [128, 8, 256], BF16)
        w_kvr = sb(nc, st, "w_kvr", [128, 8, 640], BF16)
        w_g = sb(nc, st, "w_g", [128, 8, 16], BF16)
        w_gu = sb(nc, st, "w_gu", [33, 128], F32)
        ng = sb(nc, st, "ng", [128, 256], F32)
        tri_i = sb(nc, st, "tri_i_s", [128, 128], F32)
        tri_u = sb(nc, st, "tri_u_s", [128, 128], F32)
        maskT = sb(nc, st, "mask_s", [128, 128], F32)
        xt = [sb(nc, st, f"xt{i}", [128, 8, TT], BF16) for i in range(2)]
        g_aug = sb(nc, st, "g_aug", [33, TT], F32)
        e1 = sb(nc, st, "e1", [128, 128], F32)
        la = sb(nc, st, "la", [128, 128], F32)
        eb = sb(nc, st, "eb", [128, 128], F32)
        enb = sb(nc, st, "enb", [128, 128], F32)
        w2 = sb(nc, st, "w2", [128, 128], F32)
        qdT = sb(nc, st, "qdT", [128, 128], BF16)
        kdT = sb(nc, st, "kdT", [128, 128], BF16)
        kd2 = sb(nc, st, "kd2", [128, 128], BF16)
        v_bf = sb(nc, st, "v_bf", [128, 256], BF16)
        atm = sb(nc, st, "atm", [128, 128], BF16)
        S_f = sb(nc, st, "S_f", [128, 256], F32)
        S_b = sb(nc, st, "S_b", [128, 256], BF16)
        junk = sb(nc, st, "junk", [128, 256], F32)
        ss = sb(nc, st, "ss", [128, 1], F32)
        rstd = sb(nc, st, "rstd", [128, 1], F32)
        er = sb(nc, st, "er", [128, 256], F32)
        rs = sb(nc, st, "rs", [128, 256], F32)
        on = sb(nc, st, "on", [128, 256], F32)
        yt = [sb(nc, st, f"yt{i}", [128, 256], BF16) for i in range(2)]

        p_q = ps(nc, st, "p_q", [128, TT])
        p_k = ps(nc, st, "p_k", [128, TT])
        p_g_full = ps(nc, st, "p_g", [128, TT])
        p_g = p_g_full[0:16, :]
        p_m = ps(nc, st, "p_m", [128, 512])
        p_kv = ps(nc, st, "p_kv", [128, 512])
        p_ro = ps(nc, st, "p_ro", [128, 512])
        p_st_full = ps(nc, st, "p_st", [128, 512])
        p_st = p_st_full[:, 0:256]

        kb.dma("pool", w_qk[:], wqk.rearrange("(kc p) n -> p kc n", p=128), writes=["w_qk"])
        kb.dma("pool", w_kvr[:], wkvr.rearrange("(kc p) n -> p kc n", p=128), writes=["w_kvr"])
        kb.dma("pool", w_g[:], wg.rearrange("(kc p) n -> p kc n", p=128), writes=["w_g"])
        kb.dma("sp", w_gu[:], wgu, writes=["w_gu"])
        kb.dma("sp", ng[:], normg.partition_broadcast(128), writes=["ng"])
        kb.dma("sp", tri_i[:], tri_i_d, writes=["tri_i"])
        kb.dma("sp", tri_u[:], tri_u_d, writes=["tri_u"])
        kb.dma("sp", maskT[:], mask_d, writes=["maskT"])
        kb.op("dve", lambda: V.memset(S_f[:], 0.0), writes=["S_f"])
        kb.op("dve", lambda: V.memset(S_b[:], 0.0), writes=["S_b"])
        kb.op("dve", lambda: V.memset(g_aug[:], 1.0), writes=["g_aug"])
        eps_t = sb(nc, st, "eps_t", [128, 1], F32)
        kb.op("dve", lambda: V.memset(eps_t[:], LN_EPS), writes=["eps_t"])

        xTv = xT.rearrange("(kc p) t -> p kc t", p=128)
        n_tiles = n_tok // TT
        for T in range(n_tiles):
            x_t = xt[T % 2]
            xk = f"xt{T % 2}"
            kb.dma("pool", x_t[:], xTv[:, :, T * TT:(T + 1) * TT], writes=[xk])
            for kc in range(8):
                kb.mm(p_q[:], w_qk[:, kc, 0:128], x_t[:, kc, :], kc == 0, kc == 7,
                      reads=["w_qk", xk], writes=["p_q"])
            for kc in range(8):
                kb.mm(p_k[:], w_qk[:, kc, 128:256], x_t[:, kc, :], kc == 0, kc == 7,
                      reads=["w_qk", xk], writes=["p_k"])
            for kc in range(8):
                kb.mm(p_g, w_g[:, kc, :], x_t[:, kc, :], kc == 0, kc == 7,
                      reads=["w_g", xk], writes=["p_g"])
            kb.op("dve", lambda: V.tensor_copy(g_aug[0:16, :], p_g), reads=["p_g"], writes=["g_aug"])
            for c in range(TT // GC):
                cs = slice(c * GC, (c + 1) * GC)
                kb.mm(p_m[:, 0:128], g_aug[:, cs], w_gu[:], True, True,
                      reads=["g_aug", "w_gu"], writes=["p_m"])
                kb.act(e1[:], p_m[:, 0:128], AF.Exp, reads=["p_m"], writes=["e1"], scale=-1.0)
                kb.act(la[:], e1[:], AF.Ln, reads=["e1"], writes=["la"], bias=1.0)
                kb.mm(p_m[:, 128:256], la[:], tri_i[:], True, True, reads=["la", "tri_i"], writes=["p_m"])
                kb.mm(p_m[:, 256:384], tri_u[:], la[:], True, True, reads=["la", "tri_u"], writes=["p_m"])
                kb.act(eb[:], p_m[:, 128:256], AF.Exp, reads=["p_m"], writes=["eb"])
                kb.act(enb[:], p_m[:, 128:256], AF.Exp, reads=["p_m"], writes=["enb"], scale=-1.0)
                kb.act(w2[:], p_m[:, 256:384], AF.Exp, reads=["p_m"], writes=["w2"])
                kb.op("dve", lambda: V.scalar_tensor_tensor(
                    out=qdT[:], in0=p_q[:, cs], scalar=float(GLA_DK ** -0.5), in1=eb[:],
                    op0=ALU.mult, op1=ALU.mult), reads=["p_q", "eb"], writes=["qdT"])
                kb.op("dve", lambda: V.tensor_tensor(out=kdT[:], in0=p_k[:, cs], in1=enb[:], op=ALU.mult),
                      reads=["p_k", "enb"], writes=["kdT"])
                for kc in range(8):
                    kb.mm(p_kv[:, 0:384], x_t[:, kc, cs], w_kvr[:, kc, 0:384], kc == 0, kc == 7,
                          reads=[xk, "w_kvr"], writes=["p_kv"])
                for kc in range(8):
                    kb.mm(p_ro[:, 0:256], x_t[:, kc, cs], w_kvr[:, kc, 384:640], kc == 0, kc == 7,
                          reads=[xk, "w_kvr"], writes=["p_ro"])
                kb.op("dve", lambda: V.tensor_tensor(out=kd2[:], in0=p_kv[:, 0:128], in1=w2[:], op=ALU.mult),
                      reads=["p_kv", "w2"], writes=["kd2"])
                kb.op("dve", lambda: V.tensor_copy(v_bf[:], p_kv[:, 128:384]), reads=["p_kv"], writes=["v_bf"])
                kb.mm(p_m[:, 384:512], kdT[:], qdT[:], True, True, reads=["kdT", "qdT"], writes=["p_m"])
                kb.op("dve", lambda: V.tensor_tensor(out=atm[:], in0=p_m[:, 384:512], in1=maskT[:], op=ALU.mult),
                      reads=["p_m", "maskT"], writes=["atm"])
                kb.mm(p_ro[:, 256:512], atm[:], v_bf[:], True, False, reads=["atm", "v_bf"], writes=["p_ro"])
                kb.mm(p_ro[:, 256:512], qdT[:], S_b[:], False, True, reads=["qdT", "S_b"], writes=["p_ro"])
                kb.mm(p_st, kd2[:], v_bf[:], True, True, reads=["kd2", "v_bf"], writes=["p_st"])
                kb.op("dve", lambda: V.scalar_tensor_tensor(
                    out=S_f[:], in0=S_f[:], scalar=eb[:, 127:128], in1=p_st,
                    op0=ALU.mult, op1=ALU.add), reads=["S_f", "eb", "p_st"], writes=["S_f"])
                kb.op("pool", lambda: nc.gpsimd.tensor_copy(S_b[:], S_f[:]), reads=["S_f"], writes=["S_b"])
                kb.act(junk[:], p_ro[:, 256:512], AF.Square, reads=["p_ro"], writes=["junk", "ss"],
                       scale=1.0 / 16.0, accum_out=ss[:])
                kb.act(rstd[:], ss[:], AF.Ln, reads=["ss"], writes=["rstd"], bias=eps_t[:])
                kb.act(rstd[:], rstd[:], AF.Exp, reads=["rstd"], writes=["rstd"], scale=-0.5)
                kb.act(er[:], p_ro[:, 0:256], AF.Exp, reads=["p_ro"], writes=["er"], scale=-1.0)
                kb.op("dve", lambda: V.tensor_scalar_add(out=er[:], in0=er[:], scalar1=1.0),
                      reads=["er"], writes=["er"])
                kb.op("dve", lambda: V.reciprocal(out=er[:], in_=er[:]), reads=["er"], writes=["er"])
                kb.op("dve", lambda: V.tensor_tensor(out=rs[:], in0=p_ro[:, 0:256], in1=er[:], op=ALU.mult),
                      reads=["p_ro", "er"], writes=["rs"])
                kb.op("dve", lambda: V.scalar_tensor_tensor(
                    out=on[:], in0=p_ro[:, 256:512], scalar=rstd[:, 0:1], in1=ng[:],
                    op0=ALU.mult, op1=ALU.mult), reads=["p_ro", "rstd", "ng"], writes=["on"])
                ci = T * (TT // GC) + c
                y_t = yt[ci % 2]
                yk = f"yt{ci % 2}"
                kb.op("pool", lambda: nc.gpsimd.tensor_tensor(out=y_t[:], in0=on[:], in1=rs[:], op=ALU.mult),
                      reads=["on", "rs"], writes=[yk])
                kb.dma("sp", y[ci * GC:(ci + 1) * GC, :], y_t[:], reads=[yk], writes=["y_out"])
        if limit is not None:
            kb.dma("sp", y[0:128, :], yt[0][:], reads=["yt0"], writes=["y_out"], force=True)
            print("n_inst", kb.n_inst)
        kb.barrier()


def gla_inputs(x, w_in, w_gate_up, b_gate, norm_g):
    tri_i, tri_u, mask = gla_consts()
    maps = []
    for c in range(NCORES):
        b, h = c // 4, c % 4
        q = w_in[:, h * 128:(h + 1) * 128]
        k = w_in[:, 512 + h * 128:512 + (h + 1) * 128]
        v = w_in[:, 1024 + h * 256:1024 + (h + 1) * 256]
        g = w_in[:, 2048:2064]
        r = w_in[:, 2064 + h * 256:2064 + (h + 1) * 256]
        wgu = np.zeros((33, 128), np.float32)
        wgu[0:16] = w_gate_up[:, h * 128:(h + 1) * 128]
        wgu[32] = b_gate[h * 128:(h + 1) * 128]
        maps.append({
            "xT": np.ascontiguousarray(x[b].T),
            "wqk": np.ascontiguousarray(np.concatenate([q, k], axis=1)),
            "wkvr": np.ascontiguousarray(np.concatenate([k, v, r], axis=1)),
            "wg": np.ascontiguousarray(g),
            "wgu": wgu,
            "normg": np.ascontiguousarray(norm_g.reshape(1, 256)),
            "tri_i": tri_i, "tri_u": tri_u, "maskT": mask,
        })
    return maps


NTB = 2048
NE = 16
DFF = 256


def moe_consts():
    sel = np.zeros((16, 16, 128), np.float32)
    for e in range(16):
        sel[e, e, :] = 1.0
    ident = np.eye(128, dtype=np.float32)
    return sel, ident


def build_ffn(n_tok=NTB, limit=None, n_exp=NE):
    return _standalone(emit_ffn, n_tok=n_tok, limit=limit, n_exp=n_exp)


def emit_ffn(ctx, n_tok=NTB, n_exp=NE, fused=None):
    nc, kb = ctx.nc, ctx.kb
    limit = kb.limit
    if fused is None:
        yT = ctx.din("yT", [D, n_tok], BF16)
    xres = ctx.din("xres", [n_tok, D])
    wout = ctx.din("wout", [D, D])
    lnp = ctx.din("lnp", [4, D])
    wr = ctx.din("wr", [D, NE])
    br = ctx.din("br", [1, NE])
    wgd = ctx.din("wg", [NE, D, DFF])
    wud = ctx.din("wu", [NE, D, DFF])
    wdd = ctx.din("wd", [NE, DFF, D])
    sel_d = ctx.din("sel", [16, 16, 128])
    ident_d = ctx.din("ident", [128, 128])
    out = ctx.dout("out", [n_tok, D]) if (fused is None or "x1_d" not in fused) else None
    n_sub = n_tok // 128
    n_tile = n_tok // 512

    with ExitStack() as st:
        V, A, G = nc.vector, nc.scalar, nc.gpsimd
        w_o = sb(nc, st, "w_o", [128, 8, D], BF16)
        lng = [sb(nc, st, f"lnp{i}", [128, D], F32) for i in range(4)]
        w_r = sb(nc, st, "w_r", [128, 8, NE], F32)
        b_r = sb(nc, st, "b_r", [128, NE], F32)
        sel = sb(nc, st, "sel_s", [16, 16, 128], BF16)
        ident = sb(nc, st, "ident_s", [128, 128], F32)
        eps_t = sb(nc, st, "eps_t", [128, 1], F32)
        acc = sb(nc, st, "acc", [128, n_sub, D], F32)
        x1T = sb(nc, st, "x1T", [128, 8, n_tok], BF16)
        gT = sb(nc, st, "gT", [16, n_tok], BF16)
        y_t = [sb(nc, st, "y_t0", [128, 8, 128], BF16)] * 2
        xr = [sb(nc, st, "xr0", [128, D], F32)] * 2
        u = sb(nc, st, "u", [128, D], F32)
        x1 = sb(nc, st, "x1", [128, D], F32)
        stats = sb(nc, st, "stats", [128, 2, 6], F32)
        mv = sb(nc, st, "mv", [128, 2], F32)
        rstd = sb(nc, st, "rstd", [128, 1], F32)
        xTf = sb(nc, st, "xTf", [128, 8, 128], F32)
        sg_ = sb(nc, st, "r_s", [128, 16], F32)
        bi_ = sb(nc, st, "r_bi", [128, 16], F32)
        b2_ = sb(nc, st, "r_b2", [128, 16], F32)
        eq_ = sb(nc, st, "r_eq", [128, 16], F32)
        m1_ = sb(nc, st, "r_m1", [128, 4], F32)
        m2_ = sb(nc, st, "r_m2", [128, 4], F32)
        gs_ = sb(nc, st, "r_gs", [128, 4], F32)
        gm_ = sb(nc, st, "r_gm", [128, 1], F32)
        ig_ = sb(nc, st, "r_ig", [128, 4], F32)
        se_ = sb(nc, st, "r_se", [128, 16], F32)
        ws_ = sb(nc, st, "r_ws", [128, 1], F32)
        gate = sb(nc, st, "gate", [128, 16], F32)
        wg_s = [sb(nc, st, f"wg_s{i}", [128, 8, DFF], BF16) for i in range(2)]
        wu_s = [sb(nc, st, f"wu_s{i}", [128, 8, DFF], BF16) for i in range(2)]
        wd_s = [sb(nc, st, f"wd_s{i}", [128, 2, D], BF16) for i in range(2)]
        gb = [sb(nc, st, f"gb{i}", [128, 512], BF16) for i in range(2)]
        sgl = [sb(nc, st, f"sgl{i}", [128, 512], BF16) for i in range(2)]
        t1 = [sb(nc, st, f"t1{i}", [128, 512], BF16) for i in range(2)]
        hT = [sb(nc, st, f"hT{i}", [128, 2, 512], BF16) for i in range(2)]
        ot = [sb(nc, st, "ot0", [128, D], F32)] * 2

        pb = [ps(nc, st, f"pb{i}", [128, 512]) for i in range(8)]
        PK = [f"pb{i}" for i in range(8)]

        kb.dma("pool", w_o[:], wout.rearrange("(kc p) n -> p kc n", p=128), writes=["w_o"])
        for i in range(4):
            kb.dma("sp", lng[i][:], lnp[i:i + 1, :].partition_broadcast(128), writes=[f"lnp{i}"])
        kb.dma("sp", w_r[:], wr.rearrange("(kc p) n -> p kc n", p=128), writes=["w_r"])
        kb.dma("sp", b_r[:], br.partition_broadcast(128), writes=["b_r"])
        kb.dma("pool", sel[:], sel_d, writes=["sel"])
        kb.dma("sp", ident[:], ident_d, writes=["ident"])
        kb.op("dve", lambda: V.memset(eps_t[:], LN_EPS), writes=["eps_t"])

        def load_expert(e):
            i = e % 2
            kb.dma("pool", wg_s[i][:], wgd[e].rearrange("(kc p) f -> p kc f", p=128), writes=[f"wg{i}"])
            kb.dma("pool", wu_s[i][:], wud[e].rearrange("(kc p) f -> p kc f", p=128), writes=[f"wu{i}"])
            kb.dma("pool", wd_s[i][:], wdd[e].rearrange("(fc p) d -> p fc d", p=128), writes=[f"wd{i}"])

        def layer_norm(src, dst, gi, eng2):
            s_t, s_k = src
            d_t, d_k = dst
            for hh in range(2):
                kb.op("dve", lambda: V.bn_stats(stats[:, hh, :], s_t[:, hh * 512:(hh + 1) * 512]),
                      reads=[s_k], writes=["stats"])
            kb.op("dve", lambda: V.bn_aggr(mv[:], stats[:]), reads=["stats"], writes=["mv"])
            kb.act(rstd[:], mv[:, 1:2], AF.Sqrt, reads=["mv"], writes=["rstd"], bias=eps_t[:])
            kb.op("dve", lambda: V.reciprocal(rstd[:], rstd[:]), reads=["rstd"], writes=["rstd"])
            kb.op("dve", lambda: V.tensor_scalar(out=d_t, in0=s_t, scalar1=mv[:, 0:1], scalar2=rstd[:, 0:1],
                                                 op0=ALU.subtract, op1=ALU.mult),
                  reads=[s_k, "mv", "rstd"], writes=[d_k])
            kb.op("pool", lambda: G.tensor_tensor(out=d_t, in0=d_t, in1=lng[gi][:], op=ALU.mult),
                  reads=[d_k, f"lnp{gi}"], writes=[d_k])
            kb.op("pool", lambda: G.tensor_tensor(out=d_t, in0=d_t, in1=lng[gi + 1][:], op=ALU.add),
                  reads=[d_k, f"lnp{gi + 1}"], writes=[d_k])

        load_expert(0)
        if fused is None:
            yTv = yT.rearrange("(kc p) t -> p kc t", p=128)
        else:
            g_v = [gq.rearrange("(h t) c -> t h c", h=4) for gq in fused["g"]]
            cand = [sb(nc, st, "cand0", [128, 4, D], BF16)] * 2
            ysel = sb(nc, st, "ysel", [128, D], BF16)
            qsel = sb(nc, st, "qsel_s", [128, 4], F32)
            identb = sb(nc, st, "identb", [128, 128], BF16)
            xo = [sb(nc, st, "xo0", [128, 8, 128], BF16)] * 2
            kb.dma("sp", qsel[:], fused["qsel"], writes=["qsel"])
            kb.dma("pool", identb[:], ident_d, writes=["identb"])
        for sub in range(n_sub):
            ts_ = slice(sub * 128, (sub + 1) * 128)
            yk = "y_t0"
            xk = "xr0"
            if fused is None:
                kb.dma("sp", y_t[sub % 2][:], yTv[:, :, ts_], writes=[yk])
            else:
                ck = "cand0"
                for qq in range(4):
                    kb.dma("sp", cand[sub % 2][:, qq, :].rearrange("p (h c) -> p h c", h=4),
                           g_v[qq][sub * 128:(sub + 1) * 128, :, :], writes=[ck])
                kb.op("pool", lambda: G.tensor_scalar(out=ysel[:], in0=cand[sub % 2][:, 0, :], scalar1=qsel[:, 0:1],
                                                      scalar2=None, op0=ALU.mult), reads=[ck, "qsel"], writes=["ysel"])
                for qq in range(1, 4):
                    kb.op("dve", lambda: V.scalar_tensor_tensor(out=ysel[:], in0=cand[sub % 2][:, qq, :],
                                                                 scalar=qsel[:, qq:qq + 1], in1=ysel[:],
                                                                 op0=ALU.mult, op1=ALU.add),
                          reads=[ck, "qsel", "ysel"], writes=["ysel"])
                pTb = pb[7][:].bitcast(BF16)
                for kc in range(8):
                    kb.op("pe", lambda: nc.tensor.transpose(pTb[:, kc * 128:(kc + 1) * 128],
                                                            ysel[:, kc * 128:(kc + 1) * 128], identb[:]),
                          reads=["ysel", "identb"], writes=[PK[7]])
                kb.op("dve", lambda: V.tensor_copy(y_t[sub % 2][:], pTb.rearrange("p (k t) -> p k t", k=8)),
                      reads=[PK[7]], writes=[yk])
            kb.dma("sp", xr[sub % 2][:], xres[ts_, :], writes=[xk])
            for hh in range(2):
                for kc in range(8):
                    kb.mm(pb[hh][:], y_t[sub % 2][:, kc, :], w_o[:, kc, hh * 512:(hh + 1) * 512],
                          kc == 0, kc == 7, reads=[yk, "w_o"], writes=[PK[hh]])
            for hh in range(2):
                kb.op("dve", lambda: V.scalar_tensor_tensor(
                    out=u[:, hh * 512:(hh + 1) * 512], in0=xr[sub % 2][:, hh * 512:(hh + 1) * 512],
                    scalar=float(DN_ALPHA), in1=pb[hh][:], op0=ALU.mult, op1=ALU.add),
                    reads=[xk, PK[hh]], writes=["u"])
            layer_norm((u[:], "u"), (x1[:], "x1"), 0, None)
            kb.op("pool", lambda: G.tensor_scalar(out=acc[:, sub, :], in0=x1[:], scalar1=float(DN_ALPHA),
                                                  scalar2=None, op0=ALU.mult),
                  reads=["x1"], writes=[f"acc{sub}"])
            for kc in range(8):
                bank = 2 + kc // 4
                kb.op("pe", lambda: nc.tensor.transpose(pb[bank][:, (kc % 4) * 128:(kc % 4 + 1) * 128],
                                                        x1[:, kc * 128:(kc + 1) * 128], ident[:]),
                      reads=["x1", "ident"], writes=[PK[bank]])
            for q in range(2):
                kb.op("dve" if q == 0 else "act",
                      (lambda: V.tensor_copy(xTf[:, 0:4, :], pb[2][:].rearrange("p (k t) -> p k t", k=4))) if q == 0
                      else (lambda: A.copy(xTf[:, 4:8, :], pb[3][:].rearrange("p (k t) -> p k t", k=4))),
                      reads=[PK[2 + q]], writes=[f"xTf{q}"])
            kb.op("pool", lambda: G.tensor_copy(x1T[:, :, ts_], xTf[:]), reads=["xTf0", "xTf1"], writes=["x1T"])
            for kc in range(8):
                kb.mm(pb[4][:, 0:16], xTf[:, kc, :], w_r[:, kc, :], kc == 0, kc == 7,
                      reads=["xTf0", "xTf1", "w_r"], writes=[PK[4]])
            kb.act(sg_[:], pb[4][:, 0:16], AF.Sigmoid, reads=[PK[4]], writes=["r_s"])
            kb.op("dve", lambda: V.tensor_tensor(out=bi_[:], in0=sg_[:], in1=b_r[:], op=ALU.add),
                  reads=["r_s", "b_r"], writes=["r_bi"])
            bi3 = bi_[:].rearrange("p (g e) -> p g e", g=4)
            kb.op("dve", lambda: V.tensor_reduce(out=m1_[:], in_=bi3, axis=AX.X, op=ALU.max),
                  reads=["r_bi"], writes=["r_m1"])
            kb.op("dve", lambda: V.tensor_tensor(out=eq_[:].rearrange("p (g e) -> p g e", g=4), in0=bi3,
                                                 in1=m1_[:].unsqueeze(2).to_broadcast([128, 4, 4]), op=ALU.is_equal),
                  reads=["r_bi", "r_m1"], writes=["r_eq"])
            kb.op("dve", lambda: V.scalar_tensor_tensor(out=b2_[:], in0=eq_[:], scalar=-1e30, in1=bi_[:],
                                                        op0=ALU.mult, op1=ALU.add),
                  reads=["r_eq", "r_bi"], writes=["r_b2"])
            kb.op("dve", lambda: V.tensor_reduce(out=m2_[:], in_=b2_[:].rearrange("p (g e) -> p g e", g=4),
                                                 axis=AX.X, op=ALU.max),
                  reads=["r_b2"], writes=["r_m2"])
            kb.op("dve", lambda: V.tensor_tensor(out=gs_[:], in0=m1_[:], in1=m2_[:], op=ALU.add),
                  reads=["r_m1", "r_m2"], writes=["r_gs"])
            kb.op("dve", lambda: V.tensor_reduce(out=gm_[:], in_=gs_[:], axis=AX.X, op=ALU.max),
                  reads=["r_gs"], writes=["r_gm"])
            kb.op("dve", lambda: V.tensor_scalar(out=ig_[:], in0=gs_[:], scalar1=gm_[:, 0:1], scalar2=None,
                                                 op0=ALU.is_ge),
                  reads=["r_gs", "r_gm"], writes=["r_ig"])
            kb.op("dve", lambda: V.tensor_tensor(out=se_[:].rearrange("p (g e) -> p g e", g=4), in0=bi3,
                                                 in1=m2_[:].unsqueeze(2).to_broadcast([128, 4, 4]), op=ALU.is_ge),
                  reads=["r_bi", "r_m2"], writes=["r_se"])
            kb.op("dve", lambda: V.tensor_tensor(out=se_[:].rearrange("p (g e) -> p g e", g=4),
                                                 in0=se_[:].rearrange("p (g e) -> p g e", g=4),
                                                 in1=ig_[:].unsqueeze(2).to_broadcast([128, 4, 4]), op=ALU.mult),
                  reads=["r_se", "r_ig"], writes=["r_se"])
            kb.op("dve", lambda: V.tensor_tensor(out=se_[:], in0=se_[:], in1=sg_[:], op=ALU.mult),
                  reads=["r_se", "r_s"], writes=["r_se"])
            kb.op("dve", lambda: V.tensor_reduce(out=ws_[:], in_=se_[:], axis=AX.X, op=ALU.add),
                  reads=["r_se"], writes=["r_ws"])
            kb.op("dve", lambda: V.reciprocal(ws_[:], ws_[:]), reads=["r_ws"], writes=["r_ws"])
            kb.op("dve", lambda: V.tensor_scalar(out=gate[:], in0=se_[:], scalar1=ws_[:, 0:1], scalar2=None,
                                                 op0=ALU.mult),
                  reads=["r_se", "r_ws"], writes=["gate"])
            kb.op("pe", lambda: nc.tensor.transpose(pb[5][0:16, 0:128], gate[:], ident[:]),
                  reads=["gate", "ident"], writes=[PK[5]])
            kb.op("dve", lambda: V.tensor_copy(gT[:, ts_], pb[5][0:16, 0:128]), reads=[PK[5]], writes=["gT"])

        for e in range(n_exp):
            i = e % 2
            if e + 1 < n_exp:
                load_expert(e + 1)
            for T in range(n_tile):
                Ts = slice(T * 512, (T + 1) * 512)
                j = (e * n_tile + T) % 2
                kb.mm(pb[4][:], sel[:, e, :], gT[:, Ts], True, True, reads=["sel", "gT"], writes=[PK[4]])
                kb.op("act", lambda: A.copy(gb[j][:], pb[4][:]), reads=[PK[4]], writes=[f"gb{j}"])
                for fc in range(2):
                    for kc in range(8):
                        kb.mm(pb[fc][:], wg_s[i][:, kc, fc * 128:(fc + 1) * 128], x1T[:, kc, Ts],
                              kc == 0, kc == 7, reads=[f"wg{i}", "x1T"], writes=[PK[fc]])
                    for kc in range(8):
                        kb.mm(pb[2 + fc][:], wu_s[i][:, kc, fc * 128:(fc + 1) * 128], x1T[:, kc, Ts],
                              kc == 0, kc == 7, reads=[f"wu{i}", "x1T"], writes=[PK[2 + fc]])
                for fc in range(2):
                    kb.act(sgl[fc][:], pb[fc][:], AF.Silu, reads=[PK[fc]], writes=[f"sgl{fc}"])
                    kb.op("dve", lambda: V.tensor_tensor(out=t1[fc][:], in0=sgl[fc][:], in1=pb[2 + fc][:], op=ALU.mult),
                          reads=[f"sgl{fc}", PK[2 + fc]], writes=[f"t1{fc}"])
                    kb.op("pool", lambda: G.tensor_tensor(out=hT[j][:, fc, :], in0=t1[fc][:], in1=gb[j][:], op=ALU.mult),
                          reads=[f"t1{fc}", f"gb{j}"], writes=[f"hT{j}"])
                for s4 in range(4):
                    sub = T * 4 + s4
                    for hh in range(2):
                        bank = 5 + (s4 * 2 + hh) % 3
                        for fc in range(2):
                            kb.mm(pb[bank][:], hT[j][:, fc, s4 * 128:(s4 + 1) * 128],
                                  wd_s[i][:, fc, hh * 512:(hh + 1) * 512], fc == 0, fc == 1,
                                  reads=[f"hT{j}", f"wd{i}"], writes=[PK[bank]])
                        kb.op("dve", lambda: V.tensor_tensor(out=acc[:, sub, hh * 512:(hh + 1) * 512],
                                                             in0=acc[:, sub, hh * 512:(hh + 1) * 512],
                                                             in1=pb[bank][:], op=ALU.add),
                              reads=[f"acc{sub}", PK[bank]], writes=[f"acc{sub}"])

        for sub in range(n_sub):
            o_t = ot[sub % 2]
            layer_norm((acc[:, sub, :], f"acc{sub}"), (o_t[:], "ot0"), 2, None)
            if out is not None:
                kb.dma("sp", out[sub * 128:(sub + 1) * 128, :], o_t[:], reads=["ot0"], writes=["out"], force=True)
            else:
                ok_ = "ot0"
                kb.dma("sp", fused["x1_d"][sub * 128:(sub + 1) * 128, :], o_t[:], reads=[ok_], writes=["x1_d"])
                for kc in range(8):
                    bank = 2 + kc // 4
                    kb.op("pe", lambda: nc.tensor.transpose(pb[bank][:, (kc % 4) * 128:(kc % 4 + 1) * 128],
                                                            o_t[:, kc * 128:(kc + 1) * 128], ident[:]),
                          reads=[ok_, "ident"], writes=[PK[bank]])
                xk_ = "xo0"
                kb.op("dve", lambda: V.tensor_copy(xo[sub % 2][:, 0:4, :], pb[2][:].rearrange("p (k t) -> p k t", k=4)),
                      reads=[PK[2]], writes=[xk_])
                kb.op("dve", lambda: V.tensor_copy(xo[sub % 2][:, 4:8, :], pb[3][:].rearrange("p (k t) -> p k t", k=4)),
                      reads=[PK[3]], writes=[xk_])
                kb.dma("sp", fused["x1T_d"].rearrange("(kc p) t -> p kc t", p=128)[:, :, sub * 128:(sub + 1) * 128],
                       xo[sub % 2][:], reads=[xk_], writes=["x1T_d"])
        kb.barrier()


def ffn_inputs(y_tok_major, xres, w_out, ln_g, ln_b, w_router, b_router, w_gate, w_up, w_down):
    sel, ident = moe_consts()
    lnp = np.ascontiguousarray(np.stack([ln_g[0], ln_b[0], ln_g[1], ln_b[1]]).astype(np.float32))
    maps = []
    for c in range(NCORES):
        rs_ = slice(c * NTB, (c + 1) * NTB)
        maps.append({
            "yT": np.ascontiguousarray(y_tok_major[rs_].T),
            "xres": np.ascontiguousarray(xres[rs_]),
            "wout": w_out, "lnp": lnp, "wr": w_router,
            "br": np.ascontiguousarray(b_router.reshape(1, NE)),
            "wg": w_gate, "wu": w_up, "wd": w_down, "sel": sel, "ident": ident,
        })
    return maps


NSA_SCALE = 0.125
MASK_NEG = -240000.0


def nsa_consts(n_tok=S):
    nqb = n_tok // 128
    inv = np.power(500000.0, -np.arange(8, dtype=np.float32) * (2.0 / 16.0)).astype(np.float32)
    def cs(pos):
        ang = pos.astype(np.float32)[:, None] * inv[None, :]
        return np.concatenate([np.cos(ang), np.sin(ang)], axis=1).astype(np.float32)
    def pl(a):
        n = a.shape[0] // 128
        return np.ascontiguousarray(a.reshape(n, 128, 16).transpose(1, 0, 2).reshape(128, n * 16))
    cs_tok = pl(cs(np.arange(n_tok)))
    cs_cmp = pl(cs(np.arange(512) * 16 + 31))
    wimp = np.zeros((512, 128), np.float32)
    for s_ in range(128):
        for o, wgt in enumerate([1, 2, 2, 2, 1]):
            c = 4 * s_ + o
            if c < 511:
                wimp[c, s_] = wgt
    texp = (np.arange(n_tok)[None, :] // 64 == np.arange(128)[:, None]).astype(np.float32)
    k = np.arange(128)[:, None]
    q = np.arange(128)[None, :]
    causal = (k <= q).astype(np.float32)
    strict = (k > q).astype(np.float32)
    cmask = np.zeros((nqb, 2, 128, 128), np.float32)
    for qi in range(nqb):
        jl = (8 * qi + 6) // 128
        for slot, jt in ((0, jl - 1), (1, jl)):
            if jt < 0:
                continue
            j = 128 * jt + k
            cmask[qi, slot] = (16 * j + 31 <= 128 * qi + q)
    ident = np.eye(128, dtype=np.float32)
    return dict(cs_tok=cs_tok, cs_cmp=cs_cmp, wimp=wimp, texp=texp, causal=causal, strict=strict,
                cmask=cmask, ident=ident)


def build_nsa(n_tok=S, limit=None):
    return _standalone(emit_nsa, n_tok=n_tok, limit=limit)


def emit_nsa(ctx, n_tok=S, g2=None):
    nc, kb = ctx.nc, ctx.kb
    limit = kb.limit
    nqb = n_tok // 128
    ncb = n_tok // 16 - 1
    ncp = ((ncb + 127) // 128) * 128
    njt_all = ncp // 128
    din = ctx.din
    xT = din("xT", [D, n_tok]) if g2 is None else None
    wn = din("wn", [D, 652])
    cs_tok_d = din("cs_tok", [128, nqb * 16])
    cs_cmp_d = din("cs_cmp", [128, 64])
    wimp_d = din("wimp", [512, 128])
    texp_d = din("texp", [128, n_tok])
    causal_d = din("causal", [128, 128])
    strict_d = din("strict", [128, 128])
    cmask_d = din("cmask", [nqb, 2, 128, 128])
    ident_d = din("ident", [128, 128])
    wk1_d = din("wk1", [2048, 256])
    wk2_d = din("wk2", [256, 64])
    wv1_d = din("wv1", [2048, 256])
    wv2_d = din("wv2", [256, 64])
    peT_d = din("peT", [64, 32])
    y = ctx.dout("y", [n_tok, 256], BF16)

    with ExitStack() as st:
        V, A, G = nc.vector, nc.scalar, nc.gpsimd
        identb = sb(nc, st, "identb", [128, 128], BF16)
        QT = sb(nc, st, "QT", [64, 4, n_tok], BF16)
        KST = sb(nc, st, "KST", [64, n_tok], BF16)
        KWT = sb(nc, st, "KWT", [64, n_tok], BF16)
        VS = sb(nc, st, "VS", [128, nqb, 65], BF16)
        VW = sb(nc, st, "VW", [128, nqb, 65], BF16)
        G_all = sb(nc, st, "G_all", [128, nqb, 12], F32)
        KCMT = sb(nc, st, "KCMT", [64, ncp], BF16)
        RC = sb(nc, st, "RC", [128, njt_all, 193], BF16)
        st1 = ExitStack()
        w_n = sb(nc, st1, "w_n", [128, 8, 652], BF16)
        cs_tok = sb(nc, st1, "cs_tok_s", [128, nqb, 16], F32)
        cs_cmp = sb(nc, st1, "cs_cmp_s", [128, 4, 16], F32)
        KCT = sb(nc, st1, "KCT", [64, n_tok], BF16)
        VCT = sb(nc, st1, "VCT", [64, n_tok], BF16)
        xt = [sb(nc, st1, f"xt{i}", [128, 8, 128], BF16) for i in range(2)]
        pr = sb(nc, st1, "pr", [128, 652], F32)
        rp = sb(nc, st1, "rp", [128, 8, 64], BF16)
        ra = sb(nc, st1, "ra", [128, 6, 8], F32)
        rb = sb(nc, st1, "rb", [128, 6, 8], F32)
        w1 = sb(nc, st1, "w1", [64, 32, 256], BF16)
        w2 = sb(nc, st1, "w2", [128, 2, 64], BF16)
        peT = sb(nc, st1, "peT_s", [64, 32], BF16)
        hb = sb(nc, st1, "hb", [128, 2], F32)
        h1T = sb(nc, st1, "h1T", [128, 2, ncp], BF16)
        kc_f = sb(nc, st1, "kc_f", [128, 64], F32)
        kc_b = sb(nc, st1, "kc_b", [128, 64], BF16)

        pA = ps(nc, st, "pA", [128, 512])
        pB = ps(nc, st, "pB", [128, 512])
        pT = ps(nc, st, "pT", [128, 1024], BF16)
        pS = [ps(nc, st, f"pS{i}", [128, 512]) for i in range(3)]
        pC = pA
        pSL = ps(nc, st, "pSL", [128, 512])
        pW = ps(nc, st, "pW", [128, 512])

        kb.dma("pool", w_n[:], wn.rearrange("(kc p) n -> p kc n", p=128), writes=["w_n"])
        kb.dma("sp", cs_tok[:], cs_tok_d.rearrange("p (n c) -> p n c", c=16), writes=["cs_tok"])
        kb.dma("sp", cs_cmp[:], cs_cmp_d.rearrange("p (n c) -> p n c", c=16), writes=["cs_cmp"])
        kb.dma("pool", identb[:], ident_d, writes=["identb"])
        kb.dma("pool", peT[:], peT_d, writes=["peT"])
        kb.op("dve", lambda: V.memset(VS[:, :, 64:65], 1.0), writes=["VS"])
        kb.op("dve", lambda: V.memset(VW[:, :, 64:65], 1.0), writes=["VW"])
        kb.op("dve", lambda: V.memset(RC[:, :, 64:65], 1.0), writes=["RC"])
        kb.op("dve", lambda: V.memset(h1T[:], 0.0), writes=["h1T"])
        kb.dma("pool", RC[:, :, 65:193], wimp_d[0:ncp, :].rearrange("(n p) s -> p n s", p=128), writes=["RC"])

        if g2 is None:
            xTv = xT.rearrange("(kc p) t -> p kc t", p=128)
        else:
            g2v = [gj.rearrange("(q k2 p) t -> q p k2 t", q=4, k2=2, p=128) for gj in g2]

        def rope(src3, dst3, cs_ap, nh, csk):
            cosb = cs_ap[:, 0:8].unsqueeze(1).to_broadcast([128, nh, 8])
            sinb = cs_ap[:, 8:16].unsqueeze(1).to_broadcast([128, nh, 8])
            a_, b_ = ra[:, 0:nh, :], rb[:, 0:nh, :]
            kb.op("pool", lambda: G.tensor_tensor(out=a_, in0=src3[:, :, 0:8], in1=cosb, op=ALU.mult),
                  reads=["rsrc", csk], writes=["ra"])
            kb.op("pool", lambda: G.tensor_tensor(out=b_, in0=src3[:, :, 8:16], in1=sinb, op=ALU.mult),
                  reads=["rsrc", csk], writes=["rb"])
            kb.op("pool", lambda: G.tensor_tensor(out=dst3[:, :, 0:8], in0=a_, in1=b_, op=ALU.subtract),
                  reads=["ra", "rb"], writes=["rdst"])
            kb.op("pool", lambda: G.tensor_tensor(out=a_, in0=src3[:, :, 8:16], in1=cosb, op=ALU.mult),
                  reads=["rsrc", csk, "rdst"], writes=["ra"])
            kb.op("pool", lambda: G.tensor_tensor(out=b_, in0=src3[:, :, 0:8], in1=sinb, op=ALU.mult),
                  reads=["rsrc", csk, "rdst"], writes=["rb"])
            kb.op("pool", lambda: G.tensor_tensor(out=dst3[:, :, 8:16], in0=a_, in1=b_, op=ALU.add),
                  reads=["ra", "rb"], writes=["rdst"])
            kb.op("pool", lambda: G.tensor_copy(dst3[:, :, 16:64], src3[:, :, 16:64]),
                  reads=["rsrc"], writes=["rdst"])

        for T in range(nqb):
            Ts = slice(T * 128, (T + 1) * 128)
            x_t = xt[T % 2]
            xk = f"xt{T % 2}"
            if g2 is None:
                kb.dma("pool", x_t[:], xTv[:, :, Ts], writes=[xk])
            else:
                qq, tl = T // 16, T % 16
                for j in range(4):
                    kb.dma("sp", x_t[:, 2 * j:2 * j + 2, :], g2v[j][qq][:, :, tl * 128:(tl + 1) * 128], writes=[xk])
            for kc in range(8):
                kb.mm(pA[:], x_t[:, kc, :], w_n[:, kc, 0:512], kc == 0, kc == 7, reads=[xk, "w_n"], writes=["pA"])
            for kc in range(8):
                kb.mm(pB[:, 0:140], x_t[:, kc, :], w_n[:, kc, 512:652], kc == 0, kc == 7,
                      reads=[xk, "w_n"], writes=["pB"])
            kb.op("dve", lambda: V.tensor_copy(pr[:, 0:512], pA[:]), reads=["pA", "rdst"], writes=["rsrc"])
            kb.op("dve", lambda: V.tensor_copy(pr[:, 512:640], pB[:, 0:128]), reads=["pB"], writes=["prv"])
            kb.act(G_all[:, T, :], pB[:, 128:140], AF.Sigmoid, reads=["pB"], writes=["G_all"])
            kb.op("pool", lambda: G.tensor_copy(VS[:, T, 0:64], pr[:, 512:576]), reads=["prv"], writes=["VS"])
            kb.op("pool", lambda: G.tensor_copy(VW[:, T, 0:64], pr[:, 576:640]), reads=["prv"], writes=["VW"])
            rope(pr[:, 0:384].rearrange("p (s d) -> p s d", s=6), rp[:, 0:6, :], cs_tok[:, T, :], 6, "cs_tok")
            kb.op("pool", lambda: G.tensor_copy(rp[:, 6:8, :], pr[:, 384:512].rearrange("p (s d) -> p s d", s=2)),
                  reads=["rsrc"], writes=["rdst"])
            for s_ in range(8):
                kb.op("pe", lambda: nc.tensor.transpose(pT[0:64, s_ * 128:(s_ + 1) * 128], rp[:, s_, :], identb[:]),
                      reads=["rdst", "identb"], writes=["pT"])
            kb.op("dve", lambda: V.tensor_copy(QT[:, :, Ts], pT[0:64, 0:512].rearrange("p (h t) -> p h t", h=4)),
                  reads=["pT"], writes=["QT"])
            kb.op("dve", lambda: V.tensor_copy(KST[:, Ts], pT[0:64, 512:640]), reads=["pT"], writes=["KST"])
            kb.op("dve", lambda: V.tensor_copy(KWT[:, Ts], pT[0:64, 640:768]), reads=["pT"], writes=["KWT"])
            kb.op("dve", lambda: V.tensor_copy(KCT[:, Ts], pT[0:64, 768:896]), reads=["pT"], writes=["KCT"])
            kb.op("dve", lambda: V.tensor_copy(VCT[:, Ts], pT[0:64, 896:1024]), reads=["pT"], writes=["VCT"])

        for which, (w1d, w2d, srcT, srck) in enumerate(((wk1_d, wk2_d, KCT, "KCT"), (wv1_d, wv2_d, VCT, "VCT"))):
            kb.dma("pool", w1[:], w1d.rearrange("(r d) h -> d r h", d=64), writes=["w1"])
            kb.dma("pool", w2[:], w2d.rearrange("(hc p) d -> p hc d", p=128), writes=["w2"])
            for hc in range(2):
                for r in range(32):
                    kb.mm(pS[0][:, 0:1], w1[:, r, hc * 128:(hc + 1) * 128], peT[:, r:r + 1], r == 0, r == 31,
                          reads=["w1", "peT"], writes=["pS0"])
                kb.op("dve", lambda: V.tensor_copy(hb[:, hc:hc + 1], pS[0][:, 0:1]), reads=["pS0"], writes=["hb"])
            for hc in range(2):
                for c0 in range(0, ncb, 512):
                    n_ = min(512, ncb - c0)
                    for r in range(32):
                        kb.mm(pS[1][:, 0:n_], w1[:, r, hc * 128:(hc + 1) * 128],
                              srcT[:, 16 * c0 + r:16 * c0 + r + 16 * (n_ - 1) + 1:16], r == 0, r == 31,
                              reads=["w1", srck], writes=["pS1"])
                    kb.act(h1T[:, hc, c0:c0 + n_], pS[1][:, 0:n_], AF.Silu, reads=["pS1", "hb"], writes=["h1T"],
                           bias=hb[:, hc:hc + 1])
            for jt in range(njt_all):
                for hc in range(2):
                    kb.mm(pS[2][:, 0:64], h1T[:, hc, jt * 128:(jt + 1) * 128], w2[:, hc, :], hc == 0, hc == 1,
                          reads=["h1T", "w2"], writes=["pS2"])
                if which == 0:
                    kb.op("dve", lambda: V.tensor_copy(kc_f[:], pS[2][:, 0:64]), reads=["pS2", "rdst"], writes=["rsrc"])
                    rope(kc_f[:].unsqueeze(1), kc_b[:].unsqueeze(1), cs_cmp[:, jt, :], 1, "cs_cmp")
                    kb.op("pe", lambda: nc.tensor.transpose(pT[0:64, 0:128], kc_b[:], identb[:]),
                          reads=["rdst", "identb"], writes=["pT"])
                    kb.op("dve", lambda: V.tensor_copy(KCMT[:, jt * 128:(jt + 1) * 128], pT[0:64, 0:128]),
                          reads=["pT"], writes=["KCMT"])
                else:
                    kb.op("dve", lambda: V.tensor_copy(RC[:, jt, 0:64], pS[2][:, 0:64]), reads=["pS2"], writes=["RC"])

        if limit is not None:
            print("n_inst after stage 2:", kb.n_inst)
        kb.barrier()
        st1.close()
        texp = sb(nc, st, "texp_s", [128, n_tok], BF16)
        causal = sb(nc, st, "causal_s", [128, 128], BF16)
        strict = sb(nc, st, "strict_s", [128, 128], BF16)
        e_sb = [sb(nc, st, f"e_sb{i}", [128, 512], BF16) for i in range(3)]
        cm_sb = [sb(nc, st, f"cm_sb{i}", [128, 2, 128], BF16) for i in range(2)]
        impm = sb(nc, st, "impm", [128, 128], F32)
        impw = sb(nc, st, "impw", [128, 128], F32)
        m8 = sb(nc, st, "m8", [128, 16], F32)
        selb = sb(nc, st, "selb", [128, 128], BF16)
        nsT = sb(nc, st, "nsT", [128, 512], BF16)
        zz = sb(nc, st, "zz", [128, 12], F32)
        coef = sb(nc, st, "coef", [128, 12], F32)
        o_acc = sb(nc, st, "o_acc", [128, 256], F32)
        y_sb = [sb(nc, st, f"y_sb{i}", [128, 256], BF16) for i in range(2)]
        kb.dma("pool", texp[:], texp_d, writes=["texp"])
        kb.dma("pool", causal[:], causal_d, writes=["causal"])
        kb.dma("pool", strict[:], strict_d, writes=["strict"])

        def exp_tile(bank, ei):
            kb.act(e_sb[ei][:], pS[bank][:], AF.Exp, reads=[f"pS{bank}"], writes=[f"e{ei}"], scale=NSA_SCALE)

        def mask_tile(ei, mask_ap, mkeys):
            e3 = e_sb[ei][:].rearrange("p (h q) -> p h q", h=4)
            kb.op("pool", lambda: G.tensor_tensor(out=e3, in0=e3, in1=mask_ap.unsqueeze(1).to_broadcast([128, 4, 128]),
                                                  op=ALU.mult),
                  reads=[f"e{ei}"] + mkeys, writes=[f"e{ei}"])

        rot = [0]

        def nxt():
            rot[0] += 1
            return rot[0] % 3

        for qi in range(nqb):
            Qs = slice(qi * 128, (qi + 1) * 128)
            Qv = QT[:, :, Qs]
            jl = (8 * qi + 6) // 128
            cmk = f"cm{qi % 2}"
            kb.dma("pool", cm_sb[qi % 2][:], cmask_d[qi].rearrange("s j q -> j s q"), writes=[cmk])
            for jt in range(jl + 1):
                r_ = nxt()
                kb.mm(pS[r_][:], KCMT[:, jt * 128:(jt + 1) * 128], Qv, True, True, reads=["KCMT", "QT"], writes=[f"pS{r_}"])
                exp_tile(r_, r_)
                if jt >= jl - 1:
                    mask_tile(r_, cm_sb[qi % 2][:, 1 - (jl - jt), :], [cmk])
                for h in range(4):
                    bank = pA if h < 2 else pB
                    kb.mm(bank[:, (h % 2) * 193:(h % 2) * 193 + 193], e_sb[r_][:, h * 128:(h + 1) * 128], RC[:, jt, :],
                          jt == 0 and h % 2 == 0, jt == jl and h % 2 == 1, reads=[f"e{r_}", "RC"], writes=["pA" if h < 2 else "pB"])
            for h in range(4):
                bank = pA if h < 2 else pB
                c0 = (h % 2) * 193
                kb.op("dve", lambda: V.tensor_scalar_max(out=zz[:, h:h + 1], in0=bank[:, c0 + 64:c0 + 65], scalar1=1e-30),
                      reads=["pA" if h < 2 else "pB"], writes=["zz"])
            kb.op("dve", lambda: V.reciprocal(zz[:, 0:4], zz[:, 0:4]), reads=["zz"], writes=["zz"])
            Gq = G_all[:, qi, :].rearrange("p (h j) -> p h j", h=4)
            kb.op("dve", lambda: V.tensor_tensor(out=coef[:, 0:4], in0=zz[:, 0:4], in1=Gq[:, :, 0], op=ALU.mult),
                  reads=["zz", "G_all"], writes=["coef"])
            for h in range(4):
                bank = pA if h < 2 else pB
                bk = "pA" if h < 2 else "pB"
                c0 = (h % 2) * 193
                kb.op("dve", lambda: V.tensor_scalar(out=o_acc[:, h * 64:(h + 1) * 64], in0=bank[:, c0:c0 + 64],
                                                     scalar1=coef[:, h:h + 1], scalar2=None, op0=ALU.mult),
                      reads=[bk, "coef"], writes=["o_acc"])
                if qi >= 8:
                    if h == 0:
                        kb.op("dve", lambda: V.tensor_scalar(out=impm[:], in0=bank[:, c0 + 65:c0 + 193],
                                                             scalar1=zz[:, 0:1], scalar2=None, op0=ALU.mult),
                              reads=[bk, "zz"], writes=["impm"])
                    else:
                        kb.op("dve", lambda: V.scalar_tensor_tensor(out=impm[:], in0=bank[:, c0 + 65:c0 + 193],
                                                                    scalar=zz[:, h:h + 1], in1=impm[:],
                                                                    op0=ALU.mult, op1=ALU.add),
                              reads=[bk, "zz", "impm"], writes=["impm"])
            use_sel = qi >= 8
            if use_sel:
                c2 = 2 * qi
                kb.op("dve", lambda: V.memset(impm[:, 0:1], 3e30), reads=[], writes=["impm"])
                kb.op("dve", lambda: V.memset(impm[:, c2:c2 + 1], 2e30), writes=["impm"])
                kb.op("dve", lambda: V.memset(impm[0:64, c2 - 1:c2], 1e30), writes=["impm"])
                kb.op("dve", lambda: V.memset(impm[0:64, c2 + 1:c2 + 2], -1e30), writes=["impm"])
                kb.op("dve", lambda: V.memset(impm[64:128, c2 + 1:c2 + 2], 2.5e30), writes=["impm"])
                if c2 + 2 < 128:
                    kb.op("dve", lambda: V.memset(impm[:, c2 + 2:128], -1e30), writes=["impm"])
                kb.op("dve", lambda: V.max(out=m8[:, 0:8], in_=impm[:]), reads=["impm"], writes=["m8"])
                kb.op("dve", lambda: V.match_replace(out=impw[:], in_to_replace=m8[:, 0:8], in_values=impm[:],
                                                     imm_value=-1e30), reads=["impm", "m8"], writes=["impw"])
                kb.op("dve", lambda: V.max(out=m8[:, 8:16], in_=impw[:]), reads=["impw"], writes=["m8"])
                kb.op("dve", lambda: V.tensor_scalar(out=selb[:], in0=impm[:], scalar1=m8[:, 15:16], scalar2=MASK_NEG,
                                                     op0=ALU.is_lt, op1=ALU.mult),
                      reads=["impm", "m8"], writes=["selb"])
                kb.op("pe", lambda: nc.tensor.transpose(pT[:, 0:128], selb[:], identb[:]),
                      reads=["selb", "identb"], writes=["pT"])
                kb.op("dve", lambda: V.tensor_copy(nsT[:].rearrange("p (h q) -> p h q", h=4),
                                                   pT[:, 0:128].unsqueeze(1).to_broadcast([128, 4, 128])),
                      reads=["pT"], writes=["nsT"])
            for kt in range(qi + 1):
                r_ = nxt()
                Ks = slice(kt * 128, (kt + 1) * 128)
                kb.mm(pS[r_][:], KST[:, Ks], Qv, True, not use_sel, reads=["KST", "QT"], writes=[f"pS{r_}"])
                if use_sel:
                    kb.mm(pS[r_][:], texp[:, Ks], nsT[:], False, True, reads=["texp", "nsT"], writes=[f"pS{r_}"])
                exp_tile(r_, r_)
                if kt == qi:
                    mask_tile(r_, causal[:], ["causal"])
                for h in range(4):
                    kb.mm(pSL[:, h * 65:(h + 1) * 65], e_sb[r_][:, h * 128:(h + 1) * 128], VS[:, kt, :],
                          kt == 0 and h == 0, kt == qi and h == 3, reads=[f"e{r_}", "VS"], writes=["pSL"])
            k0 = max(0, qi - 4)
            for kt in range(k0, qi + 1):
                r_ = nxt()
                Ks = slice(kt * 128, (kt + 1) * 128)
                kb.mm(pS[r_][:], KWT[:, Ks], Qv, True, True, reads=["KWT", "QT"], writes=[f"pS{r_}"])
                exp_tile(r_, r_)
                if kt == qi:
                    mask_tile(r_, causal[:], ["causal"])
                elif kt == qi - 4:
                    mask_tile(r_, strict[:], ["strict"])
                for h in range(4):
                    kb.mm(pW[:, h * 65:(h + 1) * 65], e_sb[r_][:, h * 128:(h + 1) * 128], VW[:, kt, :],
                          kt == k0 and h == 0, kt == qi and h == 3, reads=[f"e{r_}", "VW"], writes=["pW"])
            for bi, (bank, bk) in enumerate(((pSL, "pSL"), (pW, "pW"))):
                b3 = bank[:, 0:260].rearrange("p (h c) -> p h c", h=4)
                kb.op("dve", lambda: V.reciprocal(zz[:, 4 + 4 * bi:8 + 4 * bi], b3[:, :, 64]), reads=[bk], writes=["zz"])
                kb.op("dve", lambda: V.tensor_tensor(out=coef[:, 4 + 4 * bi:8 + 4 * bi], in0=zz[:, 4 + 4 * bi:8 + 4 * bi],
                                                     in1=Gq[:, :, 1 + bi], op=ALU.mult),
                      reads=["zz", "G_all"], writes=["coef"])
                for h in range(4):
                    kb.op("dve", lambda: V.scalar_tensor_tensor(
                        out=o_acc[:, h * 64:(h + 1) * 64], in0=b3[:, h, 0:64],
                        scalar=coef[:, 4 + 4 * bi + h:5 + 4 * bi + h], in1=o_acc[:, h * 64:(h + 1) * 64],
                        op0=ALU.mult, op1=ALU.add), reads=[bk, "coef", "o_acc"], writes=["o_acc"])
            yk = f"y_sb{qi % 2}"
            kb.op("pool", lambda: G.tensor_copy(y_sb[qi % 2][:], o_acc[:]), reads=["o_acc"], writes=[yk])
            kb.dma("sp", y[Qs, :], y_sb[qi % 2][:], reads=[yk], writes=["y_out"], force=True)
            if limit is not None:
                print("n_inst after qi", qi, kb.n_inst)
        kb.barrier()


def nsa_inputs(x1, w_in, w_ck1, w_ck2, w_cv1, w_cv2, cmp_pe, n_tok=S):
    cst = nsa_consts(n_tok)
    maps = []
    for c in range(NCORES):
        b, g = c // 4, c % 4
        def col(base, width=64, mult=64):
            return w_in[:, base + g * mult: base + g * mult + width]
        q = w_in[:, g * 256:(g + 1) * 256]
        kc, vc, ks, vs, kw, vw = (col(1024), col(1280), col(1536), col(1792), col(2048), col(2304))
        gt = w_in[:, 2560 + g * 12:2560 + (g + 1) * 12]
        wn = np.ascontiguousarray(np.concatenate([q, ks, kw, kc, vc, vs, vw, gt], axis=1))
        m = {"xT": np.ascontiguousarray(x1[b].T[:, :n_tok]), "wn": wn,
             "wk1": w_ck1, "wk2": w_ck2, "wv1": w_cv1, "wv2": w_cv2,
             "peT": np.ascontiguousarray(cmp_pe.T)}
        m.update(cst)
        maps.append(m)
    return maps


GROUPS = [[0, 1, 2, 3], [4, 5, 6, 7]]


def build_fused():
    nc = bass.Bass("TRN2", target_bir_lowering=False)
    with ExitStack() as st0:
        kb = KB(nc, st0)
        cc_sem = st0.enter_context(nc.semaphore("cc_sem"))
        n_cc = [0]
        internal = lambda name, shape, dt: nc.dram_tensor(name, list(shape), dt).ap()
        y0_d = internal("y0_d", [S, 256], BF16)
        g1 = [internal(f"g1_{j}", [4 * NTB, 256], BF16) for j in range(4)]
        x1_d = internal("x1_d", [NTB, D], F32)
        x1T_d = internal("x1T_d", [D, NTB], BF16)
        g2 = [internal(f"g2_{j}", [4 * 256, NTB], BF16) for j in range(4)]
        y1_d = internal("y1_d", [S, 256], BF16)
        g3 = [internal(f"g3_{j}", [4 * NTB, 256], BF16) for j in range(4)]
        qsel = nc.dram_tensor("qsel", [128, 4], F32, kind="ExternalInput").ap()

        def all_gather(srcs, dsts):
            kb.barrier()
            for s_, d_ in zip(srcs, dsts):
                n_cc[0] += 1
                nc.gpsimd.collective_compute("AllGather", ALU.bypass, replica_groups=GROUPS,
                                             ins=[s_], outs=[d_]).then_inc(cc_sem, 1)
            for e in kb.engs.values():
                e.wait_ge(cc_sem, n_cc[0])

        emit_gla(Ctx(nc, kb, "a0_", {"y": y0_d}))
        all_gather([y0_d[j * NTB:(j + 1) * NTB, :] for j in range(4)], g1)
        emit_ffn(Ctx(nc, kb, "b0_"), fused={"g": g1, "qsel": qsel, "x1_d": x1_d, "x1T_d": x1T_d})
        all_gather([x1T_d[j * 256:(j + 1) * 256, :] for j in range(4)], g2)
        emit_nsa(Ctx(nc, kb, "a1_", {"y": y1_d}), g2=g2)
        all_gather([y1_d[j * NTB:(j + 1) * NTB, :] for j in range(4)], g3)
        emit_ffn(Ctx(nc, kb, "b1_", {"xres": x1_d}), fused={"g": g3, "qsel": qsel})
        kb.barrier()
    return nc


_PROGS = {}


def _prog(name, fn):
    if name not in _PROGS:
        _PROGS[name] = fn()
    return _PROGS[name]


def _run(nc, maps):
    res = run_bass_kernel_spmd(nc, maps, core_ids=list(range(NCORES)))
    return res.results


def _gather_heads(results, key="y"):
    first = np.asarray(results[0][key])
    full = np.empty((B * S, D), dtype=first.dtype)
    for c in range(NCORES):
        b, h = c // 4, c % 4
        full[b * S:(b + 1) * S, h * 256:(h + 1) * 256] = np.asarray(results[c][key])
    return full


def _pref(prefix, m, drop=()):
    return {prefix + k: v for k, v in m.items() if k not in drop}


def fused_inputs(x, gla_w_in, gla_w_gate_up, gla_b_gate, gla_norm_g, gla_w_out,
                 nsa_w_in, nsa_w_cmp_k1, nsa_w_cmp_k2, nsa_w_cmp_v1, nsa_w_cmp_v2, nsa_cmp_pe, nsa_w_out,
                 moe_w_router, moe_b_router, moe_w_gate, moe_w_up, moe_w_down, ln_g, ln_b):
    a0 = gla_inputs(x, gla_w_in[0], gla_w_gate_up[0], gla_b_gate[0], gla_norm_g[0])
    dummy_y = np.zeros((B * S, 1), np.float32)
    b0 = ffn_inputs(dummy_y, x.reshape(B * S, D), gla_w_out[0], ln_g[0], ln_b[0], moe_w_router, moe_b_router,
                    moe_w_gate[0], moe_w_up[0], moe_w_down[0])
    a1 = nsa_inputs(np.zeros((B, 1, S), np.float32), nsa_w_in[0], nsa_w_cmp_k1[0], nsa_w_cmp_k2[0],
                    nsa_w_cmp_v1[0], nsa_w_cmp_v2[0], nsa_cmp_pe[0])
    b1 = ffn_inputs(dummy_y, np.zeros((B * S, 1), np.float32), nsa_w_out[0], ln_g[1], ln_b[1], moe_w_router,
                    moe_b_router, moe_w_gate[1], moe_w_up[1], moe_w_down[1])
    maps = []
    for c in range(NCORES):
        m = {}
        m.update(_pref("a0_", a0[c]))
        m.update(_pref("b0_", b0[c], drop=("yT",)))
        m.update(_pref("a1_", a1[c], drop=("xT",)))
        m.update(_pref("b1_", b1[c], drop=("yT", "xres")))
        qs = np.zeros((128, 4), np.float32)
        qs[:, c % 4] = 1.0
        m["qsel"] = qs
        maps.append(m)
    return maps


def kernel(x, gla_w_in, gla_w_gate_up, gla_b_gate, gla_norm_g, gla_w_out,
           nsa_w_in, nsa_w_cmp_k1, nsa_w_cmp_k2, nsa_w_cmp_v1, nsa_w_cmp_v2, nsa_cmp_pe, nsa_w_out,
           moe_w_router, moe_b_router, moe_w_gate, moe_w_up, moe_w_down, ln_g, ln_b):
    f = lambda a: np.ascontiguousarray(np.asarray(a, dtype=np.float32))
    maps = fused_inputs(f(x), f(gla_w_in), f(gla_w_gate_up), f(gla_b_gate), f(gla_norm_g), f(gla_w_out),
                        f(nsa_w_in), f(nsa_w_cmp_k1), f(nsa_w_cmp_k2), f(nsa_w_cmp_v1), f(nsa_w_cmp_v2),
                        f(nsa_cmp_pe), f(nsa_w_out), f(moe_w_router), f(moe_b_router), f(moe_w_gate),
                        f(moe_w_up), f(moe_w_down), f(ln_g), f(ln_b))
    r = _run(_prog("fused", build_fused), maps)
    out = np.concatenate([np.asarray(r[c]["b1_out"]) for c in range(NCORES)], axis=0)
    return out.reshape(B, S, D).astype(np.float32)
```

```python
from contextlib import ExitStack

import numpy as np
import concourse.bass as bass
import concourse.mybir as mybir
from concourse.bass_utils import run_bass_kernel_spmd

F32 = mybir.dt.float32
BF16 = mybir.dt.bfloat16
AF = mybir.ActivationFunctionType
ALU = mybir.AluOpType
AX = mybir.AxisListType

D = 1024
B = 2
S = 8192
NCORES = 8
DN_ALPHA = 4.0 ** 0.25
LN_EPS = 1e-5


class KB:
    NDMA = 12

    def __init__(self, nc, stack):
        self.nc = nc
        self.stack = stack
        self.engs = {"pe": nc.tensor, "act": nc.scalar, "dve": nc.vector,
                     "pool": nc.gpsimd, "sp": nc.sync}
        self.sems = {}
        self.cnt = {}
        for e in ["pe", "act", "dve", "pool"]:
            self.sems[e] = stack.enter_context(nc.semaphore("c_" + e))
            self.cnt[e] = 0
        self.dq = {}
        for q in ["sp", "act", "pool"]:
            for i in range(self.NDMA):
                nm = f"d_{q}{i}"
                self.sems[nm] = stack.enter_context(nc.semaphore(nm))
                self.cnt[nm] = 0
            self.dq[q] = 0
        self.waited = {e: {} for e in self.engs}
        self.last_w = {}
        self.readers = {}
        self.n_inst = 0
        self.limit = None

    def _need(self, eng, dep):
        sem, val = dep
        if eng == "pe" and sem == "pe":
            return
        if self.waited[eng].get(sem, 0) >= val:
            return
        self.engs[eng].wait_ge(self.sems[sem], val)
        self.waited[eng][sem] = val

    def _deps(self, eng, reads, writes):
        for k in reads:
            d = self.last_w.get(k)
            if d is not None:
                self._need(eng, d)
            if k.startswith("p"):
                for d in self.readers.get(k, ()):
                    if d[0] != eng:
                        self._need(eng, d)
        for k in writes:
            d = self.last_w.get(k)
            if d is not None:
                self._need(eng, d)
            for d in self.readers.get(k, ()):
                self._need(eng, d)

    def _commit(self, tok, reads, writes):
        for k in reads:
            self.readers.setdefault(k, []).append(tok)
        for k in writes:
            self.last_w[k] = tok
            self.readers[k] = []

    def op(self, eng, fn, reads=(), writes=()):
        if self.limit is not None and self.n_inst >= self.limit:
            return None
        self._deps(eng, reads, writes)
        ins = fn()
        self.cnt[eng] += 1
        ins.then_inc(self.sems[eng], 1)
        self._commit((eng, self.cnt[eng]), reads, writes)
        self.n_inst += 1
        return ins

    def dma(self, q, out, in_, reads=(), writes=(), **kw):
        if self.limit is not None and self.n_inst >= self.limit and not kw.pop("force", False):
            return None
        kw.pop("force", None)
        i = self.dq[q] % self.NDMA
        self.dq[q] += 1
        nm = f"d_{q}{i}"
        if self.cnt[nm] > 0:
            self._need(q, (nm, self.cnt[nm]))
        self._deps(q, reads, writes)
        ins = self.engs[q].dma_start(out=out, in_=in_, **kw)
        self.cnt[nm] += 16
        ins.then_inc(self.sems[nm], 16)
        self._commit((nm, self.cnt[nm]), reads, writes)
        self.n_inst += 1
        return ins

    def finish(self, keys, eng="sp"):
        for k in keys:
            d = self.last_w.get(k)
            if d is not None:
                self._need(eng, d)

    def barrier(self):
        for e in self.engs:
            for c, v in self.cnt.items():
                if v > 0:
                    self._need(e, (c, v))

    def mm(self, out, lhsT, rhs, start, stop, reads, writes):
        nc = self.nc
        return self.op("pe", lambda: nc.tensor.matmul(out, lhsT, rhs, start=start, stop=stop),
                       reads=reads, writes=writes)

    def act(self, out, in_, func, reads, writes, **kw):
        nc = self.nc
        return self.op("act", lambda: nc.scalar.activation(out, in_, func, **kw),
                       reads=reads, writes=writes)


class Ctx:
    def __init__(self, nc, kb, prefix="", over=None):
        self.nc, self.kb, self.prefix, self.over = nc, kb, prefix, dict(over or {})

    def din(self, name, shape, dt=F32):
        if name in self.over:
            return self.over[name]
        return self.nc.dram_tensor(self.prefix + name, list(shape), dt, kind="ExternalInput").ap()

    def dout(self, name, shape, dt=F32):
        if name in self.over:
            return self.over[name]
        return self.nc.dram_tensor(self.prefix + name, list(shape), dt, kind="ExternalOutput").ap()


def _standalone(emit, **kw):
    nc = bass.Bass("TRN2", target_bir_lowering=False)
    with ExitStack() as st0:
        kb = KB(nc, st0)
        kb.limit = kw.pop("limit", None)
        emit(Ctx(nc, kb), **kw)
    return nc


_UID = [0]


def _uniq(name):
    _UID[0] += 1
    return f"{name}_{_UID[0]}"


def sb(nc, st, name, shape, dt):
    return st.enter_context(nc.sbuf_tensor(_uniq(name), list(shape), dt))


def ps(nc, st, name, shape, dt=F32):
    return st.enter_context(nc.psum_tensor(_uniq(name), list(shape), dt))


GLA_DK = 128
GLA_DV = 256
GC = 128
TT = 512


def gla_consts():
    j = np.arange(128)[:, None]
    i = np.arange(128)[None, :]
    tri_i = np.where(j <= i, -1.0 / 16.0, 0.0).astype(np.float32)
    tri_u = np.where(j > i, -1.0 / 16.0, 0.0).astype(np.float32)
    mask = np.where(j <= i, 1.0, 0.0).astype(np.float32)
    return tri_i, tri_u, mask


def build_gla(n_tok=S, limit=None):
    return _standalone(emit_gla, n_tok=n_tok, limit=limit)


def emit_gla(ctx, n_tok=S):
    nc, kb = ctx.nc, ctx.kb
    limit = kb.limit
    xT = ctx.din("xT", [D, n_tok])
    wqk = ctx.din("wqk", [D, 256])
    wkvr = ctx.din("wkvr", [D, 640])
    wg = ctx.din("wg", [D, 16])
    wgu = ctx.din("wgu", [33, 128])
    normg = ctx.din("normg", [1, 256])
    tri_i_d = ctx.din("tri_i", [128, 128])
    tri_u_d = ctx.din("tri_u", [128, 128])
    mask_d = ctx.din("maskT", [128, 128])
    y = ctx.dout("y", [n_tok, 256], BF16)

    with ExitStack() as st:
        V, A = nc.vector, nc.scalar
        w_qk = sb(nc, st, "w_qk", [128, 8, 256], BF16)
        w_kvr = sb(nc, st, "w_kvr", [128, 8, 640], BF16)
        w_g = sb(nc, st, "w_g", [128, 8, 16], BF16)
        w_gu = sb(nc, st, "w_gu", [33, 128], F32)
        ng = sb(nc, st, "ng", [128, 256], F32)
        tri_i = sb(nc, st, "tri_i_s", [128, 128], F32)
        tri_u = sb(nc, st, "tri_u_s", [128, 128], F32)
        maskT = sb(nc, st, "mask_s", [128, 128], F32)
        xt = [sb(nc, st, f"xt{i}", [128, 8, TT], BF16) for i in range(2)]
        g_aug = sb(nc, st, "g_aug", [33, TT], F32)
        e1 = sb(nc, st, "e1", [128, 128], F32)
        la = sb(nc, st, "la", [128, 128], F32)
        eb = sb(nc, st, "eb", [128, 128], F32)
        enb = sb(nc, st, "enb", [128, 128], F32)
        w2 = sb(nc, st, "w2", [128, 128], F32)
        qdT = sb(nc, st, "qdT", [128, 128], BF16)
        kdT = sb(nc, st, "kdT", [128, 128], BF16)
        kd2 = sb(nc, st, "kd2", [128, 128], BF16)
        v_bf = sb(nc, st, "v_bf", [128, 256], BF16)
        atm = sb(nc, st, "atm", [128, 128], BF16)
        S_f = sb(nc, st, "S_f", [128, 256], F32)
        S_b = sb(nc, st, "S_b", [128, 256], BF16)
        junk = sb(nc, st, "junk", [128, 256], F32)
        ss = sb(nc, st, "ss", [128, 1], F32)
        rstd = sb(nc, st, "rstd", [128, 1], F32)
        er = sb(nc, st, "er", [128, 256], F32)
        rs = sb(nc, st, "rs", [128, 256], F32)
        on = sb(nc, st, "on", [128, 256], F32)
        yt = [sb(nc, st, f"yt{i}", [128, 256], BF16) for i in range(2)]

        p_q = ps(nc, st, "p_q", [128, TT])
        p_k = ps(nc, st, "p_k", [128, TT])
        p_g_full = ps(nc, st, "p_g", [128, TT])
        p_g = p_g_full[0:16, :]
        p_m = ps(nc, st, "p_m", [128, 512])
        p_kv = ps(nc, st, "p_kv", [128, 512])
        p_ro = ps(nc, st, "p_ro", [128, 512])
        p_st_full = ps(nc, st, "p_st", [128, 512])
        p_st = p_st_full[:, 0:256]

        kb.dma("pool", w_qk[:], wqk.rearrange("(kc p) n -> p kc n", p=128), writes=["w_qk"])
        kb.dma("pool", w_kvr[:], wkvr.rearrange("(kc p) n -> p kc n", p=128), writes=["w_kvr"])
        kb.dma("pool", w_g[:], wg.rearrange("(kc p) n -> p kc n", p=128), writes=["w_g"])
        kb.dma("sp", w_gu[:], wgu, writes=["w_gu"])
        kb.dma("sp", ng[:], normg.partition_broadcast(128), writes=["ng"])
        kb.dma("sp", tri_i[:], tri_i_d, writes=["tri_i"])
        kb.dma("sp", tri_u[:], tri_u_d, writes=["tri_u"])
        kb.dma("sp", maskT[:], mask_d, writes=["maskT"])
        kb.op("dve", lambda: V.memset(S_f[:], 0.0), writes=["S_f"])
        kb.op("dve", lambda: V.memset(S_b[:], 0.0), writes=["S_b"])
        kb.op("dve", lambda: V.memset(g_aug[:], 1.0), writes=["g_aug"])
        eps_t = sb(nc, st, "eps_t", [128, 1], F32)
        kb.op("dve", lambda: V.memset(eps_t[:], LN_EPS), writes=["eps_t"])

        xTv = xT.rearrange("(kc p) t -> p kc t", p=128)
        n_tiles = n_tok // TT
        for T in range(n_tiles):
            x_t = xt[T % 2]
            xk = f"xt{T % 2}"
            kb.dma("pool", x_t[:], xTv[:, :, T * TT:(T + 1) * TT], writes=[xk])
            for kc in range(8):
                kb.mm(p_q[:], w_qk[:, kc, 0:128], x_t[:, kc, :], kc == 0, kc == 7,
                      reads=["w_qk", xk], writes=["p_q"])
            for kc in range(8):
                kb.mm(p_k[:], w_qk[:, kc, 128:256], x_t[:, kc, :], kc == 0, kc == 7,
                      reads=["w_qk", xk], writes=["p_k"])
            for kc in range(8):
                kb.mm(p_g, w_g[:, kc, :], x_t[:, kc, :], kc == 0, kc == 7,
                      reads=["w_g", xk], writes=["p_g"])
            kb.op("dve", lambda: V.tensor_copy(g_aug[0:16, :], p_g), reads=["p_g"], writes=["g_aug"])
            for c in range(TT // GC):
                cs = slice(c * GC, (c + 1) * GC)
                kb.mm(p_m[:, 0:128], g_aug[:, cs], w_gu[:], True, True,
                      reads=["g_aug", "w_gu"], writes=["p_m"])
                kb.act(e1[:], p_m[:, 0:128], AF.Exp, reads=["p_m"], writes=["e1"], scale=-1.0)
                kb.act(la[:], e1[:], AF.Ln, reads=["e1"], writes=["la"], bias=1.0)
                kb.mm(p_m[:, 128:256], la[:], tri_i[:], True, True, reads=["la", "tri_i"], writes=["p_m"])
                kb.mm(p_m[:, 256:384], tri_u[:], la[:], True, True, reads=["la", "tri_u"], writes=["p_m"])
                kb.act(eb[:], p_m[:, 128:256], AF.Exp, reads=["p_m"], writes=["eb"])
                kb.act(enb[:], p_m[:, 128:256], AF.Exp, reads=["p_m"], writes=["enb"], scale=-1.0)
                kb.act(w2[:], p_m[:, 256:384], AF.Exp, reads=["p_m"], writes=["w2"])
                kb.op("dve", lambda: V.scalar_tensor_tensor(
                    out=qdT[:], in0=p_q[:, cs], scalar=float(GLA_DK ** -0.5), in1=eb[:],
                    op0=ALU.mult, op1=ALU.mult), reads=["p_q", "eb"], writes=["qdT"])
                kb.op("dve", lambda: V.tensor_tensor(out=kdT[:], in0=p_k[:, cs], in1=enb[:], op=ALU.mult),
                      reads=["p_k", "enb"], writes=["kdT"])
                for kc in range(8):
                    kb.mm(p_kv[:, 0:384], x_t[:, kc, cs], w_kvr[:, kc, 0:384], kc == 0, kc == 7,
                          reads=[xk, "w_kvr"], writes=["p_kv"])
                for kc in range(8):
                    kb.mm(p_ro[:, 0:256], x_t[:, kc, cs], w_kvr[:, kc, 384:640], kc == 0, kc == 7,
                          reads=[xk, "w_kvr"], writes=["p_ro"])
                kb.op("dve", lambda: V.tensor_tensor(out=kd2[:], in0=p_kv[:, 0:128], in1=w2[:], op=ALU.mult),
                      reads=["p_kv", "w2"], writes=["kd2"])
                kb.op("dve", lambda: V.tensor_copy(v_bf[:], p_kv[:, 128:384]), reads=["p_kv"], writes=["v_bf"])
                kb.mm(p_m[:, 384:512], kdT[:], qdT[:], True, True, reads=["kdT", "qdT"], writes=["p_m"])
                kb.op("dve", lambda: V.tensor_tensor(out=atm[:], in0=p_m[:, 384:512], in1=maskT[:], op=ALU.mult),
                      reads=["p_m", "maskT"], writes=["atm"])
                kb.mm(p_ro[:, 256:512], atm[:], v_bf[:], True, False, reads=["atm", "v_bf"], writes=["p_ro"])
                kb.mm(p_ro[:, 256:512], qdT[:], S_b[:], False, True, reads=["qdT", "S_b"], writes=["p_ro"])
                kb.mm(p_st, kd2[:], v_bf[:], True, True, reads=["kd2", "v_bf"], writes=["p_st"])
                kb.op("dve", lambda: V.scalar_tensor_tensor(
                    out=S_f[:], in0=S_f[:], scalar=eb[:, 127:128], in1=p_st,
                    op0=ALU.mult, op1=ALU.add), reads=["S_f", "eb", "p_st"], writes=["S_f"])
                kb.op("pool", lambda: nc.gpsimd.tensor_copy(S_b[:], S_f[:]), reads=["S_f"], writes=["S_b"])
                kb.act(junk[:], p_ro[:, 256:512], AF.Square, reads=["p_ro"], writes=["junk", "ss"],
                       scale=1.0 / 16.0, accum_out=ss[:])
                kb.act(rstd[:], ss[:], AF.Ln, reads=["ss"], writes=["rstd"], bias=eps_t[:])
                kb.act(rstd[:], rstd[:], AF.Exp, reads=["rstd"], writes=["rstd"], scale=-0.5)
                kb.act(er[:], p_ro[:, 0:256], AF.Exp, reads=["p_ro"], writes=["er"], scale=-1.0)
                kb.op("dve", lambda: V.tensor_scalar_add(out=er[:], in0=er[:], scalar1=1.0),
                      reads=["er"], writes=["er"])
                kb.op("dve", lambda: V.reciprocal(out=er[:], in_=er[:]), reads=["er"], writes=["er"])
                kb.op("dve", lambda: V.tensor_tensor(out=rs[:], in0=p_ro[:, 0:256], in1=er[:], op=ALU.mult),
                      reads=["p_ro", "er"], writes=["rs"])
                kb.op("dve", lambda: V.scalar_tensor_tensor(
                    out=on[:], in0=p_ro[:, 256:512], scalar=rstd[:, 0:1], in1=ng[:],
                    op0=ALU.mult, op1=ALU.mult), reads=["p_ro", "rstd", "ng"], writes=["on"])
                ci = T * (TT // GC) + c
                y_t = yt[ci % 2]
                yk = f"yt{ci % 2}"
                kb.op("pool", lambda: nc.gpsimd.tensor_tensor(out=y_t[:], in0=on[:], in1=rs[:], op=ALU.mult),
                      reads=["on", "rs"], writes=[yk])
                kb.dma("sp", y[ci * GC:(ci + 1) * GC, :], y_t[:], reads=[yk], writes=["y_out"])
        if limit is not None:
            kb.dma("sp", y[0:128, :], yt[0][:], reads=["yt0"], writes=["y_out"], force=True)
            print("n_inst", kb.n_inst)
        kb.barrier()


def gla_inputs(x, w_in, w_gate_up, b_gate, norm_g):
    tri_i, tri_u, mask = gla_consts()
    maps = []
    for c in range(NCORES):
        b, h = c // 4, c % 4
        q = w_in[:, h * 128:(h + 1) * 128]
        k = w_in[:, 512 + h * 128:512 + (h + 1) * 128]
        v = w_in[:, 1024 + h * 256:1024 + (h + 1) * 256]
        g = w_in[:, 2048:2064]
        r = w_in[:, 2064 + h * 256:2064 + (h + 1) * 256]
        wgu = np.zeros((33, 128), np.float32)
        wgu[0:16] = w_gate_up[:, h * 128:(h + 1) * 128]
        wgu[32] = b_gate[h * 128:(h + 1) * 128]
        maps.append({
            "xT": np.ascontiguousarray(x[b].T),
            "wqk": np.ascontiguousarray(np.concatenate([q, k], axis=1)),
            "wkvr": np.ascontiguousarray(np.concatenate([k, v, r], axis=1)),
            "wg": np.ascontiguousarray(g),
            "wgu": wgu,
            "normg": np.ascontiguousarray(norm_g.reshape(1, 256)),
            "tri_i": tri_i, "tri_u": tri_u, "maskT": mask,
        })
    return maps


NTB = 2048
NE = 16
DFF = 256


def moe_consts():
    sel = np.zeros((16, 16, 128), np.float32)
    for e in range(16):
        sel[e, e, :] = 1.0
    ident = np.eye(128, dtype=np.float32)
    return sel, ident


def build_ffn(n_tok=NTB, limit=None, n_exp=NE):
    return _standalone(emit_ffn, n_tok=n_tok, limit=limit, n_exp=n_exp)


def emit_ffn(ctx, n_tok=NTB, n_exp=NE, fused=None):
    nc, kb = ctx.nc, ctx.kb
    limit = kb.limit
    if fused is None:
        yT = ctx.din("yT", [D, n_tok], BF16)
    xres = ctx.din("xres", [n_tok, D])
    wout = ctx.din("wout", [D, D])
    lnp = ctx.din("lnp", [4, D])
    wr = ctx.din("wr", [D, NE])
    br = ctx.din("br", [1, NE])
    wgd = ctx.din("wg", [NE, D, DFF])
    wud = ctx.din("wu", [NE, D, DFF])
    wdd = ctx.din("wd", [NE, DFF, D])
    sel_d = ctx.din("sel", [16, 16, 128])
    ident_d = ctx.din("ident", [128, 128])
    out = ctx.dout("out", [n_tok, D]) if (fused is None or "x1_d" not in fused) else None
    n_sub = n_tok // 128
    n_tile = n_tok // 512

    with ExitStack() as st:
        V, A, G = nc.vector, nc.scalar, nc.gpsimd
        w_o = sb(nc, st, "w_o", [128, 8, D], BF16)
        lng = [sb(nc, st, f"lnp{i}", [128, D], F32) for i in range(4)]
        w_r = sb(nc, st, "w_r", [128, 8, NE], F32)
        b_r = sb(nc, st, "b_r", [128, NE], F32)
        sel = sb(nc, st, "sel_s", [16, 16, 128], BF16)
        ident = sb(nc, st, "ident_s", [128, 128], F32)
        eps_t = sb(nc, st, "eps_t", [128, 1], F32)
        acc = sb(nc, st, "acc", [128, n_sub, D], F32)
        x1T = sb(nc, st, "x1T", [128, 8, n_tok], BF16)
        gT = sb(nc, st, "gT", [16, n_tok], BF16)
        y_t = [sb(nc, st, "y_t0", [128, 8, 128], BF16)] * 2
        xr = [sb(nc, st, "xr0", [128, D], F32)] * 2
        u = sb(nc, st, "u", [128, D], F32)
        x1 = sb(nc, st, "x1", [128, D], F32)
        stats = sb(nc, st, "stats", [128, 2, 6], F32)
        mv = sb(nc, st, "mv", [128, 2], F32)
        rstd = sb(nc, st, "rstd", [128, 1], F32)
        xTf = sb(nc, st, "xTf", [128, 8, 128], F32)
        sg_ = sb(nc, st, "r_s", [128, 16], F32)
        bi_ = sb(nc, st, "r_bi", [128, 16], F32)
        b2_ = sb(nc, st, "r_b2", [128, 16], F32)
        eq_ = sb(nc, st, "r_eq", [128, 16], F32)
        m1_ = sb(nc, st, "r_m1", [128, 4], F32)
        m2_ = sb(nc, st, "r_m2", [128, 4], F32)
        gs_ = sb(nc, st, "r_gs", [128, 4], F32)
        gm_ = sb(nc, st, "r_gm", [128, 1], F32)
        ig_ = sb(nc, st, "r_ig", [128, 4], F32)
        se_ = sb(nc, st, "r_se", [128, 16], F32)
        ws_ = sb(nc, st, "r_ws", [128, 1], F32)
        gate = sb(nc, st, "gate", [128, 16], F32)
        wg_s = [sb(nc, st, f"wg_s{i}", [128, 8, DFF], BF16) for i in range(2)]
        wu_s = [sb(nc, st, f"wu_s{i}", [128, 8, DFF], BF16) for i in range(2)]
        wd_s = [sb(nc, st, f"wd_s{i}", [128, 2, D], BF16) for i in range(2)]
        gb = [sb(nc, st, f"gb{i}", [128, 512], BF16) for i in range(2)]
        sgl = [sb(nc, st, f"sgl{i}", [128, 512], BF16) for i in range(2)]
        t1 = [sb(nc, st, f"t1{i}", [128, 512], BF16) for i in range(2)]
        hT = [sb(nc, st, f"hT{i}", [128, 2, 512], BF16) for i in range(2)]
        ot = [sb(nc, st, "ot0", [128, D], F32)] * 2

        pb = [ps(nc, st, f"pb{i}", [128, 512]) for i in range(8)]
        PK = [f"pb{i}" for i in range(8)]

        kb.dma("pool", w_o[:], wout.rearrange("(kc p) n -> p kc n", p=128), writes=["w_o"])
        for i in range(4):
            kb.dma("sp", lng[i][:], lnp[i:i + 1, :].partition_broadcast(128), writes=[f"lnp{i}"])
        kb.dma("sp", w_r[:], wr.rearrange("(kc p) n -> p kc n", p=128), writes=["w_r"])
        kb.dma("sp", b_r[:], br.partition_broadcast(128), writes=["b_r"])
        kb.dma("pool", sel[:], sel_d, writes=["sel"])
        kb.dma("sp", ident[:], ident_d, writes=["ident"])
        kb.op("dve", lambda: V.memset(eps_t[:], LN_EPS), writes=["eps_t"])

        def load_expert(e):
            i = e % 2
            kb.dma("pool", wg_s[i][:], wgd[e].rearrange("(kc p) f -> p kc f", p=128), writes=[f"wg{i}"])
            kb.dma("pool", wu_s[i][:], wud[e].rearrange("(kc p) f -> p kc f", p=128), writes=[f"wu{i}"])
            kb.dma("pool", wd_s[i][:], wdd[e].rearrange("(fc p) d -> p fc d", p=128), writes=[f"wd{i}"])

        def layer_norm(src, dst, gi, eng2):
            s_t, s_k = src
            d_t, d_k = dst
            for hh in range(2):
                kb.op("dve", lambda: V.bn_stats(stats[:, hh, :], s_t[:, hh * 512:(hh + 1) * 512]),
                      reads=[s_k], writes=["stats"])
            kb.op("dve", lambda: V.bn_aggr(mv[:], stats[:]), reads=["stats"], writes=["mv"])
            kb.act(rstd[:], mv[:, 1:2], AF.Sqrt, reads=["mv"], writes=["rstd"], bias=eps_t[:])
            kb.op("dve", lambda: V.reciprocal(rstd[:], rstd[:]), reads=["rstd"], writes=["rstd"])
            kb.op("dve", lambda: V.tensor_scalar(out=d_t, in0=s_t, scalar1=mv[:, 0:1], scalar2=rstd[:, 0:1],
                                                 op0=ALU.subtract, op1=ALU.mult),
                  reads=[s_k, "mv", "rstd"], writes=[d_k])
            kb.op("pool", lambda: G.tensor_tensor(out=d_t, in0=d_t, in1=lng[gi][:], op=ALU.mult),
                  reads=[d_k, f"lnp{gi}"], writes=[d_k])
            kb.op("pool", lambda: G.tensor_tensor(out=d_t, in0=d_t, in1=lng[gi + 1][:], op=ALU.add),
                  reads=[d_k, f"lnp{gi + 1}"], writes=[d_k])

        load_expert(0)
        if fused is None:
            yTv = yT.rearrange("(kc p) t -> p kc t", p=128)
        else:
            g_v = [gq.rearrange("(h t) c -> t h c", h=4) for gq in fused["g"]]
            cand = [sb(nc, st, "cand0", [128, 4, D], BF16)] * 2
            ysel = sb(nc, st, "ysel", [128, D], BF16)
            qsel = sb(nc, st, "qsel_s", [128, 4], F32)
            identb = sb(nc, st, "identb", [128, 128], BF16)
            xo = [sb(nc, st, "xo0", [128, 8, 128], BF16)] * 2
            kb.dma("sp", qsel[:], fused["qsel"], writes=["qsel"])
            kb.dma("pool", identb[:], ident_d, writes=["identb"])
        for sub in range(n_sub):
            ts_ = slice(sub * 128, (sub + 1) * 128)
            yk = "y_t0"
            xk = "xr0"
            if fused is None:
                kb.dma("sp", y_t[sub % 2][:], yTv[:, :, ts_], writes=[yk])
            else:
                ck = "cand0"
                for qq in range(4):
                    kb.dma("sp", cand[sub % 2][:, qq, :].rearrange("p (h c) -> p h c", h=4),
                           g_v[qq][sub * 128:(sub + 1) * 128, :, :], writes=[ck])
                kb.op("pool", lambda: G.tensor_scalar(out=ysel[:], in0=cand[sub % 2][:, 0, :], scalar1=qsel[:, 0:1],
                                                      scalar2=None, op0=ALU.mult), reads=[ck, "qsel"], writes=["ysel"])
                for qq in range(1, 4):
                    kb.op("dve", lambda: V.scalar_tensor_tensor(out=ysel[:], in0=cand[sub % 2][:, qq, :],
                                                                 scalar=qsel[:, qq:qq + 1], in1=ysel[:],
                                                                 op0=ALU.mult, op1=ALU.add),
                          reads=[ck, "qsel", "ysel"], writes=["ysel"])
                pTb = pb[7][:].bitcast(BF16)
                for kc in range(8):
                    kb.op("pe", lambda: nc.tensor.transpose(pTb[:, kc * 128:(kc + 1) * 128],
                                                            ysel[:, kc * 128:(kc + 1) * 128], identb[:]),
                          reads=["ysel", "identb"], writes=[PK[7]])
                kb.op("dve", lambda: V.tensor_copy(y_t[sub % 2][:], pTb.rearrange("p (k t) -> p k t", k=8)),
                      reads=[PK[7]], writes=[yk])
            kb.dma("sp", xr[sub % 2][:], xres[ts_, :], writes=[xk])
            for hh in range(2):
                for kc in range(8):
                    kb.mm(pb[hh][:], y_t[sub % 2][:, kc, :], w_o[:, kc, hh * 512:(hh + 1) * 512],
                          kc == 0, kc == 7, reads=[yk, "w_o"], writes=[PK[hh]])
            for hh in range(2):
                kb.op("dve", lambda: V.scalar_tensor_tensor(
                    out=u[:, hh * 512:(hh + 1) * 512], in0=xr[sub % 2][:, hh * 512:(hh + 1) * 512],
                    scalar=float(DN_ALPHA), in1=pb[hh][:], op0=ALU.mult, op1=ALU.add),
                    reads=[xk, PK[hh]], writes=["u"])
            layer_norm((u[:], "u"), (x1[:], "x1"), 0, None)
            kb.op("pool", lambda: G.tensor_scalar(out=acc[:, sub, :], in0=x1[:], scalar1=float(DN_ALPHA),
                                                  scalar2=None, op0=ALU.mult),
                  reads=["x1"], writes=[f"acc{sub}"])
            for kc in range(8):
                bank = 2 + kc // 4
                kb.op("pe", lambda: nc.tensor.transpose(pb[bank][:, (kc % 4) * 128:(kc % 4 + 1) * 128],
                                                        x1[:, kc * 128:(kc + 1) * 128], ident[:]),
                      reads=["x1", "ident"], writes=[PK[bank]])
            for q in range(2):
                kb.op("dve" if q == 0 else "act",
                      (lambda: V.tensor_copy(xTf[:, 0:4, :], pb[2][:].rearrange("p (k t) -> p k t", k=4))) if q == 0
                      else (lambda: A.copy(xTf[:, 4:8, :], pb[3][:].rearrange("p (k t) -> p k t", k=4))),
                      reads=[PK[2 + q]], writes=[f"xTf{q}"])
            kb.op("pool", lambda: G.tensor_copy(x1T[:, :, ts_], xTf[:]), reads=["xTf0", "xTf1"], writes=["x1T"])
            for kc in range(8):
                kb.mm(pb[4][:, 0:16], xTf[:, kc, :], w_r[:, kc, :], kc == 0, kc == 7,
                      reads=["xTf0", "xTf1", "w_r"], writes=[PK[4]])
            kb.act(sg_[:], pb[4][:, 0:16], AF.Sigmoid, reads=[PK[4]], writes=["r_s"])
            kb.op("dve", lambda: V.tensor_tensor(out=bi_[:], in0=sg_[:], in1=b_r[:], op=ALU.add),
                  reads=["r_s", "b_r"], writes=["r_bi"])
            bi3 = bi_[:].rearrange("p (g e) -> p g e", g=4)
            kb.op("dve", lambda: V.tensor_reduce(out=m1_[:], in_=bi3, axis=AX.X, op=ALU.max),
                  reads=["r_bi"], writes=["r_m1"])
            kb.op("dve", lambda: V.tensor_tensor(out=eq_[:].rearrange("p (g e) -> p g e", g=4), in0=bi3,
                                                 in1=m1_[:].unsqueeze(2).to_broadcast([128, 4, 4]), op=ALU.is_equal),
                  reads=["r_bi", "r_m1"], writes=["r_eq"])
            kb.op("dve", lambda: V.scalar_tensor_tensor(out=b2_[:], in0=eq_[:], scalar=-1e30, in1=bi_[:],
                                                        op0=ALU.mult, op1=ALU.add),
                  reads=["r_eq", "r_bi"], writes=["r_b2"])
            kb.op("dve", lambda: V.tensor_reduce(out=m2_[:], in_=b2_[:].rearrange("p (g e) -> p g e", g=4),
                                                 axis=AX.X, op=ALU.max),
                  reads=["r_b2"], writes=["r_m2"])
            kb.op("dve", lambda: V.tensor_tensor(out=gs_[:], in0=m1_[:], in1=m2_[:], op=ALU.add),
                  reads=["r_m1", "r_m2"], writes=["r_gs"])
            kb.op("dve", lambda: V.tensor_reduce(out=gm_[:], in_=gs_[:], axis=AX.X, op=ALU.max),
                  reads=["r_gs"], writes=["r_gm"])
            kb.op("dve", lambda: V.tensor_scalar(out=ig_[:], in0=gs_[:], scalar1=gm_[:, 0:1], scalar2=None,
                                                 op0=ALU.is_ge),
                  reads=["r_gs", "r_gm"], writes=["r_ig"])
            kb.op("dve", lambda: V.tensor_tensor(out=se_[:].rearrange("p (g e) -> p g e", g=4), in0=bi3,
                                                 in1=m2_[:].unsqueeze(2).to_broadcast([128, 4, 4]), op=ALU.is_ge),
                  reads=["r_bi", "r_m2"], writes=["r_se"])
            kb.op("dve", lambda: V.tensor_tensor(out=se_[:].rearrange("p (g e) -> p g e", g=4),
                                                 in0=se_[:].rearrange("p (g e) -> p g e", g=4),
                                                 in1=ig_[:].unsqueeze(2).to_broadcast([128, 4, 4]), op=ALU.mult),
                  reads=["r_se", "r_ig"], writes=["r_se"])
            kb.op("dve", lambda: V.tensor_tensor(out=se_[:], in0=se_[:], in1=sg_[:], op=ALU.mult),
                  reads=["r_se", "r_s"], writes=["r_se"])
            kb.op("dve", lambda: V.tensor_reduce(out=ws_[:], in_=se_[:], axis=AX.X, op=ALU.add),
                  reads=["r_se"], writes=["r_ws"])
            kb.op("dve", lambda: V.reciprocal(ws_[:], ws_[:]), reads=["r_ws"], writes=["r_ws"])
            kb.op("dve", lambda: V.tensor_scalar(out=gate[:], in0=se_[:], scalar1=ws_[:, 0:1], scalar2=None,
                                                 op0=ALU.mult),
                  reads=["r_se", "r_ws"], writes=["gate"])
            kb.op("pe", lambda: nc.tensor.transpose(pb[5][0:16, 0:128], gate[:], ident[:]),
                  reads=["gate", "ident"], writes=[PK[5]])
            kb.op("dve", lambda: V.tensor_copy(gT[:, ts_], pb[5][0:16, 0:128]), reads=[PK[5]], writes=["gT"])

        for e in range(n_exp):
            i = e % 2
            if e + 1 < n_exp:
                load_expert(e + 1)
            for T in range(n_tile):
                Ts = slice(T * 512, (T + 1) * 512)
                j = (e * n_tile + T) % 2
                kb.mm(pb[4][:], sel[:, e, :], gT[:, Ts], True, True, reads=["sel", "gT"], writes=[PK[4]])
                kb.op("act", lambda: A.copy(gb[j][:], pb[4][:]), reads=[PK[4]], writes=[f"gb{j}"])
                for fc in range(2):
                    for kc in range(8):
                        kb.mm(pb[fc][:], wg_s[i][:, kc, fc * 128:(fc + 1) * 128], x1T[:, kc, Ts],
                              kc == 0, kc == 7, reads=[f"wg{i}", "x1T"], writes=[PK[fc]])
                    for kc in range(8):
                        kb.mm(pb[2 + fc][:], wu_s[i][:, kc, fc * 128:(fc + 1) * 128], x1T[:, kc, Ts],
                              kc == 0, kc == 7, reads=[f"wu{i}", "x1T"], writes=[PK[2 + fc]])
                for fc in range(2):
                    kb.act(sgl[fc][:], pb[fc][:], AF.Silu, reads=[PK[fc]], writes=[f"sgl{fc}"])
                    kb.op("dve", lambda: V.tensor_tensor(out=t1[fc][:], in0=sgl[fc][:], in1=pb[2 + fc][:], op=ALU.mult),
                          reads=[f"sgl{fc}", PK[2 + fc]], writes=[f"t1{fc}"])
                    kb.op("pool", lambda: G.tensor_tensor(out=hT[j][:, fc, :], in0=t1[fc][:], in1=gb[j][:], op=ALU.mult),
                          reads=[f"t1{fc}", f"gb{j}"], writes=[f"hT{j}"])
                for s4 in range(4):
                    sub = T * 4 + s4
                    for hh in range(2):
                        bank = 5 + (s4 * 2 + hh) % 3
                        for fc in range(2):
                            kb.mm(pb[bank][:], hT[j][:, fc, s4 * 128:(s4 + 1) * 128],
                                  wd_s[i][:, fc, hh * 512:(hh + 1) * 512], fc == 0, fc == 1,
                                  reads=[f"hT{j}", f"wd{i}"], writes=[PK[bank]])
                        kb.op("dve", lambda: V.tensor_tensor(out=acc[:, sub, hh * 512:(hh + 1) * 512],
                                                             in0=acc[:, sub, hh * 512:(hh + 1) * 512],
                                                             in1=pb[bank][:], op=ALU.add),
                              reads=[f"acc{sub}", PK[bank]], writes=[f"acc{sub}"])

        for sub in range(n_sub):
            o_t = ot[sub % 2]
            layer_norm((acc[:, sub, :], f"acc{sub}"), (o_t[:], "ot0"), 2, None)
            if out is not None:
                kb.dma("sp", out[sub * 128:(sub + 1) * 128, :], o_t[:], reads=["ot0"], writes=["out"], force=True)
            else:
                ok_ = "ot0"
                kb.dma("sp", fused["x1_d"][sub * 128:(sub + 1) * 128, :], o_t[:], reads=[ok_], writes=["x1_d"])
                for kc in range(8):
                    bank = 2 + kc // 4
                    kb.op("pe", lambda: nc.tensor.transpose(pb[bank][:, (kc % 4) * 128:(kc % 4 + 1) * 128],
                                                            o_t[:, kc * 128:(kc + 1) * 128], ident[:]),
                          reads=[ok_, "ident"], writes=[PK[bank]])
                xk_ = "xo0"
                kb.op("dve", lambda: V.tensor_copy(xo[sub % 2][:, 0:4, :], pb[2][:].rearrange("p (k t) -> p k t", k=4)),
                      reads=[PK[2]], writes=[xk_])
                kb.op("dve", lambda: V.tensor_copy(xo[sub % 2][:, 4:8, :], pb[3][:].rearrange("p (k t) -> p k t", k=4)),
                      reads=[PK[3]], writes=[xk_])
                kb.dma("sp", fused["x1T_d"].rearrange("(kc p) t -> p kc t", p=128)[:, :, sub * 128:(sub + 1) * 128],
                       xo[sub % 2][:], reads=[xk_], writes=["x1T_d"])
        kb.barrier()


def ffn_inputs(y_tok_major, xres, w_out, ln_g, ln_b, w_router, b_router, w_gate, w_up, w_down):
    sel, ident = moe_consts()
    lnp = np.ascontiguousarray(np.stack([ln_g[0], ln_b[0], ln_g[1], ln_b[1]]).astype(np.float32))
    maps = []
    for c in range(NCORES):
        rs_ = slice(c * NTB, (c + 1) * NTB)
        maps.append({
            "yT": np.ascontiguousarray(y_tok_major[rs_].T),
            "xres": np.ascontiguousarray(xres[rs_]),
            "wout": w_out, "lnp": lnp, "wr": w_router,
            "br": np.ascontiguousarray(b_router.reshape(1, NE)),
            "wg": w_gate, "wu": w_up, "wd": w_down, "sel": sel, "ident": ident,
        })
    return maps


NSA_SCALE = 0.125
MASK_NEG = -240000.0


def nsa_consts(n_tok=S):
    nqb = n_tok // 128
    inv = np.power(500000.0, -np.arange(8, dtype=np.float32) * (2.0 / 16.0)).astype(np.float32)
    def cs(pos):
        ang = pos.astype(np.float32)[:, None] * inv[None, :]
        return np.concatenate([np.cos(ang), np.sin(ang)], axis=1).astype(np.float32)
    def pl(a):
        n = a.shape[0] // 128
        return np.ascontiguousarray(a.reshape(n, 128, 16).transpose(1, 0, 2).reshape(128, n * 16))
    cs_tok = pl(cs(np.arange(n_tok)))
    cs_cmp = pl(cs(np.arange(512) * 16 + 31))
    wimp = np.zeros((512, 128), np.float32)
    for s_ in range(128):
        for o, wgt in enumerate([1, 2, 2, 2, 1]):
            c = 4 * s_ + o
            if c < 511:
                wimp[c, s_] = wgt
    texp = (np.arange(n_tok)[None, :] // 64 == np.arange(128)[:, None]).astype(np.float32)
    k = np.arange(128)[:, None]
    q = np.arange(128)[None, :]
    causal = (k <= q).astype(np.float32)
    strict = (k > q).astype(np.float32)
    cmask = np.zeros((nqb, 2, 128, 128), np.float32)
    for qi in range(nqb):
        jl = (8 * qi + 6) // 128
        for slot, jt in ((0, jl - 1), (1, jl)):
            if jt < 0:
                continue
            j = 128 * jt + k
            cmask[qi, slot] = (16 * j + 31 <= 128 * qi + q)
    ident = np.eye(128, dtype=np.float32)
    return dict(cs_tok=cs_tok, cs_cmp=cs_cmp, wimp=wimp, texp=texp, causal=causal, strict=strict,
                cmask=cmask, ident=ident)


def build_nsa(n_tok=S, limit=None):
    return _standalone(emit_nsa, n_tok=n_tok, limit=limit)


def emit_nsa(ctx, n_tok=S, g2=None):
    nc, kb = ctx.nc, ctx.kb
    limit = kb.limit
    nqb = n_tok // 128
    ncb = n_tok // 16 - 1
    ncp = ((ncb + 127) // 128) * 128
    njt_all = ncp // 128
    din = ctx.din
    xT = din("xT", [D, n_tok]) if g2 is None else None
    wn = din("wn", [D, 652])
    cs_tok_d = din("cs_tok", [128, nqb * 16])
    cs_cmp_d = din("cs_cmp", [128, 64])
    wimp_d = din("wimp", [512, 128])
    texp_d = din("texp", [128, n_tok])
    causal_d = din("causal", [128, 128])
    strict_d = din("strict", [128, 128])
    cmask_d = din("cmask", [nqb, 2, 128, 128])
    ident_d = din("ident", [128, 128])
    wk1_d = din("wk1", [2048, 256])
    wk2_d = din("wk2", [256, 64])
    wv1_d = din("wv1", [2048, 256])
    wv2_d = din("wv2", [256, 64])
    peT_d = din("peT", [64, 32])
    y = ctx.dout("y", [n_tok, 256], BF16)

    with ExitStack() as st:
        V, A, G = nc.vector, nc.scalar, nc.gpsimd
        identb = sb(nc, st, "identb", [128, 128], BF16)
        QT = sb(nc, st, "QT", [64, 4, n_tok], BF16)
        KST = sb(nc, st, "KST", [64, n_tok], BF16)
        KWT = sb(nc, st, "KWT", [64, n_tok], BF16)
        VS = sb(nc, st, "VS", [128, nqb, 65], BF16)
        VW = sb(nc, st, "VW", [128, nqb, 65], BF16)
        G_all = sb(nc, st, "G_all", [128, nqb, 12], F32)
        KCMT = sb(nc, st, "KCMT", [64, ncp], BF16)
        RC = sb(nc, st, "RC", [128, njt_all, 193], BF16)
        st1 = ExitStack()
        w_n = sb(nc, st1, "w_n", [128, 8, 652], BF16)
        cs_tok = sb(nc, st1, "cs_tok_s", [128, nqb, 16], F32)
        cs_cmp = sb(nc, st1, "cs_cmp_s", [128, 4, 16], F32)
        KCT = sb(nc, st1, "KCT", [64, n_tok], BF16)
        VCT = sb(nc, st1, "VCT", [64, n_tok], BF16)
        xt = [sb(nc, st1, f"xt{i}", [128, 8, 128], BF16) for i in range(2)]
        pr = sb(nc, st1, "pr", [128, 652], F32)
        rp = sb(nc, st1, "rp", [128, 8, 64], BF16)
        ra = sb(nc, st1, "ra", [128, 6, 8], F32)
        rb = sb(nc, st1, "rb", [128, 6, 8], F32)
        w1 = sb(nc, st1, "w1", [64, 32, 256], BF16)
        w2 = sb(nc, st1, "w2", [128, 2, 64], BF16)
        peT = sb(nc, st1, "peT_s", [64, 32], BF16)
        hb = sb(nc, st1, "hb", [128, 2], F32)
        h1T = sb(nc, st1, "h1T", [128, 2, ncp], BF16)
        kc_f = sb(nc, st1, "kc_f", [128, 64], F32)
        kc_b = sb(nc, st1, "kc_b", [128, 64], BF16)

        pA = ps(nc, st, "pA", [128, 512])
        pB = ps(nc, st, "pB", [128, 512])
        pT = ps(nc, st, "pT", [128, 1024], BF16)
        pS = [ps(nc, st, f"pS{i}", [128, 512]) for i in range(3)]
        pC = pA
        pSL = ps(nc, st, "pSL", [128, 512])
        pW = ps(nc, st, "pW", [128, 512])

        kb.dma("pool", w_n[:], wn.rearrange("(kc p) n -> p kc n", p=128), writes=["w_n"])
        kb.dma("sp", cs_tok[:], cs_tok_d.rearrange("p (n c) -> p n c", c=16), writes=["cs_tok"])
        kb.dma("sp", cs_cmp[:], cs_cmp_d.rearrange("p (n c) -> p n c", c=16), writes=["cs_cmp"])
        kb.dma("pool", identb[:], ident_d, writes=["identb"])
        kb.dma("pool", peT[:], peT_d, writes=["peT"])
        kb.op("dve", lambda: V.memset(VS[:, :, 64:65], 1.0), writes=["VS"])
        kb.op("dve", lambda: V.memset(VW[:, :, 64:65], 1.0), writes=["VW"])
        kb.op("dve", lambda: V.memset(RC[:, :, 64:65], 1.0), writes=["RC"])
        kb.op("dve", lambda: V.memset(h1T[:], 0.0), writes=["h1T"])
        kb.dma("pool", RC[:, :, 65:193], wimp_d[0:ncp, :].rearrange("(n p) s -> p n s", p=128), writes=["RC"])

        if g2 is None:
            xTv = xT.rearrange("(kc p) t -> p kc t", p=128)
        else:
            g2v = [gj.rearrange("(q k2 p) t -> q p k2 t", q=4, k2=2, p=128) for gj in g2]

        def rope(src3, dst3, cs_ap, nh, csk):
            cosb = cs_ap[:, 0:8].unsqueeze(1).to_broadcast([128, nh, 8])
            sinb = cs_ap[:, 8:16].unsqueeze(1).to_broadcast([128, nh, 8])
            a_, b_ = ra[:, 0:nh, :], rb[:, 0:nh, :]
            kb.op("pool", lambda: G.tensor_tensor(out=a_, in0=src3[:, :, 0:8], in1=cosb, op=ALU.mult),
                  reads=["rsrc", csk], writes=["ra"])
            kb.op("pool", lambda: G.tensor_tensor(out=b_, in0=src3[:, :, 8:16], in1=sinb, op=ALU.mult),
                  reads=["rsrc", csk], writes=["rb"])
            kb.op("pool", lambda: G.tensor_tensor(out=dst3[:, :, 0:8], in0=a_, in1=b_, op=ALU.subtract),
                  reads=["ra", "rb"], writes=["rdst"])
            kb.op("pool", lambda: G.tensor_tensor(out=a_, in0=src3[:, :, 8:16], in1=cosb, op=ALU.mult),
                  reads=["rsrc", csk, "rdst"], writes=["ra"])
            kb.op("pool", lambda: G.tensor_tensor(out=b_, in0=src3[:, :, 0:8], in1=sinb, op=ALU.mult),
                  reads=["rsrc", csk, "rdst"], writes=["rb"])
            kb.op("pool", lambda: G.tensor_tensor(out=dst3[:, :, 8:16], in0=a_, in1=b_, op=ALU.add),
                  reads=["ra", "rb"], writes=["rdst"])
            kb.op("pool", lambda: G.tensor_copy(dst3[:, :, 16:64], src3[:, :, 16:64]),
                  reads=["rsrc"], writes=["rdst"])

        for T in range(nqb):
            Ts = slice(T * 128, (T + 1) * 128)
            x_t = xt[T % 2]
            xk = f"xt{T % 2}"
            if g2 is None:
                kb.dma("pool", x_t[:], xTv[:, :, Ts], writes=[xk])
            else:
                qq, tl = T // 16, T % 16
                for j in range(4):
                    kb.dma("sp", x_t[:, 2 * j:2 * j + 2, :], g2v[j][qq][:, :, tl * 128:(tl + 1) * 128], writes=[xk])
            for kc in range(8):
                kb.mm(pA[:], x_t[:, kc, :], w_n[:, kc, 0:512], kc == 0, kc == 7, reads=[xk, "w_n"], writes=["pA"])
            for kc in range(8):
                kb.mm(pB[:, 0:140], x_t[:, kc, :], w_n[:, kc, 512:652], kc == 0, kc == 7,
                      reads=[xk, "w_n"], writes=["pB"])
            kb.op("dve", lambda: V.tensor_copy(pr[:, 0:512], pA[:]), reads=["pA", "rdst"], writes=["rsrc"])
            kb.op("dve", lambda: V.tensor_copy(pr[:, 512:640], pB[:, 0:128]), reads=["pB"], writes=["prv"])
            kb.act(G_all[:, T, :], pB[:, 128:140], AF.Sigmoid, reads=["pB"], writes=["G_all"])
            kb.op("pool", lambda: G.tensor_copy(VS[:, T, 0:64], pr[:, 512:576]), reads=["prv"], writes=["VS"])
            kb.op("pool", lambda: G.tensor_copy(VW[:, T, 0:64], pr[:, 576:640]), reads=["prv"], writes=["VW"])
            rope(pr[:, 0:384].rearrange("p (s d) -> p s d", s=6), rp[:, 0:6, :], cs_tok[:, T, :], 6, "cs_tok")
            kb.op("pool", lambda: G.tensor_copy(rp[:, 6:8, :], pr[:, 384:512].rearrange("p (s d) -> p s d", s=2)),
                  reads=["rsrc"], writes=["rdst"])
            for s_ in range(8):
                kb.op("pe", lambda: nc.tensor.transpose(pT[0:64, s_ * 128:(s_ + 1) * 128], rp[:, s_, :], identb[:]),
                      reads=["rdst", "identb"], writes=["pT"])
            kb.op("dve", lambda: V.tensor_copy(QT[:, :, Ts], pT[0:64, 0:512].rearrange("p (h t) -> p h t", h=4)),
                  reads=["pT"], writes=["QT"])
            kb.op("dve", lambda: V.tensor_copy(KST[:, Ts], pT[0:64, 512:640]), reads=["pT"], writes=["KST"])
            kb.op("dve", lambda: V.tensor_copy(KWT[:, Ts], pT[0:64, 640:768]), reads=["pT"], writes=["KWT"])
            kb.op("dve", lambda: V.tensor_copy(KCT[:, Ts], pT[0:64, 768:896]), reads=["pT"], writes=["KCT"])
            kb.op("dve", lambda: V.tensor_copy(VCT[:, Ts], pT[0:64, 896:1024]), reads=["pT"], writes=["VCT"])

        for which, (w1d, w2d, srcT, srck) in enumerate(((wk1_d, wk2_d, KCT, "KCT"), (wv1_d, wv2_d, VCT, "VCT"))):
            kb.dma("pool", w1[:], w1d.rearrange("(r d) h -> d r h", d=64), writes=["w1"])
            kb.dma("pool", w2[:], w2d.rearrange("(hc p) d -> p hc d", p=128), writes=["w2"])
            for hc in range(2):
                for r in range(32):
                    kb.mm(pS[0][:, 0:1], w1[:, r, hc * 128:(hc + 1) * 128], peT[:, r:r + 1], r == 0, r == 31,
                          reads=["w1", "peT"], writes=["pS0"])
                kb.op("dve", lambda: V.tensor_copy(hb[:, hc:hc + 1], pS[0][:, 0:1]), reads=["pS0"], writes=["hb"])
            for hc in range(2):
                for c0 in range(0, ncb, 512):
                    n_ = min(512, ncb - c0)
                    for r in range(32):
                        kb.mm(pS[1][:, 0:n_], w1[:, r, hc * 128:(hc + 1) * 128],
                              srcT[:, 16 * c0 + r:16 * c0 + r + 16 * (n_ - 1) + 1:16], r == 0, r == 31,
                              reads=["w1", srck], writes=["pS1"])
                    kb.act(h1T[:, hc, c0:c0 + n_], pS[1][:, 0:n_], AF.Silu, reads=["pS1", "hb"], writes=["h1T"],
                           bias=hb[:, hc:hc + 1])
            for jt in range(njt_all):
                for hc in range(2):
                    kb.mm(pS[2][:, 0:64], h1T[:, hc, jt * 128:(jt + 1) * 128], w2[:, hc, :], hc == 0, hc == 1,
                          reads=["h1T", "w2"], writes=["pS2"])
                if which == 0:
                    kb.op("dve", lambda: V.tensor_copy(kc_f[:], pS[2][:, 0:64]), reads=["pS2", "rdst"], writes=["rsrc"])
                    rope(kc_f[:].unsqueeze(1), kc_b[:].unsqueeze(1), cs_cmp[:, jt, :], 1, "cs_cmp")
                    kb.op("pe", lambda: nc.tensor.transpose(pT[0:64, 0:128], kc_b[:], identb[:]),
                          reads=["rdst", "identb"], writes=["pT"])
                    kb.op("dve", lambda: V.tensor_copy(KCMT[:, jt * 128:(jt + 1) * 128], pT[0:64, 0:128]),
                          reads=["pT"], writes=["KCMT"])
                else:
                    kb.op("dve", lambda: V.tensor_copy(RC[:, jt, 0:64], pS[2][:, 0:64]), reads=["pS2"], writes=["RC"])

        if limit is not None:
            print("n_inst after stage 2:", kb.n_inst)
        kb.barrier()
        st1.close()
        texp = sb(nc, st, "texp_s", [128, n_tok], BF16)
        causal = sb(nc, st, "causal_s", [128, 128], BF16)
        strict = sb(nc, st, "strict_s", [128, 128], BF16)
        e_sb = [sb(nc, st, f"e_sb{i}", [128, 512], BF16) for i in range(3)]
        cm_sb = [sb(nc, st, f"cm_sb{i}", [128, 2, 128], BF16) for i in range(2)]
        impm = sb(nc, st, "impm", [128, 128], F32)
        impw = sb(nc, st, "impw", [128, 128], F32)
        m8 = sb(nc, st, "m8", [128, 16], F32)
        selb = sb(nc, st, "selb", [128, 128], BF16)
        nsT = sb(nc, st, "nsT", [128, 512], BF16)
        zz = sb(nc, st, "zz", [128, 12], F32)
        coef = sb(nc, st, "coef", [128, 12], F32)
        o_acc = sb(nc, st, "o_acc", [128, 256], F32)
        y_sb = [sb(nc, st, f"y_sb{i}", [128, 256], BF16) for i in range(2)]
        kb.dma("pool", texp[:], texp_d, writes=["texp"])
        kb.dma("pool", causal[:], causal_d, writes=["causal"])
        kb.dma("pool", strict[:], strict_d, writes=["strict"])

        def exp_tile(bank, ei):
            kb.act(e_sb[ei][:], pS[bank][:], AF.Exp, reads=[f"pS{bank}"], writes=[f"e{ei}"], scale=NSA_SCALE)

        def mask_tile(ei, mask_ap, mkeys):
            e3 = e_sb[ei][:].rearrange("p (h q) -> p h q", h=4)
            kb.op("pool", lambda: G.tensor_tensor(out=e3, in0=e3, in1=mask_ap.unsqueeze(1).to_broadcast([128, 4, 128]),
                                                  op=ALU.mult),
                  reads=[f"e{ei}"] + mkeys, writes=[f"e{ei}"])

        rot = [0]

        def nxt():
            rot[0] += 1
            return rot[0] % 3

        PIPE = 2
        pipe = []

        def drain_one():
            pv0, post0 = pipe.pop(0)
            pv0()
            if post0 is not None:
                post0()

        def push_tile(qk, pv, post=None):
            qk()
            pipe.append((pv, post))
            while len(pipe) > PIPE:
                drain_one()

        def tile_a(qi, jt, jl, Qv, cmk):
            r_ = nxt()

            def qk():
                kb.mm(pS[r_][:], KCMT[:, jt * 128:(jt + 1) * 128], Qv, True, True, reads=["KCMT", "QT"], writes=[f"pS{r_}"])
                exp_tile(r_, r_)
                if jt >= jl - 1:
                    mask_tile(r_, cm_sb[qi % 2][:, 1 - (jl - jt), :], [cmk])

            def pv():
                for h in range(4):
                    bank = pA if h < 2 else pB
                    kb.mm(bank[:, (h % 2) * 193:(h % 2) * 193 + 193], e_sb[r_][:, h * 128:(h + 1) * 128], RC[:, jt, :],
                          jt == 0 and h % 2 == 0, jt == jl and h % 2 == 1, reads=[f"e{r_}", "RC"],
                          writes=["pA" if h < 2 else "pB"])
            return qk, pv

        def post_a1(qi):
            use_sel = qi >= 8
            Gq = G_all[:, qi, :].rearrange("p (h j) -> p h j", h=4)

            def post():
                for h in range(4):
                    bank = pA if h < 2 else pB
                    c0 = (h % 2) * 193
                    kb.op("dve", lambda: V.tensor_scalar_max(out=zz[:, h:h + 1], in0=bank[:, c0 + 64:c0 + 65], scalar1=1e-30),
                          reads=["pA" if h < 2 else "pB"], writes=["zz"])
                kb.op("dve", lambda: V.reciprocal(zz[:, 0:4], zz[:, 0:4]), reads=["zz"], writes=["zz"])
                if use_sel:
                    for h in range(4):
                        bank = pA if h < 2 else pB
                        bk = "pA" if h < 2 else "pB"
                        c0 = (h % 2) * 193
                        if h == 0:
                            kb.op("dve", lambda: V.tensor_scalar(out=impm[:], in0=bank[:, c0 + 65:c0 + 193],
                                                                 scalar1=zz[:, 0:1], scalar2=None, op0=ALU.mult),
                                  reads=[bk, "zz"], writes=["impm"])
                        else:
                            kb.op("dve", lambda: V.scalar_tensor_tensor(out=impm[:], in0=bank[:, c0 + 65:c0 + 193],
                                                                        scalar=zz[:, h:h + 1], in1=impm[:],
                                                                        op0=ALU.mult, op1=ALU.add),
                                  reads=[bk, "zz", "impm"], writes=["impm"])
                    c2 = 2 * qi
                    kb.op("dve", lambda: V.memset(impm[:, 0:1], 3e30), reads=[], writes=["impm"])
                    kb.op("dve", lambda: V.memset(impm[:, c2:c2 + 1], 2e30), writes=["impm"])
                    kb.op("dve", lambda: V.memset(impm[0:64, c2 - 1:c2], 1e30), writes=["impm"])
                    kb.op("dve", lambda: V.memset(impm[0:64, c2 + 1:c2 + 2], -1e30), writes=["impm"])
                    kb.op("dve", lambda: V.memset(impm[64:128, c2 + 1:c2 + 2], 2.5e30), writes=["impm"])
                    if c2 + 2 < 128:
                        kb.op("dve", lambda: V.memset(impm[:, c2 + 2:128], -1e30), writes=["impm"])
                    kb.op("dve", lambda: V.max(out=m8[:, 0:8], in_=impm[:]), reads=["impm"], writes=["m8"])
                    kb.op("dve", lambda: V.match_replace(out=impw[:], in_to_replace=m8[:, 0:8], in_values=impm[:],
                                                         imm_value=-1e30), reads=["impm", "m8"], writes=["impw"])
                    kb.op("dve", lambda: V.max(out=m8[:, 8:16], in_=impw[:]), reads=["impw"], writes=["m8"])
                    kb.op("dve", lambda: V.tensor_scalar(out=selb[:], in0=impm[:], scalar1=m8[:, 15:16], scalar2=MASK_NEG,
                                                         op0=ALU.is_lt, op1=ALU.mult),
                          reads=["impm", "m8"], writes=["selb"])
                kb.op("dve", lambda: V.tensor_tensor(out=coef[:, 0:4], in0=zz[:, 0:4], in1=Gq[:, :, 0], op=ALU.mult),
                      reads=["zz", "G_all"], writes=["coef"])
                for h in range(4):
                    bank = pA if h < 2 else pB
                    bk = "pA" if h < 2 else "pB"
                    c0 = (h % 2) * 193
                    kb.op("dve", lambda: V.tensor_scalar(out=o_acc[:, h * 64:(h + 1) * 64], in0=bank[:, c0:c0 + 64],
                                                         scalar1=coef[:, h:h + 1], scalar2=None, op0=ALU.mult),
                          reads=[bk, "coef"], writes=["o_acc"])
            return post

        def post_a2():
            kb.op("pe", lambda: nc.tensor.transpose(pT[:, 0:128], selb[:], identb[:]),
                  reads=["selb", "identb"], writes=["pT"])
            kb.op("dve", lambda: V.tensor_copy(nsT[:].rearrange("p (h q) -> p h q", h=4),
                                               pT[:, 0:128].unsqueeze(1).to_broadcast([128, 4, 128])),
                  reads=["pT"], writes=["nsT"])

        def tile_c(qi, kt, Qv, use_sel):
            r_ = nxt()
            Ks = slice(kt * 128, (kt + 1) * 128)

            def qk():
                kb.mm(pS[r_][:], KST[:, Ks], Qv, True, not use_sel, reads=["KST", "QT"], writes=[f"pS{r_}"])
                if use_sel:
                    kb.mm(pS[r_][:], texp[:, Ks], nsT[:], False, True, reads=["texp", "nsT"], writes=[f"pS{r_}"])
                exp_tile(r_, r_)
                if kt == qi:
                    mask_tile(r_, causal[:], ["causal"])

            def pv():
                for h in range(4):
                    kb.mm(pSL[:, h * 65:(h + 1) * 65], e_sb[r_][:, h * 128:(h + 1) * 128], VS[:, kt, :],
                          kt == 0 and h == 0, kt == qi and h == 3, reads=[f"e{r_}", "VS"], writes=["pSL"])
            return qk, pv

        def tile_d(qi, kt, k0, Qv):
            r_ = nxt()
            Ks = slice(kt * 128, (kt + 1) * 128)

            def qk():
                kb.mm(pS[r_][:], KWT[:, Ks], Qv, True, True, reads=["KWT", "QT"], writes=[f"pS{r_}"])
                exp_tile(r_, r_)
                if kt == qi:
                    mask_tile(r_, causal[:], ["causal"])
                elif kt == qi - 4:
                    mask_tile(r_, strict[:], ["strict"])

            def pv():
                for h in range(4):
                    kb.mm(pW[:, h * 65:(h + 1) * 65], e_sb[r_][:, h * 128:(h + 1) * 128], VW[:, kt, :],
                          kt == k0 and h == 0, kt == qi and h == 3, reads=[f"e{r_}", "VW"], writes=["pW"])
            return qk, pv

        def post_combine(qi, bi, final):
            Gq = G_all[:, qi, :].rearrange("p (h j) -> p h j", h=4)
            bank, bk = ((pSL, "pSL"), (pW, "pW"))[bi]
            Qs = slice(qi * 128, (qi + 1) * 128)

            def post():
                b3 = bank[:, 0:260].rearrange("p (h c) -> p h c", h=4)
                kb.op("dve", lambda: V.reciprocal(zz[:, 4 + 4 * bi:8 + 4 * bi], b3[:, :, 64]), reads=[bk], writes=["zz"])
                kb.op("dve", lambda: V.tensor_tensor(out=coef[:, 4 + 4 * bi:8 + 4 * bi], in0=zz[:, 4 + 4 * bi:8 + 4 * bi],
                                                     in1=Gq[:, :, 1 + bi], op=ALU.mult),
                      reads=["zz", "G_all"], writes=["coef"])
                for h in range(4):
                    kb.op("dve", lambda: V.scalar_tensor_tensor(
                        out=o_acc[:, h * 64:(h + 1) * 64], in0=b3[:, h, 0:64],
                        scalar=coef[:, 4 + 4 * bi + h:5 + 4 * bi + h], in1=o_acc[:, h * 64:(h + 1) * 64],
                        op0=ALU.mult, op1=ALU.add), reads=[bk, "coef", "o_acc"], writes=["o_acc"])
                if final:
                    yk = f"y_sb{qi % 2}"
                    kb.op("pool", lambda: G.tensor_copy(y_sb[qi % 2][:], o_acc[:]), reads=["o_acc"], writes=[yk])
                    kb.dma("sp", y[Qs, :], y_sb[qi % 2][:], reads=[yk], writes=["y_out"], force=True)
            return post

        for qi in range(nqb):
            Qs = slice(qi * 128, (qi + 1) * 128)
            Qv = QT[:, :, Qs]
            use_sel = qi >= 8
            jl = (8 * qi + 6) // 128
            cmk = f"cm{qi % 2}"
            kb.dma("pool", cm_sb[qi % 2][:], cmask_d[qi].rearrange("s j q -> j s q"), writes=[cmk])
            for jt in range(jl + 1):
                qk, pv = tile_a(qi, jt, jl, Qv, cmk)
                push_tile(qk, pv, post_a1(qi) if jt == jl else None)
            k0 = max(0, qi - 4)
            for kt in range(k0, qi + 1):
                qk, pv = tile_d(qi, kt, k0, Qv)
                post = None
                if kt == qi:
                    post = post_combine(qi, 1, False)
                elif use_sel and kt == k0 + 2:
                    post = post_a2
                push_tile(qk, pv, post)
            for kt in range(qi + 1):
                qk, pv = tile_c(qi, kt, Qv, use_sel)
                push_tile(qk, pv, post_combine(qi, 0, True) if kt == qi else None)
        while pipe:
            drain_one()
        kb.barrier()


def nsa_inputs(x1, w_in, w_ck1, w_ck2, w_cv1, w_cv2, cmp_pe, n_tok=S):
    cst = nsa_consts(n_tok)
    maps = []
    for c in range(NCORES):
        b, g = c // 4, c % 4
        def col(base, width=64, mult=64):
            return w_in[:, base + g * mult: base + g * mult + width]
        q = w_in[:, g * 256:(g + 1) * 256]
        kc, vc, ks, vs, kw, vw = (col(1024), col(1280), col(1536), col(1792), col(2048), col(2304))
        gt = w_in[:, 2560 + g * 12:2560 + (g + 1) * 12]
        wn = np.ascontiguousarray(np.concatenate([q, ks, kw, kc, vc, vs, vw, gt], axis=1))
        m = {"xT": np.ascontiguousarray(x1[b].T[:, :n_tok]), "wn": wn,
             "wk1": w_ck1, "wk2": w_ck2, "wv1": w_cv1, "wv2": w_cv2,
             "peT": np.ascontiguousarray(cmp_pe.T)}
        m.update(cst)
        maps.append(m)
    return maps


GROUPS = [[0, 1, 2, 3], [4, 5, 6, 7]]


def build_fused():
    nc = bass.Bass("TRN2", target_bir_lowering=False)
    with ExitStack() as st0:
        kb = KB(nc, st0)
        cc_sem = st0.enter_context(nc.semaphore("cc_sem"))
        n_cc = [0]
        internal = lambda name, shape, dt: nc.dram_tensor(name, list(shape), dt).ap()
        y0_d = internal("y0_d", [S, 256], BF16)
        g1 = [internal(f"g1_{j}", [4 * NTB, 256], BF16) for j in range(4)]
        x1_d = internal("x1_d", [NTB, D], F32)
        x1T_d = internal("x1T_d", [D, NTB], BF16)
        g2 = [internal(f"g2_{j}", [4 * 256, NTB], BF16) for j in range(4)]
        y1_d = internal("y1_d", [S, 256], BF16)
        g3 = [internal(f"g3_{j}", [4 * NTB, 256], BF16) for j in range(4)]
        qsel = nc.dram_tensor("qsel", [128, 4], F32, kind="ExternalInput").ap()

        def all_gather(srcs, dsts):
            kb.barrier()
            for s_, d_ in zip(srcs, dsts):
                n_cc[0] += 1
                nc.gpsimd.collective_compute("AllGather", ALU.bypass, replica_groups=GROUPS,
                                             ins=[s_], outs=[d_]).then_inc(cc_sem, 1)
            for e in kb.engs.values():
                e.wait_ge(cc_sem, n_cc[0])

        emit_gla(Ctx(nc, kb, "a0_", {"y": y0_d}))
        all_gather([y0_d[j * NTB:(j + 1) * NTB, :] for j in range(4)], g1)
        emit_ffn(Ctx(nc, kb, "b0_"), fused={"g": g1, "qsel": qsel, "x1_d": x1_d, "x1T_d": x1T_d})
        all_gather([x1T_d[j * 256:(j + 1) * 256, :] for j in range(4)], g2)
        emit_nsa(Ctx(nc, kb, "a1_", {"y": y1_d}), g2=g2)
        all_gather([y1_d[j * NTB:(j + 1) * NTB, :] for j in range(4)], g3)
        emit_ffn(Ctx(nc, kb, "b1_", {"xres": x1_d}), fused={"g": g3, "qsel": qsel})
        kb.barrier()
    return nc


_PROGS = {}


def _prog(name, fn):
    if name not in _PROGS:
        _PROGS[name] = fn()
    return _PROGS[name]


def _run(nc, maps):
    res = run_bass_kernel_spmd(nc, maps, core_ids=list(range(NCORES)))
    return res.results


def _gather_heads(results, key="y"):
    first = np.asarray(results[0][key])
    full = np.empty((B * S, D), dtype=first.dtype)
    for c in range(NCORES):
        b, h = c // 4, c % 4
        full[b * S:(b + 1) * S, h * 256:(h + 1) * 256] = np.asarray(results[c][key])
    return full


def _pref(prefix, m, drop=()):
    return {prefix + k: v for k, v in m.items() if k not in drop}


def fused_inputs(x, gla_w_in, gla_w_gate_up, gla_b_gate, gla_norm_g, gla_w_out,
                 nsa_w_in, nsa_w_cmp_k1, nsa_w_cmp_k2, nsa_w_cmp_v1, nsa_w_cmp_v2, nsa_cmp_pe, nsa_w_out,
                 moe_w_router, moe_b_router, moe_w_gate, moe_w_up, moe_w_down, ln_g, ln_b):
    a0 = gla_inputs(x, gla_w_in[0], gla_w_gate_up[0], gla_b_gate[0], gla_norm_g[0])
    dummy_y = np.zeros((B * S, 1), np.float32)
    b0 = ffn_inputs(dummy_y, x.reshape(B * S, D), gla_w_out[0], ln_g[0], ln_b[0], moe_w_router, moe_b_router,
                    moe_w_gate[0], moe_w_up[0], moe_w_down[0])
    a1 = nsa_inputs(np.zeros((B, 1, S), np.float32), nsa_w_in[0], nsa_w_cmp_k1[0], nsa_w_cmp_k2[0],
                    nsa_w_cmp_v1[0], nsa_w_cmp_v2[0], nsa_cmp_pe[0])
    b1 = ffn_inputs(dummy_y, np.zeros((B * S, 1), np.float32), nsa_w_out[0], ln_g[1], ln_b[1], moe_w_router,
                    moe_b_router, moe_w_gate[1], moe_w_up[1], moe_w_down[1])
    maps = []
    for c in range(NCORES):
        m = {}
        m.update(_pref("a0_", a0[c]))
        m.update(_pref("b0_", b0[c], drop=("yT",)))
        m.update(_pref("a1_", a1[c], drop=("xT",)))
        m.update(_pref("b1_", b1[c], drop=("yT", "xres")))
        qs = np.zeros((128, 4), np.float32)
        qs[:, c % 4] = 1.0
        m["qsel"] = qs
        maps.append(m)
    return maps


def kernel(x, gla_w_in, gla_w_gate_up, gla_b_gate, gla_norm_g, gla_w_out,
           nsa_w_in, nsa_w_cmp_k1, nsa_w_cmp_k2, nsa_w_cmp_v1, nsa_w_cmp_v2, nsa_cmp_pe, nsa_w_out,
           moe_w_router, moe_b_router, moe_w_gate, moe_w_up, moe_w_down, ln_g, ln_b):
    f = lambda a: np.ascontiguousarray(np.asarray(a, dtype=np.float32))
    maps = fused_inputs(f(x), f(gla_w_in), f(gla_w_gate_up), f(gla_b_gate), f(gla_norm_g), f(gla_w_out),
                        f(nsa_w_in), f(nsa_w_cmp_k1), f(nsa_w_cmp_k2), f(nsa_w_cmp_v1), f(nsa_w_cmp_v2),
                        f(nsa_cmp_pe), f(nsa_w_out), f(moe_w_router), f(moe_b_router), f(moe_w_gate),
                        f(moe_w_up), f(moe_w_down), f(ln_g), f(ln_b))
    r = _run(_prog("fused", build_fused), maps)
    out = np.concatenate([np.asarray(r[c]["b1_out"]) for c in range(NCORES)], axis=0)
    return out.reshape(B, S, D).astype(np.float32)
```

```python
from contextlib import ExitStack

import numpy as np
import concourse.bass as bass
import concourse.mybir as mybir
from concourse.bass_utils import run_bass_kernel_spmd

F32 = mybir.dt.float32
BF16 = mybir.dt.bfloat16
AF = mybir.ActivationFunctionType
ALU = mybir.AluOpType
AX = mybir.AxisListType

D = 1024
B = 2
S = 8192
NCORES = 8
DN_ALPHA = 4.0 ** 0.25
LN_EPS = 1e-5


class KB:
    NDMA = 12

    def __init__(self, nc, stack):
        self.nc = nc
        self.stack = stack
        self.engs = {"pe": nc.tensor, "act": nc.scalar, "dve": nc.vector,
                     "pool": nc.gpsimd, "sp": nc.sync}
        self.sems = {}
        self.cnt = {}
        for e in ["pe", "act", "dve", "pool"]:
            self.sems[e] = stack.enter_context(nc.semaphore("c_" + e))
            self.cnt[e] = 0
        self.dq = {}
        for q in ["sp", "act", "pool"]:
            for i in range(self.NDMA):
                nm = f"d_{q}{i}"
                self.sems[nm] = stack.enter_context(nc.semaphore(nm))
                self.cnt[nm] = 0
            self.dq[q] = 0
        self.waited = {e: {} for e in self.engs}
        self.last_w = {}
        self.readers = {}
        self.n_inst = 0
        self.limit = None

    def _need(self, eng, dep):
        sem, val = dep
        if eng == "pe" and sem == "pe":
            return
        if self.waited[eng].get(sem, 0) >= val:
            return
        self.engs[eng].wait_ge(self.sems[sem], val)
        self.waited[eng][sem] = val

    def _deps(self, eng, reads, writes):
        for k in reads:
            d = self.last_w.get(k)
            if d is not None:
                self._need(eng, d)
            if k.startswith("p"):
                for d in self.readers.get(k, ()):
                    if d[0] != eng:
                        self._need(eng, d)
        for k in writes:
            d = self.last_w.get(k)
            if d is not None:
                self._need(eng, d)
            for d in self.readers.get(k, ()):
                self._need(eng, d)

    def _commit(self, tok, reads, writes):
        for k in reads:
            self.readers.setdefault(k, []).append(tok)
        for k in writes:
            self.last_w[k] = tok
            self.readers[k] = []

    def op(self, eng, fn, reads=(), writes=()):
        if self.limit is not None and self.n_inst >= self.limit:
            return None
        self._deps(eng, reads, writes)
        ins = fn()
        self.cnt[eng] += 1
        ins.then_inc(self.sems[eng], 1)
        self._commit((eng, self.cnt[eng]), reads, writes)
        self.n_inst += 1
        return ins

    def dma(self, q, out, in_, reads=(), writes=(), **kw):
        if self.limit is not None and self.n_inst >= self.limit and not kw.pop("force", False):
            return None
        kw.pop("force", None)
        i = self.dq[q] % self.NDMA
        self.dq[q] += 1
        nm = f"d_{q}{i}"
        if self.cnt[nm] > 0:
            self._need(q, (nm, self.cnt[nm]))
        self._deps(q, reads, writes)
        ins = self.engs[q].dma_start(out=out, in_=in_, **kw)
        self.cnt[nm] += 16
        ins.then_inc(self.sems[nm], 16)
        self._commit((nm, self.cnt[nm]), reads, writes)
        self.n_inst += 1
        return ins

    def finish(self, keys, eng="sp"):
        for k in keys:
            d = self.last_w.get(k)
            if d is not None:
                self._need(eng, d)

    def barrier(self):
        for e in self.engs:
            for c, v in self.cnt.items():
                if v > 0:
                    self._need(e, (c, v))

    def mm(self, out, lhsT, rhs, start, stop, reads, writes):
        nc = self.nc
        return self.op("pe", lambda: nc.tensor.matmul(out, lhsT, rhs, start=start, stop=stop),
                       reads=reads, writes=writes)

    def act(self, out, in_, func, reads, writes, **kw):
        nc = self.nc
        return self.op("act", lambda: nc.scalar.activation(out, in_, func, **kw),
                       reads=reads, writes=writes)


class Ctx:
    def __init__(self, nc, kb, prefix="", over=None):
        self.nc, self.kb, self.prefix, self.over = nc, kb, prefix, dict(over or {})

    def din(self, name, shape, dt=F32):
        if name in self.over:
            return self.over[name]
        return self.nc.dram_tensor(self.prefix + name, list(shape), dt, kind="ExternalInput").ap()

    def dout(self, name, shape, dt=F32):
        if name in self.over:
            return self.over[name]
        return self.nc.dram_tensor(self.prefix + name, list(shape), dt, kind="ExternalOutput").ap()


def _standalone(emit, **kw):
    nc = bass.Bass("TRN2", target_bir_lowering=False)
    with ExitStack() as st0:
        kb = KB(nc, st0)
        kb.limit = kw.pop("limit", None)
        emit(Ctx(nc, kb), **kw)
    return nc


_UID = [0]


def _uniq(name):
    _UID[0] += 1
    return f"{name}_{_UID[0]}"


def sb(nc, st, name, shape, dt):
    return st.enter_context(nc.sbuf_tensor(_uniq(name), list(shape), dt))


def ps(nc, st, name, shape, dt=F32):
    return st.enter_context(nc.psum_tensor(_uniq(name), list(shape), dt))


GLA_DK = 128
GLA_DV = 256
GC = 128
TT = 512


def gla_consts():
    j = np.arange(128)[:, None]
    i = np.arange(128)[None, :]
    tri_i = np.where(j <= i, -1.0 / 16.0, 0.0).astype(np.float32)
    tri_u = np.where(j > i, -1.0 / 16.0, 0.0).astype(np.float32)
    mask = np.where(j <= i, 1.0, 0.0).astype(np.float32)
    return tri_i, tri_u, mask


def build_gla(n_tok=S, limit=None):
    return _standalone(emit_gla, n_tok=n_tok, limit=limit)


def emit_gla(ctx, n_tok=S):
    nc, kb = ctx.nc, ctx.kb
    limit = kb.limit
    xT = ctx.din("xT", [D, n_tok])
    wqk = ctx.din("wqk", [D, 256])
    wkvr = ctx.din("wkvr", [D, 640])
    wg = ctx.din("wg", [D, 16])
    wgu = ctx.din("wgu", [33, 128])
    normg = ctx.din("normg", [1, 256])
    tri_i_d = ctx.din("tri_i", [128, 128])
    tri_u_d = ctx.din("tri_u", [128, 128])
    mask_d = ctx.din("maskT", [128, 128])
    y = ctx.dout("y", [n_tok, 256], BF16)

    with ExitStack() as st:
        V, A = nc.vector, nc.scalar
        w_qk = sb(nc, st, "w_qk", [128, 8, 256], BF16)
        w_kvr = sb(nc, st, "w_kvr", [128, 8, 640], BF16)
        w_g = sb(nc, st, "w_g", [128, 8, 16], BF16)
        w_gu = sb(nc, st, "w_gu", [33, 128], F32)
        ng = sb(nc, st, "ng", [128, 256], F32)
        tri_i = sb(nc, st, "tri_i_s", [128, 128], F32)
        tri_u = sb(nc, st, "tri_u_s", [128, 128], F32)
        maskT = sb(nc, st, "mask_s", [128, 128], F32)
        xt = [sb(nc, st, f"xt{i}", [128, 8, TT], BF16) for i in range(2)]
        g_aug = sb(nc, st, "g_aug", [33, TT], F32)
        e1 = sb(nc, st, "e1", [128, 128], F32)
        la = sb(nc, st, "la", [128, 128], F32)
        eb = sb(nc, st, "eb", [128, 128], F32)
        enb = sb(nc, st, "enb", [128, 128], F32)
        w2 = sb(nc, st, "w2", [128, 128], F32)
        qdT = sb(nc, st, "qdT", [128, 128], BF16)
        kdT = sb(nc, st, "kdT", [128, 128], BF16)
        kd2 = sb(nc, st, "kd2", [128, 128], BF16)
        v_bf = sb(nc, st, "v_bf", [128, 256], BF16)
        atm = sb(nc, st, "atm", [128, 128], BF16)
        S_f = sb(nc, st, "S_f", [128, 256], F32)
        S_b = sb(nc, st, "S_b", [128, 256], BF16)
        junk = sb(nc, st, "junk", [128, 256], F32)
        ss = sb(nc, st, "ss", [128, 1], F32)
        rstd = sb(nc, st, "rstd", [128, 1], F32)
        er = sb(nc, st, "er", [128, 256], F32)
        rs = sb(nc, st, "rs", [128, 256], F32)
        on = sb(nc, st, "on", [128, 256], F32)
        yt = [sb(nc, st, f"yt{i}", [128, 256], BF16) for i in range(2)]

        p_q = ps(nc, st, "p_q", [128, TT])
        p_k = ps(nc, st, "p_k", [128, TT])
        p_g_full = ps(nc, st, "p_g", [128, TT])
        p_g = p_g_full[0:16, :]
        p_m = ps(nc, st, "p_m", [128, 512])
        p_kv = ps(nc, st, "p_kv", [128, 512])
        p_ro = ps(nc, st, "p_ro", [128, 512])
        p_st_full = ps(nc, st, "p_st", [128, 512])
        p_st = p_st_full[:, 0:256]

        kb.dma("pool", w_qk[:], wqk.rearrange("(kc p) n -> p kc n", p=128), writes=["w_qk"])
        kb.dma("pool", w_kvr[:], wkvr.rearrange("(kc p) n -> p kc n", p=128), writes=["w_kvr"])
        kb.dma("pool", w_g[:], wg.rearrange("(kc p) n -> p kc n", p=128), writes=["w_g"])
        kb.dma("sp", w_gu[:], wgu, writes=["w_gu"])
        kb.dma("sp", ng[:], normg.partition_broadcast(128), writes=["ng"])
        kb.dma("sp", tri_i[:], tri_i_d, writes=["tri_i"])
        kb.dma("sp", tri_u[:], tri_u_d, writes=["tri_u"])
        kb.dma("sp", maskT[:], mask_d, writes=["maskT"])
        kb.op("dve", lambda: V.memset(S_f[:], 0.0), writes=["S_f"])
        kb.op("dve", lambda: V.memset(S_b[:], 0.0), writes=["S_b"])
        kb.op("dve", lambda: V.memset(g_aug[:], 1.0), writes=["g_aug"])
        eps_t = sb(nc, st, "eps_t", [128, 1], F32)
        kb.op("dve", lambda: V.memset(eps_t[:], LN_EPS), writes=["eps_t"])

        xTv = xT.rearrange("(kc p) t -> p kc t", p=128)
        n_tiles = n_tok // TT
        for T in range(n_tiles):
            x_t = xt[T % 2]
            xk = f"xt{T % 2}"
            kb.dma("pool", x_t[:], xTv[:, :, T * TT:(T + 1) * TT], writes=[xk])
            for kc in range(8):
                kb.mm(p_q[:], w_qk[:, kc, 0:128], x_t[:, kc, :], kc == 0, kc == 7,
                      reads=["w_qk", xk], writes=["p_q"])
            for kc in range(8):
                kb.mm(p_k[:], w_qk[:, kc, 128:256], x_t[:, kc, :], kc == 0, kc == 7,
                      reads=["w_qk", xk], writes=["p_k"])
            for kc in range(8):
                kb.mm(p_g, w_g[:, kc, :], x_t[:, kc, :], kc == 0, kc == 7,
                      reads=["w_g", xk], writes=["p_g"])
            kb.op("dve", lambda: V.tensor_copy(g_aug[0:16, :], p_g), reads=["p_g"], writes=["g_aug"])
            for c in range(TT // GC):
                cs = slice(c * GC, (c + 1) * GC)
                kb.mm(p_m[:, 0:128], g_aug[:, cs], w_gu[:], True, True,
                      reads=["g_aug", "w_gu"], writes=["p_m"])
                kb.act(e1[:], p_m[:, 0:128], AF.Exp, reads=["p_m"], writes=["e1"], scale=-1.0)
                kb.act(la[:], e1[:], AF.Ln, reads=["e1"], writes=["la"], bias=1.0)
                kb.mm(p_m[:, 128:256], la[:], tri_i[:], True, True, reads=["la", "tri_i"], writes=["p_m"])
                kb.mm(p_m[:, 256:384], tri_u[:], la[:], True, True, reads=["la", "tri_u"], writes=["p_m"])
                kb.act(eb[:], p_m[:, 128:256], AF.Exp, reads=["p_m"], writes=["eb"])
                kb.act(enb[:], p_m[:, 128:256], AF.Exp, reads=["p_m"], writes=["enb"], scale=-1.0)
                kb.act(w2[:], p_m[:, 256:384], AF.Exp, reads=["p_m"], writes=["w2"])
                kb.op("dve", lambda: V.scalar_tensor_tensor(
                    out=qdT[:], in0=p_q[:, cs], scalar=float(GLA_DK ** -0.5), in1=eb[:],
                    op0=ALU.mult, op1=ALU.mult), reads=["p_q", "eb"], writes=["qdT"])
                kb.op("dve", lambda: V.tensor_tensor(out=kdT[:], in0=p_k[:, cs], in1=enb[:], op=ALU.mult),
                      reads=["p_k", "enb"], writes=["kdT"])
                for kc in range(8):
                    kb.mm(p_kv[:, 0:384], x_t[:, kc, cs], w_kvr[:, kc, 0:384], kc == 0, kc == 7,
                          reads=[xk, "w_kvr"], writes=["p_kv"])
                for kc in range(8):
                    kb.mm(p_ro[:, 0:256], x_t[:, kc, cs], w_kvr[:, kc, 384:640], kc == 0, kc == 7,
                          reads=[xk, "w_kvr"], writes=["p_ro"])
                kb.op("dve", lambda: V.tensor_tensor(out=kd2[:], in0=p_kv[:, 0:128], in1=w2[:], op=ALU.mult),
                      reads=["p_kv", "w2"], writes=["kd2"])
                kb.op("dve", lambda: V.tensor_copy(v_bf[:], p_kv[:, 128:384]), reads=["p_kv"], writes=["v_bf"])
                kb.mm(p_m[:, 384:512], kdT[:], qdT[:], True, True, reads=["kdT", "qdT"], writes=["p_m"])
                kb.op("dve", lambda: V.tensor_tensor(out=atm[:], in0=p_m[:, 384:512], in1=maskT[:], op=ALU.mult),
                      reads=["p_m", "maskT"], writes=["atm"])
                kb.mm(p_ro[:, 256:512], atm[:], v_bf[:], True, False, reads=["atm", "v_bf"], writes=["p_ro"])
                kb.mm(p_ro[:, 256:512], qdT[:], S_b[:], False, True, reads=["qdT", "S_b"], writes=["p_ro"])
                kb.mm(p_st, kd2[:], v_bf[:], True, True, reads=["kd2", "v_bf"], writes=["p_st"])
                kb.op("dve", lambda: V.scalar_tensor_tensor(
                    out=S_f[:], in0=S_f[:], scalar=eb[:, 127:128], in1=p_st,
                    op0=ALU.mult, op1=ALU.add), reads=["S_f", "eb", "p_st"], writes=["S_f"])
                kb.op("pool", lambda: nc.gpsimd.tensor_copy(S_b[:], S_f[:]), reads=["S_f"], writes=["S_b"])
                kb.act(junk[:], p_ro[:, 256:512], AF.Square, reads=["p_ro"], writes=["junk", "ss"],
                       scale=1.0 / 16.0, accum_out=ss[:])
                kb.act(rstd[:], ss[:], AF.Ln, reads=["ss"], writes=["rstd"], bias=eps_t[:])
                kb.act(rstd[:], rstd[:], AF.Exp, reads=["rstd"], writes=["rstd"], scale=-0.5)
                kb.act(er[:], p_ro[:, 0:256], AF.Exp, reads=["p_ro"], writes=["er"], scale=-1.0)
                kb.op("dve", lambda: V.tensor_scalar_add(out=er[:], in0=er[:], scalar1=1.0),
                      reads=["er"], writes=["er"])
                kb.op("dve", lambda: V.reciprocal(out=er[:], in_=er[:]), reads=["er"], writes=["er"])
                kb.op("dve", lambda: V.tensor_tensor(out=rs[:], in0=p_ro[:, 0:256], in1=er[:], op=ALU.mult),
                      reads=["p_ro", "er"], writes=["rs"])
                kb.op("dve", lambda: V.scalar_tensor_tensor(
                    out=on[:], in0=p_ro[:, 256:512], scalar=rstd[:, 0:1], in1=ng[:],
                    op0=ALU.mult, op1=ALU.mult), reads=["p_ro", "rstd", "ng"], writes=["on"])
                ci = T * (TT // GC) + c
                y_t = yt[ci % 2]
                yk = f"yt{ci % 2}"
                kb.op("pool", lambda: nc.gpsimd.tensor_tensor(out=y_t[:], in0=on[:], in1=rs[:], op=ALU.mult),
                      reads=["on", "rs"], writes=[yk])
                kb.dma("sp", y[ci * GC:(ci + 1) * GC, :], y_t[:], reads=[yk], writes=["y_out"])
        if limit is not None:
            kb.dma("sp", y[0:128, :], yt[0][:], reads=["yt0"], writes=["y_out"], force=True)
            print("n_inst", kb.n_inst)
        kb.barrier()


def gla_inputs(x, w_in, w_gate_up, b_gate, norm_g):
    tri_i, tri_u, mask = gla_consts()
    maps = []
    for c in range(NCORES):
        b, h = c // 4, c % 4
        q = w_in[:, h * 128:(h + 1) * 128]
        k = w_in[:, 512 + h * 128:512 + (h + 1) * 128]
        v = w_in[:, 1024 + h * 256:1024 + (h + 1) * 256]
        g = w_in[:, 2048:2064]
        r = w_in[:, 2064 + h * 256:2064 + (h + 1) * 256]
        wgu = np.zeros((33, 128), np.float32)
        wgu[0:16] = w_gate_up[:, h * 128:(h + 1) * 128]
        wgu[32] = b_gate[h * 128:(h + 1) * 128]
        maps.append({
            "xT": np.ascontiguousarray(x[b].T),
            "wqk": np.ascontiguousarray(np.concatenate([q, k], axis=1)),
            "wkvr": np.ascontiguousarray(np.concatenate([k, v, r], axis=1)),
            "wg": np.ascontiguousarray(g),
            "wgu": wgu,
            "normg": np.ascontiguousarray(norm_g.reshape(1, 256)),
            "tri_i": tri_i, "tri_u": tri_u, "maskT": mask,
        })
    return maps


NTB = 2048
NE = 16
DFF = 256


def moe_consts():
    sel = np.zeros((16, 16, 128), np.float32)
    for e in range(16):
        sel[e, e, :] = 1.0
    ident = np.eye(128, dtype=np.float32)
    return sel, ident


def build_ffn(n_tok=NTB, limit=None, n_exp=NE):
    return _standalone(emit_ffn, n_tok=n_tok, limit=limit, n_exp=n_exp)


def emit_ffn(ctx, n_tok=NTB, n_exp=NE, fused=None):
    nc, kb = ctx.nc, ctx.kb
    limit = kb.limit
    if fused is None:
        yT = ctx.din("yT", [D, n_tok], BF16)
    xres = ctx.din("xres", [n_tok, D])
    wout = ctx.din("wout", [D, D])
    lnp = ctx.din("lnp", [4, D])
    wr = ctx.din("wr", [D, NE])
    br = ctx.din("br", [1, NE])
    wgd = ctx.din("wg", [NE, D, DFF])
    wud = ctx.din("wu", [NE, D, DFF])
    wdd = ctx.din("wd", [NE, DFF, D])
    sel_d = ctx.din("sel", [16, 16, 128])
    ident_d = ctx.din("ident", [128, 128])
    out = ctx.dout("out", [n_tok, D]) if (fused is None or "x1_d" not in fused) else None
    n_sub = n_tok // 128
    n_tile = n_tok // 512

    with ExitStack() as st:
        V, A, G = nc.vector, nc.scalar, nc.gpsimd
        w_o = sb(nc, st, "w_o", [128, 8, D], BF16)
        lng = [sb(nc, st, f"lnp{i}", [128, D], F32) for i in range(4)]
        w_r = sb(nc, st, "w_r", [128, 8, NE], F32)
        b_r = sb(nc, st, "b_r", [128, NE], F32)
        sel = sb(nc, st, "sel_s", [16, 16, 128], BF16)
        ident = sb(nc, st, "ident_s", [128, 128], F32)
        eps_t = sb(nc, st, "eps_t", [128, 1], F32)
        acc = sb(nc, st, "acc", [128, n_sub, D], F32)
        x1T = sb(nc, st, "x1T", [128, 8, n_tok], BF16)
        gT = sb(nc, st, "gT", [16, n_tok], BF16)
        y_t = [sb(nc, st, "y_t0", [128, 8, 128], BF16)] * 2
        xr = [sb(nc, st, "xr0", [128, D], F32)] * 2
        u = sb(nc, st, "u", [128, D], F32)
        x1 = sb(nc, st, "x1", [128, D], F32)
        stats = sb(nc, st, "stats", [128, 2, 6], F32)
        mv = sb(nc, st, "mv", [128, 2], F32)
        rstd = sb(nc, st, "rstd", [128, 1], F32)
        xTf = sb(nc, st, "xTf", [128, 8, 128], F32)
        sg_ = sb(nc, st, "r_s", [128, 16], F32)
        bi_ = sb(nc, st, "r_bi", [128, 16], F32)
        b2_ = sb(nc, st, "r_b2", [128, 16], F32)
        eq_ = sb(nc, st, "r_eq", [128, 16], F32)
        m1_ = sb(nc, st, "r_m1", [128, 4], F32)
        m2_ = sb(nc, st, "r_m2", [128, 4], F32)
        gs_ = sb(nc, st, "r_gs", [128, 4], F32)
        gm_ = sb(nc, st, "r_gm", [128, 1], F32)
        ig_ = sb(nc, st, "r_ig", [128, 4], F32)
        se_ = sb(nc, st, "r_se", [128, 16], F32)
        ws_ = sb(nc, st, "r_ws", [128, 1], F32)
        gate = sb(nc, st, "gate", [128, 16], F32)
        wg_s = [sb(nc, st, f"wg_s{i}", [128, 8, DFF], BF16) for i in range(2)]
        wu_s = [sb(nc, st, f"wu_s{i}", [128, 8, DFF], BF16) for i in range(2)]
        wd_s = [sb(nc, st, f"wd_s{i}", [128, 2, D], BF16) for i in range(2)]
        gb = [sb(nc, st, f"gb{i}", [128, 512], BF16) for i in range(2)]
        sgl = [sb(nc, st, f"sgl{i}", [128, 512], BF16) for i in range(2)]
        t1 = [sb(nc, st, f"t1{i}", [128, 512], BF16) for i in range(2)]
        hT = [sb(nc, st, f"hT{i}", [128, 2, 512], BF16) for i in range(2)]
        ot = [sb(nc, st, "ot0", [128, D], F32)] * 2

        pb = [ps(nc, st, f"pb{i}", [128, 512]) for i in range(8)]
        PK = [f"pb{i}" for i in range(8)]

        kb.dma("pool", w_o[:], wout.rearrange("(kc p) n -> p kc n", p=128), writes=["w_o"])
        for i in range(4):
            kb.dma("sp", lng[i][:], lnp[i:i + 1, :].partition_broadcast(128), writes=[f"lnp{i}"])
        kb.dma("sp", w_r[:], wr.rearrange("(kc p) n -> p kc n", p=128), writes=["w_r"])
        kb.dma("sp", b_r[:], br.partition_broadcast(128), writes=["b_r"])
        kb.dma("pool", sel[:], sel_d, writes=["sel"])
        kb.dma("sp", ident[:], ident_d, writes=["ident"])
        kb.op("dve", lambda: V.memset(eps_t[:], LN_EPS), writes=["eps_t"])

        def load_expert(e):
            i = e % 2
            kb.dma("pool", wg_s[i][:], wgd[e].rearrange("(kc p) f -> p kc f", p=128), writes=[f"wg{i}"])
            kb.dma("pool", wu_s[i][:], wud[e].rearrange("(kc p) f -> p kc f", p=128), writes=[f"wu{i}"])
            kb.dma("pool", wd_s[i][:], wdd[e].rearrange("(fc p) d -> p fc d", p=128), writes=[f"wd{i}"])

        def layer_norm(src, dst, gi, eng2):
            s_t, s_k = src
            d_t, d_k = dst
            for hh in range(2):
                kb.op("dve", lambda: V.bn_stats(stats[:, hh, :], s_t[:, hh * 512:(hh + 1) * 512]),
                      reads=[s_k], writes=["stats"])
            kb.op("dve", lambda: V.bn_aggr(mv[:], stats[:]), reads=["stats"], writes=["mv"])
            kb.act(rstd[:], mv[:, 1:2], AF.Sqrt, reads=["mv"], writes=["rstd"], bias=eps_t[:])
            kb.op("dve", lambda: V.reciprocal(rstd[:], rstd[:]), reads=["rstd"], writes=["rstd"])
            kb.op("dve", lambda: V.tensor_scalar(out=d_t, in0=s_t, scalar1=mv[:, 0:1], scalar2=rstd[:, 0:1],
                                                 op0=ALU.subtract, op1=ALU.mult),
                  reads=[s_k, "mv", "rstd"], writes=[d_k])
            kb.op("pool", lambda: G.tensor_tensor(out=d_t, in0=d_t, in1=lng[gi][:], op=ALU.mult),
                  reads=[d_k, f"lnp{gi}"], writes=[d_k])
            kb.op("pool", lambda: G.tensor_tensor(out=d_t, in0=d_t, in1=lng[gi + 1][:], op=ALU.add),
                  reads=[d_k, f"lnp{gi + 1}"], writes=[d_k])

        load_expert(0)
        if fused is None:
            yTv = yT.rearrange("(kc p) t -> p kc t", p=128)
        else:
            g_v = [gq.rearrange("(h t) c -> t h c", h=4) for gq in fused["g"]]
            cand = [sb(nc, st, "cand0", [128, 4, D], BF16)] * 2
            ysel = sb(nc, st, "ysel", [128, D], BF16)
            qsel = sb(nc, st, "qsel_s", [128, 4], F32)
            identb = sb(nc, st, "identb", [128, 128], BF16)
            xo = [sb(nc, st, "xo0", [128, 8, 128], BF16)] * 2
            kb.dma("sp", qsel[:], fused["qsel"], writes=["qsel"])
            kb.dma("pool", identb[:], ident_d, writes=["identb"])
        for sub in range(n_sub):
            ts_ = slice(sub * 128, (sub + 1) * 128)
            yk = "y_t0"
            xk = "xr0"
            if fused is None:
                kb.dma("sp", y_t[sub % 2][:], yTv[:, :, ts_], writes=[yk])
            else:
                ck = "cand0"
                for qq in range(4):
                    kb.dma("sp", cand[sub % 2][:, qq, :].rearrange("p (h c) -> p h c", h=4),
                           g_v[qq][sub * 128:(sub + 1) * 128, :, :], writes=[ck])
                kb.op("pool", lambda: G.tensor_scalar(out=ysel[:], in0=cand[sub % 2][:, 0, :], scalar1=qsel[:, 0:1],
                                                      scalar2=None, op0=ALU.mult), reads=[ck, "qsel"], writes=["ysel"])
                for qq in range(1, 4):
                    kb.op("dve", lambda: V.scalar_tensor_tensor(out=ysel[:], in0=cand[sub % 2][:, qq, :],
                                                                 scalar=qsel[:, qq:qq + 1], in1=ysel[:],
                                                                 op0=ALU.mult, op1=ALU.add),
                          reads=[ck, "qsel", "ysel"], writes=["ysel"])
                pTb = pb[7][:].bitcast(BF16)
                for kc in range(8):
                    kb.op("pe", lambda: nc.tensor.transpose(pTb[:, kc * 128:(kc + 1) * 128],
                                                            ysel[:, kc * 128:(kc + 1) * 128], identb[:]),
                          reads=["ysel", "identb"], writes=[PK[7]])
                kb.op("dve", lambda: V.tensor_copy(y_t[sub % 2][:], pTb.rearrange("p (k t) -> p k t", k=8)),
                      reads=[PK[7]], writes=[yk])
            kb.dma("sp", xr[sub % 2][:], xres[ts_, :], writes=[xk])
            for hh in range(2):
                for kc in range(8):
                    kb.mm(pb[hh][:], y_t[sub % 2][:, kc, :], w_o[:, kc, hh * 512:(hh + 1) * 512],
                          kc == 0, kc == 7, reads=[yk, "w_o"], writes=[PK[hh]])
            for hh in range(2):
                kb.op("dve", lambda: V.scalar_tensor_tensor(
                    out=u[:, hh * 512:(hh + 1) * 512], in0=xr[sub % 2][:, hh * 512:(hh + 1) * 512],
                    scalar=float(DN_ALPHA), in1=pb[hh][:], op0=ALU.mult, op1=ALU.add),
                    reads=[xk, PK[hh]], writes=["u"])
            layer_norm((u[:], "u"), (x1[:], "x1"), 0, None)
            kb.op("pool", lambda: G.tensor_scalar(out=acc[:, sub, :], in0=x1[:], scalar1=float(DN_ALPHA),
                                                  scalar2=None, op0=ALU.mult),
                  reads=["x1"], writes=[f"acc{sub}"])
            for kc in range(8):
                bank = 2 + kc // 4
                kb.op("pe", lambda: nc.tensor.transpose(pb[bank][:, (kc % 4) * 128:(kc % 4 + 1) * 128],
                                                        x1[:, kc * 128:(kc + 1) * 128], ident[:]),
                      reads=["x1", "ident"], writes=[PK[bank]])
            for q in range(2):
                kb.op("dve" if q == 0 else "act",
                      (lambda: V.tensor_copy(xTf[:, 0:4, :], pb[2][:].rearrange("p (k t) -> p k t", k=4))) if q == 0
                      else (lambda: A.copy(xTf[:, 4:8, :], pb[3][:].rearrange("p (k t) -> p k t", k=4))),
                      reads=[PK[2 + q]], writes=[f"xTf{q}"])
            kb.op("pool", lambda: G.tensor_copy(x1T[:, :, ts_], xTf[:]), reads=["xTf0", "xTf1"], writes=["x1T"])
            for kc in range(8):
                kb.mm(pb[4][:, 0:16], xTf[:, kc, :], w_r[:, kc, :], kc == 0, kc == 7,
                      reads=["xTf0", "xTf1", "w_r"], writes=[PK[4]])
            kb.act(sg_[:], pb[4][:, 0:16], AF.Sigmoid, reads=[PK[4]], writes=["r_s"])
            kb.op("dve", lambda: V.tensor_tensor(out=bi_[:], in0=sg_[:], in1=b_r[:], op=ALU.add),
                  reads=["r_s", "b_r"], writes=["r_bi"])
            bi3 = bi_[:].rearrange("p (g e) -> p g e", g=4)
            kb.op("dve", lambda: V.tensor_reduce(out=m1_[:], in_=bi3, axis=AX.X, op=ALU.max),
                  reads=["r_bi"], writes=["r_m1"])
            kb.op("dve", lambda: V.tensor_tensor(out=eq_[:].rearrange("p (g e) -> p g e", g=4), in0=bi3,
                                                 in1=m1_[:].unsqueeze(2).to_broadcast([128, 4, 4]), op=ALU.is_equal),
                  reads=["r_bi", "r_m1"], writes=["r_eq"])
            kb.op("dve", lambda: V.scalar_tensor_tensor(out=b2_[:], in0=eq_[:], scalar=-1e30, in1=bi_[:],
                                                        op0=ALU.mult, op1=ALU.add),
                  reads=["r_eq", "r_bi"], writes=["r_b2"])
            kb.op("dve", lambda: V.tensor_reduce(out=m2_[:], in_=b2_[:].rearrange("p (g e) -> p g e", g=4),
                                                 axis=AX.X, op=ALU.max),
                  reads=["r_b2"], writes=["r_m2"])
            kb.op("dve", lambda: V.tensor_tensor(out=gs_[:], in0=m1_[:], in1=m2_[:], op=ALU.add),
                  reads=["r_m1", "r_m2"], writes=["r_gs"])
            kb.op("dve", lambda: V.tensor_reduce(out=gm_[:], in_=gs_[:], axis=AX.X, op=ALU.max),
                  reads=["r_gs"], writes=["r_gm"])
            kb.op("dve", lambda: V.tensor_scalar(out=ig_[:], in0=gs_[:], scalar1=gm_[:, 0:1], scalar2=None,
                                                 op0=ALU.is_ge),
                  reads=["r_gs", "r_gm"], writes=["r_ig"])
            kb.op("dve", lambda: V.tensor_tensor(out=se_[:].rearrange("p (g e) -> p g e", g=4), in0=bi3,
                                                 in1=m2_[:].unsqueeze(2).to_broadcast([128, 4, 4]), op=ALU.is_ge),
                  reads=["r_bi", "r_m2"], writes=["r_se"])
            kb.op("dve", lambda: V.tensor_tensor(out=se_[:].rearrange("p (g e) -> p g e", g=4),
                                                 in0=se_[:].rearrange("p (g e) -> p g e", g=4),
                                                 in1=ig_[:].unsqueeze(2).to_broadcast([128, 4, 4]), op=ALU.mult),
                  reads=["r_se", "r_ig"], writes=["r_se"])
            kb.op("dve", lambda: V.tensor_tensor(out=se_[:], in0=se_[:], in1=sg_[:], op=ALU.mult),
                  reads=["r_se", "r_s"], writes=["r_se"])
            kb.op("dve", lambda: V.tensor_reduce(out=ws_[:], in_=se_[:], axis=AX.X, op=ALU.add),
                  reads=["r_se"], writes=["r_ws"])
            kb.op("dve", lambda: V.reciprocal(ws_[:], ws_[:]), reads=["r_ws"], writes=["r_ws"])
            kb.op("dve", lambda: V.tensor_scalar(out=gate[:], in0=se_[:], scalar1=ws_[:, 0:1], scalar2=None,
                                                 op0=ALU.mult),
                  reads=["r_se", "r_ws"], writes=["gate"])
            kb.op("pe", lambda: nc.tensor.transpose(pb[5][0:16, 0:128], gate[:], ident[:]),
                  reads=["gate", "ident"], writes=[PK[5]])
            kb.op("dve", lambda: V.tensor_copy(gT[:, ts_], pb[5][0:16, 0:128]), reads=[PK[5]], writes=["gT"])

        for e in range(n_exp):
            i = e % 2
            if e + 1 < n_exp:
                load_expert(e + 1)
            for T in range(n_tile):
                Ts = slice(T * 512, (T + 1) * 512)
                j = (e * n_tile + T) % 2
                kb.mm(pb[4][:], sel[:, e, :], gT[:, Ts], True, True, reads=["sel", "gT"], writes=[PK[4]])
                kb.op("act", lambda: A.copy(gb[j][:], pb[4][:]), reads=[PK[4]], writes=[f"gb{j}"])
                for fc in range(2):
                    for kc in range(8):
                        kb.mm(pb[fc][:], wg_s[i][:, kc, fc * 128:(fc + 1) * 128], x1T[:, kc, Ts],
                              kc == 0, kc == 7, reads=[f"wg{i}", "x1T"], writes=[PK[fc]])
                    for kc in range(8):
                        kb.mm(pb[2 + fc][:], wu_s[i][:, kc, fc * 128:(fc + 1) * 128], x1T[:, kc, Ts],
                              kc == 0, kc == 7, reads=[f"wu{i}", "x1T"], writes=[PK[2 + fc]])
                for fc in range(2):
                    kb.act(sgl[fc][:], pb[fc][:], AF.Silu, reads=[PK[fc]], writes=[f"sgl{fc}"])
                    kb.op("dve", lambda: V.tensor_tensor(out=t1[fc][:], in0=sgl[fc][:], in1=pb[2 + fc][:], op=ALU.mult),
                          reads=[f"sgl{fc}", PK[2 + fc]], writes=[f"t1{fc}"])
                    kb.op("pool", lambda: G.tensor_tensor(out=hT[j][:, fc, :], in0=t1[fc][:], in1=gb[j][:], op=ALU.mult),
                          reads=[f"t1{fc}", f"gb{j}"], writes=[f"hT{j}"])
                for s4 in range(4):
                    sub = T * 4 + s4
                    for hh in range(2):
                        bank = 5 + (s4 * 2 + hh) % 3
                        for fc in range(2):
                            kb.mm(pb[bank][:], hT[j][:, fc, s4 * 128:(s4 + 1) * 128],
                                  wd_s[i][:, fc, hh * 512:(hh + 1) * 512], fc == 0, fc == 1,
                                  reads=[f"hT{j}", f"wd{i}"], writes=[PK[bank]])
                        kb.op("dve", lambda: V.tensor_tensor(out=acc[:, sub, hh * 512:(hh + 1) * 512],
                                                             in0=acc[:, sub, hh * 512:(hh + 1) * 512],
                                                             in1=pb[bank][:], op=ALU.add),
                              reads=[f"acc{sub}", PK[bank]], writes=[f"acc{sub}"])

        for sub in range(n_sub):
            o_t = ot[sub % 2]
            layer_norm((acc[:, sub, :], f"acc{sub}"), (o_t[:], "ot0"), 2, None)
            if out is not None:
                kb.dma("sp", out[sub * 128:(sub + 1) * 128, :], o_t[:], reads=["ot0"], writes=["out"], force=True)
            else:
                ok_ = "ot0"
                kb.dma("sp", fused["x1_d"][sub * 128:(sub + 1) * 128, :], o_t[:], reads=[ok_], writes=["x1_d"])
                for kc in range(8):
                    bank = 2 + kc // 4
                    kb.op("pe", lambda: nc.tensor.transpose(pb[bank][:, (kc % 4) * 128:(kc % 4 + 1) * 128],
                                                            o_t[:, kc * 128:(kc + 1) * 128], ident[:]),
                          reads=[ok_, "ident"], writes=[PK[bank]])
                xk_ = "xo0"
                kb.op("dve", lambda: V.tensor_copy(xo[sub % 2][:, 0:4, :], pb[2][:].rearrange("p (k t) -> p k t", k=4)),
                      reads=[PK[2]], writes=[xk_])
                kb.op("dve", lambda: V.tensor_copy(xo[sub % 2][:, 4:8, :], pb[3][:].rearrange("p (k t) -> p k t", k=4)),
                      reads=[PK[3]], writes=[xk_])
                kb.dma("sp", fused["x1T_d"].rearrange("(kc p) t -> p kc t", p=128)[:, :, sub * 128:(sub + 1) * 128],
                       xo[sub % 2][:], reads=[xk_], writes=["x1T_d"])
        kb.barrier()


def ffn_inputs(y_tok_major, xres, w_out, ln_g, ln_b, w_router, b_router, w_gate, w_up, w_down):
    sel, ident = moe_consts()
    lnp = np.ascontiguousarray(np.stack([ln_g[0], ln_b[0], ln_g[1], ln_b[1]]).astype(np.float32))
    maps = []
    for c in range(NCORES):
        rs_ = slice(c * NTB, (c + 1) * NTB)
        maps.append({
            "yT": np.ascontiguousarray(y_tok_major[rs_].T),
            "xres": np.ascontiguousarray(xres[rs_]),
            "wout": w_out, "lnp": lnp, "wr": w_router,
            "br": np.ascontiguousarray(b_router.reshape(1, NE)),
            "wg": w_gate, "wu": w_up, "wd": w_down, "sel": sel, "ident": ident,
        })
    return maps


NSA_SCALE = 0.125
MASK_NEG = -240000.0


def nsa_consts(n_tok=S):
    nqb = n_tok // 128
    inv = np.power(500000.0, -np.arange(8, dtype=np.float32) * (2.0 / 16.0)).astype(np.float32)
    def cs(pos):
        ang = pos.astype(np.float32)[:, None] * inv[None, :]
        return np.concatenate([np.cos(ang), np.sin(ang)], axis=1).astype(np.float32)
    def pl(a):
        n = a.shape[0] // 128
        return np.ascontiguousarray(a.reshape(n, 128, 16).transpose(1, 0, 2).reshape(128, n * 16))
    cs_tok = pl(cs(np.arange(n_tok)))
    cs_cmp = pl(cs(np.arange(512) * 16 + 31))
    wimp = np.zeros((512, 128), np.float32)
    for s_ in range(128):
        for o, wgt in enumerate([1, 2, 2, 2, 1]):
            c = 4 * s_ + o
            if c < 511:
                wimp[c, s_] = wgt
    texp = ((np.arange(n_tok)[None, :] // 64) % 64 == np.arange(64)[:, None]).astype(np.float32)
    k = np.arange(128)[:, None]
    q = np.arange(128)[None, :]
    causal = (k <= q).astype(np.float32)
    strict = (k > q).astype(np.float32)
    cmask = np.zeros((nqb, 2, 128, 128), np.float32)
    for qi in range(nqb):
        jl = (8 * qi + 6) // 128
        for slot, jt in ((0, jl - 1), (1, jl)):
            if jt < 0:
                continue
            j = 128 * jt + k
            cmask[qi, slot] = (16 * j + 31 <= 128 * qi + q)
    ident = np.eye(128, dtype=np.float32)
    return dict(cs_tok=cs_tok, cs_cmp=cs_cmp, wimp=wimp, texp=texp, causal=causal, strict=strict,
                cmask=cmask, ident=ident)


def build_nsa(n_tok=S, limit=None):
    return _standalone(emit_nsa, n_tok=n_tok, limit=limit)


def emit_nsa(ctx, n_tok=S, g2=None):
    nc, kb = ctx.nc, ctx.kb
    limit = kb.limit
    nqb = n_tok // 128
    ncb = n_tok // 16 - 1
    ncp = ((ncb + 127) // 128) * 128
    njt_all = ncp // 128
    din = ctx.din
    xT = din("xT", [D, n_tok]) if g2 is None else None
    wn = din("wn", [D, 652])
    cs_tok_d = din("cs_tok", [128, nqb * 16])
    cs_cmp_d = din("cs_cmp", [128, 64])
    wimp_d = din("wimp", [512, 128])
    texp_d = din("texp", [64, n_tok])
    causal_d = din("causal", [128, 128])
    strict_d = din("strict", [128, 128])
    cmask_d = din("cmask", [nqb, 2, 128, 128])
    ident_d = din("ident", [128, 128])
    wk1_d = din("wk1", [2048, 256])
    wk2_d = din("wk2", [256, 64])
    wv1_d = din("wv1", [2048, 256])
    wv2_d = din("wv2", [256, 64])
    peT_d = din("peT", [64, 32])
    y = ctx.dout("y", [n_tok, 256], BF16)

    with ExitStack() as st:
        V, A, G = nc.vector, nc.scalar, nc.gpsimd
        identb = sb(nc, st, "identb", [128, 128], BF16)
        QT = sb(nc, st, "QT", [64, 4, n_tok], BF16)
        KST = sb(nc, st, "KST", [128, n_tok], BF16)
        KWT = sb(nc, st, "KWT", [64, n_tok], BF16)
        VS = sb(nc, st, "VS", [128, nqb, 65], BF16)
        VW = sb(nc, st, "VW", [128, nqb, 65], BF16)
        G_all = sb(nc, st, "G_all", [128, nqb, 12], F32)
        KCMT = sb(nc, st, "KCMT", [64, ncp], BF16)
        RC = sb(nc, st, "RC", [128, njt_all, 193], BF16)
        st1 = ExitStack()
        w_n = sb(nc, st1, "w_n", [128, 8, 652], BF16)
        cs_tok = sb(nc, st1, "cs_tok_s", [128, nqb, 16], F32)
        cs_cmp = sb(nc, st1, "cs_cmp_s", [128, 4, 16], F32)
        KCT = sb(nc, st1, "KCT", [64, n_tok], BF16)
        VCT = sb(nc, st1, "VCT", [64, n_tok], BF16)
        xt = [sb(nc, st1, f"xt{i}", [128, 8, 128], BF16) for i in range(2)]
        pr = sb(nc, st1, "pr", [128, 652], F32)
        rp = sb(nc, st1, "rp", [128, 8, 64], BF16)
        ra = sb(nc, st1, "ra", [128, 6, 8], F32)
        rb = sb(nc, st1, "rb", [128, 6, 8], F32)
        w1 = sb(nc, st1, "w1", [64, 32, 256], BF16)
        w2 = sb(nc, st1, "w2", [128, 2, 64], BF16)
        peT = sb(nc, st1, "peT_s", [64, 32], BF16)
        hb = sb(nc, st1, "hb", [128, 2], F32)
        h1T = sb(nc, st1, "h1T", [128, 2, ncp], BF16)
        kc_f = sb(nc, st1, "kc_f", [128, 64], F32)
        kc_b = sb(nc, st1, "kc_b", [128, 64], BF16)

        pA = ps(nc, st, "pA", [128, 512])
        pB = ps(nc, st, "pB", [128, 512])
        pT = ps(nc, st, "pT", [128, 1024], BF16)
        pS = [ps(nc, st, f"pS{i}", [128, 512]) for i in range(3)]
        pC = pA
        pSL = ps(nc, st, "pSL", [128, 512])
        pW = ps(nc, st, "pW", [128, 512])

        kb.dma("pool", w_n[:], wn.rearrange("(kc p) n -> p kc n", p=128), writes=["w_n"])
        kb.dma("sp", cs_tok[:], cs_tok_d.rearrange("p (n c) -> p n c", c=16), writes=["cs_tok"])
        kb.dma("sp", cs_cmp[:], cs_cmp_d.rearrange("p (n c) -> p n c", c=16), writes=["cs_cmp"])
        kb.dma("pool", identb[:], ident_d, writes=["identb"])
        kb.dma("pool", peT[:], peT_d, writes=["peT"])
        kb.op("dve", lambda: V.memset(VS[:, :, 64:65], 1.0), writes=["VS"])
        kb.op("dve", lambda: V.memset(VW[:, :, 64:65], 1.0), writes=["VW"])
        kb.op("dve", lambda: V.memset(RC[:, :, 64:65], 1.0), writes=["RC"])
        kb.op("dve", lambda: V.memset(h1T[:], 0.0), writes=["h1T"])
        kb.dma("pool", RC[:, :, 65:193], wimp_d[0:ncp, :].rearrange("(n p) s -> p n s", p=128), writes=["RC"])
        kb.dma("pool", KST[64:128, :], texp_d, writes=["KSTa"])

        if g2 is None:
            xTv = xT.rearrange("(kc p) t -> p kc t", p=128)
        else:
            g2v = [gj.rearrange("(q k2 p) t -> q p k2 t", q=4, k2=2, p=128) for gj in g2]

        def rope(src3, dst3, cs_ap, nh, csk):
            cosb = cs_ap[:, 0:8].unsqueeze(1).to_broadcast([128, nh, 8])
            sinb = cs_ap[:, 8:16].unsqueeze(1).to_broadcast([128, nh, 8])
            a_, b_ = ra[:, 0:nh, :], rb[:, 0:nh, :]
            kb.op("pool", lambda: G.tensor_tensor(out=a_, in0=src3[:, :, 0:8], in1=cosb, op=ALU.mult),
                  reads=["rsrc", csk], writes=["ra"])
            kb.op("pool", lambda: G.tensor_tensor(out=b_, in0=src3[:, :, 8:16], in1=sinb, op=ALU.mult),
                  reads=["rsrc", csk], writes=["rb"])
            kb.op("pool", lambda: G.tensor_tensor(out=dst3[:, :, 0:8], in0=a_, in1=b_, op=ALU.subtract),
                  reads=["ra", "rb"], writes=["rdst"])
            kb.op("pool", lambda: G.tensor_tensor(out=a_, in0=src3[:, :, 8:16], in1=cosb, op=ALU.mult),
                  reads=["rsrc", csk, "rdst"], writes=["ra"])
            kb.op("pool", lambda: G.tensor_tensor(out=b_, in0=src3[:, :, 0:8], in1=sinb, op=ALU.mult),
                  reads=["rsrc", csk, "rdst"], writes=["rb"])
            kb.op("pool", lambda: G.tensor_tensor(out=dst3[:, :, 8:16], in0=a_, in1=b_, op=ALU.add),
                  reads=["ra", "rb"], writes=["rdst"])
            kb.op("pool", lambda: G.tensor_copy(dst3[:, :, 16:64], src3[:, :, 16:64]),
                  reads=["rsrc"], writes=["rdst"])

        for T in range(nqb):
            Ts = slice(T * 128, (T + 1) * 128)
            x_t = xt[T % 2]
            xk = f"xt{T % 2}"
            if g2 is None:
                kb.dma("pool", x_t[:], xTv[:, :, Ts], writes=[xk])
            else:
                qq, tl = T // 16, T % 16
                for j in range(4):
                    kb.dma("sp", x_t[:, 2 * j:2 * j + 2, :], g2v[j][qq][:, :, tl * 128:(tl + 1) * 128], writes=[xk])
            for kc in range(8):
                kb.mm(pA[:], x_t[:, kc, :], w_n[:, kc, 0:512], kc == 0, kc == 7, reads=[xk, "w_n"], writes=["pA"])
            for kc in range(8):
                kb.mm(pB[:, 0:140], x_t[:, kc, :], w_n[:, kc, 512:652], kc == 0, kc == 7,
                      reads=[xk, "w_n"], writes=["pB"])
            kb.op("dve", lambda: V.tensor_copy(pr[:, 0:512], pA[:]), reads=["pA", "rdst"], writes=["rsrc"])
            kb.op("dve", lambda: V.tensor_copy(pr[:, 512:640], pB[:, 0:128]), reads=["pB"], writes=["prv"])
            kb.act(G_all[:, T, :], pB[:, 128:140], AF.Sigmoid, reads=["pB"], writes=["G_all"])
            kb.op("pool", lambda: G.tensor_copy(VS[:, T, 0:64], pr[:, 512:576]), reads=["prv"], writes=["VS"])
            kb.op("pool", lambda: G.tensor_copy(VW[:, T, 0:64], pr[:, 576:640]), reads=["prv"], writes=["VW"])
            rope(pr[:, 0:384].rearrange("p (s d) -> p s d", s=6), rp[:, 0:6, :], cs_tok[:, T, :], 6, "cs_tok")
            kb.op("pool", lambda: G.tensor_copy(rp[:, 6:8, :], pr[:, 384:512].rearrange("p (s d) -> p s d", s=2)),
                  reads=["rsrc"], writes=["rdst"])
            for s_ in range(8):
                kb.op("pe", lambda: nc.tensor.transpose(pT[0:64, s_ * 128:(s_ + 1) * 128], rp[:, s_, :], identb[:]),
                      reads=["rdst", "identb"], writes=["pT"])
            kb.op("dve", lambda: V.tensor_copy(QT[:, :, Ts], pT[0:64, 0:512].rearrange("p (h t) -> p h t", h=4)),
                  reads=["pT"], writes=["QT"])
            kb.op("dve", lambda: V.tensor_copy(KST[0:64, Ts], pT[0:64, 512:640]), reads=["pT"], writes=["KST"])
            kb.op("dve", lambda: V.tensor_copy(KWT[:, Ts], pT[0:64, 640:768]), reads=["pT"], writes=["KWT"])
            kb.op("dve", lambda: V.tensor_copy(KCT[:, Ts], pT[0:64, 768:896]), reads=["pT"], writes=["KCT"])
            kb.op("dve", lambda: V.tensor_copy(VCT[:, Ts], pT[0:64, 896:1024]), reads=["pT"], writes=["VCT"])

        for which, (w1d, w2d, srcT, srck) in enumerate(((wk1_d, wk2_d, KCT, "KCT"), (wv1_d, wv2_d, VCT, "VCT"))):
            kb.dma("pool", w1[:], w1d.rearrange("(r d) h -> d r h", d=64), writes=["w1"])
            kb.dma("pool", w2[:], w2d.rearrange("(hc p) d -> p hc d", p=128), writes=["w2"])
            for hc in range(2):
                for r in range(32):
                    kb.mm(pS[0][:, 0:1], w1[:, r, hc * 128:(hc + 1) * 128], peT[:, r:r + 1], r == 0, r == 31,
                          reads=["w1", "peT"], writes=["pS0"])
                kb.op("dve", lambda: V.tensor_copy(hb[:, hc:hc + 1], pS[0][:, 0:1]), reads=["pS0"], writes=["hb"])
            for hc in range(2):
                for c0 in range(0, ncb, 512):
                    n_ = min(512, ncb - c0)
                    for r in range(32):
                        kb.mm(pS[1][:, 0:n_], w1[:, r, hc * 128:(hc + 1) * 128],
                              srcT[:, 16 * c0 + r:16 * c0 + r + 16 * (n_ - 1) + 1:16], r == 0, r == 31,
                              reads=["w1", srck], writes=["pS1"])
                    kb.act(h1T[:, hc, c0:c0 + n_], pS[1][:, 0:n_], AF.Silu, reads=["pS1", "hb"], writes=["h1T"],
                           bias=hb[:, hc:hc + 1])
            for jt in range(njt_all):
                for hc in range(2):
                    kb.mm(pS[2][:, 0:64], h1T[:, hc, jt * 128:(jt + 1) * 128], w2[:, hc, :], hc == 0, hc == 1,
                          reads=["h1T", "w2"], writes=["pS2"])
                if which == 0:
                    kb.op("dve", lambda: V.tensor_copy(kc_f[:], pS[2][:, 0:64]), reads=["pS2", "rdst"], writes=["rsrc"])
                    rope(kc_f[:].unsqueeze(1), kc_b[:].unsqueeze(1), cs_cmp[:, jt, :], 1, "cs_cmp")
                    kb.op("pe", lambda: nc.tensor.transpose(pT[0:64, 0:128], kc_b[:], identb[:]),
                          reads=["rdst", "identb"], writes=["pT"])
                    kb.op("dve", lambda: V.tensor_copy(KCMT[:, jt * 128:(jt + 1) * 128], pT[0:64, 0:128]),
                          reads=["pT"], writes=["KCMT"])
                else:
                    kb.op("dve", lambda: V.tensor_copy(RC[:, jt, 0:64], pS[2][:, 0:64]), reads=["pS2"], writes=["RC"])

        if limit is not None:
            print("n_inst after stage 2:", kb.n_inst)
        kb.barrier()
        st1.close()
        causal = sb(nc, st, "causal_s", [128, 128], BF16)
        strict = sb(nc, st, "strict_s", [128, 128], BF16)
        e_sb = [sb(nc, st, f"e_sb{i}", [128, 512], BF16) for i in range(3)]
        cm_sb = [sb(nc, st, f"cm_sb{i}", [128, 2, 128], BF16) for i in range(2)]
        impm = sb(nc, st, "impm", [128, 128], F32)
        impw = sb(nc, st, "impw", [128, 128], F32)
        m8 = sb(nc, st, "m8", [128, 16], F32)
        selb = sb(nc, st, "selb", [128, 192], BF16)
        QaLo = [sb(nc, st, f"QaLo{i}", [128, 512], BF16) for i in range(2)]
        QaHi = [sb(nc, st, f"QaHi{i}", [128, 512], BF16) for i in range(2)]
        kb.op("dve", lambda: V.memset(selb[:], 0.0), writes=["selb"])
        zz = sb(nc, st, "zz", [128, 12], F32)
        coef = sb(nc, st, "coef", [128, 12], F32)
        o_acc = sb(nc, st, "o_acc", [128, 256], F32)
        y_sb = [sb(nc, st, f"y_sb{i}", [128, 256], BF16) for i in range(2)]
        kb.dma("pool", causal[:], causal_d, writes=["causal"])
        kb.dma("pool", strict[:], strict_d, writes=["strict"])

        def exp_tile(bank, ei):
            kb.act(e_sb[ei][:], pS[bank][:], AF.Exp, reads=[f"pS{bank}"], writes=[f"e{ei}"], scale=NSA_SCALE)

        def mask_tile(ei, mask_ap, mkeys):
            e3 = e_sb[ei][:].rearrange("p (h q) -> p h q", h=4)
            kb.op("pool", lambda: G.tensor_tensor(out=e3, in0=e3, in1=mask_ap.unsqueeze(1).to_broadcast([128, 4, 128]),
                                                  op=ALU.mult),
                  reads=[f"e{ei}"] + mkeys, writes=[f"e{ei}"])

        rot = [0]

        def nxt():
            rot[0] += 1
            return rot[0] % 3

        PIPE = 2
        pipe = []

        def drain_one():
            pv0, post0 = pipe.pop(0)
            pv0()
            if post0 is not None:
                post0()

        def push_tile(qk, pv, post=None):
            qk()
            pipe.append((pv, post))
            while len(pipe) > PIPE:
                drain_one()

        def tile_a(qi, jt, jl, Qv, cmk):
            r_ = nxt()

            def qk():
                kb.mm(pS[r_][:], KCMT[:, jt * 128:(jt + 1) * 128], Qv, True, True, reads=["KCMT", "QT"], writes=[f"pS{r_}"])
                exp_tile(r_, r_)
                if jt >= jl - 1:
                    mask_tile(r_, cm_sb[qi % 2][:, 1 - (jl - jt), :], [cmk])

            def pv():
                for h in range(4):
                    bank = pA if h < 2 else pB
                    kb.mm(bank[:, (h % 2) * 193:(h % 2) * 193 + 193], e_sb[r_][:, h * 128:(h + 1) * 128], RC[:, jt, :],
                          jt == 0 and h % 2 == 0, jt == jl and h % 2 == 1, reads=[f"e{r_}", "RC"],
                          writes=["pA" if h < 2 else "pB"])
            return qk, pv

        def post_a1(qi):
            use_sel = qi >= 8
            Gq = G_all[:, qi, :].rearrange("p (h j) -> p h j", h=4)

            def post():
                for h in range(4):
                    bank = pA if h < 2 else pB
                    c0 = (h % 2) * 193
                    kb.op("dve", lambda: V.tensor_scalar_max(out=zz[:, h:h + 1], in0=bank[:, c0 + 64:c0 + 65], scalar1=1e-30),
                          reads=["pA" if h < 2 else "pB"], writes=["zz"])
                kb.op("dve", lambda: V.reciprocal(zz[:, 0:4], zz[:, 0:4]), reads=["zz"], writes=["zz"])
                if use_sel:
                    for h in range(4):
                        bank = pA if h < 2 else pB
                        bk = "pA" if h < 2 else "pB"
                        c0 = (h % 2) * 193
                        if h == 0:
                            kb.op("dve", lambda: V.tensor_scalar(out=impm[:], in0=bank[:, c0 + 65:c0 + 193],
                                                                 scalar1=zz[:, 0:1], scalar2=None, op0=ALU.mult),
                                  reads=[bk, "zz"], writes=["impm"])
                        else:
                            kb.op("dve", lambda: V.scalar_tensor_tensor(out=impm[:], in0=bank[:, c0 + 65:c0 + 193],
                                                                        scalar=zz[:, h:h + 1], in1=impm[:],
                                                                        op0=ALU.mult, op1=ALU.add),
                                  reads=[bk, "zz", "impm"], writes=["impm"])
                    c2 = 2 * qi
                    kb.op("dve", lambda: V.memset(impm[:, 0:1], 3e30), reads=[], writes=["impm"])
                    kb.op("dve", lambda: V.memset(impm[:, c2:c2 + 1], 2e30), writes=["impm"])
                    kb.op("dve", lambda: V.memset(impm[0:64, c2 - 1:c2], 1e30), writes=["impm"])
                    kb.op("dve", lambda: V.memset(impm[0:64, c2 + 1:c2 + 2], -1e30), writes=["impm"])
                    kb.op("dve", lambda: V.memset(impm[64:128, c2 + 1:c2 + 2], 2.5e30), writes=["impm"])
                    if c2 + 2 < 128:
                        kb.op("dve", lambda: V.memset(impm[:, c2 + 2:128], -1e30), writes=["impm"])
                    kb.op("dve", lambda: V.max(out=m8[:, 0:8], in_=impm[:]), reads=["impm"], writes=["m8"])
                    kb.op("dve", lambda: V.match_replace(out=impw[:], in_to_replace=m8[:, 0:8], in_values=impm[:],
                                                         imm_value=-1e30), reads=["impm", "m8"], writes=["impw"])
                    kb.op("dve", lambda: V.max(out=m8[:, 8:16], in_=impw[:]), reads=["impw"], writes=["m8"])
                    kb.op("dve", lambda: V.tensor_scalar(out=selb[:, 64:192], in0=impm[:], scalar1=m8[:, 15:16], scalar2=MASK_NEG,
                                                         op0=ALU.is_lt, op1=ALU.mult),
                          reads=["impm", "m8"], writes=["selb"])
                kb.op("dve", lambda: V.tensor_tensor(out=coef[:, 0:4], in0=zz[:, 0:4], in1=Gq[:, :, 0], op=ALU.mult),
                      reads=["zz", "G_all"], writes=["coef"])
                for h in range(4):
                    bank = pA if h < 2 else pB
                    bk = "pA" if h < 2 else "pB"
                    c0 = (h % 2) * 193
                    kb.op("dve", lambda: V.tensor_scalar(out=o_acc[:, h * 64:(h + 1) * 64], in0=bank[:, c0:c0 + 64],
                                                         scalar1=coef[:, h:h + 1], scalar2=None, op0=ALU.mult),
                          reads=[bk, "coef"], writes=["o_acc"])
            return post

        def post_a2(qi):
            b_ = qi % 2

            def post():
                kb.op("pe", lambda: nc.tensor.transpose(pT[:, 0:128], selb[:, 0:128], identb[:]),
                      reads=["selb", "identb"], writes=["pT"])
                if qi >= 32:
                    kb.op("pe", lambda: nc.tensor.transpose(pT[:, 128:256], selb[:, 64:192], identb[:]),
                          reads=["selb", "identb"], writes=["pT"])
                kb.op("dve", lambda: V.tensor_copy(QaLo[b_][64:128, :].rearrange("p (h q) -> p h q", h=4),
                                                   pT[64:128, 0:128].unsqueeze(1).to_broadcast([64, 4, 128])),
                      reads=["pT"], writes=[f"QaLoM{b_}"])
                if qi >= 32:
                    kb.op("dve", lambda: V.tensor_copy(QaHi[b_][64:128, :].rearrange("p (h q) -> p h q", h=4),
                                                       pT[64:128, 128:256].unsqueeze(1).to_broadcast([64, 4, 128])),
                          reads=["pT"], writes=[f"QaHiM{b_}"])
            return post

        def tile_c(qi, kt, Qv, use_sel):
            r_ = nxt()
            Ks = slice(kt * 128, (kt + 1) * 128)

            def qk():
                if use_sel:
                    b_ = qi % 2
                    if kt < 32:
                        kb.mm(pS[r_][:], KST[:, Ks], QaLo[b_][:], True, True,
                              reads=["KST", "KSTa", f"QaLoQ{b_}", f"QaLoM{b_}"], writes=[f"pS{r_}"])
                    else:
                        kb.mm(pS[r_][:], KST[:, Ks], QaHi[b_][:], True, True,
                              reads=["KST", "KSTa", f"QaHiQ{b_}", f"QaHiM{b_}"], writes=[f"pS{r_}"])
                else:
                    kb.mm(pS[r_][:], KST[0:64, Ks], Qv, True, True, reads=["KST", "QT"], writes=[f"pS{r_}"])
                exp_tile(r_, r_)
                if kt == qi:
                    mask_tile(r_, causal[:], ["causal"])

            def pv():
                for h in range(4):
                    kb.mm(pSL[:, h * 65:(h + 1) * 65], e_sb[r_][:, h * 128:(h + 1) * 128], VS[:, kt, :],
                          kt == 0 and h == 0, kt == qi and h == 3, reads=[f"e{r_}", "VS"], writes=["pSL"])
            return qk, pv

        def tile_d(qi, kt, k0, Qv):
            r_ = nxt()
            Ks = slice(kt * 128, (kt + 1) * 128)

            def qk():
                kb.mm(pS[r_][:], KWT[:, Ks], Qv, True, True, reads=["KWT", "QT"], writes=[f"pS{r_}"])
                exp_tile(r_, r_)
                if kt == qi:
                    mask_tile(r_, causal[:], ["causal"])
                elif kt == qi - 4:
                    mask_tile(r_, strict[:], ["strict"])

            def pv():
                for h in range(4):
                    kb.mm(pW[:, h * 65:(h + 1) * 65], e_sb[r_][:, h * 128:(h + 1) * 128], VW[:, kt, :],
                          kt == k0 and h == 0, kt == qi and h == 3, reads=[f"e{r_}", "VW"], writes=["pW"])
            return qk, pv

        def post_combine(qi, bi, final):
            Gq = G_all[:, qi, :].rearrange("p (h j) -> p h j", h=4)
            bank, bk = ((pSL, "pSL"), (pW, "pW"))[bi]
            Qs = slice(qi * 128, (qi + 1) * 128)

            def post():
                b3 = bank[:, 0:260].rearrange("p (h c) -> p h c", h=4)
                kb.op("dve", lambda: V.reciprocal(zz[:, 4 + 4 * bi:8 + 4 * bi], b3[:, :, 64]), reads=[bk], writes=["zz"])
                kb.op("dve", lambda: V.tensor_tensor(out=coef[:, 4 + 4 * bi:8 + 4 * bi], in0=zz[:, 4 + 4 * bi:8 + 4 * bi],
                                                     in1=Gq[:, :, 1 + bi], op=ALU.mult),
                      reads=["zz", "G_all"], writes=["coef"])
                for h in range(4):
                    kb.op("dve", lambda: V.scalar_tensor_tensor(
                        out=o_acc[:, h * 64:(h + 1) * 64], in0=b3[:, h, 0:64],
                        scalar=coef[:, 4 + 4 * bi + h:5 + 4 * bi + h], in1=o_acc[:, h * 64:(h + 1) * 64],
                        op0=ALU.mult, op1=ALU.add), reads=[bk, "coef", "o_acc"], writes=["o_acc"])
                if final:
                    yk = f"y_sb{qi % 2}"
                    kb.op("pool", lambda: G.tensor_copy(y_sb[qi % 2][:], o_acc[:]), reads=["o_acc"], writes=[yk])
                    kb.dma("sp", y[Qs, :], y_sb[qi % 2][:], reads=[yk], writes=["y_out"], force=True)
            return post

        for qi in range(nqb):
            Qs = slice(qi * 128, (qi + 1) * 128)
            Qv = QT[:, :, Qs]
            use_sel = qi >= 8
            jl = (8 * qi + 6) // 128
            cmk = f"cm{qi % 2}"
            kb.dma("pool", cm_sb[qi % 2][:], cmask_d[qi].rearrange("s j q -> j s q"), writes=[cmk])
            if use_sel:
                kb.op("pool", lambda: G.tensor_copy(QaLo[qi % 2][0:64, :].rearrange("p (h q) -> p h q", h=4), Qv),
                      reads=["QT"], writes=[f"QaLoQ{qi % 2}"])
                if qi >= 32:
                    kb.op("pool", lambda: G.tensor_copy(QaHi[qi % 2][0:64, :].rearrange("p (h q) -> p h q", h=4), Qv),
                          reads=["QT"], writes=[f"QaHiQ{qi % 2}"])
            for jt in range(jl + 1):
                qk, pv = tile_a(qi, jt, jl, Qv, cmk)
                push_tile(qk, pv, post_a1(qi) if jt == jl else None)
            k0 = max(0, qi - 4)
            for kt in range(k0, qi + 1):
                qk, pv = tile_d(qi, kt, k0, Qv)
                post = None
                if kt == qi:
                    post = post_combine(qi, 1, False)
                elif use_sel and kt == k0 + 2:
                    post = post_a2(qi)
                push_tile(qk, pv, post)
            for kt in range(qi + 1):
                qk, pv = tile_c(qi, kt, Qv, use_sel)
                push_tile(qk, pv, post_combine(qi, 0, True) if kt == qi else None)
        while pipe:
            drain_one()
        kb.barrier()


def nsa_inputs(x1, w_in, w_ck1, w_ck2, w_cv1, w_cv2, cmp_pe, n_tok=S):
    cst = nsa_consts(n_tok)
    maps = []
    for c in range(NCORES):
        b, g = c // 4, c % 4
        def col(base, width=64, mult=64):
            return w_in[:, base + g * mult: base + g * mult + width]
        q = w_in[:, g * 256:(g + 1) * 256]
        kc, vc, ks, vs, kw, vw = (col(1024), col(1280), col(1536), col(1792), col(2048), col(2304))
        gt = w_in[:, 2560 + g * 12:2560 + (g + 1) * 12]
        wn = np.ascontiguousarray(np.concatenate([q, ks, kw, kc, vc, vs, vw, gt], axis=1))
        m = {"xT": np.ascontiguousarray(x1[b].T[:, :n_tok]), "wn": wn,
             "wk1": w_ck1, "wk2": w_ck2, "wv1": w_cv1, "wv2": w_cv2,
             "peT": np.ascontiguousarray(cmp_pe.T)}
        m.update(cst)
        maps.append(m)
    return maps


GROUPS = [[0, 1, 2, 3], [4, 5, 6, 7]]


def build_fused():
    nc = bass.Bass("TRN2", target_bir_lowering=False)
    with ExitStack() as st0:
        kb = KB(nc, st0)
        cc_sem = st0.enter_context(nc.semaphore("cc_sem"))
        n_cc = [0]
        internal = lambda name, shape, dt: nc.dram_tensor(name, list(shape), dt).ap()
        y0_d = internal("y0_d", [S, 256], BF16)
        g1 = [internal(f"g1_{j}", [4 * NTB, 256], BF16) for j in range(4)]
        x1_d = internal("x1_d", [NTB, D], F32)
        x1T_d = internal("x1T_d", [D, NTB], BF16)
        g2 = [internal(f"g2_{j}", [4 * 256, NTB], BF16) for j in range(4)]
        y1_d = internal("y1_d", [S, 256], BF16)
        g3 = [internal(f"g3_{j}", [4 * NTB, 256], BF16) for j in range(4)]
        qsel = nc.dram_tensor("qsel", [128, 4], F32, kind="ExternalInput").ap()

        def all_gather(srcs, dsts):
            kb.barrier()
            for s_, d_ in zip(srcs, dsts):
                n_cc[0] += 1
                nc.gpsimd.collective_compute("AllGather", ALU.bypass, replica_groups=GROUPS,
                                             ins=[s_], outs=[d_]).then_inc(cc_sem, 1)
            for e in kb.engs.values():
                e.wait_ge(cc_sem, n_cc[0])

        emit_gla(Ctx(nc, kb, "a0_", {"y": y0_d}))
        all_gather([y0_d[j * NTB:(j + 1) * NTB, :] for j in range(4)], g1)
        emit_ffn(Ctx(nc, kb, "b0_"), fused={"g": g1, "qsel": qsel, "x1_d": x1_d, "x1T_d": x1T_d})
        all_gather([x1T_d[j * 256:(j + 1) * 256, :] for j in range(4)], g2)
        emit_nsa(Ctx(nc, kb, "a1_", {"y": y1_d}), g2=g2)
        all_gather([y1_d[j * NTB:(j + 1) * NTB, :] for j in range(4)], g3)
        emit_ffn(Ctx(nc, kb, "b1_", {"xres": x1_d}), fused={"g": g3, "qsel": qsel})
        kb.barrier()
    return nc


_PROGS = {}


def _prog(name, fn):
    if name not in _PROGS:
        _PROGS[name] = fn()
    return _PROGS[name]


def _run(nc, maps):
    res = run_bass_kernel_spmd(nc, maps, core_ids=list(range(NCORES)))
    return res.results


def _gather_heads(results, key="y"):
    first = np.asarray(results[0][key])
    full = np.empty((B * S, D), dtype=first.dtype)
    for c in range(NCORES):
        b, h = c // 4, c % 4
        full[b * S:(b + 1) * S, h * 256:(h + 1) * 256] = np.asarray(results[c][key])
    return full


def _pref(prefix, m, drop=()):
    return {prefix + k: v for k, v in m.items() if k not in drop}


def fused_inputs(x, gla_w_in, gla_w_gate_up, gla_b_gate, gla_norm_g, gla_w_out,
                 nsa_w_in, nsa_w_cmp_k1, nsa_w_cmp_k2, nsa_w_cmp_v1, nsa_w_cmp_v2, nsa_cmp_pe, nsa_w_out,
                 moe_w_router, moe_b_router, moe_w_gate, moe_w_up, moe_w_down, ln_g, ln_b):
    a0 = gla_inputs(x, gla_w_in[0], gla_w_gate_up[0], gla_b_gate[0], gla_norm_g[0])
    dummy_y = np.zeros((B * S, 1), np.float32)
    b0 = ffn_inputs(dummy_y, x.reshape(B * S, D), gla_w_out[0], ln_g[0], ln_b[0], moe_w_router, moe_b_router,
                    moe_w_gate[0], moe_w_up[0], moe_w_down[0])
    a1 = nsa_inputs(np.zeros((B, 1, S), np.float32), nsa_w_in[0], nsa_w_cmp_k1[0], nsa_w_cmp_k2[0],
                    nsa_w_cmp_v1[0], nsa_w_cmp_v2[0], nsa_cmp_pe[0])
    b1 = ffn_inputs(dummy_y, np.zeros((B * S, 1), np.float32), nsa_w_out[0], ln_g[1], ln_b[1], moe_w_router,
                    moe_b_router, moe_w_gate[1], moe_w_up[1], moe_w_down[1])
    maps = []
    for c in range(NCORES):
        m = {}
        m.update(_pref("a0_", a0[c]))
        m.update(_pref("b0_", b0[c], drop=("yT",)))
        m.update(_pref("a1_", a1[c], drop=("xT",)))
        m.update(_pref("b1_", b1[c], drop=("yT", "xres")))
        qs = np.zeros((128, 4), np.float32)
        qs[:, c % 4] = 1.0
        m["qsel"] = qs
        maps.append(m)
    return maps


def kernel(x, gla_w_in, gla_w_gate_up, gla_b_gate, gla_norm_g, gla_w_out,
           nsa_w_in, nsa_w_cmp_k1, nsa_w_cmp_k2, nsa_w_cmp_v1, nsa_w_cmp_v2, nsa_cmp_pe, nsa_w_out,
           moe_w_router, moe_b_router, moe_w_gate, moe_w_up, moe_w_down, ln_g, ln_b):
    f = lambda a: np.ascontiguousarray(np.asarray(a, dtype=np.float32))
    maps = fused_inputs(f(x), f(gla_w_in), f(gla_w_gate_up), f(gla_b_gate), f(gla_norm_g), f(gla_w_out),
                        f(nsa_w_in), f(nsa_w_cmp_k1), f(nsa_w_cmp_k2), f(nsa_w_cmp_v1), f(nsa_w_cmp_v2),
                        f(nsa_cmp_pe), f(nsa_w_out), f(moe_w_router), f(moe_b_router), f(moe_w_gate),
                        f(moe_w_up), f(moe_w_down), f(ln_g), f(ln_b))
    r = _run(_prog("fused", build_fused), maps)
    out = np.concatenate([np.asarray(r[c]["b1_out"]) for c in range(NCORES)], axis=0)
    return out.reshape(B, S, D).astype(np.float32)
```

```python
from contextlib import ExitStack

import numpy as np
import concourse.bass as bass
import concourse.mybir as mybir
from concourse.bass_utils import run_bass_kernel_spmd

F32 = mybir.dt.float32
BF16 = mybir.dt.bfloat16
AF = mybir.ActivationFunctionType
ALU = mybir.AluOpType
AX = mybir.AxisListType

D = 1024
B = 2
S = 8192
NCORES = 8
DN_ALPHA = 4.0 ** 0.25
LN_EPS = 1e-5


class KB:
    NDMA = 12

    def __init__(self, nc, stack):
        self.nc = nc
        self.stack = stack
        self.engs = {"pe": nc.tensor, "act": nc.scalar, "dve": nc.vector,
                     "pool": nc.gpsimd, "sp": nc.sync}
        self.sems = {}
        self.cnt = {}
        for e in ["pe", "act", "dve", "pool"]:
            self.sems[e] = stack.enter_context(nc.semaphore("c_" + e))
            self.cnt[e] = 0
        self.dq = {}
        for q in ["sp", "act", "pool"]:
            for i in range(self.NDMA):
                nm = f"d_{q}{i}"
                self.sems[nm] = stack.enter_context(nc.semaphore(nm))
                self.cnt[nm] = 0
            self.dq[q] = 0
        self.waited = {e: {} for e in self.engs}
        self.last_w = {}
        self.readers = {}
        self.n_inst = 0
        self.limit = None

    def _need(self, eng, dep):
        sem, val = dep
        if eng == "pe" and sem == "pe":
            return
        if self.waited[eng].get(sem, 0) >= val:
            return
        self.engs[eng].wait_ge(self.sems[sem], val)
        self.waited[eng][sem] = val

    def _deps(self, eng, reads, writes):
        for k in reads:
            d = self.last_w.get(k)
            if d is not None:
                self._need(eng, d)
            if k.startswith("p"):
                for d in self.readers.get(k, ()):
                    if d[0] != eng:
                        self._need(eng, d)
        for k in writes:
            d = self.last_w.get(k)
            if d is not None:
                self._need(eng, d)
            for d in self.readers.get(k, ()):
                self._need(eng, d)

    def _commit(self, tok, reads, writes):
        for k in reads:
            self.readers.setdefault(k, []).append(tok)
        for k in writes:
            self.last_w[k] = tok
            self.readers[k] = []

    def op(self, eng, fn, reads=(), writes=()):
        if self.limit is not None and self.n_inst >= self.limit:
            return None
        self._deps(eng, reads, writes)
        ins = fn()
        self.cnt[eng] += 1
        ins.then_inc(self.sems[eng], 1)
        self._commit((eng, self.cnt[eng]), reads, writes)
        self.n_inst += 1
        return ins

    def dma(self, q, out, in_, reads=(), writes=(), **kw):
        if self.limit is not None and self.n_inst >= self.limit and not kw.pop("force", False):
            return None
        kw.pop("force", None)
        i = self.dq[q] % self.NDMA
        self.dq[q] += 1
        nm = f"d_{q}{i}"
        if self.cnt[nm] > 0:
            self._need(q, (nm, self.cnt[nm]))
        self._deps(q, reads, writes)
        ins = self.engs[q].dma_start(out=out, in_=in_, **kw)
        self.cnt[nm] += 16
        ins.then_inc(self.sems[nm], 16)
        self._commit((nm, self.cnt[nm]), reads, writes)
        self.n_inst += 1
        return ins

    def finish(self, keys, eng="sp"):
        for k in keys:
            d = self.last_w.get(k)
            if d is not None:
                self._need(eng, d)

    def barrier(self):
        for e in self.engs:
            for c, v in self.cnt.items():
                if v > 0:
                    self._need(e, (c, v))

    def mm(self, out, lhsT, rhs, start, stop, reads, writes):
        nc = self.nc
        return self.op("pe", lambda: nc.tensor.matmul(out, lhsT, rhs, start=start, stop=stop),
                       reads=reads, writes=writes)

    def act(self, out, in_, func, reads, writes, **kw):
        nc = self.nc
        return self.op("act", lambda: nc.scalar.activation(out, in_, func, **kw),
                       reads=reads, writes=writes)


class Ctx:
    def __init__(self, nc, kb, prefix="", over=None):
        self.nc, self.kb, self.prefix, self.over = nc, kb, prefix, dict(over or {})

    def din(self, name, shape, dt=F32):
        if name in self.over:
            return self.over[name]
        return self.nc.dram_tensor(self.prefix + name, list(shape), dt, kind="ExternalInput").ap()

    def dout(self, name, shape, dt=F32):
        if name in self.over:
            return self.over[name]
        return self.nc.dram_tensor(self.prefix + name, list(shape), dt, kind="ExternalOutput").ap()


def _standalone(emit, **kw):
    nc = bass.Bass("TRN2", target_bir_lowering=False)
    with ExitStack() as st0:
        kb = KB(nc, st0)
        kb.limit = kw.pop("limit", None)
        emit(Ctx(nc, kb), **kw)
    return nc


_UID = [0]


def _uniq(name):
    _UID[0] += 1
    return f"{name}_{_UID[0]}"


def sb(nc, st, name, shape, dt):
    return st.enter_context(nc.sbuf_tensor(_uniq(name), list(shape), dt))


def ps(nc, st, name, shape, dt=F32):
    return st.enter_context(nc.psum_tensor(_uniq(name), list(shape), dt))


GLA_DK = 128
GLA_DV = 256
GC = 128
TT = 512


def gla_consts():
    j = np.arange(128)[:, None]
    i = np.arange(128)[None, :]
    tri_i = np.where(j <= i, -1.0 / 16.0, 0.0).astype(np.float32)
    tri_u = np.where(j > i, -1.0 / 16.0, 0.0).astype(np.float32)
    mask = np.where(j <= i, 1.0, 0.0).astype(np.float32)
    return tri_i, tri_u, mask


def build_gla(n_tok=S, limit=None):
    return _standalone(emit_gla, n_tok=n_tok, limit=limit)


def emit_gla(ctx, n_tok=S):
    nc, kb = ctx.nc, ctx.kb
    limit = kb.limit
    xT = ctx.din("xT", [D, n_tok])
    wqk = ctx.din("wqk", [D, 256])
    wkvr = ctx.din("wkvr", [D, 640])
    wg = ctx.din("wg", [D, 16])
    wgu = ctx.din("wgu", [33, 128])
    normg = ctx.din("normg", [1, 256])
    tri_i_d = ctx.din("tri_i", [128, 128])
    tri_u_d = ctx.din("tri_u", [128, 128])
    mask_d = ctx.din("maskT", [128, 128])
    y = ctx.dout("y", [n_tok, 256], BF16)

    with ExitStack() as st:
        V, A = nc.vector, nc.scalar
        w_qk = sb(nc, st, "w_qk", [128, 8, 256], BF16)
        w_kvr = sb(nc, st, "w_kvr", [128, 8, 640], BF16)
        w_g = sb(nc, st, "w_g", [128, 8, 16], BF16)
        w_gu = sb(nc, st, "w_gu", [33, 128], F32)
        ng = sb(nc, st, "ng", [128, 256], F32)
        tri_i = sb(nc, st, "tri_i_s", [128, 128], F32)
        tri_u = sb(nc, st, "tri_u_s", [128, 128], F32)
        maskT = sb(nc, st, "mask_s", [128, 128], F32)
        xt = [sb(nc, st, f"xt{i}", [128, 8, TT], BF16) for i in range(2)]
        g_aug = sb(nc, st, "g_aug", [33, TT], F32)
        e1 = sb(nc, st, "e1", [128, 128], F32)
        la = sb(nc, st, "la", [128, 128], F32)
        eb = sb(nc, st, "eb", [128, 128], F32)
        enb = sb(nc, st, "enb", [128, 128], F32)
        w2 = sb(nc, st, "w2", [128, 128], F32)
        qdT = sb(nc, st, "qdT", [128, 128], BF16)
        kdT = sb(nc, st, "kdT", [128, 128], BF16)
        kd2 = sb(nc, st, "kd2", [128, 128], BF16)
        v_bf = sb(nc, st, "v_bf", [128, 256], BF16)
        atm = sb(nc, st, "atm", [128, 128], BF16)
        S_f = sb(nc, st, "S_f", [128, 256], F32)
        S_b = sb(nc, st, "S_b", [128, 256], BF16)
        junk = sb(nc, st, "junk", [128, 256], F32)
        ss = sb(nc, st, "ss", [128, 1], F32)
        rstd = sb(nc, st, "rstd", [128, 1], F32)
        er = sb(nc, st, "er", [128, 256], F32)
        rs = sb(nc, st, "rs", [128, 256], F32)
        on = sb(nc, st, "on", [128, 256], F32)
        yt = [sb(nc, st, f"yt{i}", [128, 256], BF16) for i in range(2)]

        p_q = ps(nc, st, "p_q", [128, TT])
        p_k = ps(nc, st, "p_k", [128, TT])
        p_g_full = ps(nc, st, "p_g", [128, TT])
        p_g = p_g_full[0:16, :]
        p_m = ps(nc, st, "p_m", [128, 512])
        p_kv = ps(nc, st, "p_kv", [128, 512])
        p_ro = ps(nc, st, "p_ro", [128, 512])
        p_st_full = ps(nc, st, "p_st", [128, 512])
        p_st = p_st_full[:, 0:256]

        kb.dma("pool", w_qk[:], wqk.rearrange("(kc p) n -> p kc n", p=128), writes=["w_qk"])
        kb.dma("pool", w_kvr[:], wkvr.rearrange("(kc p) n -> p kc n", p=128), writes=["w_kvr"])
        kb.dma("pool", w_g[:], wg.rearrange("(kc p) n -> p kc n", p=128), writes=["w_g"])
        kb.dma("sp", w_gu[:], wgu, writes=["w_gu"])
        kb.dma("sp", ng[:], normg.partition_broadcast(128), writes=["ng"])
        kb.dma("sp", tri_i[:], tri_i_d, writes=["tri_i"])
        kb.dma("sp", tri_u[:], tri_u_d, writes=["tri_u"])
        kb.dma("sp", maskT[:], mask_d, writes=["maskT"])
        kb.op("dve", lambda: V.memset(S_f[:], 0.0), writes=["S_f"])
        kb.op("dve", lambda: V.memset(S_b[:], 0.0), writes=["S_b"])
        kb.op("dve", lambda: V.memset(g_aug[:], 1.0), writes=["g_aug"])
        eps_t = sb(nc, st, "eps_t", [128, 1], F32)
        kb.op("dve", lambda: V.memset(eps_t[:], LN_EPS), writes=["eps_t"])

        xTv = xT.rearrange("(kc p) t -> p kc t", p=128)
        n_tiles = n_tok // TT
        for T in range(n_tiles):
            x_t = xt[T % 2]
            xk = f"xt{T % 2}"
            kb.dma("pool", x_t[:], xTv[:, :, T * TT:(T + 1) * TT], writes=[xk])
            for kc in range(8):
                kb.mm(p_q[:], w_qk[:, kc, 0:128], x_t[:, kc, :], kc == 0, kc == 7,
                      reads=["w_qk", xk], writes=["p_q"])
            for kc in range(8):
                kb.mm(p_k[:], w_qk[:, kc, 128:256], x_t[:, kc, :], kc == 0, kc == 7,
                      reads=["w_qk", xk], writes=["p_k"])
            for kc in range(8):
                kb.mm(p_g, w_g[:, kc, :], x_t[:, kc, :], kc == 0, kc == 7,
                      reads=["w_g", xk], writes=["p_g"])
            kb.op("dve", lambda: V.tensor_copy(g_aug[0:16, :], p_g), reads=["p_g"], writes=["g_aug"])
            for c in range(TT // GC):
                cs = slice(c * GC, (c + 1) * GC)
                kb.mm(p_m[:, 0:128], g_aug[:, cs], w_gu[:], True, True,
                      reads=["g_aug", "w_gu"], writes=["p_m"])
                kb.act(e1[:], p_m[:, 0:128], AF.Exp, reads=["p_m"], writes=["e1"], scale=-1.0)
                kb.act(la[:], e1[:], AF.Ln, reads=["e1"], writes=["la"], bias=1.0)
                kb.mm(p_m[:, 128:256], la[:], tri_i[:], True, True, reads=["la", "tri_i"], writes=["p_m"])
                kb.mm(p_m[:, 256:384], tri_u[:], la[:], True, True, reads=["la", "tri_u"], writes=["p_m"])
                kb.act(eb[:], p_m[:, 128:256], AF.Exp, reads=["p_m"], writes=["eb"])
                kb.act(enb[:], p_m[:, 128:256], AF.Exp, reads=["p_m"], writes=["enb"], scale=-1.0)
                kb.act(w2[:], p_m[:, 256:384], AF.Exp, reads=["p_m"], writes=["w2"])
                kb.op("dve", lambda: V.scalar_tensor_tensor(
                    out=qdT[:], in0=p_q[:, cs], scalar=float(GLA_DK ** -0.5), in1=eb[:],
                    op0=ALU.mult, op1=ALU.mult), reads=["p_q", "eb"], writes=["qdT"])
                kb.op("dve", lambda: V.tensor_tensor(out=kdT[:], in0=p_k[:, cs], in1=enb[:], op=ALU.mult),
                      reads=["p_k", "enb"], writes=["kdT"])
                for kc in range(8):
                    kb.mm(p_kv[:, 0:384], x_t[:, kc, cs], w_kvr[:, kc, 0:384], kc == 0, kc == 7,
                          reads=[xk, "w_kvr"], writes=["p_kv"])
                for kc in range(8):
                    kb.mm(p_ro[:, 0:256], x_t[:, kc, cs], w_kvr[:, kc, 384:640], kc == 0, kc == 7,
                          reads=[xk, "w_kvr"], writes=["p_ro"])
                kb.op("dve", lambda: V.tensor_tensor(out=kd2[:], in0=p_kv[:, 0:128], in1=w2[:], op=ALU.mult),
                      reads=["p_kv", "w2"], writes=["kd2"])
                kb.op("dve", lambda: V.tensor_copy(v_bf[:], p_kv[:, 128:384]), reads=["p_kv"], writes=["v_bf"])
                kb.mm(p_m[:, 384:512], kdT[:], qdT[:], True, True, reads=["kdT", "qdT"], writes=["p_m"])
                kb.op("dve", lambda: V.tensor_tensor(out=atm[:], in0=p_m[:, 384:512], in1=maskT[:], op=ALU.mult),
                      reads=["p_m", "maskT"], writes=["atm"])
                kb.mm(p_ro[:, 256:512], atm[:], v_bf[:], True, False, reads=["atm", "v_bf"], writes=["p_ro"])
                kb.mm(p_ro[:, 256:512], qdT[:], S_b[:], False, True, reads=["qdT", "S_b"], writes=["p_ro"])
                kb.mm(p_st, kd2[:], v_bf[:], True, True, reads=["kd2", "v_bf"], writes=["p_st"])
                kb.op("dve", lambda: V.scalar_tensor_tensor(
                    out=S_f[:], in0=S_f[:], scalar=eb[:, 127:128], in1=p_st,
                    op0=ALU.mult, op1=ALU.add), reads=["S_f", "eb", "p_st"], writes=["S_f"])
                kb.op("pool", lambda: nc.gpsimd.tensor_copy(S_b[:], S_f[:]), reads=["S_f"], writes=["S_b"])
                kb.act(junk[:], p_ro[:, 256:512], AF.Square, reads=["p_ro"], writes=["junk", "ss"],
                       scale=1.0 / 16.0, accum_out=ss[:])
                kb.act(rstd[:], ss[:], AF.Ln, reads=["ss"], writes=["rstd"], bias=eps_t[:])
                kb.act(rstd[:], rstd[:], AF.Exp, reads=["rstd"], writes=["rstd"], scale=-0.5)
                kb.act(er[:], p_ro[:, 0:256], AF.Exp, reads=["p_ro"], writes=["er"], scale=-1.0)
                kb.op("dve", lambda: V.tensor_scalar_add(out=er[:], in0=er[:], scalar1=1.0),
                      reads=["er"], writes=["er"])
                kb.op("dve", lambda: V.reciprocal(out=er[:], in_=er[:]), reads=["er"], writes=["er"])
                kb.op("dve", lambda: V.tensor_tensor(out=rs[:], in0=p_ro[:, 0:256], in1=er[:], op=ALU.mult),
                      reads=["p_ro", "er"], writes=["rs"])
                kb.op("dve", lambda: V.scalar_tensor_tensor(
                    out=on[:], in0=p_ro[:, 256:512], scalar=rstd[:, 0:1], in1=ng[:],
                    op0=ALU.mult, op1=ALU.mult), reads=["p_ro", "rstd", "ng"], writes=["on"])
                ci = T * (TT // GC) + c
                y_t = yt[ci % 2]
                yk = f"yt{ci % 2}"
                kb.op("pool", lambda: nc.gpsimd.tensor_tensor(out=y_t[:], in0=on[:], in1=rs[:], op=ALU.mult),
                      reads=["on", "rs"], writes=[yk])
                kb.dma("sp", y[ci * GC:(ci + 1) * GC, :], y_t[:], reads=[yk], writes=["y_out"])
        if limit is not None:
            kb.dma("sp", y[0:128, :], yt[0][:], reads=["yt0"], writes=["y_out"], force=True)
            print("n_inst", kb.n_inst)
        kb.barrier()


def gla_inputs(x, w_in, w_gate_up, b_gate, norm_g):
    tri_i, tri_u, mask = gla_consts()
    maps = []
    for c in range(NCORES):
        b, h = c // 4, c % 4
        q = w_in[:, h * 128:(h + 1) * 128]
        k = w_in[:, 512 + h * 128:512 + (h + 1) * 128]
        v = w_in[:, 1024 + h * 256:1024 + (h + 1) * 256]
        g = w_in[:, 2048:2064]
        r = w_in[:, 2064 + h * 256:2064 + (h + 1) * 256]
        wgu = np.zeros((33, 128), np.float32)
        wgu[0:16] = w_gate_up[:, h * 128:(h + 1) * 128]
        wgu[32] = b_gate[h * 128:(h + 1) * 128]
        maps.append({
            "xT": np.ascontiguousarray(x[b].T),
            "wqk": np.ascontiguousarray(np.concatenate([q, k], axis=1)),
            "wkvr": np.ascontiguousarray(np.concatenate([k, v, r], axis=1)),
            "wg": np.ascontiguousarray(g),
            "wgu": wgu,
            "normg": np.ascontiguousarray(norm_g.reshape(1, 256)),
            "tri_i": tri_i, "tri_u": tri_u, "maskT": mask,
        })
    return maps


NTB = 2048
NE = 16
DFF = 256


def moe_consts():
    sel = np.zeros((16, 16, 128), np.float32)
    for e in range(16):
        sel[e, e, :] = 1.0
    ident = np.eye(128, dtype=np.float32)
    return sel, ident


def build_ffn(n_tok=NTB, limit=None, n_exp=NE):
    return _standalone(emit_ffn, n_tok=n_tok, limit=limit, n_exp=n_exp)


def emit_ffn(ctx, n_tok=NTB, n_exp=NE, fused=None):
    nc, kb = ctx.nc, ctx.kb
    limit = kb.limit
    if fused is None:
        yT = ctx.din("yT", [D, n_tok], BF16)
    xres = ctx.din("xres", [n_tok, D])
    wout = ctx.din("wout", [D, D])
    lnp = ctx.din("lnp", [4, D])
    wr = ctx.din("wr", [D, NE])
    br = ctx.din("br", [1, NE])
    wgd = ctx.din("wg", [NE, D, DFF])
    wud = ctx.din("wu", [NE, D, DFF])
    wdd = ctx.din("wd", [NE, DFF, D])
    sel_d = ctx.din("sel", [16, 16, 128])
    ident_d = ctx.din("ident", [128, 128])
    out = ctx.dout("out", [n_tok, D]) if (fused is None or "x1_d" not in fused) else None
    n_sub = n_tok // 128
    n_tile = n_tok // 512

    with ExitStack() as st:
        V, A, G = nc.vector, nc.scalar, nc.gpsimd
        w_o = sb(nc, st, "w_o", [128, 8, D], BF16)
        lng = [sb(nc, st, f"lnp{i}", [128, D], F32) for i in range(4)]
        w_r = sb(nc, st, "w_r", [128, 8, NE], F32)
        b_r = sb(nc, st, "b_r", [128, NE], F32)
        sel = sb(nc, st, "sel_s", [16, 16, 128], BF16)
        ident = sb(nc, st, "ident_s", [128, 128], F32)
        eps_t = sb(nc, st, "eps_t", [128, 1], F32)
        acc = sb(nc, st, "acc", [128, n_sub, D], F32)
        x1T = sb(nc, st, "x1T", [128, 8, n_tok], BF16)
        gT = sb(nc, st, "gT", [16, n_tok], BF16)
        y_t = [sb(nc, st, "y_t0", [128, 8, 128], BF16)] * 2
        xr = [sb(nc, st, "xr0", [128, D], F32)] * 2
        u = sb(nc, st, "u", [128, D], F32)
        x1 = sb(nc, st, "x1", [128, D], F32)
        stats = sb(nc, st, "stats", [128, 2, 6], F32)
        mv = sb(nc, st, "mv", [128, 2], F32)
        rstd = sb(nc, st, "rstd", [128, 1], F32)
        xTf = sb(nc, st, "xTf", [128, 8, 128], F32)
        sg_ = sb(nc, st, "r_s", [128, 16], F32)
        bi_ = sb(nc, st, "r_bi", [128, 16], F32)
        b2_ = sb(nc, st, "r_b2", [128, 16], F32)
        eq_ = sb(nc, st, "r_eq", [128, 16], F32)
        m1_ = sb(nc, st, "r_m1", [128, 4], F32)
        m2_ = sb(nc, st, "r_m2", [128, 4], F32)
        gs_ = sb(nc, st, "r_gs", [128, 4], F32)
        gm_ = sb(nc, st, "r_gm", [128, 1], F32)
        ig_ = sb(nc, st, "r_ig", [128, 4], F32)
        se_ = sb(nc, st, "r_se", [128, 16], F32)
        ws_ = sb(nc, st, "r_ws", [128, 1], F32)
        gate = sb(nc, st, "gate", [128, 16], F32)
        wg_s = [sb(nc, st, f"wg_s{i}", [128, 8, DFF], BF16) for i in range(2)]
        wu_s = [sb(nc, st, f"wu_s{i}", [128, 8, DFF], BF16) for i in range(2)]
        wd_s = [sb(nc, st, f"wd_s{i}", [128, 2, D], BF16) for i in range(2)]
        gb = [sb(nc, st, f"gb{i}", [128, 512], BF16) for i in range(2)]
        sgl = [sb(nc, st, f"sgl{i}", [128, 512], BF16) for i in range(2)]
        t1 = [sb(nc, st, f"t1{i}", [128, 512], BF16) for i in range(2)]
        hT = [sb(nc, st, f"hT{i}", [128, 2, 512], BF16) for i in range(2)]
        ot = [sb(nc, st, "ot0", [128, D], F32)] * 2

        pb = [ps(nc, st, f"pb{i}", [128, 512]) for i in range(8)]
        PK = [f"pb{i}" for i in range(8)]

        kb.dma("pool", w_o[:], wout.rearrange("(kc p) n -> p kc n", p=128), writes=["w_o"])
        for i in range(4):
            kb.dma("sp", lng[i][:], lnp[i:i + 1, :].partition_broadcast(128), writes=[f"lnp{i}"])
        kb.dma("sp", w_r[:], wr.rearrange("(kc p) n -> p kc n", p=128), writes=["w_r"])
        kb.dma("sp", b_r[:], br.partition_broadcast(128), writes=["b_r"])
        kb.dma("pool", sel[:], sel_d, writes=["sel"])
        kb.dma("sp", ident[:], ident_d, writes=["ident"])
        kb.op("dve", lambda: V.memset(eps_t[:], LN_EPS), writes=["eps_t"])

        def load_expert(e):
            i = e % 2
            kb.dma("pool", wg_s[i][:], wgd[e].rearrange("(kc p) f -> p kc f", p=128), writes=[f"wg{i}"])
            kb.dma("pool", wu_s[i][:], wud[e].rearrange("(kc p) f -> p kc f", p=128), writes=[f"wu{i}"])
            kb.dma("pool", wd_s[i][:], wdd[e].rearrange("(fc p) d -> p fc d", p=128), writes=[f"wd{i}"])

        def layer_norm(src, dst, gi, eng2):
            s_t, s_k = src
            d_t, d_k = dst
            for hh in range(2):
                kb.op("dve", lambda: V.bn_stats(stats[:, hh, :], s_t[:, hh * 512:(hh + 1) * 512]),
                      reads=[s_k], writes=["stats"])
            kb.op("dve", lambda: V.bn_aggr(mv[:], stats[:]), reads=["stats"], writes=["mv"])
            kb.act(rstd[:], mv[:, 1:2], AF.Sqrt, reads=["mv"], writes=["rstd"], bias=eps_t[:])
            kb.op("dve", lambda: V.reciprocal(rstd[:], rstd[:]), reads=["rstd"], writes=["rstd"])
            kb.op("dve", lambda: V.tensor_scalar(out=d_t, in0=s_t, scalar1=mv[:, 0:1], scalar2=rstd[:, 0:1],
                                                 op0=ALU.subtract, op1=ALU.mult),
                  reads=[s_k, "mv", "rstd"], writes=[d_k])
            kb.op("pool", lambda: G.tensor_tensor(out=d_t, in0=d_t, in1=lng[gi][:], op=ALU.mult),
                  reads=[d_k, f"lnp{gi}"], writes=[d_k])
            kb.op("pool", lambda: G.tensor_tensor(out=d_t, in0=d_t, in1=lng[gi + 1][:], op=ALU.add),
                  reads=[d_k, f"lnp{gi + 1}"], writes=[d_k])

        load_expert(0)
        if fused is None:
            yTv = yT.rearrange("(kc p) t -> p kc t", p=128)
        else:
            g_v = [gq.rearrange("(h t) c -> t h c", h=4) for gq in fused["g"]]
            cand = [sb(nc, st, "cand0", [128, 4, D], BF16)] * 2
            ysel = sb(nc, st, "ysel", [128, D], BF16)
            qsel = sb(nc, st, "qsel_s", [128, 4], F32)
            identb = sb(nc, st, "identb", [128, 128], BF16)
            xo = [sb(nc, st, "xo0", [128, 8, 128], BF16)] * 2
            kb.dma("sp", qsel[:], fused["qsel"], writes=["qsel"])
            kb.dma("pool", identb[:], ident_d, writes=["identb"])
        for sub in range(n_sub):
            ts_ = slice(sub * 128, (sub + 1) * 128)
            yk = "y_t0"
            xk = "xr0"
            if fused is None:
                kb.dma("sp", y_t[sub % 2][:], yTv[:, :, ts_], writes=[yk])
            else:
                ck = "cand0"
                for qq in range(4):
                    kb.dma("sp", cand[sub % 2][:, qq, :].rearrange("p (h c) -> p h c", h=4),
                           g_v[qq][sub * 128:(sub + 1) * 128, :, :], writes=[ck])
                kb.op("pool", lambda: G.tensor_scalar(out=ysel[:], in0=cand[sub % 2][:, 0, :], scalar1=qsel[:, 0:1],
                                                      scalar2=None, op0=ALU.mult), reads=[ck, "qsel"], writes=["ysel"])
                for qq in range(1, 4):
                    kb.op("dve", lambda: V.scalar_tensor_tensor(out=ysel[:], in0=cand[sub % 2][:, qq, :],
                                                                 scalar=qsel[:, qq:qq + 1], in1=ysel[:],
                                                                 op0=ALU.mult, op1=ALU.add),
                          reads=[ck, "qsel", "ysel"], writes=["ysel"])
                pTb = pb[7][:].bitcast(BF16)
                for kc in range(8):
                    kb.op("pe", lambda: nc.tensor.transpose(pTb[:, kc * 128:(kc + 1) * 128],
                                                            ysel[:, kc * 128:(kc + 1) * 128], identb[:]),
                          reads=["ysel", "identb"], writes=[PK[7]])
                kb.op("dve", lambda: V.tensor_copy(y_t[sub % 2][:], pTb.rearrange("p (k t) -> p k t", k=8)),
                      reads=[PK[7]], writes=[yk])
            kb.dma("sp", xr[sub % 2][:], xres[ts_, :], writes=[xk])
            for hh in range(2):
                for kc in range(8):
                    kb.mm(pb[hh][:], y_t[sub % 2][:, kc, :], w_o[:, kc, hh * 512:(hh + 1) * 512],
                          kc == 0, kc == 7, reads=[yk, "w_o"], writes=[PK[hh]])
            for hh in range(2):
                kb.op("dve", lambda: V.scalar_tensor_tensor(
                    out=u[:, hh * 512:(hh + 1) * 512], in0=xr[sub % 2][:, hh * 512:(hh + 1) * 512],
                    scalar=float(DN_ALPHA), in1=pb[hh][:], op0=ALU.mult, op1=ALU.add),
                    reads=[xk, PK[hh]], writes=["u"])
            layer_norm((u[:], "u"), (x1[:], "x1"), 0, None)
            kb.op("pool", lambda: G.tensor_scalar(out=acc[:, sub, :], in0=x1[:], scalar1=float(DN_ALPHA),
                                                  scalar2=None, op0=ALU.mult),
                  reads=["x1"], writes=[f"acc{sub}"])
            for kc in range(8):
                bank = 2 + kc // 4
                kb.op("pe", lambda: nc.tensor.transpose(pb[bank][:, (kc % 4) * 128:(kc % 4 + 1) * 128],
                                                        x1[:, kc * 128:(kc + 1) * 128], ident[:]),
                      reads=["x1", "ident"], writes=[PK[bank]])
            for q in range(2):
                kb.op("dve" if q == 0 else "act",
                      (lambda: V.tensor_copy(xTf[:, 0:4, :], pb[2][:].rearrange("p (k t) -> p k t", k=4))) if q == 0
                      else (lambda: A.copy(xTf[:, 4:8, :], pb[3][:].rearrange("p (k t) -> p k t", k=4))),
                      reads=[PK[2 + q]], writes=[f"xTf{q}"])
            kb.op("pool", lambda: G.tensor_copy(x1T[:, :, ts_], xTf[:]), reads=["xTf0", "xTf1"], writes=["x1T"])
            for kc in range(8):
                kb.mm(pb[4][:, 0:16], xTf[:, kc, :], w_r[:, kc, :], kc == 0, kc == 7,
                      reads=["xTf0", "xTf1", "w_r"], writes=[PK[4]])
            kb.act(sg_[:], pb[4][:, 0:16], AF.Sigmoid, reads=[PK[4]], writes=["r_s"])
            kb.op("dve", lambda: V.tensor_tensor(out=bi_[:], in0=sg_[:], in1=b_r[:], op=ALU.add),
                  reads=["r_s", "b_r"], writes=["r_bi"])
            bi3 = bi_[:].rearrange("p (g e) -> p g e", g=4)
            kb.op("dve", lambda: V.tensor_reduce(out=m1_[:], in_=bi3, axis=AX.X, op=ALU.max),
                  reads=["r_bi"], writes=["r_m1"])
            kb.op("dve", lambda: V.tensor_tensor(out=eq_[:].rearrange("p (g e) -> p g e", g=4), in0=bi3,
                                                 in1=m1_[:].unsqueeze(2).to_broadcast([128, 4, 4]), op=ALU.is_equal),
                  reads=["r_bi", "r_m1"], writes=["r_eq"])
            kb.op("dve", lambda: V.scalar_tensor_tensor(out=b2_[:], in0=eq_[:], scalar=-1e30, in1=bi_[:],
                                                        op0=ALU.mult, op1=ALU.add),
                  reads=["r_eq", "r_bi"], writes=["r_b2"])
            kb.op("dve", lambda: V.tensor_reduce(out=m2_[:], in_=b2_[:].rearrange("p (g e) -> p g e", g=4),
                                                 axis=AX.X, op=ALU.max),
                  reads=["r_b2"], writes=["r_m2"])
            kb.op("dve", lambda: V.tensor_tensor(out=gs_[:], in0=m1_[:], in1=m2_[:], op=ALU.add),
                  reads=["r_m1", "r_m2"], writes=["r_gs"])
            kb.op("dve", lambda: V.tensor_reduce(out=gm_[:], in_=gs_[:], axis=AX.X, op=ALU.max),
                  reads=["r_gs"], writes=["r_gm"])
            kb.op("dve", lambda: V.tensor_scalar(out=ig_[:], in0=gs_[:], scalar1=gm_[:, 0:1], scalar2=None,
                                                 op0=ALU.is_ge),
                  reads=["r_gs", "r_gm"], writes=["r_ig"])
            kb.op("dve", lambda: V.tensor_tensor(out=se_[:].rearrange("p (g e) -> p g e", g=4), in0=bi3,
                                                 in1=m2_[:].unsqueeze(2).to_broadcast([128, 4, 4]), op=ALU.is_ge),
                  reads=["r_bi", "r_m2"], writes=["r_se"])
            kb.op("dve", lambda: V.tensor_tensor(out=se_[:].rearrange("p (g e) -> p g e", g=4),
                                                 in0=se_[:].rearrange("p (g e) -> p g e", g=4),
                                                 in1=ig_[:].unsqueeze(2).to_broadcast([128, 4, 4]), op=ALU.mult),
                  reads=["r_se", "r_ig"], writes=["r_se"])
            kb.op("dve", lambda: V.tensor_tensor(out=se_[:], in0=se_[:], in1=sg_[:], op=ALU.mult),
                  reads=["r_se", "r_s"], writes=["r_se"])
            kb.op("dve", lambda: V.tensor_reduce(out=ws_[:], in_=se_[:], axis=AX.X, op=ALU.add),
                  reads=["r_se"], writes=["r_ws"])
            kb.op("dve", lambda: V.reciprocal(ws_[:], ws_[:]), reads=["r_ws"], writes=["r_ws"])
            kb.op("dve", lambda: V.tensor_scalar(out=gate[:], in0=se_[:], scalar1=ws_[:, 0:1], scalar2=None,
                                                 op0=ALU.mult),
                  reads=["r_se", "r_ws"], writes=["gate"])
            kb.op("pe", lambda: nc.tensor.transpose(pb[5][0:16, 0:128], gate[:], ident[:]),
                  reads=["gate", "ident"], writes=[PK[5]])
            kb.op("dve", lambda: V.tensor_copy(gT[:, ts_], pb[5][0:16, 0:128]), reads=[PK[5]], writes=["gT"])

        pend = []

        def down_part(e, T, i, j):
            def f():
                for s4 in range(4):
                    sub = T * 4 + s4
                    for hh in range(2):
                        bank = 5 + (s4 * 2 + hh) % 3
                        for fc in range(2):
                            kb.mm(pb[bank][:], hT[j][:, fc, s4 * 128:(s4 + 1) * 128],
                                  wd_s[i][:, fc, hh * 512:(hh + 1) * 512], fc == 0, fc == 1,
                                  reads=[f"hT{j}", f"wd{i}"], writes=[PK[bank]])
                        kb.op("dve", lambda: V.tensor_tensor(out=acc[:, sub, hh * 512:(hh + 1) * 512],
                                                             in0=acc[:, sub, hh * 512:(hh + 1) * 512],
                                                             in1=pb[bank][:], op=ALU.add),
                              reads=[f"acc{sub}", PK[bank]], writes=[f"acc{sub}"])
            return f

        for e in range(n_exp):
            i = e % 2
            for T in range(n_tile):
                Ts = slice(T * 512, (T + 1) * 512)
                j = (e * n_tile + T) % 2
                kb.mm(pb[4][:], sel[:, e, :], gT[:, Ts], True, True, reads=["sel", "gT"], writes=[PK[4]])
                kb.op("act", lambda: A.copy(gb[j][:], pb[4][:]), reads=[PK[4]], writes=[f"gb{j}"])
                for fc in range(2):
                    for kc in range(8):
                        kb.mm(pb[fc][:], wg_s[i][:, kc, fc * 128:(fc + 1) * 128], x1T[:, kc, Ts],
                              kc == 0, kc == 7, reads=[f"wg{i}", "x1T"], writes=[PK[fc]])
                    for kc in range(8):
                        kb.mm(pb[2 + fc][:], wu_s[i][:, kc, fc * 128:(fc + 1) * 128], x1T[:, kc, Ts],
                              kc == 0, kc == 7, reads=[f"wu{i}", "x1T"], writes=[PK[2 + fc]])
                for fc in range(2):
                    kb.act(sgl[fc][:], pb[fc][:], AF.Silu, reads=[PK[fc]], writes=[f"sgl{fc}"])
                    kb.op("dve", lambda: V.tensor_tensor(out=t1[fc][:], in0=sgl[fc][:], in1=pb[2 + fc][:], op=ALU.mult),
                          reads=[f"sgl{fc}", PK[2 + fc]], writes=[f"t1{fc}"])
                    kb.op("pool", lambda: G.tensor_tensor(out=hT[j][:, fc, :], in0=t1[fc][:], in1=gb[j][:], op=ALU.mult),
                          reads=[f"t1{fc}", f"gb{j}"], writes=[f"hT{j}"])
                while pend:
                    pend.pop(0)()
                if T == 0 and e + 1 < n_exp:
                    load_expert(e + 1)
                pend.append(down_part(e, T, i, j))
        while pend:
            pend.pop(0)()

        for sub in range(n_sub):
            o_t = ot[sub % 2]
            layer_norm((acc[:, sub, :], f"acc{sub}"), (o_t[:], "ot0"), 2, None)
            if out is not None:
                kb.dma("sp", out[sub * 128:(sub + 1) * 128, :], o_t[:], reads=["ot0"], writes=["out"], force=True)
            else:
                ok_ = "ot0"
                kb.dma("sp", fused["x1_d"][sub * 128:(sub + 1) * 128, :], o_t[:], reads=[ok_], writes=["x1_d"])
                for kc in range(8):
                    bank = 2 + kc // 4
                    kb.op("pe", lambda: nc.tensor.transpose(pb[bank][:, (kc % 4) * 128:(kc % 4 + 1) * 128],
                                                            o_t[:, kc * 128:(kc + 1) * 128], ident[:]),
                          reads=[ok_, "ident"], writes=[PK[bank]])
                xk_ = "xo0"
                kb.op("dve", lambda: V.tensor_copy(xo[sub % 2][:, 0:4, :], pb[2][:].rearrange("p (k t) -> p k t", k=4)),
                      reads=[PK[2]], writes=[xk_])
                kb.op("dve", lambda: V.tensor_copy(xo[sub % 2][:, 4:8, :], pb[3][:].rearrange("p (k t) -> p k t", k=4)),
                      reads=[PK[3]], writes=[xk_])
                kb.dma("sp", fused["x1T_d"].rearrange("(kc p) t -> p kc t", p=128)[:, :, sub * 128:(sub + 1) * 128],
                       xo[sub % 2][:], reads=[xk_], writes=["x1T_d"])
        kb.barrier()


def ffn_inputs(y_tok_major, xres, w_out, ln_g, ln_b, w_router, b_router, w_gate, w_up, w_down):
    sel, ident = moe_consts()
    lnp = np.ascontiguousarray(np.stack([ln_g[0], ln_b[0], ln_g[1], ln_b[1]]).astype(np.float32))
    maps = []
    for c in range(NCORES):
        rs_ = slice(c * NTB, (c + 1) * NTB)
        maps.append({
            "yT": np.ascontiguousarray(y_tok_major[rs_].T),
            "xres": np.ascontiguousarray(xres[rs_]),
            "wout": w_out, "lnp": lnp, "wr": w_router,
            "br": np.ascontiguousarray(b_router.reshape(1, NE)),
            "wg": w_gate, "wu": w_up, "wd": w_down, "sel": sel, "ident": ident,
        })
    return maps


NSA_SCALE = 0.125
MASK_NEG = -240000.0


def nsa_consts(n_tok=S):
    nqb = n_tok // 128
    inv = np.power(500000.0, -np.arange(8, dtype=np.float32) * (2.0 / 16.0)).astype(np.float32)
    def cs(pos):
        ang = pos.astype(np.float32)[:, None] * inv[None, :]
        return np.concatenate([np.cos(ang), np.sin(ang)], axis=1).astype(np.float32)
    def pl(a):
        n = a.shape[0] // 128
        return np.ascontiguousarray(a.reshape(n, 128, 16).transpose(1, 0, 2).reshape(128, n * 16))
    cs_tok = pl(cs(np.arange(n_tok)))
    cs_cmp = pl(cs(np.arange(512) * 16 + 31))
    wimp = np.zeros((512, 128), np.float32)
    for s_ in range(128):
        for o, wgt in enumerate([1, 2, 2, 2, 1]):
            c = 4 * s_ + o
            if c < 511:
                wimp[c, s_] = wgt
    texp = ((np.arange(n_tok)[None, :] // 64) % 64 == np.arange(64)[:, None]).astype(np.float32)
    k = np.arange(128)[:, None]
    q = np.arange(128)[None, :]
    causal = (k <= q).astype(np.float32)
    strict = (k > q).astype(np.float32)
    cmask = np.zeros((nqb, 2, 128, 128), np.float32)
    for qi in range(nqb):
        jl = (8 * qi + 6) // 128
        for slot, jt in ((0, jl - 1), (1, jl)):
            if jt < 0:
                continue
            j = 128 * jt + k
            cmask[qi, slot] = (16 * j + 31 <= 128 * qi + q)
    ident = np.eye(128, dtype=np.float32)
    return dict(cs_tok=cs_tok, cs_cmp=cs_cmp, wimp=wimp, texp=texp, causal=causal, strict=strict,
                cmask=cmask, ident=ident)


def build_nsa(n_tok=S, limit=None):
    return _standalone(emit_nsa, n_tok=n_tok, limit=limit)


def emit_nsa(ctx, n_tok=S, g2=None):
    nc, kb = ctx.nc, ctx.kb
    limit = kb.limit
    nqb = n_tok // 128
    ncb = n_tok // 16 - 1
    ncp = ((ncb + 127) // 128) * 128
    njt_all = ncp // 128
    din = ctx.din
    xT = din("xT", [D, n_tok]) if g2 is None else None
    wn = din("wn", [D, 652])
    cs_tok_d = din("cs_tok", [128, nqb * 16])
    cs_cmp_d = din("cs_cmp", [128, 64])
    wimp_d = din("wimp", [512, 128])
    texp_d = din("texp", [64, n_tok])
    causal_d = din("causal", [128, 128])
    strict_d = din("strict", [128, 128])
    cmask_d = din("cmask", [nqb, 2, 128, 128])
    ident_d = din("ident", [128, 128])
    wk1_d = din("wk1", [2048, 256])
    wk2_d = din("wk2", [256, 64])
    wv1_d = din("wv1", [2048, 256])
    wv2_d = din("wv2", [256, 64])
    peT_d = din("peT", [64, 32])
    y = ctx.dout("y", [n_tok, 256], BF16)

    with ExitStack() as st:
        V, A, G = nc.vector, nc.scalar, nc.gpsimd
        identb = sb(nc, st, "identb", [128, 128], BF16)
        QT = sb(nc, st, "QT", [64, 4, n_tok], BF16)
        KST = sb(nc, st, "KST", [128, n_tok], BF16)
        KWT = sb(nc, st, "KWT", [64, n_tok], BF16)
        VS = sb(nc, st, "VS", [128, nqb, 65], BF16)
        VW = sb(nc, st, "VW", [128, nqb, 65], BF16)
        G_all = sb(nc, st, "G_all", [128, nqb, 12], F32)
        KCMT = sb(nc, st, "KCMT", [64, ncp], BF16)
        RC = sb(nc, st, "RC", [128, njt_all, 193], BF16)
        st1 = ExitStack()
        w_n = sb(nc, st1, "w_n", [128, 8, 652], BF16)
        cs_tok = sb(nc, st1, "cs_tok_s", [128, nqb, 16], F32)
        cs_cmp = sb(nc, st1, "cs_cmp_s", [128, 4, 16], F32)
        KCT = sb(nc, st1, "KCT", [64, n_tok], BF16)
        VCT = sb(nc, st1, "VCT", [64, n_tok], BF16)
        xt = [sb(nc, st1, f"xt{i}", [128, 8, 128], BF16) for i in range(2)]
        pr = sb(nc, st1, "pr", [128, 652], F32)
        rp = sb(nc, st1, "rp", [128, 8, 64], BF16)
        ra = sb(nc, st1, "ra", [128, 6, 8], F32)
        rb = sb(nc, st1, "rb", [128, 6, 8], F32)
        w1 = sb(nc, st1, "w1", [64, 32, 256], BF16)
        w2 = sb(nc, st1, "w2", [128, 2, 64], BF16)
        peT = sb(nc, st1, "peT_s", [64, 32], BF16)
        hb = sb(nc, st1, "hb", [128, 2], F32)
        h1T = sb(nc, st1, "h1T", [128, 2, ncp], BF16)
        kc_f = sb(nc, st1, "kc_f", [128, 64], F32)
        kc_b = sb(nc, st1, "kc_b", [128, 64], BF16)

        pA = ps(nc, st, "pA", [128, 512])
        pB = ps(nc, st, "pB", [128, 512])
        pT = ps(nc, st, "pT", [128, 1024], BF16)
        pS = [ps(nc, st, f"pS{i}", [128, 512]) for i in range(3)]
        pC = pA
        pSL = ps(nc, st, "pSL", [128, 512])
        pW = ps(nc, st, "pW", [128, 512])

        kb.dma("pool", w_n[:], wn.rearrange("(kc p) n -> p kc n", p=128), writes=["w_n"])
        kb.dma("sp", cs_tok[:], cs_tok_d.rearrange("p (n c) -> p n c", c=16), writes=["cs_tok"])
        kb.dma("sp", cs_cmp[:], cs_cmp_d.rearrange("p (n c) -> p n c", c=16), writes=["cs_cmp"])
        kb.dma("pool", identb[:], ident_d, writes=["identb"])
        kb.dma("pool", peT[:], peT_d, writes=["peT"])
        kb.op("dve", lambda: V.memset(VS[:, :, 64:65], 1.0), writes=["VS"])
        kb.op("dve", lambda: V.memset(VW[:, :, 64:65], 1.0), writes=["VW"])
        kb.op("dve", lambda: V.memset(RC[:, :, 64:65], 1.0), writes=["RC"])
        kb.op("dve", lambda: V.memset(h1T[:], 0.0), writes=["h1T"])
        kb.dma("pool", RC[:, :, 65:193], wimp_d[0:ncp, :].rearrange("(n p) s -> p n s", p=128), writes=["RC"])
        kb.dma("pool", KST[64:128, :], texp_d, writes=["KSTa"])

        if g2 is None:
            xTv = xT.rearrange("(kc p) t -> p kc t", p=128)
        else:
            g2v = [gj.rearrange("(q k2 p) t -> q p k2 t", q=4, k2=2, p=128) for gj in g2]

        def rope(src3, dst3, cs_ap, nh, csk):
            cosb = cs_ap[:, 0:8].unsqueeze(1).to_broadcast([128, nh, 8])
            sinb = cs_ap[:, 8:16].unsqueeze(1).to_broadcast([128, nh, 8])
            a_, b_ = ra[:, 0:nh, :], rb[:, 0:nh, :]
            kb.op("pool", lambda: G.tensor_tensor(out=a_, in0=src3[:, :, 0:8], in1=cosb, op=ALU.mult),
                  reads=["rsrc", csk], writes=["ra"])
            kb.op("pool", lambda: G.tensor_tensor(out=b_, in0=src3[:, :, 8:16], in1=sinb, op=ALU.mult),
                  reads=["rsrc", csk], writes=["rb"])
            kb.op("pool", lambda: G.tensor_tensor(out=dst3[:, :, 0:8], in0=a_, in1=b_, op=ALU.subtract),
                  reads=["ra", "rb"], writes=["rdst"])
            kb.op("pool", lambda: G.tensor_tensor(out=a_, in0=src3[:, :, 8:16], in1=cosb, op=ALU.mult),
                  reads=["rsrc", csk, "rdst"], writes=["ra"])
            kb.op("pool", lambda: G.tensor_tensor(out=b_, in0=src3[:, :, 0:8], in1=sinb, op=ALU.mult),
                  reads=["rsrc", csk, "rdst"], writes=["rb"])
            kb.op("pool", lambda: G.tensor_tensor(out=dst3[:, :, 8:16], in0=a_, in1=b_, op=ALU.add),
                  reads=["ra", "rb"], writes=["rdst"])
            kb.op("pool", lambda: G.tensor_copy(dst3[:, :, 16:64], src3[:, :, 16:64]),
                  reads=["rsrc"], writes=["rdst"])

        for T in range(nqb):
            Ts = slice(T * 128, (T + 1) * 128)
            x_t = xt[T % 2]
            xk = f"xt{T % 2}"
            if g2 is None:
                kb.dma("pool", x_t[:], xTv[:, :, Ts], writes=[xk])
            else:
                qq, tl = T // 16, T % 16
                for j in range(4):
                    kb.dma("sp", x_t[:, 2 * j:2 * j + 2, :], g2v[j][qq][:, :, tl * 128:(tl + 1) * 128], writes=[xk])
            for kc in range(8):
                kb.mm(pA[:], x_t[:, kc, :], w_n[:, kc, 0:512], kc == 0, kc == 7, reads=[xk, "w_n"], writes=["pA"])
            for kc in range(8):
                kb.mm(pB[:, 0:140], x_t[:, kc, :], w_n[:, kc, 512:652], kc == 0, kc == 7,
                      reads=[xk, "w_n"], writes=["pB"])
            kb.op("dve", lambda: V.tensor_copy(pr[:, 0:512], pA[:]), reads=["pA", "rdst"], writes=["rsrc"])
            kb.op("dve", lambda: V.tensor_copy(pr[:, 512:640], pB[:, 0:128]), reads=["pB"], writes=["prv"])
            kb.act(G_all[:, T, :], pB[:, 128:140], AF.Sigmoid, reads=["pB"], writes=["G_all"])
            kb.op("pool", lambda: G.tensor_copy(VS[:, T, 0:64], pr[:, 512:576]), reads=["prv"], writes=["VS"])
            kb.op("pool", lambda: G.tensor_copy(VW[:, T, 0:64], pr[:, 576:640]), reads=["prv"], writes=["VW"])
            rope(pr[:, 0:384].rearrange("p (s d) -> p s d", s=6), rp[:, 0:6, :], cs_tok[:, T, :], 6, "cs_tok")
            kb.op("pool", lambda: G.tensor_copy(rp[:, 6:8, :], pr[:, 384:512].rearrange("p (s d) -> p s d", s=2)),
                  reads=["rsrc"], writes=["rdst"])
            for s_ in range(8):
                kb.op("pe", lambda: nc.tensor.transpose(pT[0:64, s_ * 128:(s_ + 1) * 128], rp[:, s_, :], identb[:]),
                      reads=["rdst", "identb"], writes=["pT"])
            kb.op("dve", lambda: V.tensor_copy(QT[:, :, Ts], pT[0:64, 0:512].rearrange("p (h t) -> p h t", h=4)),
                  reads=["pT"], writes=["QT"])
            kb.op("dve", lambda: V.tensor_copy(KST[0:64, Ts], pT[0:64, 512:640]), reads=["pT"], writes=["KST"])
            kb.op("dve", lambda: V.tensor_copy(KWT[:, Ts], pT[0:64, 640:768]), reads=["pT"], writes=["KWT"])
            kb.op("dve", lambda: V.tensor_copy(KCT[:, Ts], pT[0:64, 768:896]), reads=["pT"], writes=["KCT"])
            kb.op("dve", lambda: V.tensor_copy(VCT[:, Ts], pT[0:64, 896:1024]), reads=["pT"], writes=["VCT"])

        for which, (w1d, w2d, srcT, srck) in enumerate(((wk1_d, wk2_d, KCT, "KCT"), (wv1_d, wv2_d, VCT, "VCT"))):
            kb.dma("pool", w1[:], w1d.rearrange("(r d) h -> d r h", d=64), writes=["w1"])
            kb.dma("pool", w2[:], w2d.rearrange("(hc p) d -> p hc d", p=128), writes=["w2"])
            for hc in range(2):
                for r in range(32):
                    kb.mm(pS[0][:, 0:1], w1[:, r, hc * 128:(hc + 1) * 128], peT[:, r:r + 1], r == 0, r == 31,
                          reads=["w1", "peT"], writes=["pS0"])
                kb.op("dve", lambda: V.tensor_copy(hb[:, hc:hc + 1], pS[0][:, 0:1]), reads=["pS0"], writes=["hb"])
            for hc in range(2):
                for c0 in range(0, ncb, 512):
                    n_ = min(512, ncb - c0)
                    for r in range(32):
                        kb.mm(pS[1][:, 0:n_], w1[:, r, hc * 128:(hc + 1) * 128],
                              srcT[:, 16 * c0 + r:16 * c0 + r + 16 * (n_ - 1) + 1:16], r == 0, r == 31,
                              reads=["w1", srck], writes=["pS1"])
                    kb.act(h1T[:, hc, c0:c0 + n_], pS[1][:, 0:n_], AF.Silu, reads=["pS1", "hb"], writes=["h1T"],
                           bias=hb[:, hc:hc + 1])
            for jt in range(njt_all):
                for hc in range(2):
                    kb.mm(pS[2][:, 0:64], h1T[:, hc, jt * 128:(jt + 1) * 128], w2[:, hc, :], hc == 0, hc == 1,
                          reads=["h1T", "w2"], writes=["pS2"])
                if which == 0:
                    kb.op("dve", lambda: V.tensor_copy(kc_f[:], pS[2][:, 0:64]), reads=["pS2", "rdst"], writes=["rsrc"])
                    rope(kc_f[:].unsqueeze(1), kc_b[:].unsqueeze(1), cs_cmp[:, jt, :], 1, "cs_cmp")
                    kb.op("pe", lambda: nc.tensor.transpose(pT[0:64, 0:128], kc_b[:], identb[:]),
                          reads=["rdst", "identb"], writes=["pT"])
                    kb.op("dve", lambda: V.tensor_copy(KCMT[:, jt * 128:(jt + 1) * 128], pT[0:64, 0:128]),
                          reads=["pT"], writes=["KCMT"])
                else:
                    kb.op("dve", lambda: V.tensor_copy(RC[:, jt, 0:64], pS[2][:, 0:64]), reads=["pS2"], writes=["RC"])

        if limit is not None:
            print("n_inst after stage 2:", kb.n_inst)
        kb.barrier()
        st1.close()
        causal = sb(nc, st, "causal_s", [128, 128], BF16)
        strict = sb(nc, st, "strict_s", [128, 128], BF16)
        e_sb = [sb(nc, st, f"e_sb{i}", [128, 512], BF16) for i in range(3)]
        cm_sb = [sb(nc, st, f"cm_sb{i}", [128, 2, 128], BF16) for i in range(2)]
        impm = sb(nc, st, "impm", [128, 128], F32)
        impw = sb(nc, st, "impw", [128, 128], F32)
        m8 = sb(nc, st, "m8", [128, 16], F32)
        selb = sb(nc, st, "selb", [128, 192], BF16)
        QaLo = [sb(nc, st, f"QaLo{i}", [128, 512], BF16) for i in range(2)]
        QaHi = [sb(nc, st, f"QaHi{i}", [128, 512], BF16) for i in range(2)]
        kb.op("dve", lambda: V.memset(selb[:], 0.0), writes=["selb"])
        zz = sb(nc, st, "zz", [128, 12], F32)
        coef = sb(nc, st, "coef", [128, 12], F32)
        o_acc = sb(nc, st, "o_acc", [128, 256], F32)
        y_sb = [sb(nc, st, f"y_sb{i}", [128, 256], BF16) for i in range(2)]
        kb.dma("pool", causal[:], causal_d, writes=["causal"])
        kb.dma("pool", strict[:], strict_d, writes=["strict"])

        def exp_tile(bank, ei):
            kb.act(e_sb[ei][:], pS[bank][:], AF.Exp, reads=[f"pS{bank}"], writes=[f"e{ei}"], scale=NSA_SCALE)

        def mask_tile(ei, mask_ap, mkeys):
            e3 = e_sb[ei][:].rearrange("p (h q) -> p h q", h=4)
            kb.op("pool", lambda: G.tensor_tensor(out=e3, in0=e3, in1=mask_ap.unsqueeze(1).to_broadcast([128, 4, 128]),
                                                  op=ALU.mult),
                  reads=[f"e{ei}"] + mkeys, writes=[f"e{ei}"])

        rot = [0]

        def nxt():
            rot[0] += 1
            return rot[0] % 3

        PIPE = 2
        pipe = []

        def drain_one():
            pv0, post0 = pipe.pop(0)
            pv0()
            if post0 is not None:
                post0()

        def push_tile(qk, pv, post=None):
            qk()
            pipe.append((pv, post))
            while len(pipe) > PIPE:
                drain_one()

        def tile_a(qi, jt, jl, Qv, cmk):
            r_ = nxt()

            def qk():
                kb.mm(pS[r_][:], KCMT[:, jt * 128:(jt + 1) * 128], Qv, True, True, reads=["KCMT", "QT"], writes=[f"pS{r_}"])
                exp_tile(r_, r_)
                if jt >= jl - 1:
                    mask_tile(r_, cm_sb[qi % 2][:, 1 - (jl - jt), :], [cmk])

            def pv():
                for h in range(4):
                    bank = pA if h < 2 else pB
                    kb.mm(bank[:, (h % 2) * 193:(h % 2) * 193 + 193], e_sb[r_][:, h * 128:(h + 1) * 128], RC[:, jt, :],
                          jt == 0 and h % 2 == 0, jt == jl and h % 2 == 1, reads=[f"e{r_}", "RC"],
                          writes=["pA" if h < 2 else "pB"])
            return qk, pv

        def post_a1(qi):
            use_sel = qi >= 8
            Gq = G_all[:, qi, :].rearrange("p (h j) -> p h j", h=4)

            def post():
                for h in range(4):
                    bank = pA if h < 2 else pB
                    c0 = (h % 2) * 193
                    kb.op("dve", lambda: V.tensor_scalar_max(out=zz[:, h:h + 1], in0=bank[:, c0 + 64:c0 + 65], scalar1=1e-30),
                          reads=["pA" if h < 2 else "pB"], writes=["zz"])
                kb.op("dve", lambda: V.reciprocal(zz[:, 0:4], zz[:, 0:4]), reads=["zz"], writes=["zz"])
                if use_sel:
                    for h in range(4):
                        bank = pA if h < 2 else pB
                        bk = "pA" if h < 2 else "pB"
                        c0 = (h % 2) * 193
                        if h == 0:
                            kb.op("dve", lambda: V.tensor_scalar(out=impm[:], in0=bank[:, c0 + 65:c0 + 193],
                                                                 scalar1=zz[:, 0:1], scalar2=None, op0=ALU.mult),
                                  reads=[bk, "zz"], writes=["impm"])
                        else:
                            kb.op("dve", lambda: V.scalar_tensor_tensor(out=impm[:], in0=bank[:, c0 + 65:c0 + 193],
                                                                        scalar=zz[:, h:h + 1], in1=impm[:],
                                                                        op0=ALU.mult, op1=ALU.add),
                                  reads=[bk, "zz", "impm"], writes=["impm"])
                    c2 = 2 * qi
                    kb.op("dve", lambda: V.memset(impm[:, 0:1], 3e30), reads=[], writes=["impm"])
                    kb.op("dve", lambda: V.memset(impm[:, c2:c2 + 1], 2e30), writes=["impm"])
                    kb.op("dve", lambda: V.memset(impm[0:64, c2 - 1:c2], 1e30), writes=["impm"])
                    kb.op("dve", lambda: V.memset(impm[0:64, c2 + 1:c2 + 2], -1e30), writes=["impm"])
                    kb.op("dve", lambda: V.memset(impm[64:128, c2 + 1:c2 + 2], 2.5e30), writes=["impm"])
                    if c2 + 2 < 128:
                        kb.op("dve", lambda: V.memset(impm[:, c2 + 2:128], -1e30), writes=["impm"])
                    kb.op("dve", lambda: V.max(out=m8[:, 0:8], in_=impm[:]), reads=["impm"], writes=["m8"])
                    kb.op("dve", lambda: V.match_replace(out=impw[:], in_to_replace=m8[:, 0:8], in_values=impm[:],
                                                         imm_value=-1e30), reads=["impm", "m8"], writes=["impw"])
                    kb.op("dve", lambda: V.max(out=m8[:, 8:16], in_=impw[:]), reads=["impw"], writes=["m8"])
                    kb.op("dve", lambda: V.tensor_scalar(out=selb[:, 64:192], in0=impm[:], scalar1=m8[:, 15:16], scalar2=MASK_NEG,
                                                         op0=ALU.is_lt, op1=ALU.mult),
                          reads=["impm", "m8"], writes=["selb"])
                kb.op("dve", lambda: V.tensor_tensor(out=coef[:, 0:4], in0=zz[:, 0:4], in1=Gq[:, :, 0], op=ALU.mult),
                      reads=["zz", "G_all"], writes=["coef"])
                for h in range(4):
                    bank = pA if h < 2 else pB
                    bk = "pA" if h < 2 else "pB"
                    c0 = (h % 2) * 193
                    kb.op("dve", lambda: V.tensor_scalar(out=o_acc[:, h * 64:(h + 1) * 64], in0=bank[:, c0:c0 + 64],
                                                         scalar1=coef[:, h:h + 1], scalar2=None, op0=ALU.mult),
                          reads=[bk, "coef"], writes=["o_acc"])
            return post

        def post_a2(qi):
            b_ = qi % 2

            def post():
                kb.op("pe", lambda: nc.tensor.transpose(pT[:, 0:128], selb[:, 0:128], identb[:]),
                      reads=["selb", "identb"], writes=["pT"])
                if qi >= 32:
                    kb.op("pe", lambda: nc.tensor.transpose(pT[:, 128:256], selb[:, 64:192], identb[:]),
                          reads=["selb", "identb"], writes=["pT"])
                kb.op("dve", lambda: V.tensor_copy(QaLo[b_][64:128, :].rearrange("p (h q) -> p h q", h=4),
                                                   pT[64:128, 0:128].unsqueeze(1).to_broadcast([64, 4, 128])),
                      reads=["pT"], writes=[f"QaLoM{b_}"])
                if qi >= 32:
                    kb.op("dve", lambda: V.tensor_copy(QaHi[b_][64:128, :].rearrange("p (h q) -> p h q", h=4),
                                                       pT[64:128, 128:256].unsqueeze(1).to_broadcast([64, 4, 128])),
                          reads=["pT"], writes=[f"QaHiM{b_}"])
            return post

        def tile_c(qi, kt, Qv, use_sel):
            r_ = nxt()
            Ks = slice(kt * 128, (kt + 1) * 128)

            def qk():
                if use_sel:
                    b_ = qi % 2
                    if kt < 32:
                        kb.mm(pS[r_][:], KST[:, Ks], QaLo[b_][:], True, True,
                              reads=["KST", "KSTa", f"QaLoQ{b_}", f"QaLoM{b_}"], writes=[f"pS{r_}"])
                    else:
                        kb.mm(pS[r_][:], KST[:, Ks], QaHi[b_][:], True, True,
                              reads=["KST", "KSTa", f"QaHiQ{b_}", f"QaHiM{b_}"], writes=[f"pS{r_}"])
                else:
                    kb.mm(pS[r_][:], KST[0:64, Ks], Qv, True, True, reads=["KST", "QT"], writes=[f"pS{r_}"])
                exp_tile(r_, r_)
                if kt == qi:
                    mask_tile(r_, causal[:], ["causal"])

            def pv():
                for h in range(4):
                    kb.mm(pSL[:, h * 65:(h + 1) * 65], e_sb[r_][:, h * 128:(h + 1) * 128], VS[:, kt, :],
                          kt == 0 and h == 0, kt == qi and h == 3, reads=[f"e{r_}", "VS"], writes=["pSL"])
            return qk, pv

        def tile_d(qi, kt, k0, Qv):
            r_ = nxt()
            Ks = slice(kt * 128, (kt + 1) * 128)

            def qk():
                kb.mm(pS[r_][:], KWT[:, Ks], Qv, True, True, reads=["KWT", "QT"], writes=[f"pS{r_}"])
                exp_tile(r_, r_)
                if kt == qi:
                    mask_tile(r_, causal[:], ["causal"])
                elif kt == qi - 4:
                    mask_tile(r_, strict[:], ["strict"])

            def pv():
                for h in range(4):
                    kb.mm(pW[:, h * 65:(h + 1) * 65], e_sb[r_][:, h * 128:(h + 1) * 128], VW[:, kt, :],
                          kt == k0 and h == 0, kt == qi and h == 3, reads=[f"e{r_}", "VW"], writes=["pW"])
            return qk, pv

        def post_combine(qi, bi, final):
            Gq = G_all[:, qi, :].rearrange("p (h j) -> p h j", h=4)
            bank, bk = ((pSL, "pSL"), (pW, "pW"))[bi]
            Qs = slice(qi * 128, (qi + 1) * 128)

            def post():
                b3 = bank[:, 0:260].rearrange("p (h c) -> p h c", h=4)
                kb.op("dve", lambda: V.reciprocal(zz[:, 4 + 4 * bi:8 + 4 * bi], b3[:, :, 64]), reads=[bk], writes=["zz"])
                kb.op("dve", lambda: V.tensor_tensor(out=coef[:, 4 + 4 * bi:8 + 4 * bi], in0=zz[:, 4 + 4 * bi:8 + 4 * bi],
                                                     in1=Gq[:, :, 1 + bi], op=ALU.mult),
                      reads=["zz", "G_all"], writes=["coef"])
                for h in range(4):
                    kb.op("dve", lambda: V.scalar_tensor_tensor(
                        out=o_acc[:, h * 64:(h + 1) * 64], in0=b3[:, h, 0:64],
                        scalar=coef[:, 4 + 4 * bi + h:5 + 4 * bi + h], in1=o_acc[:, h * 64:(h + 1) * 64],
                        op0=ALU.mult, op1=ALU.add), reads=[bk, "coef", "o_acc"], writes=["o_acc"])
                if final:
                    yk = f"y_sb{qi % 2}"
                    kb.op("pool", lambda: G.tensor_copy(y_sb[qi % 2][:], o_acc[:]), reads=["o_acc"], writes=[yk])
                    kb.dma("sp", y[Qs, :], y_sb[qi % 2][:], reads=[yk], writes=["y_out"], force=True)
            return post

        for qi in range(nqb):
            Qs = slice(qi * 128, (qi + 1) * 128)
            Qv = QT[:, :, Qs]
            use_sel = qi >= 8
            jl = (8 * qi + 6) // 128
            cmk = f"cm{qi % 2}"
            kb.dma("pool", cm_sb[qi % 2][:], cmask_d[qi].rearrange("s j q -> j s q"), writes=[cmk])
            if use_sel:
                kb.op("pool", lambda: G.tensor_copy(QaLo[qi % 2][0:64, :].rearrange("p (h q) -> p h q", h=4), Qv),
                      reads=["QT"], writes=[f"QaLoQ{qi % 2}"])
                if qi >= 32:
                    kb.op("pool", lambda: G.tensor_copy(QaHi[qi % 2][0:64, :].rearrange("p (h q) -> p h q", h=4), Qv),
                          reads=["QT"], writes=[f"QaHiQ{qi % 2}"])
            for jt in range(jl + 1):
                qk, pv = tile_a(qi, jt, jl, Qv, cmk)
                push_tile(qk, pv, post_a1(qi) if jt == jl else None)
            k0 = max(0, qi - 4)
            for kt in range(k0, qi + 1):
                qk, pv = tile_d(qi, kt, k0, Qv)
                post = None
                if kt == qi:
                    post = post_combine(qi, 1, False)
                elif use_sel and kt == k0 + 2:
                    post = post_a2(qi)
                push_tile(qk, pv, post)
            for kt in range(qi + 1):
                qk, pv = tile_c(qi, kt, Qv, use_sel)
                push_tile(qk, pv, post_combine(qi, 0, True) if kt == qi else None)
        while pipe:
            drain_one()
        kb.barrier()


def nsa_inputs(x1, w_in, w_ck1, w_ck2, w_cv1, w_cv2, cmp_pe, n_tok=S):
    cst = nsa_consts(n_tok)
    maps = []
    for c in range(NCORES):
        b, g = c // 4, c % 4
        def col(base, width=64, mult=64):
            return w_in[:, base + g * mult: base + g * mult + width]
        q = w_in[:, g * 256:(g + 1) * 256]
        kc, vc, ks, vs, kw, vw = (col(1024), col(1280), col(1536), col(1792), col(2048), col(2304))
        gt = w_in[:, 2560 + g * 12:2560 + (g + 1) * 12]
        wn = np.ascontiguousarray(np.concatenate([q, ks, kw, kc, vc, vs, vw, gt], axis=1))
        m = {"xT": np.ascontiguousarray(x1[b].T[:, :n_tok]), "wn": wn,
             "wk1": w_ck1, "wk2": w_ck2, "wv1": w_cv1, "wv2": w_cv2,
             "peT": np.ascontiguousarray(cmp_pe.T)}
        m.update(cst)
        maps.append(m)
    return maps


GROUPS = [[0, 1, 2, 3], [4, 5, 6, 7]]


def build_fused():
    nc = bass.Bass("TRN2", target_bir_lowering=False)
    with ExitStack() as st0:
        kb = KB(nc, st0)
        cc_sem = st0.enter_context(nc.semaphore("cc_sem"))
        n_cc = [0]
        internal = lambda name, shape, dt: nc.dram_tensor(name, list(shape), dt).ap()
        y0_d = internal("y0_d", [S, 256], BF16)
        g1 = [internal(f"g1_{j}", [4 * NTB, 256], BF16) for j in range(4)]
        x1_d = internal("x1_d", [NTB, D], F32)
        x1T_d = internal("x1T_d", [D, NTB], BF16)
        g2 = [internal(f"g2_{j}", [4 * 256, NTB], BF16) for j in range(4)]
        y1_d = internal("y1_d", [S, 256], BF16)
        g3 = [internal(f"g3_{j}", [4 * NTB, 256], BF16) for j in range(4)]
        qsel = nc.dram_tensor("qsel", [128, 4], F32, kind="ExternalInput").ap()

        def all_gather(srcs, dsts):
            kb.barrier()
            for s_, d_ in zip(srcs, dsts):
                n_cc[0] += 1
                nc.gpsimd.collective_compute("AllGather", ALU.bypass, replica_groups=GROUPS,
                                             ins=[s_], outs=[d_]).then_inc(cc_sem, 1)
            for e in kb.engs.values():
                e.wait_ge(cc_sem, n_cc[0])

        emit_gla(Ctx(nc, kb, "a0_", {"y": y0_d}))
        all_gather([y0_d[j * NTB:(j + 1) * NTB, :] for j in range(4)], g1)
        emit_ffn(Ctx(nc, kb, "b0_"), fused={"g": g1, "qsel": qsel, "x1_d": x1_d, "x1T_d": x1T_d})
        all_gather([x1T_d[j * 256:(j + 1) * 256, :] for j in range(4)], g2)
        emit_nsa(Ctx(nc, kb, "a1_", {"y": y1_d}), g2=g2)
        all_gather([y1_d[j * NTB:(j + 1) * NTB, :] for j in range(4)], g3)
        emit_ffn(Ctx(nc, kb, "b1_", {"xres": x1_d}), fused={"g": g3, "qsel": qsel})
        kb.barrier()
    return nc


_PROGS = {}


def _prog(name, fn):
    if name not in _PROGS:
        _PROGS[name] = fn()
    return _PROGS[name]


def _run(nc, maps):
    res = run_bass_kernel_spmd(nc, maps, core_ids=list(range(NCORES)))
    return res.results


def _gather_heads(results, key="y"):
    first = np.asarray(results[0][key])
    full = np.empty((B * S, D), dtype=first.dtype)
    for c in range(NCORES):
        b, h = c // 4, c % 4
        full[b * S:(b + 1) * S, h * 256:(h + 1) * 256] = np.asarray(results[c][key])
    return full


def _pref(prefix, m, drop=()):
    return {prefix + k: v for k, v in m.items() if k not in drop}


def fused_inputs(x, gla_w_in, gla_w_gate_up, gla_b_gate, gla_norm_g, gla_w_out,
                 nsa_w_in, nsa_w_cmp_k1, nsa_w_cmp_k2, nsa_w_cmp_v1, nsa_w_cmp_v2, nsa_cmp_pe, nsa_w_out,
                 moe_w_router, moe_b_router, moe_w_gate, moe_w_up, moe_w_down, ln_g, ln_b):
    a0 = gla_inputs(x, gla_w_in[0], gla_w_gate_up[0], gla_b_gate[0], gla_norm_g[0])
    dummy_y = np.zeros((B * S, 1), np.float32)
    b0 = ffn_inputs(dummy_y, x.reshape(B * S, D), gla_w_out[0], ln_g[0], ln_b[0], moe_w_router, moe_b_router,
                    moe_w_gate[0], moe_w_up[0], moe_w_down[0])
    a1 = nsa_inputs(np.zeros((B, 1, S), np.float32), nsa_w_in[0], nsa_w_cmp_k1[0], nsa_w_cmp_k2[0],
                    nsa_w_cmp_v1[0], nsa_w_cmp_v2[0], nsa_cmp_pe[0])
    b1 = ffn_inputs(dummy_y, np.zeros((B * S, 1), np.float32), nsa_w_out[0], ln_g[1], ln_b[1], moe_w_router,
                    moe_b_router, moe_w_gate[1], moe_w_up[1], moe_w_down[1])
    maps = []
    for c in range(NCORES):
        m = {}
        m.update(_pref("a0_", a0[c]))
        m.update(_pref("b0_", b0[c], drop=("yT",)))
        m.update(_pref("a1_", a1[c], drop=("xT",)))
        m.update(_pref("b1_", b1[c], drop=("yT", "xres")))
        qs = np.zeros((128, 4), np.float32)
        qs[:, c % 4] = 1.0
        m["qsel"] = qs
        maps.append(m)
    return maps


def kernel(x, gla_w_in, gla_w_gate_up, gla_b_gate, gla_norm_g, gla_w_out,
           nsa_w_in, nsa_w_cmp_k1, nsa_w_cmp_k2, nsa_w_cmp_v1, nsa_w_cmp_v2, nsa_cmp_pe, nsa_w_out,
           moe_w_router, moe_b_router, moe_w_gate, moe_w_up, moe_w_down, ln_g, ln_b):
    f = lambda a: np.ascontiguousarray(np.asarray(a, dtype=np.float32))
    maps = fused_inputs(f(x), f(gla_w_in), f(gla_w_gate_up), f(gla_b_gate), f(gla_norm_g), f(gla_w_out),
                        f(nsa_w_in), f(nsa_w_cmp_k1), f(nsa_w_cmp_k2), f(nsa_w_cmp_v1), f(nsa_w_cmp_v2),
                        f(nsa_cmp_pe), f(nsa_w_out), f(moe_w_router), f(moe_b_router), f(moe_w_gate),
                        f(moe_w_up), f(moe_w_down), f(ln_g), f(ln_b))
    r = _run(_prog("fused", build_fused), maps)
    out = np.concatenate([np.asarray(r[c]["b1_out"]) for c in range(NCORES)], axis=0)
    return out.reshape(B, S, D).astype(np.float32)
```

```python
from contextlib import ExitStack

import numpy as np
import concourse.bass as bass
import concourse.mybir as mybir
from concourse.bass_utils import run_bass_kernel_spmd

F32 = mybir.dt.float32
BF16 = mybir.dt.bfloat16
AF = mybir.ActivationFunctionType
ALU = mybir.AluOpType
AX = mybir.AxisListType

D = 1024
B = 2
S = 8192
NCORES = 8
DN_ALPHA = 4.0 ** 0.25
LN_EPS = 1e-5


class KB:
    NDMA = 12

    def __init__(self, nc, stack):
        self.nc = nc
        self.stack = stack
        self.engs = {"pe": nc.tensor, "act": nc.scalar, "dve": nc.vector,
                     "pool": nc.gpsimd, "sp": nc.sync}
        self.sems = {}
        self.cnt = {}
        for e in ["pe", "act", "dve", "pool"]:
            self.sems[e] = stack.enter_context(nc.semaphore("c_" + e))
            self.cnt[e] = 0
        self.dq = {}
        for q in ["sp", "act", "pool"]:
            for i in range(self.NDMA):
                nm = f"d_{q}{i}"
                self.sems[nm] = stack.enter_context(nc.semaphore(nm))
                self.cnt[nm] = 0
            self.dq[q] = 0
        self.waited = {e: {} for e in self.engs}
        self.last_w = {}
        self.readers = {}
        self.n_inst = 0
        self.limit = None

    def _need(self, eng, dep):
        sem, val = dep
        if eng == "pe" and sem == "pe":
            return
        if self.waited[eng].get(sem, 0) >= val:
            return
        self.engs[eng].wait_ge(self.sems[sem], val)
        self.waited[eng][sem] = val

    def _deps(self, eng, reads, writes):
        for k in reads:
            d = self.last_w.get(k)
            if d is not None:
                self._need(eng, d)
            if k.startswith("p"):
                for d in self.readers.get(k, ()):
                    if d[0] != eng:
                        self._need(eng, d)
        for k in writes:
            d = self.last_w.get(k)
            if d is not None:
                self._need(eng, d)
            for d in self.readers.get(k, ()):
                self._need(eng, d)

    def _commit(self, tok, reads, writes):
        for k in reads:
            self.readers.setdefault(k, []).append(tok)
        for k in writes:
            self.last_w[k] = tok
            self.readers[k] = []

    def op(self, eng, fn, reads=(), writes=()):
        if self.limit is not None and self.n_inst >= self.limit:
            return None
        self._deps(eng, reads, writes)
        ins = fn()
        self.cnt[eng] += 1
        ins.then_inc(self.sems[eng], 1)
        self._commit((eng, self.cnt[eng]), reads, writes)
        self.n_inst += 1
        return ins

    def dma(self, q, out, in_, reads=(), writes=(), **kw):
        if self.limit is not None and self.n_inst >= self.limit and not kw.pop("force", False):
            return None
        kw.pop("force", None)
        i = self.dq[q] % self.NDMA
        self.dq[q] += 1
        nm = f"d_{q}{i}"
        if self.cnt[nm] > 0:
            self._need(q, (nm, self.cnt[nm]))
        self._deps(q, reads, writes)
        ins = self.engs[q].dma_start(out=out, in_=in_, **kw)
        self.cnt[nm] += 16
        ins.then_inc(self.sems[nm], 16)
        self._commit((nm, self.cnt[nm]), reads, writes)
        self.n_inst += 1
        return ins

    def finish(self, keys, eng="sp"):
        for k in keys:
            d = self.last_w.get(k)
            if d is not None:
                self._need(eng, d)

    def barrier(self):
        for e in self.engs:
            for c, v in self.cnt.items():
                if v > 0:
                    self._need(e, (c, v))

    def mm(self, out, lhsT, rhs, start, stop, reads, writes):
        nc = self.nc
        return self.op("pe", lambda: nc.tensor.matmul(out, lhsT, rhs, start=start, stop=stop),
                       reads=reads, writes=writes)

    def act(self, out, in_, func, reads, writes, **kw):
        nc = self.nc
        return self.op("act", lambda: nc.scalar.activation(out, in_, func, **kw),
                       reads=reads, writes=writes)


class Ctx:
    def __init__(self, nc, kb, prefix="", over=None):
        self.nc, self.kb, self.prefix, self.over = nc, kb, prefix, dict(over or {})

    def din(self, name, shape, dt=F32):
        if name in self.over:
            return self.over[name]
        return self.nc.dram_tensor(self.prefix + name, list(shape), dt, kind="ExternalInput").ap()

    def dout(self, name, shape, dt=F32):
        if name in self.over:
            return self.over[name]
        return self.nc.dram_tensor(self.prefix + name, list(shape), dt, kind="ExternalOutput").ap()


def _standalone(emit, **kw):
    nc = bass.Bass("TRN2", target_bir_lowering=False)
    with ExitStack() as st0:
        kb = KB(nc, st0)
        kb.limit = kw.pop("limit", None)
        emit(Ctx(nc, kb), **kw)
    return nc


_UID = [0]


def _uniq(name):
    _UID[0] += 1
    return f"{name}_{_UID[0]}"


def sb(nc, st, name, shape, dt):
    return st.enter_context(nc.sbuf_tensor(_uniq(name), list(shape), dt))


def ps(nc, st, name, shape, dt=F32):
    return st.enter_context(nc.psum_tensor(_uniq(name), list(shape), dt))


GLA_DK = 128
GLA_DV = 256
GC = 128
TT = 512


def gla_consts():
    j = np.arange(128)[:, None]
    i = np.arange(128)[None, :]
    tri_i = np.where(j <= i, -1.0 / 16.0, 0.0).astype(np.float32)
    tri_u = np.where(j > i, -1.0 / 16.0, 0.0).astype(np.float32)
    mask = np.where(j <= i, 1.0, 0.0).astype(np.float32)
    return tri_i, tri_u, mask


def build_gla(n_tok=S, limit=None):
    return _standalone(emit_gla, n_tok=n_tok, limit=limit)


def emit_gla(ctx, n_tok=S):
    nc, kb = ctx.nc, ctx.kb
    limit = kb.limit
    xT = ctx.din("xT", [D, n_tok])
    wqk = ctx.din("wqk", [D, 256])
    wkvr = ctx.din("wkvr", [D, 640])
    wg = ctx.din("wg", [D, 16])
    wgu = ctx.din("wgu", [33, 128])
    normg = ctx.din("normg", [1, 256])
    tri_i_d = ctx.din("tri_i", [128, 128])
    tri_u_d = ctx.din("tri_u", [128, 128])
    mask_d = ctx.din("maskT", [128, 128])
    y = ctx.dout("y", [n_tok, 256], BF16)

    with ExitStack() as st:
        V, A = nc.vector, nc.scalar
        w_qk = sb(nc, st, "w_qk", [128, 8, 256], BF16)
        w_kvr = sb(nc, st, "w_kvr", [128, 8, 640], BF16)
        w_g = sb(nc, st, "w_g", [128, 8, 16], BF16)
        w_gu = sb(nc, st, "w_gu", [33, 128], F32)
        ng = sb(nc, st, "ng", [128, 256], F32)
        tri_i = sb(nc, st, "tri_i_s", [128, 128], F32)
        tri_u = sb(nc, st, "tri_u_s", [128, 128], F32)
        maskT = sb(nc, st, "mask_s", [128, 128], F32)
        xt = [sb(nc, st, f"xt{i}", [128, 8, TT], BF16) for i in range(2)]
        g_aug = sb(nc, st, "g_aug", [33, TT], F32)
        e1 = sb(nc, st, "e1", [128, 128], F32)
        la = sb(nc, st, "la", [128, 128], F32)
        eb = sb(nc, st, "eb", [128, 128], F32)
        enb = sb(nc, st, "enb", [128, 128], F32)
        w2 = sb(nc, st, "w2", [128, 128], F32)
        qdT = sb(nc, st, "qdT", [128, 128], BF16)
        kdT = sb(nc, st, "kdT", [128, 128], BF16)
        kd2 = sb(nc, st, "kd2", [128, 128], BF16)
        v_bf = sb(nc, st, "v_bf", [128, 256], BF16)
        atm = sb(nc, st, "atm", [128, 128], BF16)
        S_f = sb(nc, st, "S_f", [128, 256], F32)
        S_b = sb(nc, st, "S_b", [128, 256], BF16)
        junk = sb(nc, st, "junk", [128, 256], F32)
        ss = sb(nc, st, "ss", [128, 1], F32)
        rstd = sb(nc, st, "rstd", [128, 1], F32)
        er = sb(nc, st, "er", [128, 256], F32)
        rs = sb(nc, st, "rs", [128, 256], F32)
        on = sb(nc, st, "on", [128, 256], F32)
        yt = [sb(nc, st, f"yt{i}", [128, 256], BF16) for i in range(2)]

        p_q = ps(nc, st, "p_q", [128, TT])
        p_k = ps(nc, st, "p_k", [128, TT])
        p_g_full = ps(nc, st, "p_g", [128, TT])
        p_g = p_g_full[0:16, :]
        p_m = ps(nc, st, "p_m", [128, 512])
        p_kv = ps(nc, st, "p_kv", [128, 512])
        p_ro = ps(nc, st, "p_ro", [128, 512])
        p_st_full = ps(nc, st, "p_st", [128, 512])
        p_st = p_st_full[:, 0:256]

        kb.dma("pool", w_qk[:], wqk.rearrange("(kc p) n -> p kc n", p=128), writes=["w_qk"])
        kb.dma("pool", w_kvr[:], wkvr.rearrange("(kc p) n -> p kc n", p=128), writes=["w_kvr"])
        kb.dma("pool", w_g[:], wg.rearrange("(kc p) n -> p kc n", p=128), writes=["w_g"])
        kb.dma("sp", w_gu[:], wgu, writes=["w_gu"])
        kb.dma("sp", ng[:], normg.partition_broadcast(128), writes=["ng"])
        kb.dma("sp", tri_i[:], tri_i_d, writes=["tri_i"])
        kb.dma("sp", tri_u[:], tri_u_d, writes=["tri_u"])
        kb.dma("sp", maskT[:], mask_d, writes=["maskT"])
        kb.op("dve", lambda: V.memset(S_f[:], 0.0), writes=["S_f"])
        kb.op("dve", lambda: V.memset(S_b[:], 0.0), writes=["S_b"])
        kb.op("dve", lambda: V.memset(g_aug[:], 1.0), writes=["g_aug"])
        eps_t = sb(nc, st, "eps_t", [128, 1], F32)
        kb.op("dve", lambda: V.memset(eps_t[:], LN_EPS), writes=["eps_t"])

        xTv = xT.rearrange("(kc p) t -> p kc t", p=128)
        n_tiles = n_tok // TT
        for T in range(n_tiles):
            x_t = xt[T % 2]
            xk = f"xt{T % 2}"
            kb.dma("pool", x_t[:], xTv[:, :, T * TT:(T + 1) * TT], writes=[xk])
            for kc in range(8):
                kb.mm(p_q[:], w_qk[:, kc, 0:128], x_t[:, kc, :], kc == 0, kc == 7,
                      reads=["w_qk", xk], writes=["p_q"])
            for kc in range(8):
                kb.mm(p_k[:], w_qk[:, kc, 128:256], x_t[:, kc, :], kc == 0, kc == 7,
                      reads=["w_qk", xk], writes=["p_k"])
            for kc in range(8):
                kb.mm(p_g, w_g[:, kc, :], x_t[:, kc, :], kc == 0, kc == 7,
                      reads=["w_g", xk], writes=["p_g"])
            kb.op("dve", lambda: V.tensor_copy(g_aug[0:16, :], p_g), reads=["p_g"], writes=["g_aug"])
            for c in range(TT // GC):
                cs = slice(c * GC, (c + 1) * GC)
                kb.mm(p_m[:, 0:128], g_aug[:, cs], w_gu[:], True, True,
                      reads=["g_aug", "w_gu"], writes=["p_m"])
                kb.act(e1[:], p_m[:, 0:128], AF.Exp, reads=["p_m"], writes=["e1"], scale=-1.0)
                kb.act(la[:], e1[:], AF.Ln, reads=["e1"], writes=["la"], bias=1.0)
                kb.mm(p_m[:, 128:256], la[:], tri_i[:], True, True, reads=["la", "tri_i"], writes=["p_m"])
                kb.mm(p_m[:, 256:384], tri_u[:], la[:], True, True, reads=["la", "tri_u"], writes=["p_m"])
                kb.act(eb[:], p_m[:, 128:256], AF.Exp, reads=["p_m"], writes=["eb"])
                kb.act(enb[:], p_m[:, 128:256], AF.Exp, reads=["p_m"], writes=["enb"], scale=-1.0)
                kb.act(w2[:], p_m[:, 256:384], AF.Exp, reads=["p_m"], writes=["w2"])
                kb.op("dve", lambda: V.scalar_tensor_tensor(
                    out=qdT[:], in0=p_q[:, cs], scalar=float(GLA_DK ** -0.5), in1=eb[:],
                    op0=ALU.mult, op1=ALU.mult), reads=["p_q", "eb"], writes=["qdT"])
                kb.op("dve", lambda: V.tensor_tensor(out=kdT[:], in0=p_k[:, cs], in1=enb[:], op=ALU.mult),
                      reads=["p_k", "enb"], writes=["kdT"])
                for kc in range(8):
                    kb.mm(p_kv[:, 0:384], x_t[:, kc, cs], w_kvr[:, kc, 0:384], kc == 0, kc == 7,
                          reads=[xk, "w_kvr"], writes=["p_kv"])
                for kc in range(8):
                    kb.mm(p_ro[:, 0:256], x_t[:, kc, cs], w_kvr[:, kc, 384:640], kc == 0, kc == 7,
                          reads=[xk, "w_kvr"], writes=["p_ro"])
                kb.op("dve", lambda: V.tensor_tensor(out=kd2[:], in0=p_kv[:, 0:128], in1=w2[:], op=ALU.mult),
                      reads=["p_kv", "w2"], writes=["kd2"])
                kb.op("dve", lambda: V.tensor_copy(v_bf[:], p_kv[:, 128:384]), reads=["p_kv"], writes=["v_bf"])
                kb.mm(p_m[:, 384:512], kdT[:], qdT[:], True, True, reads=["kdT", "qdT"], writes=["p_m"])
                kb.op("dve", lambda: V.tensor_tensor(out=atm[:], in0=p_m[:, 384:512], in1=maskT[:], op=ALU.mult),
                      reads=["p_m", "maskT"], writes=["atm"])
                kb.mm(p_ro[:, 256:512], atm[:], v_bf[:], True, False, reads=["atm", "v_bf"], writes=["p_ro"])
                kb.mm(p_ro[:, 256:512], qdT[:], S_b[:], False, True, reads=["qdT", "S_b"], writes=["p_ro"])
                kb.mm(p_st, kd2[:], v_bf[:], True, True, reads=["kd2", "v_bf"], writes=["p_st"])
                kb.op("dve", lambda: V.scalar_tensor_tensor(
                    out=S_f[:], in0=S_f[:], scalar=eb[:, 127:128], in1=p_st,
                    op0=ALU.mult, op1=ALU.add), reads=["S_f", "eb", "p_st"], writes=["S_f"])
                kb.op("pool", lambda: nc.gpsimd.tensor_copy(S_b[:], S_f[:]), reads=["S_f"], writes=["S_b"])
                kb.act(junk[:], p_ro[:, 256:512], AF.Square, reads=["p_ro"], writes=["junk", "ss"],
                       scale=1.0 / 16.0, accum_out=ss[:])
                kb.act(rstd[:], ss[:], AF.Ln, reads=["ss"], writes=["rstd"], bias=eps_t[:])
                kb.act(rstd[:], rstd[:], AF.Exp, reads=["rstd"], writes=["rstd"], scale=-0.5)
                kb.act(er[:], p_ro[:, 0:256], AF.Exp, reads=["p_ro"], writes=["er"], scale=-1.0)
                kb.op("dve", lambda: V.tensor_scalar_add(out=er[:], in0=er[:], scalar1=1.0),
                      reads=["er"], writes=["er"])
                kb.op("dve", lambda: V.reciprocal(out=er[:], in_=er[:]), reads=["er"], writes=["er"])
                kb.op("dve", lambda: V.tensor_tensor(out=rs[:], in0=p_ro[:, 0:256], in1=er[:], op=ALU.mult),
                      reads=["p_ro", "er"], writes=["rs"])
                kb.op("dve", lambda: V.scalar_tensor_tensor(
                    out=on[:], in0=p_ro[:, 256:512], scalar=rstd[:, 0:1], in1=ng[:],
                    op0=ALU.mult, op1=ALU.mult), reads=["p_ro", "rstd", "ng"], writes=["on"])
                ci = T * (TT // GC) + c
                y_t = yt[ci % 2]
                yk = f"yt{ci % 2}"
                kb.op("pool", lambda: nc.gpsimd.tensor_tensor(out=y_t[:], in0=on[:], in1=rs[:], op=ALU.mult),
                      reads=["on", "rs"], writes=[yk])
                kb.dma("sp", y[ci * GC:(ci + 1) * GC, :], y_t[:], reads=[yk], writes=["y_out"])
        if limit is not None:
            kb.dma("sp", y[0:128, :], yt[0][:], reads=["yt0"], writes=["y_out"], force=True)
            print("n_inst", kb.n_inst)
        kb.barrier()


def gla_inputs(x, w_in, w_gate_up, b_gate, norm_g):
    tri_i, tri_u, mask = gla_consts()
    maps = []
    for c in range(NCORES):
        b, h = c // 4, c % 4
        q = w_in[:, h * 128:(h + 1) * 128]
        k = w_in[:, 512 + h * 128:512 + (h + 1) * 128]
        v = w_in[:, 1024 + h * 256:1024 + (h + 1) * 256]
        g = w_in[:, 2048:2064]
        r = w_in[:, 2064 + h * 256:2064 + (h + 1) * 256]
        wgu = np.zeros((33, 128), np.float32)
        wgu[0:16] = w_gate_up[:, h * 128:(h + 1) * 128]
        wgu[32] = b_gate[h * 128:(h + 1) * 128]
        maps.append({
            "xT": np.ascontiguousarray(x[b].T),
            "wqk": np.ascontiguousarray(np.concatenate([q, k], axis=1)),
            "wkvr": np.ascontiguousarray(np.concatenate([k, v, r], axis=1)),
            "wg": np.ascontiguousarray(g),
            "wgu": wgu,
            "normg": np.ascontiguousarray(norm_g.reshape(1, 256)),
            "tri_i": tri_i, "tri_u": tri_u, "maskT": mask,
        })
    return maps


NTB = 2048
NE = 16
DFF = 256


def moe_consts():
    sel = np.zeros((16, 16, 128), np.float32)
    for e in range(16):
        sel[e, e, :] = 1.0
    ident = np.eye(128, dtype=np.float32)
    return sel, ident


def build_ffn(n_tok=NTB, limit=None, n_exp=NE):
    return _standalone(emit_ffn, n_tok=n_tok, limit=limit, n_exp=n_exp)


def emit_ffn(ctx, n_tok=NTB, n_exp=NE, fused=None):
    nc, kb = ctx.nc, ctx.kb
    limit = kb.limit
    if fused is None:
        yT = ctx.din("yT", [D, n_tok], BF16)
    xres = ctx.din("xres", [n_tok, D])
    wout = ctx.din("wout", [D, D])
    lnp = ctx.din("lnp", [4, D])
    wr = ctx.din("wr", [D, NE])
    br = ctx.din("br", [1, NE])
    wgd = ctx.din("wg", [NE, D, DFF])
    wud = ctx.din("wu", [NE, D, DFF])
    wdd = ctx.din("wd", [NE, DFF, D])
    sel_d = ctx.din("sel", [16, 16, 128])
    ident_d = ctx.din("ident", [128, 128])
    out = ctx.dout("out", [n_tok, D]) if (fused is None or "x1_d" not in fused) else None
    n_sub = n_tok // 128
    n_tile = n_tok // 512

    with ExitStack() as st:
        V, A, G = nc.vector, nc.scalar, nc.gpsimd
        w_o = sb(nc, st, "w_o", [128, 8, D], BF16)
        lng = [sb(nc, st, f"lnp{i}", [128, D], F32) for i in range(4)]
        w_r = sb(nc, st, "w_r", [128, 8, NE], F32)
        b_r = sb(nc, st, "b_r", [128, NE], F32)
        sel = sb(nc, st, "sel_s", [16, 16, 128], BF16)
        ident = sb(nc, st, "ident_s", [128, 128], F32)
        eps_t = sb(nc, st, "eps_t", [128, 1], F32)
        acc = sb(nc, st, "acc", [128, n_sub, D], F32)
        x1T = sb(nc, st, "x1T", [128, 8, n_tok], BF16)
        gT = sb(nc, st, "gT", [16, n_tok], BF16)
        y_t = [sb(nc, st, "y_t0", [128, 8, 128], BF16)] * 2
        xr = [sb(nc, st, "xr0", [128, D], F32)] * 2
        x1 = sb(nc, st, "x1", [128, D], F32)
        stats = sb(nc, st, "stats", [128, 2, 6], F32)
        mv = sb(nc, st, "mv", [128, 2], F32)
        rstd = sb(nc, st, "rstd", [128, 1], F32)
        xTf = sb(nc, st, "xTf", [128, 8, 128], F32)
        sg_ = sb(nc, st, "r_s", [128, 16], F32)
        bi_ = sb(nc, st, "r_bi", [128, 16], F32)
        b2_ = sb(nc, st, "r_b2", [128, 16], F32)
        eq_ = sb(nc, st, "r_eq", [128, 16], F32)
        m1_ = sb(nc, st, "r_m1", [128, 4], F32)
        m2_ = sb(nc, st, "r_m2", [128, 4], F32)
        gs_ = sb(nc, st, "r_gs", [128, 4], F32)
        gm_ = sb(nc, st, "r_gm", [128, 1], F32)
        ig_ = sb(nc, st, "r_ig", [128, 4], F32)
        se_ = sb(nc, st, "r_se", [128, 16], F32)
        ws_ = sb(nc, st, "r_ws", [128, 1], F32)
        gate = sb(nc, st, "gate", [128, 16], F32)
        wg_s = [sb(nc, st, f"wg_s{i}", [128, 8, DFF], BF16) for i in range(2)]
        wu_s = [sb(nc, st, f"wu_s{i}", [128, 8, DFF], BF16) for i in range(2)]
        wd_s = [sb(nc, st, f"wd_s{i}", [128, 2, D], BF16) for i in range(2)]
        gb = [sb(nc, st, f"gb{i}", [128, 512], BF16) for i in range(2)]
        sgl = [sb(nc, st, f"sgl{i}", [128, 512], BF16) for i in range(2)]
        t1 = [sb(nc, st, f"t1{i}", [128, 512], BF16) for i in range(2)]
        hT = [sb(nc, st, f"hT{i}", [128, 2, 512], BF16) for i in range(2)]

        pb = [ps(nc, st, f"pb{i}", [128, 512]) for i in range(8)]
        PK = [f"pb{i}" for i in range(8)]

        kb.dma("pool", w_o[:], wout.rearrange("(kc p) n -> p kc n", p=128), writes=["w_o"])
        for i in range(4):
            kb.dma("sp", lng[i][:], lnp[i:i + 1, :].partition_broadcast(128), writes=[f"lnp{i}"])
        kb.dma("sp", w_r[:], wr.rearrange("(kc p) n -> p kc n", p=128), writes=["w_r"])
        kb.dma("sp", b_r[:], br.partition_broadcast(128), writes=["b_r"])
        kb.dma("pool", sel[:], sel_d, writes=["sel"])
        kb.dma("sp", ident[:], ident_d, writes=["ident"])
        kb.op("dve", lambda: V.memset(eps_t[:], LN_EPS), writes=["eps_t"])

        def load_expert(e):
            i = e % 2
            kb.dma("pool", wg_s[i][:], wgd[e].rearrange("(kc p) f -> p kc f", p=128), writes=[f"wg{i}"])
            kb.dma("pool", wu_s[i][:], wud[e].rearrange("(kc p) f -> p kc f", p=128), writes=[f"wu{i}"])
            kb.dma("pool", wd_s[i][:], wdd[e].rearrange("(fc p) d -> p fc d", p=128), writes=[f"wd{i}"])

        def layer_norm(src, dst, gi, eng2, fast=False):
            s_t, s_k = src
            d_t, d_k = dst
            for hh in range(2):
                kb.op("dve", lambda: V.bn_stats(stats[:, hh, :], s_t[:, hh * 512:(hh + 1) * 512]),
                      reads=[s_k], writes=["stats"])
            kb.op("dve", lambda: V.bn_aggr(mv[:], stats[:]), reads=["stats"], writes=["mv"])
            kb.act(rstd[:], mv[:, 1:2], AF.Sqrt, reads=["mv"], writes=["rstd"], bias=eps_t[:])
            kb.op("dve", lambda: V.reciprocal(rstd[:], rstd[:]), reads=["rstd"], writes=["rstd"])
            if fast:
                kb.op("dve", lambda: V.scalar_tensor_tensor(out=d_t, in0=s_t, scalar=mv[:, 0:1], in1=lng[gi][:],
                                                            op0=ALU.subtract, op1=ALU.mult),
                      reads=[s_k, "mv", f"lnp{gi}"], writes=[d_k])
                kb.op("dve", lambda: V.scalar_tensor_tensor(out=d_t, in0=d_t, scalar=rstd[:, 0:1], in1=lng[gi + 1][:],
                                                            op0=ALU.mult, op1=ALU.add),
                      reads=[d_k, "rstd", f"lnp{gi + 1}"], writes=[d_k])
                return
            kb.op("dve", lambda: V.tensor_scalar(out=d_t, in0=s_t, scalar1=mv[:, 0:1], scalar2=rstd[:, 0:1],
                                                 op0=ALU.subtract, op1=ALU.mult),
                  reads=[s_k, "mv", "rstd"], writes=[d_k])
            kb.op("pool", lambda: G.tensor_tensor(out=d_t, in0=d_t, in1=lng[gi][:], op=ALU.mult),
                  reads=[d_k, f"lnp{gi}"], writes=[d_k])
            kb.op("pool", lambda: G.tensor_tensor(out=d_t, in0=d_t, in1=lng[gi + 1][:], op=ALU.add),
                  reads=[d_k, f"lnp{gi + 1}"], writes=[d_k])

        load_expert(0)
        if fused is None:
            yTv = yT.rearrange("(kc p) t -> p kc t", p=128)
        else:
            g_v = [gq.rearrange("(h t) c -> t h c", h=4) for gq in fused["g"]]
            cand = [sb(nc, st, f"cand{i}", [128, 4, D], BF16) for i in range(2)]
            ysel = sb(nc, st, "ysel", [128, D], BF16)
            qsel = sb(nc, st, "qsel_s", [128, 4], F32)
            identb = sb(nc, st, "identb", [128, 128], BF16)
            xo = [sb(nc, st, "xo0", [128, 8, 128], BF16)] * 2
            kb.dma("sp", qsel[:], fused["qsel"], writes=["qsel"])
            kb.dma("pool", identb[:], ident_d, writes=["identb"])
        for sub in range(n_sub):
            ts_ = slice(sub * 128, (sub + 1) * 128)
            yk = "y_t0"
            xk = "xr0"
            if fused is None:
                kb.dma("sp", y_t[sub % 2][:], yTv[:, :, ts_], writes=[yk])
            else:
                ck = f"cand{sub % 2}_"
                for qq in range(4):
                    kb.dma("sp" if qq % 2 == 0 else "act", cand[sub % 2][:, qq, :].rearrange("p (h c) -> p h c", h=4),
                           g_v[qq][sub * 128:(sub + 1) * 128, :, :], writes=[ck + str(qq)])
                kb.op("pool", lambda: G.tensor_scalar(out=ysel[:], in0=cand[sub % 2][:, 0, :], scalar1=qsel[:, 0:1],
                                                      scalar2=None, op0=ALU.mult), reads=[ck + "0", "qsel"], writes=["ysel"])
                for qq in range(1, 4):
                    kb.op("dve", lambda: V.scalar_tensor_tensor(out=ysel[:], in0=cand[sub % 2][:, qq, :],
                                                                 scalar=qsel[:, qq:qq + 1], in1=ysel[:],
                                                                 op0=ALU.mult, op1=ALU.add),
                          reads=[ck + str(qq), "qsel", "ysel"], writes=["ysel"])
                pTb = pb[7][:].bitcast(BF16)
                for kc in range(8):
                    kb.op("pe", lambda: nc.tensor.transpose(pTb[:, kc * 128:(kc + 1) * 128],
                                                            ysel[:, kc * 128:(kc + 1) * 128], identb[:]),
                          reads=["ysel", "identb"], writes=[PK[7]])
                kb.op("dve", lambda: V.tensor_copy(y_t[sub % 2][:], pTb.rearrange("p (k t) -> p k t", k=8)),
                      reads=[PK[7]], writes=[yk])
            kb.dma("sp", xr[sub % 2][:], xres[ts_, :], writes=[xk])
            for hh in range(2):
                for kc in range(8):
                    kb.mm(pb[hh][:], y_t[sub % 2][:, kc, :], w_o[:, kc, hh * 512:(hh + 1) * 512],
                          kc == 0, kc == 7, reads=[yk, "w_o"], writes=[PK[hh]])
            for hh in range(2):
                kb.op("dve", lambda: V.scalar_tensor_tensor(
                    out=acc[:, sub, hh * 512:(hh + 1) * 512], in0=xr[sub % 2][:, hh * 512:(hh + 1) * 512],
                    scalar=float(DN_ALPHA), in1=pb[hh][:], op0=ALU.mult, op1=ALU.add),
                    reads=[xk, PK[hh]], writes=[f"acc{sub}"])
            layer_norm((acc[:, sub, :], f"acc{sub}"), (x1[:], "x1"), 0, None, fast=True)
            kb.op("pool", lambda: G.tensor_scalar(out=acc[:, sub, :], in0=x1[:], scalar1=float(DN_ALPHA),
                                                  scalar2=None, op0=ALU.mult),
                  reads=["x1"], writes=[f"acc{sub}"])
            for kc in range(8):
                bank = 2 + kc // 4
                kb.op("pe", lambda: nc.tensor.transpose(pb[bank][:, (kc % 4) * 128:(kc % 4 + 1) * 128],
                                                        x1[:, kc * 128:(kc + 1) * 128], ident[:]),
                      reads=["x1", "ident"], writes=[PK[bank]])
            for q in range(2):
                kb.op("dve" if q == 0 else "act",
                      (lambda: V.tensor_copy(xTf[:, 0:4, :], pb[2][:].rearrange("p (k t) -> p k t", k=4))) if q == 0
                      else (lambda: A.copy(xTf[:, 4:8, :], pb[3][:].rearrange("p (k t) -> p k t", k=4))),
                      reads=[PK[2 + q]], writes=[f"xTf{q}"])
            kb.op("pool", lambda: G.tensor_copy(x1T[:, :, ts_], xTf[:]), reads=["xTf0", "xTf1"], writes=["x1T"])
            for kc in range(8):
                kb.mm(pb[4][:, 0:16], xTf[:, kc, :], w_r[:, kc, :], kc == 0, kc == 7,
                      reads=["xTf0", "xTf1", "w_r"], writes=[PK[4]])
            kb.act(sg_[:], pb[4][:, 0:16], AF.Sigmoid, reads=[PK[4]], writes=["r_s"])
            kb.op("dve", lambda: V.tensor_tensor(out=bi_[:], in0=sg_[:], in1=b_r[:], op=ALU.add),
                  reads=["r_s", "b_r"], writes=["r_bi"])
            bi3 = bi_[:].rearrange("p (g e) -> p g e", g=4)
            kb.op("dve", lambda: V.tensor_reduce(out=m1_[:], in_=bi3, axis=AX.X, op=ALU.max),
                  reads=["r_bi"], writes=["r_m1"])
            kb.op("dve", lambda: V.tensor_tensor(out=eq_[:].rearrange("p (g e) -> p g e", g=4), in0=bi3,
                                                 in1=m1_[:].unsqueeze(2).to_broadcast([128, 4, 4]), op=ALU.is_equal),
                  reads=["r_bi", "r_m1"], writes=["r_eq"])
            kb.op("dve", lambda: V.scalar_tensor_tensor(out=b2_[:], in0=eq_[:], scalar=-1e30, in1=bi_[:],
                                                        op0=ALU.mult, op1=ALU.add),
                  reads=["r_eq", "r_bi"], writes=["r_b2"])
            kb.op("dve", lambda: V.tensor_reduce(out=m2_[:], in_=b2_[:].rearrange("p (g e) -> p g e", g=4),
                                                 axis=AX.X, op=ALU.max),
                  reads=["r_b2"], writes=["r_m2"])
            kb.op("dve", lambda: V.tensor_tensor(out=gs_[:], in0=m1_[:], in1=m2_[:], op=ALU.add),
                  reads=["r_m1", "r_m2"], writes=["r_gs"])
            kb.op("dve", lambda: V.tensor_reduce(out=gm_[:], in_=gs_[:], axis=AX.X, op=ALU.max),
                  reads=["r_gs"], writes=["r_gm"])
            kb.op("dve", lambda: V.tensor_scalar(out=ig_[:], in0=gs_[:], scalar1=gm_[:, 0:1], scalar2=None,
                                                 op0=ALU.is_ge),
                  reads=["r_gs", "r_gm"], writes=["r_ig"])
            kb.op("dve", lambda: V.tensor_tensor(out=se_[:].rearrange("p (g e) -> p g e", g=4), in0=bi3,
                                                 in1=m2_[:].unsqueeze(2).to_broadcast([128, 4, 4]), op=ALU.is_ge),
                  reads=["r_bi", "r_m2"], writes=["r_se"])
            kb.op("dve", lambda: V.tensor_tensor(out=se_[:].rearrange("p (g e) -> p g e", g=4),
                                                 in0=se_[:].rearrange("p (g e) -> p g e", g=4),
                                                 in1=ig_[:].unsqueeze(2).to_broadcast([128, 4, 4]), op=ALU.mult),
                  reads=["r_se", "r_ig"], writes=["r_se"])
            kb.op("dve", lambda: V.tensor_tensor(out=se_[:], in0=se_[:], in1=sg_[:], op=ALU.mult),
                  reads=["r_se", "r_s"], writes=["r_se"])
            kb.op("dve", lambda: V.tensor_reduce(out=ws_[:], in_=se_[:], axis=AX.X, op=ALU.add),
                  reads=["r_se"], writes=["r_ws"])
            kb.op("dve", lambda: V.reciprocal(ws_[:], ws_[:]), reads=["r_ws"], writes=["r_ws"])
            kb.op("dve", lambda: V.tensor_scalar(out=gate[:], in0=se_[:], scalar1=ws_[:, 0:1], scalar2=None,
                                                 op0=ALU.mult),
                  reads=["r_se", "r_ws"], writes=["gate"])
            kb.op("pe", lambda: nc.tensor.transpose(pb[5][0:16, 0:128], gate[:], ident[:]),
                  reads=["gate", "ident"], writes=[PK[5]])
            kb.op("dve", lambda: V.tensor_copy(gT[:, ts_], pb[5][0:16, 0:128]), reads=[PK[5]], writes=["gT"])

        pend = []

        def down_part(e, T, i, j):
            def f():
                for s4 in range(4):
                    sub = T * 4 + s4
                    for hh in range(2):
                        bank = 5 + (s4 * 2 + hh) % 3
                        for fc in range(2):
                            kb.mm(pb[bank][:], hT[j][:, fc, s4 * 128:(s4 + 1) * 128],
                                  wd_s[i][:, fc, hh * 512:(hh + 1) * 512], fc == 0, fc == 1,
                                  reads=[f"hT{j}", f"wd{i}"], writes=[PK[bank]])
                        kb.op("dve", lambda: V.tensor_tensor(out=acc[:, sub, hh * 512:(hh + 1) * 512],
                                                             in0=acc[:, sub, hh * 512:(hh + 1) * 512],
                                                             in1=pb[bank][:], op=ALU.add),
                              reads=[f"acc{sub}", PK[bank]], writes=[f"acc{sub}"])
            return f

        for e in range(n_exp):
            i = e % 2
            for T in range(n_tile):
                Ts = slice(T * 512, (T + 1) * 512)
                j = (e * n_tile + T) % 2
                kb.mm(pb[4][:], sel[:, e, :], gT[:, Ts], True, True, reads=["sel", "gT"], writes=[PK[4]])
                kb.op("act", lambda: A.copy(gb[j][:], pb[4][:]), reads=[PK[4]], writes=[f"gb{j}"])
                for fc in range(2):
                    for kc in range(8):
                        kb.mm(pb[fc][:], wg_s[i][:, kc, fc * 128:(fc + 1) * 128], x1T[:, kc, Ts],
                              kc == 0, kc == 7, reads=[f"wg{i}", "x1T"], writes=[PK[fc]])
                    for kc in range(8):
                        kb.mm(pb[2 + fc][:], wu_s[i][:, kc, fc * 128:(fc + 1) * 128], x1T[:, kc, Ts],
                              kc == 0, kc == 7, reads=[f"wu{i}", "x1T"], writes=[PK[2 + fc]])
                for fc in range(2):
                    kb.act(sgl[fc][:], pb[fc][:], AF.Silu, reads=[PK[fc]], writes=[f"sgl{fc}"])
                    kb.op("dve", lambda: V.tensor_tensor(out=t1[fc][:], in0=sgl[fc][:], in1=pb[2 + fc][:], op=ALU.mult),
                          reads=[f"sgl{fc}", PK[2 + fc]], writes=[f"t1{fc}"])
                    kb.op("pool", lambda: G.tensor_tensor(out=hT[j][:, fc, :], in0=t1[fc][:], in1=gb[j][:], op=ALU.mult),
                          reads=[f"t1{fc}", f"gb{j}"], writes=[f"hT{j}"])
                while pend:
                    pend.pop(0)()
                if T == 0 and e + 1 < n_exp:
                    load_expert(e + 1)
                pend.append(down_part(e, T, i, j))
        while pend:
            pend.pop(0)()

        for sub in range(n_sub):
            o_ap = acc[:, sub, :]
            ok_ = f"acc{sub}"
            layer_norm((o_ap, ok_), (o_ap, ok_), 2, None)
            if out is not None:
                kb.dma("sp", out[sub * 128:(sub + 1) * 128, :], o_ap, reads=[ok_], writes=["out"], force=True)
            else:
                kb.dma("sp", fused["x1_d"][sub * 128:(sub + 1) * 128, :], o_ap, reads=[ok_], writes=["x1_d"])
                for kc in range(8):
                    bank = 2 + kc // 4
                    kb.op("pe", lambda: nc.tensor.transpose(pb[bank][:, (kc % 4) * 128:(kc % 4 + 1) * 128],
                                                            acc[:, sub, kc * 128:(kc + 1) * 128], ident[:]),
                          reads=[ok_, "ident"], writes=[PK[bank]])
                xk_ = "xo0"
                kb.op("dve", lambda: V.tensor_copy(xo[sub % 2][:, 0:4, :], pb[2][:].rearrange("p (k t) -> p k t", k=4)),
                      reads=[PK[2]], writes=[xk_])
                kb.op("dve", lambda: V.tensor_copy(xo[sub % 2][:, 4:8, :], pb[3][:].rearrange("p (k t) -> p k t", k=4)),
                      reads=[PK[3]], writes=[xk_])
                kb.dma("sp", fused["x1T_d"].rearrange("(kc p) t -> p kc t", p=128)[:, :, sub * 128:(sub + 1) * 128],
                       xo[sub % 2][:], reads=[xk_], writes=["x1T_d"])
        kb.barrier()


def ffn_inputs(y_tok_major, xres, w_out, ln_g, ln_b, w_router, b_router, w_gate, w_up, w_down):
    sel, ident = moe_consts()
    lnp = np.ascontiguousarray(np.stack([ln_g[0], ln_b[0], ln_g[1], ln_b[1]]).astype(np.float32))
    maps = []
    for c in range(NCORES):
        rs_ = slice(c * NTB, (c + 1) * NTB)
        maps.append({
            "yT": np.ascontiguousarray(y_tok_major[rs_].T),
            "xres": np.ascontiguousarray(xres[rs_]),
            "wout": w_out, "lnp": lnp, "wr": w_router,
            "br": np.ascontiguousarray(b_router.reshape(1, NE)),
            "wg": w_gate, "wu": w_up, "wd": w_down, "sel": sel, "ident": ident,
        })
    return maps


NSA_SCALE = 0.125
MASK_NEG = -240000.0


def nsa_consts(n_tok=S):
    nqb = n_tok // 128
    inv = np.power(500000.0, -np.arange(8, dtype=np.float32) * (2.0 / 16.0)).astype(np.float32)
    def cs(pos):
        ang = pos.astype(np.float32)[:, None] * inv[None, :]
        return np.concatenate([np.cos(ang), np.sin(ang)], axis=1).astype(np.float32)
    def pl(a):
        n = a.shape[0] // 128
        return np.ascontiguousarray(a.reshape(n, 128, 16).transpose(1, 0, 2).reshape(128, n * 16))
    cs_tok = pl(cs(np.arange(n_tok)))
    cs_cmp = pl(cs(np.arange(512) * 16 + 31))
    wimp = np.zeros((512, 128), np.float32)
    for s_ in range(128):
        for o, wgt in enumerate([1, 2, 2, 2, 1]):
            c = 4 * s_ + o
            if c < 511:
                wimp[c, s_] = wgt
    texp = ((np.arange(n_tok)[None, :] // 64) % 64 == np.arange(64)[:, None]).astype(np.float32)
    k = np.arange(128)[:, None]
    q = np.arange(128)[None, :]
    causal = (k <= q).astype(np.float32)
    strict = (k > q).astype(np.float32)
    cmask = np.zeros((nqb, 2, 128, 128), np.float32)
    for qi in range(nqb):
        jl = (8 * qi + 6) // 128
        for slot, jt in ((0, jl - 1), (1, jl)):
            if jt < 0:
                continue
            j = 128 * jt + k
            cmask[qi, slot] = (16 * j + 31 <= 128 * qi + q)
    ident = np.eye(128, dtype=np.float32)
    return dict(cs_tok=cs_tok, cs_cmp=cs_cmp, wimp=wimp, texp=texp, causal=causal, strict=strict,
                cmask=cmask, ident=ident)


def build_nsa(n_tok=S, limit=None):
    return _standalone(emit_nsa, n_tok=n_tok, limit=limit)


def emit_nsa(ctx, n_tok=S, g2=None):
    nc, kb = ctx.nc, ctx.kb
    limit = kb.limit
    nqb = n_tok // 128
    ncb = n_tok // 16 - 1
    ncp = ((ncb + 127) // 128) * 128
    njt_all = ncp // 128
    din = ctx.din
    xT = din("xT", [D, n_tok]) if g2 is None else None
    wn = din("wn", [D, 652])
    cs_tok_d = din("cs_tok", [128, nqb * 16])
    cs_cmp_d = din("cs_cmp", [128, 64])
    wimp_d = din("wimp", [512, 128])
    texp_d = din("texp", [64, n_tok])
    causal_d = din("causal", [128, 128])
    strict_d = din("strict", [128, 128])
    cmask_d = din("cmask", [nqb, 2, 128, 128])
    ident_d = din("ident", [128, 128])
    wk1_d = din("wk1", [2048, 256])
    wk2_d = din("wk2", [256, 64])
    wv1_d = din("wv1", [2048, 256])
    wv2_d = din("wv2", [256, 64])
    peT_d = din("peT", [64, 32])
    y = ctx.dout("y", [n_tok, 256], BF16)

    with ExitStack() as st:
        V, A, G = nc.vector, nc.scalar, nc.gpsimd
        identb = sb(nc, st, "identb", [128, 128], BF16)
        QT = sb(nc, st, "QT", [64, 4, n_tok], BF16)
        KST = sb(nc, st, "KST", [128, n_tok], BF16)
        KWT = sb(nc, st, "KWT", [64, n_tok], BF16)
        VS = sb(nc, st, "VS", [128, nqb, 65], BF16)
        VW = sb(nc, st, "VW", [128, nqb, 65], BF16)
        G_all = sb(nc, st, "G_all", [128, nqb, 12], F32)
        KCMT = sb(nc, st, "KCMT", [64, ncp], BF16)
        RC = sb(nc, st, "RC", [128, njt_all, 193], BF16)
        st1 = ExitStack()
        w_n = sb(nc, st1, "w_n", [128, 8, 652], BF16)
        cs_tok = sb(nc, st1, "cs_tok_s", [128, nqb, 16], F32)
        cs_cmp = sb(nc, st1, "cs_cmp_s", [128, 4, 16], F32)
        KCT = sb(nc, st1, "KCT", [64, n_tok], BF16)
        VCT = sb(nc, st1, "VCT", [64, n_tok], BF16)
        xt = [sb(nc, st1, f"xt{i}", [128, 8, 128], BF16) for i in range(2)]
        pr = sb(nc, st1, "pr", [128, 652], F32)
        rp = sb(nc, st1, "rp", [128, 8, 64], BF16)
        ra = sb(nc, st1, "ra", [128, 6, 8], F32)
        rb = sb(nc, st1, "rb", [128, 6, 8], F32)
        w1 = sb(nc, st1, "w1", [64, 32, 256], BF16)
        w2 = sb(nc, st1, "w2", [128, 2, 64], BF16)
        peT = sb(nc, st1, "peT_s", [64, 32], BF16)
        hb = sb(nc, st1, "hb", [128, 2], F32)
        h1T = sb(nc, st1, "h1T", [128, 2, ncp], BF16)
        kc_f = sb(nc, st1, "kc_f", [128, 64], F32)
        kc_b = sb(nc, st1, "kc_b", [128, 64], BF16)

        pA = ps(nc, st, "pA", [128, 512])
        pB = ps(nc, st, "pB", [128, 512])
        pT = ps(nc, st, "pT", [128, 1024], BF16)
        pS = [ps(nc, st, f"pS{i}", [128, 512]) for i in range(3)]
        pC = pA
        pSL = ps(nc, st, "pSL", [128, 512])
        pW = ps(nc, st, "pW", [128, 512])

        kb.dma("pool", w_n[:], wn.rearrange("(kc p) n -> p kc n", p=128), writes=["w_n"])
        kb.dma("sp", cs_tok[:], cs_tok_d.rearrange("p (n c) -> p n c", c=16), writes=["cs_tok"])
        kb.dma("sp", cs_cmp[:], cs_cmp_d.rearrange("p (n c) -> p n c", c=16), writes=["cs_cmp"])
        kb.dma("pool", identb[:], ident_d, writes=["identb"])
        kb.dma("pool", peT[:], peT_d, writes=["peT"])
        kb.op("dve", lambda: V.memset(VS[:, :, 64:65], 1.0), writes=["VS"])
        kb.op("dve", lambda: V.memset(VW[:, :, 64:65], 1.0), writes=["VW"])
        kb.op("dve", lambda: V.memset(RC[:, :, 64:65], 1.0), writes=["RC"])
        kb.op("dve", lambda: V.memset(h1T[:], 0.0), writes=["h1T"])
        kb.dma("pool", RC[:, :, 65:193], wimp_d[0:ncp, :].rearrange("(n p) s -> p n s", p=128), writes=["RC"])
        kb.dma("pool", KST[64:128, :], texp_d, writes=["KSTa"])

        if g2 is None:
            xTv = xT.rearrange("(kc p) t -> p kc t", p=128)
        else:
            g2v = [gj.rearrange("(q k2 p) t -> q p k2 t", q=4, k2=2, p=128) for gj in g2]

        def rope(src3, dst3, cs_ap, nh, csk):
            cosb = cs_ap[:, 0:8].unsqueeze(1).to_broadcast([128, nh, 8])
            sinb = cs_ap[:, 8:16].unsqueeze(1).to_broadcast([128, nh, 8])
            a_, b_ = ra[:, 0:nh, :], rb[:, 0:nh, :]
            kb.op("pool", lambda: G.tensor_tensor(out=a_, in0=src3[:, :, 0:8], in1=cosb, op=ALU.mult),
                  reads=["rsrc", csk], writes=["ra"])
            kb.op("pool", lambda: G.tensor_tensor(out=b_, in0=src3[:, :, 8:16], in1=sinb, op=ALU.mult),
                  reads=["rsrc", csk], writes=["rb"])
            kb.op("pool", lambda: G.tensor_tensor(out=dst3[:, :, 0:8], in0=a_, in1=b_, op=ALU.subtract),
                  reads=["ra", "rb"], writes=["rdst"])
            kb.op("pool", lambda: G.tensor_tensor(out=a_, in0=src3[:, :, 8:16], in1=cosb, op=ALU.mult),
                  reads=["rsrc", csk, "rdst"], writes=["ra"])
            kb.op("pool", lambda: G.tensor_tensor(out=b_, in0=src3[:, :, 0:8], in1=sinb, op=ALU.mult),
                  reads=["rsrc", csk, "rdst"], writes=["rb"])
            kb.op("pool", lambda: G.tensor_tensor(out=dst3[:, :, 8:16], in0=a_, in1=b_, op=ALU.add),
                  reads=["ra", "rb"], writes=["rdst"])
            kb.op("pool", lambda: G.tensor_copy(dst3[:, :, 16:64], src3[:, :, 16:64]),
                  reads=["rsrc"], writes=["rdst"])

        for T in range(nqb):
            Ts = slice(T * 128, (T + 1) * 128)
            x_t = xt[T % 2]
            xk = f"xt{T % 2}"
            if g2 is None:
                kb.dma("pool", x_t[:], xTv[:, :, Ts], writes=[xk])
            else:
                qq, tl = T // 16, T % 16
                for j in range(4):
                    kb.dma("sp", x_t[:, 2 * j:2 * j + 2, :], g2v[j][qq][:, :, tl * 128:(tl + 1) * 128], writes=[xk])
            for kc in range(8):
                kb.mm(pA[:], x_t[:, kc, :], w_n[:, kc, 0:512], kc == 0, kc == 7, reads=[xk, "w_n"], writes=["pA"])
            for kc in range(8):
                kb.mm(pB[:, 0:140], x_t[:, kc, :], w_n[:, kc, 512:652], kc == 0, kc == 7,
                      reads=[xk, "w_n"], writes=["pB"])
            kb.op("dve", lambda: V.tensor_copy(pr[:, 0:512], pA[:]), reads=["pA", "rdst"], writes=["rsrc"])
            kb.op("dve", lambda: V.tensor_copy(pr[:, 512:640], pB[:, 0:128]), reads=["pB"], writes=["prv"])
            kb.act(G_all[:, T, :], pB[:, 128:140], AF.Sigmoid, reads=["pB"], writes=["G_all"])
            kb.op("pool", lambda: G.tensor_copy(VS[:, T, 0:64], pr[:, 512:576]), reads=["prv"], writes=["VS"])
            kb.op("pool", lambda: G.tensor_copy(VW[:, T, 0:64], pr[:, 576:640]), reads=["prv"], writes=["VW"])
            rope(pr[:, 0:384].rearrange("p (s d) -> p s d", s=6), rp[:, 0:6, :], cs_tok[:, T, :], 6, "cs_tok")
            kb.op("pool", lambda: G.tensor_copy(rp[:, 6:8, :], pr[:, 384:512].rearrange("p (s d) -> p s d", s=2)),
                  reads=["rsrc"], writes=["rdst"])
            for s_ in range(8):
                kb.op("pe", lambda: nc.tensor.transpose(pT[0:64, s_ * 128:(s_ + 1) * 128], rp[:, s_, :], identb[:]),
                      reads=["rdst", "identb"], writes=["pT"])
            kb.op("dve", lambda: V.tensor_copy(QT[:, :, Ts], pT[0:64, 0:512].rearrange("p (h t) -> p h t", h=4)),
                  reads=["pT"], writes=["QT"])
            kb.op("dve", lambda: V.tensor_copy(KST[0:64, Ts], pT[0:64, 512:640]), reads=["pT"], writes=["KST"])
            kb.op("dve", lambda: V.tensor_copy(KWT[:, Ts], pT[0:64, 640:768]), reads=["pT"], writes=["KWT"])
            kb.op("dve", lambda: V.tensor_copy(KCT[:, Ts], pT[0:64, 768:896]), reads=["pT"], writes=["KCT"])
            kb.op("dve", lambda: V.tensor_copy(VCT[:, Ts], pT[0:64, 896:1024]), reads=["pT"], writes=["VCT"])

        for which, (w1d, w2d, srcT, srck) in enumerate(((wk1_d, wk2_d, KCT, "KCT"), (wv1_d, wv2_d, VCT, "VCT"))):
            kb.dma("pool", w1[:], w1d.rearrange("(r d) h -> d r h", d=64), writes=["w1"])
            kb.dma("pool", w2[:], w2d.rearrange("(hc p) d -> p hc d", p=128), writes=["w2"])
            for hc in range(2):
                for r in range(32):
                    kb.mm(pS[0][:, 0:1], w1[:, r, hc * 128:(hc + 1) * 128], peT[:, r:r + 1], r == 0, r == 31,
                          reads=["w1", "peT"], writes=["pS0"])
                kb.op("dve", lambda: V.tensor_copy(hb[:, hc:hc + 1], pS[0][:, 0:1]), reads=["pS0"], writes=["hb"])
            for hc in range(2):
                for c0 in range(0, ncb, 512):
                    n_ = min(512, ncb - c0)
                    for r in range(32):
                        kb.mm(pS[1][:, 0:n_], w1[:, r, hc * 128:(hc + 1) * 128],
                              srcT[:, 16 * c0 + r:16 * c0 + r + 16 * (n_ - 1) + 1:16], r == 0, r == 31,
                              reads=["w1", srck], writes=["pS1"])
                    kb.act(h1T[:, hc, c0:c0 + n_], pS[1][:, 0:n_], AF.Silu, reads=["pS1", "hb"], writes=["h1T"],
                           bias=hb[:, hc:hc + 1])
            for jt in range(njt_all):
                for hc in range(2):
                    kb.mm(pS[2][:, 0:64], h1T[:, hc, jt * 128:(jt + 1) * 128], w2[:, hc, :], hc == 0, hc == 1,
                          reads=["h1T", "w2"], writes=["pS2"])
                if which == 0:
                    kb.op("dve", lambda: V.tensor_copy(kc_f[:], pS[2][:, 0:64]), reads=["pS2", "rdst"], writes=["rsrc"])
                    rope(kc_f[:].unsqueeze(1), kc_b[:].unsqueeze(1), cs_cmp[:, jt, :], 1, "cs_cmp")
                    kb.op("pe", lambda: nc.tensor.transpose(pT[0:64, 0:128], kc_b[:], identb[:]),
                          reads=["rdst", "identb"], writes=["pT"])
                    kb.op("dve", lambda: V.tensor_copy(KCMT[:, jt * 128:(jt + 1) * 128], pT[0:64, 0:128]),
                          reads=["pT"], writes=["KCMT"])
                else:
                    kb.op("dve", lambda: V.tensor_copy(RC[:, jt, 0:64], pS[2][:, 0:64]), reads=["pS2"], writes=["RC"])

        if limit is not None:
            print("n_inst after stage 2:", kb.n_inst)
        kb.barrier()
        st1.close()
        causal = sb(nc, st, "causal_s", [128, 128], BF16)
        strict = sb(nc, st, "strict_s", [128, 128], BF16)
        e_sb = [sb(nc, st, f"e_sb{i}", [128, 512], BF16) for i in range(3)]
        cm_sb = [sb(nc, st, f"cm_sb{i}", [128, 2, 128], BF16) for i in range(2)]
        impm = sb(nc, st, "impm", [128, 128], F32)
        impw = sb(nc, st, "impw", [128, 128], F32)
        m8 = sb(nc, st, "m8", [128, 16], F32)
        selb = sb(nc, st, "selb", [128, 192], BF16)
        QaLo = [sb(nc, st, f"QaLo{i}", [128, 512], BF16) for i in range(2)]
        QaHi = [sb(nc, st, f"QaHi{i}", [128, 512], BF16) for i in range(2)]
        kb.op("dve", lambda: V.memset(selb[:], 0.0), writes=["selb"])
        zz = sb(nc, st, "zz", [128, 12], F32)
        coef = sb(nc, st, "coef", [128, 12], F32)
        o_accs = [sb(nc, st, f"o_acc{i}", [128, 256], F32) for i in range(2)]
        y_sb = [sb(nc, st, f"y_sb{i}", [128, 256], BF16) for i in range(2)]
        kb.dma("pool", causal[:], causal_d, writes=["causal"])
        kb.dma("pool", strict[:], strict_d, writes=["strict"])

        def exp_tile(bank, ei):
            kb.act(e_sb[ei][:], pS[bank][:], AF.Exp, reads=[f"pS{bank}"], writes=[f"e{ei}"], scale=NSA_SCALE)

        def mask_tile(ei, mask_ap, mkeys):
            e3 = e_sb[ei][:].rearrange("p (h q) -> p h q", h=4)
            kb.op("pool", lambda: G.tensor_tensor(out=e3, in0=e3, in1=mask_ap.unsqueeze(1).to_broadcast([128, 4, 128]),
                                                  op=ALU.mult),
                  reads=[f"e{ei}"] + mkeys, writes=[f"e{ei}"])

        rot = [0]

        def nxt():
            rot[0] += 1
            return rot[0] % 3

        PIPE = 2
        pipe = []

        def drain_one():
            pv0, post0 = pipe.pop(0)
            pv0()
            if post0 is not None:
                post0()

        def push_tile(qk, pv, post=None):
            qk()
            pipe.append((pv, post))
            while len(pipe) > PIPE:
                drain_one()

        def tile_a(qi, jt, jl, Qv, cmk):
            r_ = nxt()

            def qk():
                kb.mm(pS[r_][:], KCMT[:, jt * 128:(jt + 1) * 128], Qv, True, True, reads=["KCMT", "QT"], writes=[f"pS{r_}"])
                exp_tile(r_, r_)
                if jt >= jl - 1:
                    mask_tile(r_, cm_sb[qi % 2][:, 1 - (jl - jt), :], [cmk])

            def pv():
                for h in range(4):
                    bank = pA if h < 2 else pB
                    kb.mm(bank[:, (h % 2) * 193:(h % 2) * 193 + 193], e_sb[r_][:, h * 128:(h + 1) * 128], RC[:, jt, :],
                          jt == 0 and h % 2 == 0, jt == jl and h % 2 == 1, reads=[f"e{r_}", "RC"],
                          writes=["pA" if h < 2 else "pB"])
            return qk, pv

        def post_a1(qi):
            use_sel = qi >= 8
            Gq = G_all[:, qi, :].rearrange("p (h j) -> p h j", h=4)
            o_acc, oak = o_accs[qi % 2], f"o_acc{qi % 2}"

            def post():
                for h in range(4):
                    bank = pA if h < 2 else pB
                    c0 = (h % 2) * 193
                    kb.op("dve", lambda: V.tensor_scalar_max(out=zz[:, h:h + 1], in0=bank[:, c0 + 64:c0 + 65], scalar1=1e-30),
                          reads=["pA" if h < 2 else "pB"], writes=["zz"])
                kb.op("dve", lambda: V.reciprocal(zz[:, 0:4], zz[:, 0:4]), reads=["zz"], writes=["zz"])
                if use_sel:
                    for h in range(4):
                        bank = pA if h < 2 else pB
                        bk = "pA" if h < 2 else "pB"
                        c0 = (h % 2) * 193
                        if h == 0:
                            kb.op("dve", lambda: V.tensor_scalar(out=impm[:], in0=bank[:, c0 + 65:c0 + 193],
                                                                 scalar1=zz[:, 0:1], scalar2=None, op0=ALU.mult),
                                  reads=[bk, "zz"], writes=["impm"])
                        else:
                            kb.op("dve", lambda: V.scalar_tensor_tensor(out=impm[:], in0=bank[:, c0 + 65:c0 + 193],
                                                                        scalar=zz[:, h:h + 1], in1=impm[:],
                                                                        op0=ALU.mult, op1=ALU.add),
                                  reads=[bk, "zz", "impm"], writes=["impm"])
                    c2 = 2 * qi
                    kb.op("dve", lambda: V.memset(impm[:, 0:1], 3e30), reads=[], writes=["impm"])
                    kb.op("dve", lambda: V.memset(impm[:, c2:c2 + 1], 2e30), writes=["impm"])
                    kb.op("dve", lambda: V.memset(impm[0:64, c2 - 1:c2], 1e30), writes=["impm"])
                    kb.op("dve", lambda: V.memset(impm[0:64, c2 + 1:c2 + 2], -1e30), writes=["impm"])
                    kb.op("dve", lambda: V.memset(impm[64:128, c2 + 1:c2 + 2], 2.5e30), writes=["impm"])
                    if c2 + 2 < 128:
                        kb.op("dve", lambda: V.memset(impm[:, c2 + 2:128], -1e30), writes=["impm"])
                    kb.op("dve", lambda: V.max(out=m8[:, 0:8], in_=impm[:]), reads=["impm"], writes=["m8"])
                    kb.op("dve", lambda: V.match_replace(out=impw[:], in_to_replace=m8[:, 0:8], in_values=impm[:],
                                                         imm_value=-1e30), reads=["impm", "m8"], writes=["impw"])
                    kb.op("dve", lambda: V.max(out=m8[:, 8:16], in_=impw[:]), reads=["impw"], writes=["m8"])
                    kb.op("dve", lambda: V.tensor_scalar(out=selb[:, 64:192], in0=impm[:], scalar1=m8[:, 15:16], scalar2=MASK_NEG,
                                                         op0=ALU.is_lt, op1=ALU.mult),
                          reads=["impm", "m8"], writes=["selb"])
                kb.op("dve", lambda: V.tensor_tensor(out=coef[:, 0:4], in0=zz[:, 0:4], in1=Gq[:, :, 0], op=ALU.mult),
                      reads=["zz", "G_all"], writes=["coef"])
                for h in range(4):
                    bank = pA if h < 2 else pB
                    bk = "pA" if h < 2 else "pB"
                    c0 = (h % 2) * 193
                    kb.op("dve", lambda: V.tensor_scalar(out=o_acc[:, h * 64:(h + 1) * 64], in0=bank[:, c0:c0 + 64],
                                                         scalar1=coef[:, h:h + 1], scalar2=None, op0=ALU.mult),
                          reads=[bk, "coef"], writes=[oak])
            return post

        def post_a2(qi):
            b_ = qi % 2

            def post():
                kb.op("pe", lambda: nc.tensor.transpose(pT[:, 0:128], selb[:, 0:128], identb[:]),
                      reads=["selb", "identb"], writes=["pT"])
                if qi >= 32:
                    kb.op("pe", lambda: nc.tensor.transpose(pT[:, 128:256], selb[:, 64:192], identb[:]),
                          reads=["selb", "identb"], writes=["pT"])
                kb.op("dve", lambda: V.tensor_copy(QaLo[b_][64:128, :].rearrange("p (h q) -> p h q", h=4),
                                                   pT[64:128, 0:128].unsqueeze(1).to_broadcast([64, 4, 128])),
                      reads=["pT"], writes=[f"QaLoM{b_}"])
                if qi >= 32:
                    kb.op("dve", lambda: V.tensor_copy(QaHi[b_][64:128, :].rearrange("p (h q) -> p h q", h=4),
                                                       pT[64:128, 128:256].unsqueeze(1).to_broadcast([64, 4, 128])),
                          reads=["pT"], writes=[f"QaHiM{b_}"])
            return post

        def tile_c(qi, kt, Qv, use_sel):
            r_ = nxt()
            Ks = slice(kt * 128, (kt + 1) * 128)

            def qk():
                if use_sel:
                    b_ = qi % 2
                    if kt < 32:
                        kb.mm(pS[r_][:], KST[:, Ks], QaLo[b_][:], True, True,
                              reads=["KST", "KSTa", f"QaLoQ{b_}", f"QaLoM{b_}"], writes=[f"pS{r_}"])
                    else:
                        kb.mm(pS[r_][:], KST[:, Ks], QaHi[b_][:], True, True,
                              reads=["KST", "KSTa", f"QaHiQ{b_}", f"QaHiM{b_}"], writes=[f"pS{r_}"])
                else:
                    kb.mm(pS[r_][:], KST[0:64, Ks], Qv, True, True, reads=["KST", "QT"], writes=[f"pS{r_}"])
                exp_tile(r_, r_)
                if kt == qi:
                    mask_tile(r_, causal[:], ["causal"])

            def pv():
                for h in range(4):
                    kb.mm(pSL[:, h * 65:(h + 1) * 65], e_sb[r_][:, h * 128:(h + 1) * 128], VS[:, kt, :],
                          kt == 0 and h == 0, kt == qi and h == 3, reads=[f"e{r_}", "VS"], writes=["pSL"])
            return qk, pv

        def tile_d(qi, kt, k0, Qv):
            r_ = nxt()
            Ks = slice(kt * 128, (kt + 1) * 128)

            def qk():
                kb.mm(pS[r_][:], KWT[:, Ks], Qv, True, True, reads=["KWT", "QT"], writes=[f"pS{r_}"])
                exp_tile(r_, r_)
                if kt == qi:
                    mask_tile(r_, causal[:], ["causal"])
                elif kt == qi - 4:
                    mask_tile(r_, strict[:], ["strict"])

            def pv():
                for h in range(4):
                    kb.mm(pW[:, h * 65:(h + 1) * 65], e_sb[r_][:, h * 128:(h + 1) * 128], VW[:, kt, :],
                          kt == k0 and h == 0, kt == qi and h == 3, reads=[f"e{r_}", "VW"], writes=["pW"])
            return qk, pv

        def post_combine(qi, bi, final):
            Gq = G_all[:, qi, :].rearrange("p (h j) -> p h j", h=4)
            bank, bk = ((pSL, "pSL"), (pW, "pW"))[bi]
            Qs = slice(qi * 128, (qi + 1) * 128)
            o_acc, oak = o_accs[qi % 2], f"o_acc{qi % 2}"

            def post():
                b3 = bank[:, 0:260].rearrange("p (h c) -> p h c", h=4)
                kb.op("dve", lambda: V.reciprocal(zz[:, 4 + 4 * bi:8 + 4 * bi], b3[:, :, 64]), reads=[bk], writes=["zz"])
                kb.op("dve", lambda: V.tensor_tensor(out=coef[:, 4 + 4 * bi:8 + 4 * bi], in0=zz[:, 4 + 4 * bi:8 + 4 * bi],
                                                     in1=Gq[:, :, 1 + bi], op=ALU.mult),
                      reads=["zz", "G_all"], writes=["coef"])
                for h in range(4):
                    kb.op("dve", lambda: V.scalar_tensor_tensor(
                        out=o_acc[:, h * 64:(h + 1) * 64], in0=b3[:, h, 0:64],
                        scalar=coef[:, 4 + 4 * bi + h:5 + 4 * bi + h], in1=o_acc[:, h * 64:(h + 1) * 64],
                        op0=ALU.mult, op1=ALU.add), reads=[bk, "coef", oak], writes=[oak])
                if final:
                    yk = f"y_sb{qi % 2}"
                    kb.op("pool", lambda: G.tensor_copy(y_sb[qi % 2][:], o_acc[:]), reads=[oak], writes=[yk])
                    kb.dma("sp", y[Qs, :], y_sb[qi % 2][:], reads=[yk], writes=["y_out"], force=True)
            return post

        def emit_a(qi):
            Qv = QT[:, :, qi * 128:(qi + 1) * 128]
            jl = (8 * qi + 6) // 128
            cmk = f"cm{qi % 2}"
            kb.dma("pool", cm_sb[qi % 2][:], cmask_d[qi].rearrange("s j q -> j s q"), writes=[cmk])
            if qi >= 8:
                kb.op("pool", lambda: G.tensor_copy(QaLo[qi % 2][0:64, :].rearrange("p (h q) -> p h q", h=4), Qv),
                      reads=["QT"], writes=[f"QaLoQ{qi % 2}"])
                if qi >= 32:
                    kb.op("pool", lambda: G.tensor_copy(QaHi[qi % 2][0:64, :].rearrange("p (h q) -> p h q", h=4), Qv),
                          reads=["QT"], writes=[f"QaHiQ{qi % 2}"])
            for jt in range(jl + 1):
                qk, pv = tile_a(qi, jt, jl, Qv, cmk)
                push_tile(qk, pv, post_a1(qi) if jt == jl else None)

        def emit_d(qi, a2_here):
            Qv = QT[:, :, qi * 128:(qi + 1) * 128]
            k0 = max(0, qi - 4)
            for kt in range(k0, qi + 1):
                qk, pv = tile_d(qi, kt, k0, Qv)
                post = None
                if kt == qi:
                    post = post_combine(qi, 1, False)
                elif a2_here and kt == k0 + 2:
                    post = post_a2(qi)
                push_tile(qk, pv, post)

        def emit_c(qi, a2_next):
            Qv = QT[:, :, qi * 128:(qi + 1) * 128]
            for kt in range(qi + 1):
                qk, pv = tile_c(qi, kt, Qv, qi >= 8)
                post = None
                if kt == qi:
                    post = post_combine(qi, 0, True)
                elif a2_next and kt == min(qi - 1, 12):
                    post = post_a2(qi + 1)
                push_tile(qk, pv, post)

        emit_a(0)
        emit_d(0, False)
        for qi in range(nqb):
            nxt_sel = qi + 1 < nqb and qi + 1 >= 8
            if qi + 1 < nqb:
                emit_a(qi + 1)
            emit_c(qi, nxt_sel)
            if qi + 1 < nqb:
                emit_d(qi + 1, False)
        while pipe:
            drain_one()
        kb.barrier()


def nsa_inputs(x1, w_in, w_ck1, w_ck2, w_cv1, w_cv2, cmp_pe, n_tok=S):
    cst = nsa_consts(n_tok)
    maps = []
    for c in range(NCORES):
        b, g = c // 4, c % 4
        def col(base, width=64, mult=64):
            return w_in[:, base + g * mult: base + g * mult + width]
        q = w_in[:, g * 256:(g + 1) * 256]
        kc, vc, ks, vs, kw, vw = (col(1024), col(1280), col(1536), col(1792), col(2048), col(2304))
        gt = w_in[:, 2560 + g * 12:2560 + (g + 1) * 12]
        wn = np.ascontiguousarray(np.concatenate([q, ks, kw, kc, vc, vs, vw, gt], axis=1))
        m = {"xT": np.ascontiguousarray(x1[b].T[:, :n_tok]), "wn": wn,
             "wk1": w_ck1, "wk2": w_ck2, "wv1": w_cv1, "wv2": w_cv2,
             "peT": np.ascontiguousarray(cmp_pe.T)}
        m.update(cst)
        maps.append(m)
    return maps


GROUPS = [[0, 1, 2, 3], [4, 5, 6, 7]]


def build_fused():
    nc = bass.Bass("TRN2", target_bir_lowering=False)
    with ExitStack() as st0:
        kb = KB(nc, st0)
        cc_sem = st0.enter_context(nc.semaphore("cc_sem"))
        n_cc = [0]
        internal = lambda name, shape, dt: nc.dram_tensor(name, list(shape), dt).ap()
        y0_d = internal("y0_d", [S, 256], BF16)
        g1 = [internal(f"g1_{j}", [4 * NTB, 256], BF16) for j in range(4)]
        x1_d = internal("x1_d", [NTB, D], F32)
        x1T_d = internal("x1T_d", [D, NTB], BF16)
        g2 = [internal(f"g2_{j}", [4 * 256, NTB], BF16) for j in range(4)]
        y1_d = internal("y1_d", [S, 256], BF16)
        g3 = [internal(f"g3_{j}", [4 * NTB, 256], BF16) for j in range(4)]
        qsel = nc.dram_tensor("qsel", [128, 4], F32, kind="ExternalInput").ap()

        def all_gather(srcs, dsts):
            kb.barrier()
            for s_, d_ in zip(srcs, dsts):
                n_cc[0] += 1
                nc.gpsimd.collective_compute("AllGather", ALU.bypass, replica_groups=GROUPS,
                                             ins=[s_], outs=[d_]).then_inc(cc_sem, 1)
            for e in kb.engs.values():
                e.wait_ge(cc_sem, n_cc[0])

        emit_gla(Ctx(nc, kb, "a0_", {"y": y0_d}))
        all_gather([y0_d[j * NTB:(j + 1) * NTB, :] for j in range(4)], g1)
        emit_ffn(Ctx(nc, kb, "b0_"), fused={"g": g1, "qsel": qsel, "x1_d": x1_d, "x1T_d": x1T_d})
        all_gather([x1T_d[j * 256:(j + 1) * 256, :] for j in range(4)], g2)
        emit_nsa(Ctx(nc, kb, "a1_", {"y": y1_d}), g2=g2)
        all_gather([y1_d[j * NTB:(j + 1) * NTB, :] for j in range(4)], g3)
        emit_ffn(Ctx(nc, kb, "b1_", {"xres": x1_d}), fused={"g": g3, "qsel": qsel})
        kb.barrier()
    return nc


_PROGS = {}


def _prog(name, fn):
    if name not in _PROGS:
        _PROGS[name] = fn()
    return _PROGS[name]


def _run(nc, maps):
    res = run_bass_kernel_spmd(nc, maps, core_ids=list(range(NCORES)))
    return res.results


def _gather_heads(results, key="y"):
    first = np.asarray(results[0][key])
    full = np.empty((B * S, D), dtype=first.dtype)
    for c in range(NCORES):
        b, h = c // 4, c % 4
        full[b * S:(b + 1) * S, h * 256:(h + 1) * 256] = np.asarray(results[c][key])
    return full


def _pref(prefix, m, drop=()):
    return {prefix + k: v for k, v in m.items() if k not in drop}


def fused_inputs(x, gla_w_in, gla_w_gate_up, gla_b_gate, gla_norm_g, gla_w_out,
                 nsa_w_in, nsa_w_cmp_k1, nsa_w_cmp_k2, nsa_w_cmp_v1, nsa_w_cmp_v2, nsa_cmp_pe, nsa_w_out,
                 moe_w_router, moe_b_router, moe_w_gate, moe_w_up, moe_w_down, ln_g, ln_b):
    a0 = gla_inputs(x, gla_w_in[0], gla_w_gate_up[0], gla_b_gate[0], gla_norm_g[0])
    dummy_y = np.zeros((B * S, 1), np.float32)
    b0 = ffn_inputs(dummy_y, x.reshape(B * S, D), gla_w_out[0], ln_g[0], ln_b[0], moe_w_router, moe_b_router,
                    moe_w_gate[0], moe_w_up[0], moe_w_down[0])
    a1 = nsa_inputs(np.zeros((B, 1, S), np.float32), nsa_w_in[0], nsa_w_cmp_k1[0], nsa_w_cmp_k2[0],
                    nsa_w_cmp_v1[0], nsa_w_cmp_v2[0], nsa_cmp_pe[0])
    b1 = ffn_inputs(dummy_y, np.zeros((B * S, 1), np.float32), nsa_w_out[0], ln_g[1], ln_b[1], moe_w_router,
                    moe_b_router, moe_w_gate[1], moe_w_up[1], moe_w_down[1])
    maps = []
    for c in range(NCORES):
        m = {}
        m.update(_pref("a0_", a0[c]))
        m.update(_pref("b0_", b0[c], drop=("yT",)))
        m.update(_pref("a1_", a1[c], drop=("xT",)))
        m.update(_pref("b1_", b1[c], drop=("yT", "xres")))
        qs = np.zeros((128, 4), np.float32)
        qs[:, c % 4] = 1.0
        m["qsel"] = qs
        maps.append(m)
    return maps


def kernel(x, gla_w_in, gla_w_gate_up, gla_b_gate, gla_norm_g, gla_w_out,
           nsa_w_in, nsa_w_cmp_k1, nsa_w_cmp_k2, nsa_w_cmp_v1, nsa_w_cmp_v2, nsa_cmp_pe, nsa_w_out,
           moe_w_router, moe_b_router, moe_w_gate, moe_w_up, moe_w_down, ln_g, ln_b):
    f = lambda a: np.ascontiguousarray(np.asarray(a, dtype=np.float32))
    maps = fused_inputs(f(x), f(gla_w_in), f(gla_w_gate_up), f(gla_b_gate), f(gla_norm_g), f(gla_w_out),
                        f(nsa_w_in), f(nsa_w_cmp_k1), f(nsa_w_cmp_k2), f(nsa_w_cmp_v1), f(nsa_w_cmp_v2),
                        f(nsa_cmp_pe), f(nsa_w_out), f(moe_w_router), f(moe_b_router), f(moe_w_gate),
                        f(moe_w_up), f(moe_w_down), f(ln_g), f(ln_b))
    r = _run(_prog("fused", build_fused), maps)
    out = np.concatenate([np.asarray(r[c]["b1_out"]) for c in range(NCORES)], axis=0)
    return out.reshape(B, S, D).astype(np.float32)
```

```python
from contextlib import ExitStack

import numpy as np
import concourse.bass as bass
import concourse.mybir as mybir
from concourse.bass_utils import run_bass_kernel_spmd

F32 = mybir.dt.float32
BF16 = mybir.dt.bfloat16
AF = mybir.ActivationFunctionType
ALU = mybir.AluOpType
AX = mybir.AxisListType

D = 1024
B = 2
S = 8192
NCORES = 8
DN_ALPHA = 4.0 ** 0.25
LN_EPS = 1e-5


class KB:
    NDMA = 12

    def __init__(self, nc, stack):
        self.nc = nc
        self.stack = stack
        self.engs = {"pe": nc.tensor, "act": nc.scalar, "dve": nc.vector,
                     "pool": nc.gpsimd, "sp": nc.sync}
        self.sems = {}
        self.cnt = {}
        for e in ["pe", "act", "dve", "pool"]:
            self.sems[e] = stack.enter_context(nc.semaphore("c_" + e))
            self.cnt[e] = 0
        self.dq = {}
        for q in ["sp", "act", "pool"]:
            for i in range(self.NDMA):
                nm = f"d_{q}{i}"
                self.sems[nm] = stack.enter_context(nc.semaphore(nm))
                self.cnt[nm] = 0
            self.dq[q] = 0
        self.waited = {e: {} for e in self.engs}
        self.last_w = {}
        self.readers = {}
        self.n_inst = 0
        self.limit = None

    def _need(self, eng, dep):
        sem, val = dep
        if eng == "pe" and sem == "pe":
            return
        if self.waited[eng].get(sem, 0) >= val:
            return
        self.engs[eng].wait_ge(self.sems[sem], val)
        self.waited[eng][sem] = val

    def _deps(self, eng, reads, writes):
        for k in reads:
            d = self.last_w.get(k)
            if d is not None:
                self._need(eng, d)
            if k.startswith("p"):
                for d in self.readers.get(k, ()):
                    if d[0] != eng:
                        self._need(eng, d)
        for k in writes:
            d = self.last_w.get(k)
            if d is not None:
                self._need(eng, d)
            for d in self.readers.get(k, ()):
                self._need(eng, d)

    def _commit(self, tok, reads, writes):
        for k in reads:
            self.readers.setdefault(k, []).append(tok)
        for k in writes:
            self.last_w[k] = tok
            self.readers[k] = []

    def op(self, eng, fn, reads=(), writes=()):
        if self.limit is not None and self.n_inst >= self.limit:
            return None
        self._deps(eng, reads, writes)
        ins = fn()
        self.cnt[eng] += 1
        ins.then_inc(self.sems[eng], 1)
        self._commit((eng, self.cnt[eng]), reads, writes)
        self.n_inst += 1
        return ins

    def dma(self, q, out, in_, reads=(), writes=(), **kw):
        if self.limit is not None and self.n_inst >= self.limit and not kw.pop("force", False):
            return None
        kw.pop("force", None)
        i = self.dq[q] % self.NDMA
        self.dq[q] += 1
        nm = f"d_{q}{i}"
        if self.cnt[nm] > 0:
            self._need(q, (nm, self.cnt[nm]))
        self._deps(q, reads, writes)
        ins = self.engs[q].dma_start(out=out, in_=in_, **kw)
        self.cnt[nm] += 16
        ins.then_inc(self.sems[nm], 16)
        self._commit((nm, self.cnt[nm]), reads, writes)
        self.n_inst += 1
        return ins

    def finish(self, keys, eng="sp"):
        for k in keys:
            d = self.last_w.get(k)
            if d is not None:
                self._need(eng, d)

    def barrier(self):
        for e in self.engs:
            for c, v in self.cnt.items():
                if v > 0:
                    self._need(e, (c, v))

    def mm(self, out, lhsT, rhs, start, stop, reads, writes):
        nc = self.nc
        return self.op("pe", lambda: nc.tensor.matmul(out, lhsT, rhs, start=start, stop=stop),
                       reads=reads, writes=writes)

    def act(self, out, in_, func, reads, writes, **kw):
        nc = self.nc
        return self.op("act", lambda: nc.scalar.activation(out, in_, func, **kw),
                       reads=reads, writes=writes)


class Ctx:
    def __init__(self, nc, kb, prefix="", over=None):
        self.nc, self.kb, self.prefix, self.over = nc, kb, prefix, dict(over or {})

    def din(self, name, shape, dt=F32):
        if name in self.over:
            return self.over[name]
        return self.nc.dram_tensor(self.prefix + name, list(shape), dt, kind="ExternalInput").ap()

    def dout(self, name, shape, dt=F32):
        if name in self.over:
            return self.over[name]
        return self.nc.dram_tensor(self.prefix + name, list(shape), dt, kind="ExternalOutput").ap()


def _standalone(emit, **kw):
    nc = bass.Bass("TRN2", target_bir_lowering=False)
    with ExitStack() as st0:
        kb = KB(nc, st0)
        kb.limit = kw.pop("limit", None)
        emit(Ctx(nc, kb), **kw)
    return nc


_UID = [0]


def _uniq(name):
    _UID[0] += 1
    return f"{name}_{_UID[0]}"


def sb(nc, st, name, shape, dt):
    return st.enter_context(nc.sbuf_tensor(_uniq(name), list(shape), dt))


def ps(nc, st, name, shape, dt=F32):
    return st.enter_context(nc.psum_tensor(_uniq(name), list(shape), dt))


GLA_DK = 128
GLA_DV = 256
GC = 128
TT = 512


def gla_consts():
    j = np.arange(128)[:, None]
    i = np.arange(128)[None, :]
    tri_i = np.where(j <= i, -1.0 / 16.0, 0.0).astype(np.float32)
    tri_u = np.where(j > i, -1.0 / 16.0, 0.0).astype(np.float32)
    mask = np.where(j <= i, 1.0, 0.0).astype(np.float32)
    return tri_i, tri_u, mask


def build_gla(n_tok=S, limit=None):
    return _standalone(emit_gla, n_tok=n_tok, limit=limit)


def emit_gla(ctx, n_tok=S):
    nc, kb = ctx.nc, ctx.kb
    limit = kb.limit
    xT = ctx.din("xT", [D, n_tok])
    wqk = ctx.din("wqk", [D, 256])
    wkvr = ctx.din("wkvr", [D, 640])
    wg = ctx.din("wg", [D, 16])
    wgu = ctx.din("wgu", [33, 128])
    normg = ctx.din("normg", [1, 256])
    tri_i_d = ctx.din("tri_i", [128, 128])
    tri_u_d = ctx.din("tri_u", [128, 128])
    mask_d = ctx.din("maskT", [128, 128])
    y = ctx.dout("y", [n_tok, 256], BF16)

    with ExitStack() as st:
        V, A = nc.vector, nc.scalar
        w_qk = sb(nc, st, "w_qk", [128, 8, 256], BF16)
        w_kvr = sb(nc, st, "w_kvr", [128, 8, 640], BF16)
        w_g = sb(nc, st, "w_g", [128, 8, 16], BF16)
        w_gu = sb(nc, st, "w_gu", [33, 128], F32)
        ng = sb(nc, st, "ng", [128, 256], F32)
        tri_i = sb(nc, st, "tri_i_s", [128, 128], F32)
        tri_u = sb(nc, st, "tri_u_s", [128, 128], F32)
        maskT = sb(nc, st, "mask_s", [128, 128], F32)
        xt = [sb(nc, st, f"xt{i}", [128, 8, TT], BF16) for i in range(2)]
        g_aug = sb(nc, st, "g_aug", [33, TT], F32)
        e1 = sb(nc, st, "e1", [128, 128], F32)
        la = sb(nc, st, "la", [128, 128], F32)
        eb = sb(nc, st, "eb", [128, 128], F32)
        enb = sb(nc, st, "enb", [128, 128], F32)
        w2 = sb(nc, st, "w2", [128, 128], F32)
        qdT = sb(nc, st, "qdT", [128, 128], BF16)
        kdT = sb(nc, st, "kdT", [128, 128], BF16)
        kd2 = sb(nc, st, "kd2", [128, 128], BF16)
        v_bf = sb(nc, st, "v_bf", [128, 256], BF16)
        atm = sb(nc, st, "atm", [128, 128], BF16)
        S_f = sb(nc, st, "S_f", [128, 256], F32)
        S_b = sb(nc, st, "S_b", [128, 256], BF16)
        junk = sb(nc, st, "junk", [128, 256], F32)
        ss = sb(nc, st, "ss", [128, 1], F32)
        rstd = sb(nc, st, "rstd", [128, 1], F32)
        er = sb(nc, st, "er", [128, 256], F32)
        rs = sb(nc, st, "rs", [128, 256], F32)
        on = sb(nc, st, "on", [128, 256], F32)
        yt = [sb(nc, st, f"yt{i}", [128, 256], BF16) for i in range(2)]

        p_q = ps(nc, st, "p_q", [128, TT])
        p_k = ps(nc, st, "p_k", [128, TT])
        p_g_full = ps(nc, st, "p_g", [128, TT])
        p_g = p_g_full[0:16, :]
        p_m = ps(nc, st, "p_m", [128, 512])
        p_kv = ps(nc, st, "p_kv", [128, 512])
        p_ro = ps(nc, st, "p_ro", [128, 512])
        p_st_full = ps(nc, st, "p_st", [128, 512])
        p_st = p_st_full[:, 0:256]

        kb.dma("pool", w_qk[:], wqk.rearrange("(kc p) n -> p kc n", p=128), writes=["w_qk"])
        kb.dma("pool", w_kvr[:], wkvr.rearrange("(kc p) n -> p kc n", p=128), writes=["w_kvr"])
        kb.dma("pool", w_g[:], wg.rearrange("(kc p) n -> p kc n", p=128), writes=["w_g"])
        kb.dma("sp", w_gu[:], wgu, writes=["w_gu"])
        kb.dma("sp", ng[:], normg.partition_broadcast(128), writes=["ng"])
        kb.dma("sp", tri_i[:], tri_i_d, writes=["tri_i"])
        kb.dma("sp", tri_u[:], tri_u_d, writes=["tri_u"])
        kb.dma("sp", maskT[:], mask_d, writes=["maskT"])
        kb.op("dve", lambda: V.memset(S_f[:], 0.0), writes=["S_f"])
        kb.op("dve", lambda: V.memset(S_b[:], 0.0), writes=["S_b"])
        kb.op("dve", lambda: V.memset(g_aug[:], 1.0), writes=["g_aug"])
        eps_t = sb(nc, st, "eps_t", [128, 1], F32)
        kb.op("dve", lambda: V.memset(eps_t[:], LN_EPS), writes=["eps_t"])

        xTv = xT.rearrange("(kc p) t -> p kc t", p=128)
        n_tiles = n_tok // TT
        for T in range(n_tiles):
            x_t = xt[T % 2]
            xk = f"xt{T % 2}"
            kb.dma("pool", x_t[:], xTv[:, :, T * TT:(T + 1) * TT], writes=[xk])
            for kc in range(8):
                kb.mm(p_q[:], w_qk[:, kc, 0:128], x_t[:, kc, :], kc == 0, kc == 7,
                      reads=["w_qk", xk], writes=["p_q"])
            for kc in range(8):
                kb.mm(p_k[:], w_qk[:, kc, 128:256], x_t[:, kc, :], kc == 0, kc == 7,
                      reads=["w_qk", xk], writes=["p_k"])
            for kc in range(8):
                kb.mm(p_g, w_g[:, kc, :], x_t[:, kc, :], kc == 0, kc == 7,
                      reads=["w_g", xk], writes=["p_g"])
            kb.op("dve", lambda: V.tensor_copy(g_aug[0:16, :], p_g), reads=["p_g"], writes=["g_aug"])
            for c in range(TT // GC):
                cs = slice(c * GC, (c + 1) * GC)
                kb.mm(p_m[:, 0:128], g_aug[:, cs], w_gu[:], True, True,
                      reads=["g_aug", "w_gu"], writes=["p_m"])
                kb.act(e1[:], p_m[:, 0:128], AF.Exp, reads=["p_m"], writes=["e1"], scale=-1.0)
                kb.act(la[:], e1[:], AF.Ln, reads=["e1"], writes=["la"], bias=1.0)
                kb.mm(p_m[:, 128:256], la[:], tri_i[:], True, True, reads=["la", "tri_i"], writes=["p_m"])
                kb.mm(p_m[:, 256:384], tri_u[:], la[:], True, True, reads=["la", "tri_u"], writes=["p_m"])
                kb.act(eb[:], p_m[:, 128:256], AF.Exp, reads=["p_m"], writes=["eb"])
                kb.act(enb[:], p_m[:, 128:256], AF.Exp, reads=["p_m"], writes=["enb"], scale=-1.0)
                kb.act(w2[:], p_m[:, 256:384], AF.Exp, reads=["p_m"], writes=["w2"])
                kb.op("dve", lambda: V.scalar_tensor_tensor(
                    out=qdT[:], in0=p_q[:, cs], scalar=float(GLA_DK ** -0.5), in1=eb[:],
                    op0=ALU.mult, op1=ALU.mult), reads=["p_q", "eb"], writes=["qdT"])
                kb.op("dve", lambda: V.tensor_tensor(out=kdT[:], in0=p_k[:, cs], in1=enb[:], op=ALU.mult),
                      reads=["p_k", "enb"], writes=["kdT"])
                for kc in range(8):
                    kb.mm(p_kv[:, 0:384], x_t[:, kc, cs], w_kvr[:, kc, 0:384], kc == 0, kc == 7,
                          reads=[xk, "w_kvr"], writes=["p_kv"])
                for kc in range(8):
                    kb.mm(p_ro[:, 0:256], x_t[:, kc, cs], w_kvr[:, kc, 384:640], kc == 0, kc == 7,
                          reads=[xk, "w_kvr"], writes=["p_ro"])
                kb.op("dve", lambda: V.tensor_tensor(out=kd2[:], in0=p_kv[:, 0:128], in1=w2[:], op=ALU.mult),
                      reads=["p_kv", "w2"], writes=["kd2"])
                kb.op("dve", lambda: V.tensor_copy(v_bf[:], p_kv[:, 128:384]), reads=["p_kv"], writes=["v_bf"])
                kb.mm(p_m[:, 384:512], kdT[:], qdT[:], True, True, reads=["kdT", "qdT"], writes=["p_m"])
                kb.op("dve", lambda: V.tensor_tensor(out=atm[:], in0=p_m[:, 384:512], in1=maskT[:], op=ALU.mult),
                      reads=["p_m", "maskT"], writes=["atm"])
                kb.mm(p_ro[:, 256:512], atm[:], v_bf[:], True, False, reads=["atm", "v_bf"], writes=["p_ro"])
                kb.mm(p_ro[:, 256:512], qdT[:], S_b[:], False, True, reads=["qdT", "S_b"], writes=["p_ro"])
                kb.mm(p_st, kd2[:], v_bf[:], True, True, reads=["kd2", "v_bf"], writes=["p_st"])
                kb.op("dve", lambda: V.scalar_tensor_tensor(
                    out=S_f[:], in0=S_f[:], scalar=eb[:, 127:128], in1=p_st,
                    op0=ALU.mult, op1=ALU.add), reads=["S_f", "eb", "p_st"], writes=["S_f"])
                kb.op("pool", lambda: nc.gpsimd.tensor_copy(S_b[:], S_f[:]), reads=["S_f"], writes=["S_b"])
                kb.act(junk[:], p_ro[:, 256:512], AF.Square, reads=["p_ro"], writes=["junk", "ss"],
                       scale=1.0 / 16.0, accum_out=ss[:])
                kb.act(rstd[:], ss[:], AF.Ln, reads=["ss"], writes=["rstd"], bias=eps_t[:])
                kb.act(rstd[:], rstd[:], AF.Exp, reads=["rstd"], writes=["rstd"], scale=-0.5)
                kb.act(er[:], p_ro[:, 0:256], AF.Exp, reads=["p_ro"], writes=["er"], scale=-1.0)
                kb.op("dve", lambda: V.tensor_scalar_add(out=er[:], in0=er[:], scalar1=1.0),
                      reads=["er"], writes=["er"])
                kb.op("dve", lambda: V.reciprocal(out=er[:], in_=er[:]), reads=["er"], writes=["er"])
                kb.op("dve", lambda: V.tensor_tensor(out=rs[:], in0=p_ro[:, 0:256], in1=er[:], op=ALU.mult),
                      reads=["p_ro", "er"], writes=["rs"])
                kb.op("dve", lambda: V.scalar_tensor_tensor(
                    out=on[:], in0=p_ro[:, 256:512], scalar=rstd[:, 0:1], in1=ng[:],
                    op0=ALU.mult, op1=ALU.mult), reads=["p_ro", "rstd", "ng"], writes=["on"])
                ci = T * (TT // GC) + c
                y_t = yt[ci % 2]
                yk = f"yt{ci % 2}"
                kb.op("pool", lambda: nc.gpsimd.tensor_tensor(out=y_t[:], in0=on[:], in1=rs[:], op=ALU.mult),
                      reads=["on", "rs"], writes=[yk])
                kb.dma("sp", y[ci * GC:(ci + 1) * GC, :], y_t[:], reads=[yk], writes=["y_out"])
        if limit is not None:
            kb.dma("sp", y[0:128, :], yt[0][:], reads=["yt0"], writes=["y_out"], force=True)
            print("n_inst", kb.n_inst)
        kb.barrier()


def gla_inputs(x, w_in, w_gate_up, b_gate, norm_g):
    tri_i, tri_u, mask = gla_consts()
    maps = []
    for c in range(NCORES):
        b, h = c // 4, c % 4
        q = w_in[:, h * 128:(h + 1) * 128]
        k = w_in[:, 512 + h * 128:512 + (h + 1) * 128]
        v = w_in[:, 1024 + h * 256:1024 + (h + 1) * 256]
        g = w_in[:, 2048:2064]
        r = w_in[:, 2064 + h * 256:2064 + (h + 1) * 256]
        wgu = np.zeros((33, 128), np.float32)
        wgu[0:16] = w_gate_up[:, h * 128:(h + 1) * 128]
        wgu[32] = b_gate[h * 128:(h + 1) * 128]
        maps.append({
            "xT": np.ascontiguousarray(x[b].T),
            "wqk": np.ascontiguousarray(np.concatenate([q, k], axis=1)),
            "wkvr": np.ascontiguousarray(np.concatenate([k, v, r], axis=1)),
            "wg": np.ascontiguousarray(g),
            "wgu": wgu,
            "normg": np.ascontiguousarray(norm_g.reshape(1, 256)),
            "tri_i": tri_i, "tri_u": tri_u, "maskT": mask,
        })
    return maps


NTB = 2048
NE = 16
DFF = 256


def moe_consts():
    sel = np.zeros((16, 16, 128), np.float32)
    for e in range(16):
        sel[e, e, :] = 1.0
    ident = np.eye(128, dtype=np.float32)
    return sel, ident


def build_ffn(n_tok=NTB, limit=None, n_exp=NE):
    return _standalone(emit_ffn, n_tok=n_tok, limit=limit, n_exp=n_exp)


def emit_ffn(ctx, n_tok=NTB, n_exp=NE, fused=None):
    nc, kb = ctx.nc, ctx.kb
    limit = kb.limit
    if fused is None:
        yT = ctx.din("yT", [D, n_tok], BF16)
    xres = ctx.din("xres", [n_tok, D])
    wout = ctx.din("wout", [D, D])
    lnp = ctx.din("lnp", [4, D])
    wr = ctx.din("wr", [D, NE])
    br = ctx.din("br", [1, NE])
    wgd = ctx.din("wg", [NE, D, DFF])
    wud = ctx.din("wu", [NE, D, DFF])
    wdd = ctx.din("wd", [NE, DFF, D])
    sel_d = ctx.din("sel", [16, 16, 128])
    ident_d = ctx.din("ident", [128, 128])
    out = ctx.dout("out", [n_tok, D]) if (fused is None or "x1_d" not in fused) else None
    n_sub = n_tok // 128
    n_tile = n_tok // 512

    with ExitStack() as st:
        V, A, G = nc.vector, nc.scalar, nc.gpsimd
        w_o = sb(nc, st, "w_o", [128, 8, D], BF16)
        lng = [sb(nc, st, f"lnp{i}", [128, D], F32) for i in range(4)]
        w_r = sb(nc, st, "w_r", [128, 8, NE], F32)
        b_r = sb(nc, st, "b_r", [128, NE], F32)
        sel = sb(nc, st, "sel_s", [16, 16, 128], BF16)
        ident = sb(nc, st, "ident_s", [128, 128], F32)
        eps_t = sb(nc, st, "eps_t", [128, 1], F32)
        acc = sb(nc, st, "acc", [128, n_sub, D], F32)
        x1T = sb(nc, st, "x1T", [128, 8, n_tok], BF16)
        gT = sb(nc, st, "gT", [16, n_tok], BF16)
        y_t = [sb(nc, st, "y_t0", [128, 8, 128], BF16)] * 2
        xr = [sb(nc, st, "xr0", [128, D], F32)] * 2
        x1 = sb(nc, st, "x1", [128, D], F32)
        stats = sb(nc, st, "stats", [128, 2, 6], F32)
        mv = sb(nc, st, "mv", [128, 2], F32)
        rstd = sb(nc, st, "rstd", [128, 1], F32)
        xTf = sb(nc, st, "xTf", [128, 8, 128], F32)
        sg_ = sb(nc, st, "r_s", [128, 16], F32)
        bi_ = sb(nc, st, "r_bi", [128, 16], F32)
        b2_ = sb(nc, st, "r_b2", [128, 16], F32)
        eq_ = sb(nc, st, "r_eq", [128, 16], F32)
        m1_ = sb(nc, st, "r_m1", [128, 4], F32)
        m2_ = sb(nc, st, "r_m2", [128, 4], F32)
        gs_ = sb(nc, st, "r_gs", [128, 4], F32)
        gm_ = sb(nc, st, "r_gm", [128, 1], F32)
        ig_ = sb(nc, st, "r_ig", [128, 4], F32)
        se_ = sb(nc, st, "r_se", [128, 16], F32)
        ws_ = sb(nc, st, "r_ws", [128, 1], F32)
        gate = sb(nc, st, "gate", [128, 16], F32)
        wg_s = [sb(nc, st, f"wg_s{i}", [128, 8, DFF], BF16) for i in range(2)]
        wu_s = [sb(nc, st, f"wu_s{i}", [128, 8, DFF], BF16) for i in range(2)]
        wd_s = [sb(nc, st, f"wd_s{i}", [128, 2, D], BF16) for i in range(2)]
        gb = [sb(nc, st, f"gb{i}", [128, 512], BF16) for i in range(2)]
        sgl = [sb(nc, st, f"sgl{i}", [128, 512], BF16) for i in range(2)]
        t1 = [sb(nc, st, f"t1{i}", [128, 512], BF16) for i in range(2)]
        hT = [sb(nc, st, f"hT{i}", [128, 2, 512], BF16) for i in range(2)]

        pb = [ps(nc, st, f"pb{i}", [128, 512]) for i in range(8)]
        PK = [f"pb{i}" for i in range(8)]

        kb.dma("pool", w_o[:], wout.rearrange("(kc p) n -> p kc n", p=128), writes=["w_o"])
        for i in range(4):
            kb.dma("sp", lng[i][:], lnp[i:i + 1, :].partition_broadcast(128), writes=[f"lnp{i}"])
        kb.dma("sp", w_r[:], wr.rearrange("(kc p) n -> p kc n", p=128), writes=["w_r"])
        kb.dma("sp", b_r[:], br.partition_broadcast(128), writes=["b_r"])
        kb.dma("pool", sel[:], sel_d, writes=["sel"])
        kb.dma("sp", ident[:], ident_d, writes=["ident"])
        kb.op("dve", lambda: V.memset(eps_t[:], LN_EPS), writes=["eps_t"])

        def load_expert(e):
            i = e % 2
            kb.dma("pool", wg_s[i][:], wgd[e].rearrange("(kc p) f -> p kc f", p=128), writes=[f"wg{i}"])
            kb.dma("pool", wu_s[i][:], wud[e].rearrange("(kc p) f -> p kc f", p=128), writes=[f"wu{i}"])
            kb.dma("pool", wd_s[i][:], wdd[e].rearrange("(fc p) d -> p fc d", p=128), writes=[f"wd{i}"])

        def layer_norm(src, dst, gi, eng2, fast=False):
            s_t, s_k = src
            d_t, d_k = dst
            for hh in range(2):
                kb.op("dve", lambda: V.bn_stats(stats[:, hh, :], s_t[:, hh * 512:(hh + 1) * 512]),
                      reads=[s_k], writes=["stats"])
            kb.op("dve", lambda: V.bn_aggr(mv[:], stats[:]), reads=["stats"], writes=["mv"])
            kb.act(rstd[:], mv[:, 1:2], AF.Sqrt, reads=["mv"], writes=["rstd"], bias=eps_t[:])
            kb.op("dve", lambda: V.reciprocal(rstd[:], rstd[:]), reads=["rstd"], writes=["rstd"])
            if fast:
                kb.op("dve", lambda: V.scalar_tensor_tensor(out=d_t, in0=s_t, scalar=mv[:, 0:1], in1=lng[gi][:],
                                                            op0=ALU.subtract, op1=ALU.mult),
                      reads=[s_k, "mv", f"lnp{gi}"], writes=[d_k])
                kb.op("dve", lambda: V.scalar_tensor_tensor(out=d_t, in0=d_t, scalar=rstd[:, 0:1], in1=lng[gi + 1][:],
                                                            op0=ALU.mult, op1=ALU.add),
                      reads=[d_k, "rstd", f"lnp{gi + 1}"], writes=[d_k])
                return
            kb.op("dve", lambda: V.tensor_scalar(out=d_t, in0=s_t, scalar1=mv[:, 0:1], scalar2=rstd[:, 0:1],
                                                 op0=ALU.subtract, op1=ALU.mult),
                  reads=[s_k, "mv", "rstd"], writes=[d_k])
            kb.op("pool", lambda: G.tensor_tensor(out=d_t, in0=d_t, in1=lng[gi][:], op=ALU.mult),
                  reads=[d_k, f"lnp{gi}"], writes=[d_k])
            kb.op("pool", lambda: G.tensor_tensor(out=d_t, in0=d_t, in1=lng[gi + 1][:], op=ALU.add),
                  reads=[d_k, f"lnp{gi + 1}"], writes=[d_k])

        load_expert(0)
        if fused is None:
            yTv = yT.rearrange("(kc p) t -> p kc t", p=128)
        else:
            g_v = [gq.rearrange("(h t) c -> t h c", h=4) for gq in fused["g"]]
            cand = [sb(nc, st, f"cand{i}", [128, 4, D], BF16) for i in range(2)]
            ysel = sb(nc, st, "ysel", [128, D], BF16)
            qsel = sb(nc, st, "qsel_s", [128, 4], F32)
            identb = sb(nc, st, "identb", [128, 128], BF16)
            xo = [sb(nc, st, "xo0", [128, 8, 128], BF16)] * 2
            kb.dma("sp", qsel[:], fused["qsel"], writes=["qsel"])
            kb.dma("pool", identb[:], ident_d, writes=["identb"])
        for sub in range(n_sub):
            ts_ = slice(sub * 128, (sub + 1) * 128)
            yk = "y_t0"
            xk = "xr0"
            if fused is None:
                kb.dma("sp", y_t[sub % 2][:], yTv[:, :, ts_], writes=[yk])
            else:
                ck = f"cand{sub % 2}_"
                for qq in range(4):
                    kb.dma("sp" if qq % 2 == 0 else "act", cand[sub % 2][:, qq, :].rearrange("p (h c) -> p h c", h=4),
                           g_v[qq][sub * 128:(sub + 1) * 128, :, :], writes=[ck + str(qq)])
                kb.op("pool", lambda: G.tensor_scalar(out=ysel[:], in0=cand[sub % 2][:, 0, :], scalar1=qsel[:, 0:1],
                                                      scalar2=None, op0=ALU.mult), reads=[ck + "0", "qsel"], writes=["ysel"])
                for qq in range(1, 4):
                    kb.op("dve", lambda: V.scalar_tensor_tensor(out=ysel[:], in0=cand[sub % 2][:, qq, :],
                                                                 scalar=qsel[:, qq:qq + 1], in1=ysel[:],
                                                                 op0=ALU.mult, op1=ALU.add),
                          reads=[ck + str(qq), "qsel", "ysel"], writes=["ysel"])
                pTb = pb[7][:].bitcast(BF16)
                for kc in range(8):
                    kb.op("pe", lambda: nc.tensor.transpose(pTb[:, kc * 128:(kc + 1) * 128],
                                                            ysel[:, kc * 128:(kc + 1) * 128], identb[:]),
                          reads=["ysel", "identb"], writes=[PK[7]])
                kb.op("dve", lambda: V.tensor_copy(y_t[sub % 2][:], pTb.rearrange("p (k t) -> p k t", k=8)),
                      reads=[PK[7]], writes=[yk])
            kb.dma("sp", xr[sub % 2][:], xres[ts_, :], writes=[xk])
            for hh in range(2):
                for kc in range(8):
                    kb.mm(pb[hh][:], y_t[sub % 2][:, kc, :], w_o[:, kc, hh * 512:(hh + 1) * 512],
                          kc == 0, kc == 7, reads=[yk, "w_o"], writes=[PK[hh]])
            for hh in range(2):
                kb.op("dve", lambda: V.scalar_tensor_tensor(
                    out=acc[:, sub, hh * 512:(hh + 1) * 512], in0=xr[sub % 2][:, hh * 512:(hh + 1) * 512],
                    scalar=float(DN_ALPHA), in1=pb[hh][:], op0=ALU.mult, op1=ALU.add),
                    reads=[xk, PK[hh]], writes=[f"acc{sub}"])
            layer_norm((acc[:, sub, :], f"acc{sub}"), (x1[:], "x1"), 0, None, fast=True)
            kb.op("pool", lambda: G.tensor_scalar(out=acc[:, sub, :], in0=x1[:], scalar1=float(DN_ALPHA),
                                                  scalar2=None, op0=ALU.mult),
                  reads=["x1"], writes=[f"acc{sub}"])
            for kc in range(8):
                bank = 2 + kc // 4
                kb.op("pe", lambda: nc.tensor.transpose(pb[bank][:, (kc % 4) * 128:(kc % 4 + 1) * 128],
                                                        x1[:, kc * 128:(kc + 1) * 128], ident[:]),
                      reads=["x1", "ident"], writes=[PK[bank]])
            for q in range(2):
                kb.op("dve" if q == 0 else "act",
                      (lambda: V.tensor_copy(xTf[:, 0:4, :], pb[2][:].rearrange("p (k t) -> p k t", k=4))) if q == 0
                      else (lambda: A.copy(xTf[:, 4:8, :], pb[3][:].rearrange("p (k t) -> p k t", k=4))),
                      reads=[PK[2 + q]], writes=[f"xTf{q}"])
            kb.op("pool", lambda: G.tensor_copy(x1T[:, :, ts_], xTf[:]), reads=["xTf0", "xTf1"], writes=["x1T"])
            for kc in range(8):
                kb.mm(pb[4][:, 0:16], xTf[:, kc, :], w_r[:, kc, :], kc == 0, kc == 7,
                      reads=["xTf0", "xTf1", "w_r"], writes=[PK[4]])
            kb.act(sg_[:], pb[4][:, 0:16], AF.Sigmoid, reads=[PK[4]], writes=["r_s"])
            kb.op("dve", lambda: V.tensor_tensor(out=bi_[:], in0=sg_[:], in1=b_r[:], op=ALU.add),
                  reads=["r_s", "b_r"], writes=["r_bi"])
            bi3 = bi_[:].rearrange("p (g e) -> p g e", g=4)
            kb.op("dve", lambda: V.tensor_reduce(out=m1_[:], in_=bi3, axis=AX.X, op=ALU.max),
                  reads=["r_bi"], writes=["r_m1"])
            kb.op("dve", lambda: V.tensor_tensor(out=eq_[:].rearrange("p (g e) -> p g e", g=4), in0=bi3,
                                                 in1=m1_[:].unsqueeze(2).to_broadcast([128, 4, 4]), op=ALU.is_equal),
                  reads=["r_bi", "r_m1"], writes=["r_eq"])
            kb.op("dve", lambda: V.scalar_tensor_tensor(out=b2_[:], in0=eq_[:], scalar=-1e30, in1=bi_[:],
                                                        op0=ALU.mult, op1=ALU.add),
                  reads=["r_eq", "r_bi"], writes=["r_b2"])
            kb.op("dve", lambda: V.tensor_reduce(out=m2_[:], in_=b2_[:].rearrange("p (g e) -> p g e", g=4),
                                                 axis=AX.X, op=ALU.max),
                  reads=["r_b2"], writes=["r_m2"])
            kb.op("dve", lambda: V.tensor_tensor(out=gs_[:], in0=m1_[:], in1=m2_[:], op=ALU.add),
                  reads=["r_m1", "r_m2"], writes=["r_gs"])
            kb.op("dve", lambda: V.tensor_reduce(out=gm_[:], in_=gs_[:], axis=AX.X, op=ALU.max),
                  reads=["r_gs"], writes=["r_gm"])
            kb.op("dve", lambda: V.tensor_scalar(out=ig_[:], in0=gs_[:], scalar1=gm_[:, 0:1], scalar2=None,
                                                 op0=ALU.is_ge),
                  reads=["r_gs", "r_gm"], writes=["r_ig"])
            kb.op("dve", lambda: V.tensor_tensor(out=se_[:].rearrange("p (g e) -> p g e", g=4), in0=bi3,
                                                 in1=m2_[:].unsqueeze(2).to_broadcast([128, 4, 4]), op=ALU.is_ge),
                  reads=["r_bi", "r_m2"], writes=["r_se"])
            kb.op("dve", lambda: V.tensor_tensor(out=se_[:].rearrange("p (g e) -> p g e", g=4),
                                                 in0=se_[:].rearrange("p (g e) -> p g e", g=4),
                                                 in1=ig_[:].unsqueeze(2).to_broadcast([128, 4, 4]), op=ALU.mult),
                  reads=["r_se", "r_ig"], writes=["r_se"])
            kb.op("dve", lambda: V.tensor_tensor(out=se_[:], in0=se_[:], in1=sg_[:], op=ALU.mult),
                  reads=["r_se", "r_s"], writes=["r_se"])
            kb.op("dve", lambda: V.tensor_reduce(out=ws_[:], in_=se_[:], axis=AX.X, op=ALU.add),
                  reads=["r_se"], writes=["r_ws"])
            kb.op("dve", lambda: V.reciprocal(ws_[:], ws_[:]), reads=["r_ws"], writes=["r_ws"])
            kb.op("dve", lambda: V.tensor_scalar(out=gate[:], in0=se_[:], scalar1=ws_[:, 0:1], scalar2=None,
                                                 op0=ALU.mult),
                  reads=["r_se", "r_ws"], writes=["gate"])
            kb.op("pe", lambda: nc.tensor.transpose(pb[5][0:16, 0:128], gate[:], ident[:]),
                  reads=["gate", "ident"], writes=[PK[5]])
            kb.op("dve", lambda: V.tensor_copy(gT[:, ts_], pb[5][0:16, 0:128]), reads=[PK[5]], writes=["gT"])

        pend = []

        def down_part(e, T, i, j):
            def f():
                for s4 in range(4):
                    sub = T * 4 + s4
                    for hh in range(2):
                        bank = 5 + (s4 * 2 + hh) % 3
                        for fc in range(2):
                            kb.mm(pb[bank][:], hT[j][:, fc, s4 * 128:(s4 + 1) * 128],
                                  wd_s[i][:, fc, hh * 512:(hh + 1) * 512], fc == 0, fc == 1,
                                  reads=[f"hT{j}", f"wd{i}"], writes=[PK[bank]])
                        kb.op("dve", lambda: V.tensor_tensor(out=acc[:, sub, hh * 512:(hh + 1) * 512],
                                                             in0=acc[:, sub, hh * 512:(hh + 1) * 512],
                                                             in1=pb[bank][:], op=ALU.add),
                              reads=[f"acc{sub}", PK[bank]], writes=[f"acc{sub}"])
            return f

        for e in range(n_exp):
            i = e % 2
            for T in range(n_tile):
                Ts = slice(T * 512, (T + 1) * 512)
                j = (e * n_tile + T) % 2
                kb.mm(pb[4][:], sel[:, e, :], gT[:, Ts], True, True, reads=["sel", "gT"], writes=[PK[4]])
                kb.op("act", lambda: A.copy(gb[j][:], pb[4][:]), reads=[PK[4]], writes=[f"gb{j}"])
                for fc in range(2):
                    for kc in range(8):
                        kb.mm(pb[fc][:], wg_s[i][:, kc, fc * 128:(fc + 1) * 128], x1T[:, kc, Ts],
                              kc == 0, kc == 7, reads=[f"wg{i}", "x1T"], writes=[PK[fc]])
                    for kc in range(8):
                        kb.mm(pb[2 + fc][:], wu_s[i][:, kc, fc * 128:(fc + 1) * 128], x1T[:, kc, Ts],
                              kc == 0, kc == 7, reads=[f"wu{i}", "x1T"], writes=[PK[2 + fc]])
                for fc in range(2):
                    kb.act(sgl[fc][:], pb[fc][:], AF.Silu, reads=[PK[fc]], writes=[f"sgl{fc}"])
                    kb.op("dve", lambda: V.tensor_tensor(out=t1[fc][:], in0=sgl[fc][:], in1=pb[2 + fc][:], op=ALU.mult),
                          reads=[f"sgl{fc}", PK[2 + fc]], writes=[f"t1{fc}"])
                    kb.op("pool", lambda: G.tensor_tensor(out=hT[j][:, fc, :], in0=t1[fc][:], in1=gb[j][:], op=ALU.mult),
                          reads=[f"t1{fc}", f"gb{j}"], writes=[f"hT{j}"])
                while pend:
                    pend.pop(0)()
                if T == 0 and e + 1 < n_exp:
                    load_expert(e + 1)
                pend.append(down_part(e, T, i, j))
        while pend:
            pend.pop(0)()

        for sub in range(n_sub):
            o_ap = acc[:, sub, :]
            ok_ = f"acc{sub}"
            layer_norm((o_ap, ok_), (o_ap, ok_), 2, None)
            if out is not None:
                kb.dma("sp", out[sub * 128:(sub + 1) * 128, :], o_ap, reads=[ok_], writes=["out"], force=True)
            else:
                kb.dma("sp", fused["x1_d"][sub * 128:(sub + 1) * 128, :], o_ap, reads=[ok_], writes=["x1_d"])
                for kc in range(8):
                    bank = 2 + kc // 4
                    kb.op("pe", lambda: nc.tensor.transpose(pb[bank][:, (kc % 4) * 128:(kc % 4 + 1) * 128],
                                                            acc[:, sub, kc * 128:(kc + 1) * 128], ident[:]),
                          reads=[ok_, "ident"], writes=[PK[bank]])
                xk_ = "xo0"
                kb.op("dve", lambda: V.tensor_copy(xo[sub % 2][:, 0:4, :], pb[2][:].rearrange("p (k t) -> p k t", k=4)),
                      reads=[PK[2]], writes=[xk_])
                kb.op("dve", lambda: V.tensor_copy(xo[sub % 2][:, 4:8, :], pb[3][:].rearrange("p (k t) -> p k t", k=4)),
                      reads=[PK[3]], writes=[xk_])
                kb.dma("sp", fused["x1T_d"].rearrange("(kc p) t -> p kc t", p=128)[:, :, sub * 128:(sub + 1) * 128],
                       xo[sub % 2][:], reads=[xk_], writes=["x1T_d"])
        kb.barrier()


def ffn_inputs(y_tok_major, xres, w_out, ln_g, ln_b, w_router, b_router, w_gate, w_up, w_down):
    sel, ident = moe_consts()
    lnp = np.ascontiguousarray(np.stack([ln_g[0], ln_b[0], ln_g[1], ln_b[1]]).astype(np.float32))
    maps = []
    for c in range(NCORES):
        rs_ = slice(c * NTB, (c + 1) * NTB)
        maps.append({
            "yT": np.ascontiguousarray(y_tok_major[rs_].T),
            "xres": np.ascontiguousarray(xres[rs_]),
            "wout": w_out, "lnp": lnp, "wr": w_router,
            "br": np.ascontiguousarray(b_router.reshape(1, NE)),
            "wg": w_gate, "wu": w_up, "wd": w_down, "sel": sel, "ident": ident,
        })
    return maps


NSA_SCALE = 0.125
MASK_NEG = -240000.0


def nsa_consts(n_tok=S):
    nqb = n_tok // 128
    inv = np.power(500000.0, -np.arange(8, dtype=np.float32) * (2.0 / 16.0)).astype(np.float32)
    def cs(pos):
        ang = pos.astype(np.float32)[:, None] * inv[None, :]
        return np.concatenate([np.cos(ang), np.sin(ang)], axis=1).astype(np.float32)
    def pl(a):
        n = a.shape[0] // 128
        return np.ascontiguousarray(a.reshape(n, 128, 16).transpose(1, 0, 2).reshape(128, n * 16))
    cs_tok = pl(cs(np.arange(n_tok)))
    cs_cmp = pl(cs(np.arange(512) * 16 + 31))
    wimp = np.zeros((512, 128), np.float32)
    for s_ in range(128):
        for o, wgt in enumerate([1, 2, 2, 2, 1]):
            c = 4 * s_ + o
            if c < 511:
                wimp[c, s_] = wgt
    texp = ((np.arange(n_tok)[None, :] // 64) % 64 == np.arange(64)[:, None]).astype(np.float32)
    k = np.arange(128)[:, None]
    q = np.arange(128)[None, :]
    causal = (k <= q).astype(np.float32)
    strict = (k > q).astype(np.float32)
    cmask = np.zeros((nqb, 2, 128, 128), np.float32)
    for qi in range(nqb):
        jl = (8 * qi + 6) // 128
        for slot, jt in ((0, jl - 1), (1, jl)):
            if jt < 0:
                continue
            j = 128 * jt + k
            cmask[qi, slot] = (16 * j + 31 <= 128 * qi + q)
    ident = np.eye(128, dtype=np.float32)
    return dict(cs_tok=cs_tok, cs_cmp=cs_cmp, wimp=wimp, texp=texp, causal=causal, strict=strict,
                cmask=cmask, ident=ident)


def build_nsa(n_tok=S, limit=None):
    return _standalone(emit_nsa, n_tok=n_tok, limit=limit)


def emit_nsa(ctx, n_tok=S, g2=None):
    nc, kb = ctx.nc, ctx.kb
    limit = kb.limit
    nqb = n_tok // 128
    ncb = n_tok // 16 - 1
    ncp = ((ncb + 127) // 128) * 128
    njt_all = ncp // 128
    din = ctx.din
    xT = din("xT", [D, n_tok]) if g2 is None else None
    wn = din("wn", [D, 652])
    cs_tok_d = din("cs_tok", [128, nqb * 16])
    cs_cmp_d = din("cs_cmp", [128, 64])
    wimp_d = din("wimp", [512, 128])
    texp_d = din("texp", [64, n_tok])
    causal_d = din("causal", [128, 128])
    strict_d = din("strict", [128, 128])
    cmask_d = din("cmask", [nqb, 2, 128, 128])
    ident_d = din("ident", [128, 128])
    wk1_d = din("wk1", [2048, 256])
    wk2_d = din("wk2", [256, 64])
    wv1_d = din("wv1", [2048, 256])
    wv2_d = din("wv2", [256, 64])
    peT_d = din("peT", [64, 32])
    y = ctx.dout("y", [n_tok, 256], BF16)

    with ExitStack() as st:
        V, A, G = nc.vector, nc.scalar, nc.gpsimd
        identb = sb(nc, st, "identb", [128, 128], BF16)
        QT = sb(nc, st, "QT", [64, 4, n_tok], BF16)
        KST = sb(nc, st, "KST", [128, n_tok], BF16)
        KWT = sb(nc, st, "KWT", [64, n_tok], BF16)
        VS = sb(nc, st, "VS", [128, nqb, 65], BF16)
        VW = sb(nc, st, "VW", [128, nqb, 65], BF16)
        G_all = sb(nc, st, "G_all", [128, nqb, 12], F32)
        KCMT = sb(nc, st, "KCMT", [64, ncp], BF16)
        RC = sb(nc, st, "RC", [128, njt_all, 193], BF16)
        st1 = ExitStack()
        w_n = sb(nc, st1, "w_n", [128, 8, 652], BF16)
        cs_tok = sb(nc, st1, "cs_tok_s", [128, nqb, 16], F32)
        cs_cmp = sb(nc, st1, "cs_cmp_s", [128, 4, 16], F32)
        KCT = sb(nc, st1, "KCT", [64, n_tok], BF16)
        VCT = sb(nc, st1, "VCT", [64, n_tok], BF16)
        xt = [sb(nc, st1, f"xt{i}", [128, 8, 128], BF16) for i in range(2)]
        pr = sb(nc, st1, "pr", [128, 652], F32)
        rp = sb(nc, st1, "rp", [128, 8, 64], BF16)
        ra = sb(nc, st1, "ra", [128, 6, 8], F32)
        rb = sb(nc, st1, "rb", [128, 6, 8], F32)
        w1 = sb(nc, st1, "w1", [64, 32, 256], BF16)
        w2 = sb(nc, st1, "w2", [128, 2, 64], BF16)
        peT = sb(nc, st1, "peT_s", [64, 32], BF16)
        hb = sb(nc, st1, "hb", [128, 2], F32)
        h1T = sb(nc, st1, "h1T", [128, 2, ncp], BF16)
        kc_f = sb(nc, st1, "kc_f", [128, 64], F32)
        kc_b = sb(nc, st1, "kc_b", [128, 64], BF16)

        pA = ps(nc, st, "pA", [128, 512])
        pB = ps(nc, st, "pB", [128, 512])
        pT = ps(nc, st, "pT", [128, 1024], BF16)
        pS = [ps(nc, st, f"pS{i}", [128, 512]) for i in range(3)]
        pC = pA
        pSL = ps(nc, st, "pSL", [128, 512])
        pW = ps(nc, st, "pW", [128, 512])

        kb.dma("pool", w_n[:], wn.rearrange("(kc p) n -> p kc n", p=128), writes=["w_n"])
        kb.dma("sp", cs_tok[:], cs_tok_d.rearrange("p (n c) -> p n c", c=16), writes=["cs_tok"])
        kb.dma("sp", cs_cmp[:], cs_cmp_d.rearrange("p (n c) -> p n c", c=16), writes=["cs_cmp"])
        kb.dma("pool", identb[:], ident_d, writes=["identb"])
        kb.dma("pool", peT[:], peT_d, writes=["peT"])
        kb.op("dve", lambda: V.memset(VS[:, :, 64:65], 1.0), writes=["VS"])
        kb.op("dve", lambda: V.memset(VW[:, :, 64:65], 1.0), writes=["VW"])
        kb.op("dve", lambda: V.memset(RC[:, :, 64:65], 1.0), writes=["RC"])
        kb.op("dve", lambda: V.memset(h1T[:], 0.0), writes=["h1T"])
        kb.dma("pool", RC[:, :, 65:193], wimp_d[0:ncp, :].rearrange("(n p) s -> p n s", p=128), writes=["RC"])
        kb.dma("pool", KST[64:128, :], texp_d, writes=["KSTa"])

        if g2 is None:
            xTv = xT.rearrange("(kc p) t -> p kc t", p=128)
        else:
            g2v = [gj.rearrange("(q k2 p) t -> q p k2 t", q=4, k2=2, p=128) for gj in g2]

        def rope(src3, dst3, cs_ap, nh, csk):
            cosb = cs_ap[:, 0:8].unsqueeze(1).to_broadcast([128, nh, 8])
            sinb = cs_ap[:, 8:16].unsqueeze(1).to_broadcast([128, nh, 8])
            a_, b_ = ra[:, 0:nh, :], rb[:, 0:nh, :]
            kb.op("pool", lambda: G.tensor_tensor(out=a_, in0=src3[:, :, 0:8], in1=cosb, op=ALU.mult),
                  reads=["rsrc", csk], writes=["ra"])
            kb.op("pool", lambda: G.tensor_tensor(out=b_, in0=src3[:, :, 8:16], in1=sinb, op=ALU.mult),
                  reads=["rsrc", csk], writes=["rb"])
            kb.op("pool", lambda: G.tensor_tensor(out=dst3[:, :, 0:8], in0=a_, in1=b_, op=ALU.subtract),
                  reads=["ra", "rb"], writes=["rdst"])
            kb.op("pool", lambda: G.tensor_tensor(out=a_, in0=src3[:, :, 8:16], in1=cosb, op=ALU.mult),
                  reads=["rsrc", csk, "rdst"], writes=["ra"])
            kb.op("pool", lambda: G.tensor_tensor(out=b_, in0=src3[:, :, 0:8], in1=sinb, op=ALU.mult),
                  reads=["rsrc", csk, "rdst"], writes=["rb"])
            kb.op("pool", lambda: G.tensor_tensor(out=dst3[:, :, 8:16], in0=a_, in1=b_, op=ALU.add),
                  reads=["ra", "rb"], writes=["rdst"])
            kb.op("act", lambda: A.copy(dst3[:, :, 16:64], src3[:, :, 16:64]),
                  reads=["rsrc"], writes=["rdstc"])

        for T in range(nqb):
            Ts = slice(T * 128, (T + 1) * 128)
            x_t = xt[T % 2]
            xk = f"xt{T % 2}"
            if g2 is None:
                kb.dma("pool", x_t[:], xTv[:, :, Ts], writes=[xk])
            else:
                qq, tl = T // 16, T % 16
                for j in range(4):
                    kb.dma("sp", x_t[:, 2 * j:2 * j + 2, :], g2v[j][qq][:, :, tl * 128:(tl + 1) * 128], writes=[xk])
            for kc in range(8):
                kb.mm(pA[:], x_t[:, kc, :], w_n[:, kc, 0:512], kc == 0, kc == 7, reads=[xk, "w_n"], writes=["pA"])
            for kc in range(8):
                kb.mm(pB[:, 0:140], x_t[:, kc, :], w_n[:, kc, 512:652], kc == 0, kc == 7,
                      reads=[xk, "w_n"], writes=["pB"])
            kb.op("dve", lambda: V.tensor_copy(pr[:, 0:512], pA[:]), reads=["pA", "rdst"], writes=["rsrc"])
            kb.op("dve", lambda: V.tensor_copy(pr[:, 512:640], pB[:, 0:128]), reads=["pB"], writes=["prv"])
            kb.act(G_all[:, T, :], pB[:, 128:140], AF.Sigmoid, reads=["pB"], writes=["G_all"])
            kb.op("act", lambda: A.copy(VS[:, T, 0:64], pr[:, 512:576]), reads=["prv"], writes=["VS"])
            kb.op("act", lambda: A.copy(VW[:, T, 0:64], pr[:, 576:640]), reads=["prv"], writes=["VW"])
            rope(pr[:, 0:384].rearrange("p (s d) -> p s d", s=6), rp[:, 0:6, :], cs_tok[:, T, :], 6, "cs_tok")
            kb.op("act", lambda: A.copy(rp[:, 6:8, :], pr[:, 384:512].rearrange("p (s d) -> p s d", s=2)),
                  reads=["rsrc"], writes=["rdstd"])
            for s_ in range(8):
                kb.op("pe", lambda: nc.tensor.transpose(pT[0:64, s_ * 128:(s_ + 1) * 128], rp[:, s_, :], identb[:]),
                      reads=["rdst", "rdstc", "rdstd", "identb"], writes=["pT"])
            kb.op("dve", lambda: V.tensor_copy(QT[:, :, Ts], pT[0:64, 0:512].rearrange("p (h t) -> p h t", h=4)),
                  reads=["pT"], writes=["QT"])
            kb.op("dve", lambda: V.tensor_copy(KST[0:64, Ts], pT[0:64, 512:640]), reads=["pT"], writes=["KST"])
            kb.op("dve", lambda: V.tensor_copy(KWT[:, Ts], pT[0:64, 640:768]), reads=["pT"], writes=["KWT"])
            kb.op("dve", lambda: V.tensor_copy(KCT[:, Ts], pT[0:64, 768:896]), reads=["pT"], writes=["KCT"])
            kb.op("dve", lambda: V.tensor_copy(VCT[:, Ts], pT[0:64, 896:1024]), reads=["pT"], writes=["VCT"])

        for which, (w1d, w2d, srcT, srck) in enumerate(((wk1_d, wk2_d, KCT, "KCT"), (wv1_d, wv2_d, VCT, "VCT"))):
            kb.dma("pool", w1[:], w1d.rearrange("(r d) h -> d r h", d=64), writes=["w1"])
            kb.dma("pool", w2[:], w2d.rearrange("(hc p) d -> p hc d", p=128), writes=["w2"])
            for hc in range(2):
                for r in range(32):
                    kb.mm(pS[0][:, 0:1], w1[:, r, hc * 128:(hc + 1) * 128], peT[:, r:r + 1], r == 0, r == 31,
                          reads=["w1", "peT"], writes=["pS0"])
                kb.op("dve", lambda: V.tensor_copy(hb[:, hc:hc + 1], pS[0][:, 0:1]), reads=["pS0"], writes=["hb"])
            for hc in range(2):
                for c0 in range(0, ncb, 512):
                    n_ = min(512, ncb - c0)
                    for r in range(32):
                        kb.mm(pS[1][:, 0:n_], w1[:, r, hc * 128:(hc + 1) * 128],
                              srcT[:, 16 * c0 + r:16 * c0 + r + 16 * (n_ - 1) + 1:16], r == 0, r == 31,
                              reads=["w1", srck], writes=["pS1"])
                    kb.act(h1T[:, hc, c0:c0 + n_], pS[1][:, 0:n_], AF.Silu, reads=["pS1", "hb"], writes=["h1T"],
                           bias=hb[:, hc:hc + 1])
            for jt in range(njt_all):
                for hc in range(2):
                    kb.mm(pS[2][:, 0:64], h1T[:, hc, jt * 128:(jt + 1) * 128], w2[:, hc, :], hc == 0, hc == 1,
                          reads=["h1T", "w2"], writes=["pS2"])
                if which == 0:
                    kb.op("dve", lambda: V.tensor_copy(kc_f[:], pS[2][:, 0:64]), reads=["pS2", "rdst"], writes=["rsrc"])
                    rope(kc_f[:].unsqueeze(1), kc_b[:].unsqueeze(1), cs_cmp[:, jt, :], 1, "cs_cmp")
                    kb.op("pe", lambda: nc.tensor.transpose(pT[0:64, 0:128], kc_b[:], identb[:]),
                          reads=["rdst", "rdstc", "identb"], writes=["pT"])
                    kb.op("dve", lambda: V.tensor_copy(KCMT[:, jt * 128:(jt + 1) * 128], pT[0:64, 0:128]),
                          reads=["pT"], writes=["KCMT"])
                else:
                    kb.op("dve", lambda: V.tensor_copy(RC[:, jt, 0:64], pS[2][:, 0:64]), reads=["pS2"], writes=["RC"])

        if limit is not None:
            print("n_inst after stage 2:", kb.n_inst)
        kb.barrier()
        st1.close()
        causal = sb(nc, st, "causal_s", [128, 128], BF16)
        strict = sb(nc, st, "strict_s", [128, 128], BF16)
        e_sb = [sb(nc, st, f"e_sb{i}", [128, 512], BF16) for i in range(3)]
        cm_sb = [sb(nc, st, f"cm_sb{i}", [128, 2, 128], BF16) for i in range(2)]
        impm = sb(nc, st, "impm", [128, 128], F32)
        impw = sb(nc, st, "impw", [128, 128], F32)
        m8 = sb(nc, st, "m8", [128, 16], F32)
        selb = sb(nc, st, "selb", [128, 192], BF16)
        QaLo = [sb(nc, st, f"QaLo{i}", [128, 512], BF16) for i in range(2)]
        QaHi = [sb(nc, st, f"QaHi{i}", [128, 512], BF16) for i in range(2)]
        kb.op("dve", lambda: V.memset(selb[:], 0.0), writes=["selb"])
        zz = sb(nc, st, "zz", [128, 12], F32)
        coef = sb(nc, st, "coef", [128, 12], F32)
        o_accs = [sb(nc, st, f"o_acc{i}", [128, 256], F32) for i in range(2)]
        y_sb = [sb(nc, st, f"y_sb{i}", [128, 256], BF16) for i in range(2)]
        kb.dma("pool", causal[:], causal_d, writes=["causal"])
        kb.dma("pool", strict[:], strict_d, writes=["strict"])

        def exp_tile(bank, ei):
            kb.act(e_sb[ei][:], pS[bank][:], AF.Exp, reads=[f"pS{bank}"], writes=[f"e{ei}"], scale=NSA_SCALE)

        def mask_tile(ei, mask_ap, mkeys):
            e3 = e_sb[ei][:].rearrange("p (h q) -> p h q", h=4)
            kb.op("pool", lambda: G.tensor_tensor(out=e3, in0=e3, in1=mask_ap.unsqueeze(1).to_broadcast([128, 4, 128]),
                                                  op=ALU.mult),
                  reads=[f"e{ei}"] + mkeys, writes=[f"e{ei}"])

        rot = [0]

        def nxt():
            rot[0] += 1
            return rot[0] % 3

        PIPE = 2
        pipe = []

        def drain_one():
            pv0, post0 = pipe.pop(0)
            pv0()
            if post0 is not None:
                post0()

        def push_tile(qk, pv, post=None):
            qk()
            pipe.append((pv, post))
            while len(pipe) > PIPE:
                drain_one()

        def tile_a(qi, jt, jl, Qv, cmk):
            r_ = nxt()

            def qk():
                kb.mm(pS[r_][:], KCMT[:, jt * 128:(jt + 1) * 128], Qv, True, True, reads=["KCMT", "QT"], writes=[f"pS{r_}"])
                exp_tile(r_, r_)
                if jt >= jl - 1:
                    mask_tile(r_, cm_sb[qi % 2][:, 1 - (jl - jt), :], [cmk])

            def pv():
                for h in range(4):
                    bank = pA if h < 2 else pB
                    kb.mm(bank[:, (h % 2) * 193:(h % 2) * 193 + 193], e_sb[r_][:, h * 128:(h + 1) * 128], RC[:, jt, :],
                          jt == 0 and h % 2 == 0, jt == jl and h % 2 == 1, reads=[f"e{r_}", "RC"],
                          writes=["pA" if h < 2 else "pB"])
            return qk, pv

        def post_a1(qi):
            use_sel = qi >= 8
            Gq = G_all[:, qi, :].rearrange("p (h j) -> p h j", h=4)
            o_acc, oak = o_accs[qi % 2], f"o_acc{qi % 2}"

            def post():
                for h in range(4):
                    bank = pA if h < 2 else pB
                    c0 = (h % 2) * 193
                    kb.op("dve", lambda: V.tensor_scalar_max(out=zz[:, h:h + 1], in0=bank[:, c0 + 64:c0 + 65], scalar1=1e-30),
                          reads=["pA" if h < 2 else "pB"], writes=["zz"])
                kb.op("dve", lambda: V.reciprocal(zz[:, 0:4], zz[:, 0:4]), reads=["zz"], writes=["zz"])
                if use_sel:
                    for h in range(4):
                        bank = pA if h < 2 else pB
                        bk = "pA" if h < 2 else "pB"
                        c0 = (h % 2) * 193
                        if h == 0:
                            kb.op("dve", lambda: V.tensor_scalar(out=impm[:], in0=bank[:, c0 + 65:c0 + 193],
                                                                 scalar1=zz[:, 0:1], scalar2=None, op0=ALU.mult),
                                  reads=[bk, "zz"], writes=["impm"])
                        else:
                            kb.op("dve", lambda: V.scalar_tensor_tensor(out=impm[:], in0=bank[:, c0 + 65:c0 + 193],
                                                                        scalar=zz[:, h:h + 1], in1=impm[:],
                                                                        op0=ALU.mult, op1=ALU.add),
                                  reads=[bk, "zz", "impm"], writes=["impm"])
                    c2 = 2 * qi
                    kb.op("dve", lambda: V.memset(impm[:, 0:1], 3e30), reads=[], writes=["impm"])
                    kb.op("dve", lambda: V.memset(impm[:, c2:c2 + 1], 2e30), writes=["impm"])
                    kb.op("dve", lambda: V.memset(impm[0:64, c2 - 1:c2], 1e30), writes=["impm"])
                    kb.op("dve", lambda: V.memset(impm[0:64, c2 + 1:c2 + 2], -1e30), writes=["impm"])
                    kb.op("dve", lambda: V.memset(impm[64:128, c2 + 1:c2 + 2], 2.5e30), writes=["impm"])
                    if c2 + 2 < 128:
                        kb.op("dve", lambda: V.memset(impm[:, c2 + 2:128], -1e30), writes=["impm"])
                    kb.op("dve", lambda: V.max(out=m8[:, 0:8], in_=impm[:]), reads=["impm"], writes=["m8"])
                    kb.op("dve", lambda: V.match_replace(out=impw[:], in_to_replace=m8[:, 0:8], in_values=impm[:],
                                                         imm_value=-1e30), reads=["impm", "m8"], writes=["impw"])
                    kb.op("dve", lambda: V.max(out=m8[:, 8:16], in_=impw[:]), reads=["impw"], writes=["m8"])
                    kb.op("dve", lambda: V.tensor_scalar(out=selb[:, 64:192], in0=impm[:], scalar1=m8[:, 15:16], scalar2=MASK_NEG,
                                                         op0=ALU.is_lt, op1=ALU.mult),
                          reads=["impm", "m8"], writes=["selb"])
                kb.op("dve", lambda: V.tensor_tensor(out=coef[:, 0:4], in0=zz[:, 0:4], in1=Gq[:, :, 0], op=ALU.mult),
                      reads=["zz", "G_all"], writes=["coef"])
                for h in range(4):
                    bank = pA if h < 2 else pB
                    bk = "pA" if h < 2 else "pB"
                    c0 = (h % 2) * 193
                    kb.op("dve", lambda: V.tensor_scalar(out=o_acc[:, h * 64:(h + 1) * 64], in0=bank[:, c0:c0 + 64],
                                                         scalar1=coef[:, h:h + 1], scalar2=None, op0=ALU.mult),
                          reads=[bk, "coef"], writes=[oak])
            return post

        def post_a2(qi):
            b_ = qi % 2

            def post():
                kb.op("pe", lambda: nc.tensor.transpose(pT[:, 0:128], selb[:, 0:128], identb[:]),
                      reads=["selb", "identb"], writes=["pT"])
                if qi >= 32:
                    kb.op("pe", lambda: nc.tensor.transpose(pT[:, 128:256], selb[:, 64:192], identb[:]),
                          reads=["selb", "identb"], writes=["pT"])
                kb.op("dve", lambda: V.tensor_copy(QaLo[b_][64:128, :].rearrange("p (h q) -> p h q", h=4),
                                                   pT[64:128, 0:128].unsqueeze(1).to_broadcast([64, 4, 128])),
                      reads=["pT"], writes=[f"QaLoM{b_}"])
                if qi >= 32:
                    kb.op("dve", lambda: V.tensor_copy(QaHi[b_][64:128, :].rearrange("p (h q) -> p h q", h=4),
                                                       pT[64:128, 128:256].unsqueeze(1).to_broadcast([64, 4, 128])),
                          reads=["pT"], writes=[f"QaHiM{b_}"])
            return post

        def tile_c(qi, kt, Qv, use_sel):
            r_ = nxt()
            Ks = slice(kt * 128, (kt + 1) * 128)

            def qk():
                if use_sel:
                    b_ = qi % 2
                    if kt < 32:
                        kb.mm(pS[r_][:], KST[:, Ks], QaLo[b_][:], True, True,
                              reads=["KST", "KSTa", f"QaLoQ{b_}", f"QaLoM{b_}"], writes=[f"pS{r_}"])
                    else:
                        kb.mm(pS[r_][:], KST[:, Ks], QaHi[b_][:], True, True,
                              reads=["KST", "KSTa", f"QaHiQ{b_}", f"QaHiM{b_}"], writes=[f"pS{r_}"])
                else:
                    kb.mm(pS[r_][:], KST[0:64, Ks], Qv, True, True, reads=["KST", "QT"], writes=[f"pS{r_}"])
                exp_tile(r_, r_)
                if kt == qi:
                    mask_tile(r_, causal[:], ["causal"])

            def pv():
                for h in range(4):
                    kb.mm(pSL[:, h * 65:(h + 1) * 65], e_sb[r_][:, h * 128:(h + 1) * 128], VS[:, kt, :],
                          kt == 0 and h == 0, kt == qi and h == 3, reads=[f"e{r_}", "VS"], writes=["pSL"])
            return qk, pv

        def tile_d(qi, kt, k0, Qv):
            r_ = nxt()
            Ks = slice(kt * 128, (kt + 1) * 128)

            def qk():
                kb.mm(pS[r_][:], KWT[:, Ks], Qv, True, True, reads=["KWT", "QT"], writes=[f"pS{r_}"])
                exp_tile(r_, r_)
                if kt == qi:
                    mask_tile(r_, causal[:], ["causal"])
                elif kt == qi - 4:
                    mask_tile(r_, strict[:], ["strict"])

            def pv():
                for h in range(4):
                    kb.mm(pW[:, h * 65:(h + 1) * 65], e_sb[r_][:, h * 128:(h + 1) * 128], VW[:, kt, :],
                          kt == k0 and h == 0, kt == qi and h == 3, reads=[f"e{r_}", "VW"], writes=["pW"])
            return qk, pv

        def post_combine(qi, bi, final):
            Gq = G_all[:, qi, :].rearrange("p (h j) -> p h j", h=4)
            bank, bk = ((pSL, "pSL"), (pW, "pW"))[bi]
            Qs = slice(qi * 128, (qi + 1) * 128)
            o_acc, oak = o_accs[qi % 2], f"o_acc{qi % 2}"

            def post():
                b3 = bank[:, 0:260].rearrange("p (h c) -> p h c", h=4)
                kb.op("dve", lambda: V.reciprocal(zz[:, 4 + 4 * bi:8 + 4 * bi], b3[:, :, 64]), reads=[bk], writes=["zz"])
                kb.op("dve", lambda: V.tensor_tensor(out=coef[:, 4 + 4 * bi:8 + 4 * bi], in0=zz[:, 4 + 4 * bi:8 + 4 * bi],
                                                     in1=Gq[:, :, 1 + bi], op=ALU.mult),
                      reads=["zz", "G_all"], writes=["coef"])
                for h in range(4):
                    kb.op("dve", lambda: V.scalar_tensor_tensor(
                        out=o_acc[:, h * 64:(h + 1) * 64], in0=b3[:, h, 0:64],
                        scalar=coef[:, 4 + 4 * bi + h:5 + 4 * bi + h], in1=o_acc[:, h * 64:(h + 1) * 64],
                        op0=ALU.mult, op1=ALU.add), reads=[bk, "coef", oak], writes=[oak])
                if final:
                    yk = f"y_sb{qi % 2}"
                    kb.op("pool", lambda: G.tensor_copy(y_sb[qi % 2][:], o_acc[:]), reads=[oak], writes=[yk])
                    kb.dma("sp", y[Qs, :], y_sb[qi % 2][:], reads=[yk], writes=["y_out"], force=True)
            return post

        def emit_a(qi):
            Qv = QT[:, :, qi * 128:(qi + 1) * 128]
            jl = (8 * qi + 6) // 128
            cmk = f"cm{qi % 2}"
            kb.dma("pool", cm_sb[qi % 2][:], cmask_d[qi].rearrange("s j q -> j s q"), writes=[cmk])
            if qi >= 8:
                kb.op("pool", lambda: G.tensor_copy(QaLo[qi % 2][0:64, :].rearrange("p (h q) -> p h q", h=4), Qv),
                      reads=["QT"], writes=[f"QaLoQ{qi % 2}"])
                if qi >= 32:
                    kb.op("pool", lambda: G.tensor_copy(QaHi[qi % 2][0:64, :].rearrange("p (h q) -> p h q", h=4), Qv),
                          reads=["QT"], writes=[f"QaHiQ{qi % 2}"])
            for jt in range(jl + 1):
                qk, pv = tile_a(qi, jt, jl, Qv, cmk)
                push_tile(qk, pv, post_a1(qi) if jt == jl else None)

        def emit_d(qi, a2_here):
            Qv = QT[:, :, qi * 128:(qi + 1) * 128]
            k0 = max(0, qi - 4)
            for kt in range(k0, qi + 1):
                qk, pv = tile_d(qi, kt, k0, Qv)
                post = None
                if kt == qi:
                    post = post_combine(qi, 1, False)
                elif a2_here and kt == k0 + 2:
                    post = post_a2(qi)
                push_tile(qk, pv, post)

        def emit_c(qi, a2_next):
            Qv = QT[:, :, qi * 128:(qi + 1) * 128]
            for kt in range(qi + 1):
                qk, pv = tile_c(qi, kt, Qv, qi >= 8)
                post = None
                if kt == qi:
                    post = post_combine(qi, 0, True)
                elif a2_next and kt == min(qi - 1, 12):
                    post = post_a2(qi + 1)
                push_tile(qk, pv, post)

        emit_a(0)
        emit_d(0, False)
        for qi in range(nqb):
            nxt_sel = qi + 1 < nqb and qi + 1 >= 8
            if qi + 1 < nqb:
                emit_a(qi + 1)
            emit_c(qi, nxt_sel)
            if qi + 1 < nqb:
                emit_d(qi + 1, False)
        while pipe:
            drain_one()
        kb.barrier()


def nsa_inputs(x1, w_in, w_ck1, w_ck2, w_cv1, w_cv2, cmp_pe, n_tok=S):
    cst = nsa_consts(n_tok)
    maps = []
    for c in range(NCORES):
        b, g = c // 4, c % 4
        def col(base, width=64, mult=64):
            return w_in[:, base + g * mult: base + g * mult + width]
        q = w_in[:, g * 256:(g + 1) * 256]
        kc, vc, ks, vs, kw, vw = (col(1024), col(1280), col(1536), col(1792), col(2048), col(2304))
        gt = w_in[:, 2560 + g * 12:2560 + (g + 1) * 12]
        wn = np.ascontiguousarray(np.concatenate([q, ks, kw, kc, vc, vs, vw, gt], axis=1))
        m = {"xT": np.ascontiguousarray(x1[b].T[:, :n_tok]), "wn": wn,
             "wk1": w_ck1, "wk2": w_ck2, "wv1": w_cv1, "wv2": w_cv2,
             "peT": np.ascontiguousarray(cmp_pe.T)}
        m.update(cst)
        maps.append(m)
    return maps


GROUPS = [[0, 1, 2, 3], [4, 5, 6, 7]]


def build_fused():
    nc = bass.Bass("TRN2", target_bir_lowering=False)
    with ExitStack() as st0:
        kb = KB(nc, st0)
        cc_sem = st0.enter_context(nc.semaphore("cc_sem"))
        n_cc = [0]
        internal = lambda name, shape, dt: nc.dram_tensor(name, list(shape), dt).ap()
        y0_d = internal("y0_d", [S, 256], BF16)
        g1 = [internal(f"g1_{j}", [4 * NTB, 256], BF16) for j in range(4)]
        x1_d = internal("x1_d", [NTB, D], F32)
        x1T_d = internal("x1T_d", [D, NTB], BF16)
        g2 = [internal(f"g2_{j}", [4 * 256, NTB], BF16) for j in range(4)]
        y1_d = internal("y1_d", [S, 256], BF16)
        g3 = [internal(f"g3_{j}", [4 * NTB, 256], BF16) for j in range(4)]
        qsel = nc.dram_tensor("qsel", [128, 4], F32, kind="ExternalInput").ap()

        def all_gather(srcs, dsts):
            kb.barrier()
            for s_, d_ in zip(srcs, dsts):
                n_cc[0] += 1
                nc.gpsimd.collective_compute("AllGather", ALU.bypass, replica_groups=GROUPS,
                                             ins=[s_], outs=[d_]).then_inc(cc_sem, 1)
            for e in kb.engs.values():
                e.wait_ge(cc_sem, n_cc[0])

        emit_gla(Ctx(nc, kb, "a0_", {"y": y0_d}))
        all_gather([y0_d[j * NTB:(j + 1) * NTB, :] for j in range(4)], g1)
        emit_ffn(Ctx(nc, kb, "b0_"), fused={"g": g1, "qsel": qsel, "x1_d": x1_d, "x1T_d": x1T_d})
        all_gather([x1T_d[j * 256:(j + 1) * 256, :] for j in range(4)], g2)
        emit_nsa(Ctx(nc, kb, "a1_", {"y": y1_d}), g2=g2)
        all_gather([y1_d[j * NTB:(j + 1) * NTB, :] for j in range(4)], g3)
        emit_ffn(Ctx(nc, kb, "b1_", {"xres": x1_d}), fused={"g": g3, "qsel": qsel})
        kb.barrier()
    return nc


_PROGS = {}


def _prog(name, fn):
    if name not in _PROGS:
        _PROGS[name] = fn()
    return _PROGS[name]


def _run(nc, maps):
    res = run_bass_kernel_spmd(nc, maps, core_ids=list(range(NCORES)))
    return res.results


def _gather_heads(results, key="y"):
    first = np.asarray(results[0][key])
    full = np.empty((B * S, D), dtype=first.dtype)
    for c in range(NCORES):
        b, h = c // 4, c % 4
        full[b * S:(b + 1) * S, h * 256:(h + 1) * 256] = np.asarray(results[c][key])
    return full


def _pref(prefix, m, drop=()):
    return {prefix + k: v for k, v in m.items() if k not in drop}


def fused_inputs(x, gla_w_in, gla_w_gate_up, gla_b_gate, gla_norm_g, gla_w_out,
                 nsa_w_in, nsa_w_cmp_k1, nsa_w_cmp_k2, nsa_w_cmp_v1, nsa_w_cmp_v2, nsa_cmp_pe, nsa_w_out,
                 moe_w_router, moe_b_router, moe_w_gate, moe_w_up, moe_w_down, ln_g, ln_b):
    a0 = gla_inputs(x, gla_w_in[0], gla_w_gate_up[0], gla_b_gate[0], gla_norm_g[0])
    dummy_y = np.zeros((B * S, 1), np.float32)
    b0 = ffn_inputs(dummy_y, x.reshape(B * S, D), gla_w_out[0], ln_g[0], ln_b[0], moe_w_router, moe_b_router,
                    moe_w_gate[0], moe_w_up[0], moe_w_down[0])
    a1 = nsa_inputs(np.zeros((B, 1, S), np.float32), nsa_w_in[0], nsa_w_cmp_k1[0], nsa_w_cmp_k2[0],
                    nsa_w_cmp_v1[0], nsa_w_cmp_v2[0], nsa_cmp_pe[0])
    b1 = ffn_inputs(dummy_y, np.zeros((B * S, 1), np.float32), nsa_w_out[0], ln_g[1], ln_b[1], moe_w_router,
                    moe_b_router, moe_w_gate[1], moe_w_up[1], moe_w_down[1])
    maps = []
    for c in range(NCORES):
        m = {}
        m.update(_pref("a0_", a0[c]))
        m.update(_pref("b0_", b0[c], drop=("yT",)))
        m.update(_pref("a1_", a1[c], drop=("xT",)))
        m.update(_pref("b1_", b1[c], drop=("yT", "xres")))
        qs = np.zeros((128, 4), np.float32)
        qs[:, c % 4] = 1.0
        m["qsel"] = qs
        maps.append(m)
    return maps


def kernel(x, gla_w_in, gla_w_gate_up, gla_b_gate, gla_norm_g, gla_w_out,
           nsa_w_in, nsa_w_cmp_k1, nsa_w_cmp_k2, nsa_w_cmp_v1, nsa_w_cmp_v2, nsa_cmp_pe, nsa_w_out,
           moe_w_router, moe_b_router, moe_w_gate, moe_w_up, moe_w_down, ln_g, ln_b):
    f = lambda a: np.ascontiguousarray(np.asarray(a, dtype=np.float32))
    maps = fused_inputs(f(x), f(gla_w_in), f(gla_w_gate_up), f(gla_b_gate), f(gla_norm_g), f(gla_w_out),
                        f(nsa_w_in), f(nsa_w_cmp_k1), f(nsa_w_cmp_k2), f(nsa_w_cmp_v1), f(nsa_w_cmp_v2),
                        f(nsa_cmp_pe), f(nsa_w_out), f(moe_w_router), f(moe_b_router), f(moe_w_gate),
                        f(moe_w_up), f(moe_w_down), f(ln_g), f(ln_b))
    r = _run(_prog("fused", build_fused), maps)
    out = np.concatenate([np.asarray(r[c]["b1_out"]) for c in range(NCORES)], axis=0)
    return out.reshape(B, S, D).astype(np.float32)
```
